# Optimizing a Trainium2 kernel written in Bass

```python
import math
import jax, jax.numpy as jnp
from jax import lax
import numpy as np

D_MODEL = 2048
BATCH = 2
SEQ = 4096
DEPTH = 4

GRID_W = 64
CTX_LEN = 256
N_MIXERS = 4
EPS = 1e-6
MOD_CHUNKS = 6
D_FF = 5632
FFN_CONV_WIDTH = 3
GQA_HEADS = 16
GQA_KV_HEADS = 4
HEAD_DIM = 128
ROPE_THETA = 10000.0
Q_BLOCK = 128
MLSTM_HEADS = 8
MLSTM_DQK = 128
MLSTM_DV = 256
MLSTM_CHUNK = 64
M_INIT = -1e30
CONV_INNER = D_MODEL
CONV_WIDTH = 31
NA_HEADS = 16
NA_HEAD_DIM = 128
NA_ROWS = 8
NA_COLS = 16

kernel_name = "hybrid_interleaved_diffusion_block"

F32 = jnp.float32


def rms_norm(x, w):
    x32 = x.astype(F32)
    y = x32 * lax.rsqrt(jnp.mean(x32 * x32, axis=-1, keepdims=True) + EPS)
    return (y * w.astype(F32)).astype(x.dtype)


def layer_norm(x, w, b):
    x32 = x.astype(F32)
    mu = jnp.mean(x32, axis=-1, keepdims=True)
    xc = x32 - mu
    var = jnp.mean(xc * xc, axis=-1, keepdims=True)
    return (xc * lax.rsqrt(var + EPS) * w.astype(F32) + b.astype(F32)).astype(x.dtype)


def modulate(x, shift, scale):
    return x * (1 + scale) + shift


def depthwise_conv(x, w, b):
    width = w.shape[0]
    y = lax.conv_general_dilated(
        x, w[:, None, :].astype(x.dtype), window_strides=(1,),
        padding=[(width // 2, width // 2)],
        dimension_numbers=("NWC", "WIO", "NWC"),
        feature_group_count=x.shape[-1])
    return y + b


def rope_1d(x, pos):
    half = x.shape[-1] // 2
    freqs = ROPE_THETA ** (-jnp.arange(half, dtype=F32) / half)
    ang = pos.astype(F32)[:, None] * freqs[None, :]
    cos = jnp.cos(ang)[:, None, :]
    sin = jnp.sin(ang)[:, None, :]
    x32 = x.astype(F32)
    x1, x2 = x32[..., :half], x32[..., half:]
    return jnp.concatenate([x1 * cos - x2 * sin, x1 * sin + x2 * cos], axis=-1).astype(x.dtype)


def rope_axial(x, row, col):
    h = x.shape[-1] // 2
    return jnp.concatenate([rope_1d(x[..., :h], row), rope_1d(x[..., h:], col)], axis=-1)


def grouped_attend(q, k, v):
    s = jnp.einsum("bqkgd,bskd->bkgqs", q, k).astype(F32)
    p = jax.nn.softmax(s, axis=-1).astype(v.dtype)
    return jnp.einsum("bkgqs,bskd->bqkgd", p, v)


def gqa_axial_mixer(h_lat, h_ctx, w_in, q_norm, k_norm, w_out, need_ctx):
    B, S, _ = h_lat.shape
    n_ctx = h_ctx.shape[1]
    G = GQA_HEADS // GQA_KV_HEADS
    nq = GQA_HEADS * HEAD_DIM
    pos = jnp.arange(S)
    row, col = pos // GRID_W, pos % GRID_W

    def project(h, with_q):
        n = h.shape[1]
        z = h @ (w_in if with_q else w_in[:, nq:])
        q = None
        if with_q:
            q, z = z[..., :nq], z[..., nq:]
            q = rms_norm(q.reshape(B, n, GQA_HEADS, HEAD_DIM), q_norm) * HEAD_DIM ** -0.5
        k, v = jnp.split(z, 2, axis=-1)
        k = rms_norm(k.reshape(B, n, GQA_KV_HEADS, HEAD_DIM), k_norm)
        v = v.reshape(B, n, GQA_KV_HEADS, HEAD_DIM)
        return q, k, v

    q_l, k_l, v_l = project(h_lat, True)
    q_c, k_c, v_c = project(h_ctx, need_ctx)
    q_l = rope_axial(q_l, row, col)
    k_l = rope_axial(k_l, row, col)
    k_all = jnp.concatenate([k_c, k_l], axis=1)
    v_all = jnp.concatenate([v_c, v_l], axis=1)
    nb = S // Q_BLOCK
    q_blocks = q_l.reshape(B, nb, Q_BLOCK, GQA_KV_HEADS, G, HEAD_DIM).transpose(1, 0, 2, 3, 4, 5)
    o = lax.map(lambda qb: grouped_attend(qb, k_all, v_all), q_blocks)
    o = o.transpose(1, 0, 2, 3, 4, 5).reshape(B, S, nq)
    y_lat = o @ w_out
    y_ctx = None
    if need_ctx:
        qc = q_c.reshape(B, n_ctx, GQA_KV_HEADS, G, HEAD_DIM)
        y_ctx = grouped_attend(qc, k_c, v_c).reshape(B, n_ctx, nq) @ w_out
    return y_lat, y_ctx


def mlstm_chunkwise(q, k, v, ig, lf, state, return_h):
    B, T, H, dk = q.shape
    dv = v.shape[-1]
    L = min(MLSTM_CHUNK, T)
    nc = T // L
    out_dtype = v.dtype
    q = (q.astype(F32) * dk ** -0.5).reshape(B, nc, L, H, dk)
    k = k.astype(F32).reshape(B, nc, L, H, dk)
    v = v.astype(F32).reshape(B, nc, L, H, dv)
    ig = ig.reshape(B, nc, L, H)
    lf = lf.reshape(B, nc, L, H)
    b = jnp.cumsum(lf, axis=2)
    g = b[:, :, -1]
    a = g[:, :, None] - b + ig
    m_loc = jnp.max(a, axis=2)
    w = jnp.exp(a - m_loc[:, :, None])
    kv_sum = jnp.einsum("bclh,bclhd,bclhe->bchde", w, k, v)
    k_sum = jnp.einsum("bclh,bclhd->bchd", w, k)

    def step(carry, xs):
        C, n, m = carry
        g_c, ml_c, kv_c, ks_c = xs
        m_new = jnp.maximum(g_c + m, ml_c)
        a_old = jnp.exp(g_c + m - m_new)
        a_new = jnp.exp(ml_c - m_new)
        C_new = a_old[..., None, None] * C + a_new[..., None, None] * kv_c
        n_new = a_old[..., None] * n + a_new[..., None] * ks_c
        return (C_new, n_new, m_new), ((C, n, m) if return_h else None)

    xs = (jnp.moveaxis(g, 1, 0), jnp.moveaxis(m_loc, 1, 0),
          jnp.moveaxis(kv_sum, 1, 0), jnp.moveaxis(k_sum, 1, 0))
    final, prev = lax.scan(step, state, xs)
    if not return_h:
        return None, final
    C_in, n_in, m_in = [jnp.moveaxis(t, 0, 1) for t in prev]
    lower = jnp.tril(jnp.ones((L, L), dtype=bool))
    log_d = b[:, :, :, None, :] - b[:, :, None, :, :] + ig[:, :, None, :, :]
    log_d = jnp.where(lower[None, None, :, :, None], log_d, -jnp.inf)
    log_inter = b + m_in[:, :, None, :]
    m_j = jnp.maximum(log_inter, jnp.max(log_d, axis=3))
    w_intra = jnp.exp(log_d - m_j[:, :, :, None, :])
    w_inter = jnp.exp(log_inter - m_j)
    s = jnp.einsum("bcjhd,bcshd->bcjsh", q, k) * w_intra
    num = (jnp.einsum("bcjsh,bcshe->bcjhe", s, v)
           + w_inter[..., None] * jnp.einsum("bcjhd,bchde->bcjhe", q, C_in))
    den = jnp.sum(s, axis=3) + w_inter * jnp.einsum("bcjhd,bchd->bcjh", q, n_in)
    h = num / jnp.maximum(jnp.abs(den), jnp.exp(-m_j))[..., None]
    return h.reshape(B, T, H, dv).astype(out_dtype), final


def mlstm_mixer(h_lat, h_ctx, w_in, b_gate, out_norm, w_out, need_ctx):
    B = h_lat.shape[0]
    nqk = MLSTM_HEADS * MLSTM_DQK
    nv = MLSTM_HEADS * MLSTM_DV

    def project(h):
        n = h.shape[1]
        q, k, v, o, gates = jnp.split(h @ w_in, [nqk, 2 * nqk, 2 * nqk + nv, 2 * nqk + 2 * nv], axis=-1)
        q = q.reshape(B, n, MLSTM_HEADS, MLSTM_DQK)
        k = k.reshape(B, n, MLSTM_HEADS, MLSTM_DQK)
        v = v.reshape(B, n, MLSTM_HEADS, MLSTM_DV)
        gates = (gates + b_gate).astype(F32).reshape(B, n, 4, MLSTM_HEADS)
        return q, k, v, o, gates

    def finish(h, o):
        n = h.shape[1]
        h = rms_norm(h, out_norm.reshape(MLSTM_HEADS, MLSTM_DV)).reshape(B, n, nv)
        return (h * jax.nn.sigmoid(o)) @ w_out

    def flip(t):
        return jnp.flip(t, axis=1)

    ql, kl, vl, ol, gl = project(h_lat)
    qc, kc, vc, oc, gc = project(h_ctx)
    zero = (jnp.zeros((B, MLSTM_HEADS, MLSTM_DQK, MLSTM_DV), F32),
            jnp.zeros((B, MLSTM_HEADS, MLSTM_DQK), F32),
            jnp.full((B, MLSTM_HEADS), M_INIT, F32))
    lsig = jax.nn.log_sigmoid
    hc_f, st_f = mlstm_chunkwise(qc, kc, vc, gc[:, :, 0], lsig(gc[:, :, 1]), zero, need_ctx)
    hl_f, _ = mlstm_chunkwise(ql, kl, vl, gl[:, :, 0], lsig(gl[:, :, 1]), st_f, True)
    hc_b, st_b = mlstm_chunkwise(flip(qc), flip(kc), flip(vc), flip(gc[:, :, 2]),
                                 lsig(flip(gc[:, :, 3])), zero, need_ctx)
    hl_b, _ = mlstm_chunkwise(flip(ql), flip(kl), flip(vl), flip(gl[:, :, 2]),
                              lsig(flip(gl[:, :, 3])), st_b, True)
    y_lat = finish(hl_f + flip(hl_b), ol)
    y_ctx = finish(hc_f + flip(hc_b), oc) if need_ctx else None
    return y_lat, y_ctx


def conformer_conv_mixer(h_lat, h_ctx, w_pw1, b_pw1, w_dw, b_dw, ln_w, ln_b, w_pw2, b_pw2, need_ctx):
    def branch(h):
        a, gt = jnp.split(h @ w_pw1 + b_pw1, 2, axis=-1)
        u = a * jax.nn.sigmoid(gt)
        u = depthwise_conv(u, w_dw, b_dw)
        u = jax.nn.silu(layer_norm(u, ln_w, ln_b))
        return u @ w_pw2 + b_pw2
    return branch(h_lat), (branch(h_ctx) if need_ctx else None)


def neighbourhood_mixer(h_lat, h_ctx, w_in, rpb, w_out, need_ctx):
    B, S, _ = h_lat.shape
    n_ctx = h_ctx.shape[1]
    rows = S // GRID_W
    wr = min(NA_ROWS, rows)
    nq = NA_HEADS * NA_HEAD_DIM
    scale = NA_HEAD_DIM ** -0.5
    q, k, v = jnp.split(h_lat @ w_in, [nq, 2 * nq], axis=-1)
    q = (q * scale).reshape(B, rows, GRID_W, NA_HEADS, NA_HEAD_DIM).transpose(1, 0, 2, 3, 4)
    k = k.reshape(B, rows, GRID_W, NA_HEADS, NA_HEAD_DIM)
    v = v.reshape(B, rows, GRID_W, NA_HEADS, NA_HEAD_DIM)
    k_c, v_c = jnp.split(h_ctx @ w_in[:, nq:], 2, axis=-1)
    k_c = k_c.reshape(B, n_ctx, NA_HEADS, NA_HEAD_DIM)
    v_c = v_c.reshape(B, n_ctx, NA_HEADS, NA_HEAD_DIM)
    cols = jnp.arange(GRID_W)
    c_start = jnp.clip(cols - NA_COLS // 2, 0, GRID_W - NA_COLS)
    col_idx = c_start[:, None] + jnp.arange(NA_COLS)[None, :]
    col_bias_idx = col_idx - cols[:, None] + (NA_COLS - 1)

    def row_block(args):
        r, q_r = args
        r_start = jnp.clip(r - wr // 2, 0, rows - wr)
        k_band = lax.dynamic_slice_in_dim(k, r_start, wr, axis=1)
        v_band = lax.dynamic_slice_in_dim(v, r_start, wr, axis=1)
        k_nb = k_band[:, :, col_idx]
        v_nb = v_band[:, :, col_idx]
        row_bias_idx = r_start + jnp.arange(wr) - r + (NA_ROWS - 1)
        bias = rpb[:, row_bias_idx[:, None, None], col_bias_idx[None, :, :]]
        s_nb = (jnp.einsum("bqhd,brqjhd->bhqrj", q_r, k_nb).astype(F32)
                + jnp.transpose(bias, (0, 2, 1, 3))[None].astype(F32))
        s_ctx = jnp.einsum("bqhd,bchd->bhqc", q_r, k_c).astype(F32)
        s = jnp.concatenate([s_nb.reshape(B, NA_HEADS, GRID_W, wr * NA_COLS), s_ctx], axis=-1)
        p = jax.nn.softmax(s, axis=-1).astype(v.dtype)
        p_nb = p[..., :wr * NA_COLS].reshape(B, NA_HEADS, GRID_W, wr, NA_COLS)
        p_ctx = p[..., wr * NA_COLS:]
        return (jnp.einsum("bhqrj,brqjhd->bqhd", p_nb, v_nb)
                + jnp.einsum("bhqc,bchd->bqhd", p_ctx, v_c))

    o = lax.map(row_block, (jnp.arange(rows), q))
    y_lat = o.transpose(1, 0, 2, 3, 4).reshape(B, S, nq) @ w_out
    y_ctx = None
    if need_ctx:
        q_c = (h_ctx @ w_in[:, :nq]) * scale
        q_c = q_c.reshape(B, n_ctx, NA_HEADS, 1, NA_HEAD_DIM)
        o_c = jnp.stack([grouped_attend(q_c[:, :, hh:hh + 1], k_c[:, :, hh:hh + 1], v_c[:, :, hh:hh + 1])
                         for hh in range(0)], 0) if False else None
        s_c = jnp.einsum("bqhd,bchd->bhqc", q_c[:, :, :, 0], k_c).astype(F32)
        p_c = jax.nn.softmax(s_c, axis=-1).astype(v_c.dtype)
        y_ctx = jnp.einsum("bhqc,bchd->bqhd", p_c, v_c).reshape(B, n_ctx, nq) @ w_out
    return y_lat, y_ctx


def conv_ffn(h, w_up, conv_w, conv_b, w_down):
    u = depthwise_conv(h @ w_up, conv_w, conv_b)
    val, gate = jnp.split(u, 2, axis=-1)
    return (val * jax.nn.silu(gate)) @ w_down


def setup_inputs(seed: int = 0) -> dict:
    key = jax.random.key(seed)
    keys = jax.random.split(key, 48)
    counter = [0]

    def nrm(shape, scale):
        kk = keys[counter[0]]
        counter[0] += 1
        return jax.random.normal(kk, shape, F32) * scale

    def gain(shape):
        return 1.0 + nrm(shape, 0.05)

    D = D_MODEL
    nA, nB, nC, nD = [len(range(m, DEPTH, N_MIXERS)) for m in range(N_MIXERS)]
    gqa_cols = (GQA_HEADS + 2 * GQA_KV_HEADS) * HEAD_DIM
    ml_cols = 2 * MLSTM_HEADS * MLSTM_DQK + 2 * MLSTM_HEADS * MLSTM_DV + 4 * MLSTM_HEADS
    f_bias = jnp.linspace(3.0, 6.0, MLSTM_HEADS, dtype=F32)
    gate_base = jnp.array([0.0, 1.0, 0.0, 1.0], F32)[:, None] * f_bias[None, :]
    inp = {}
    inp["x"] = nrm((BATCH, SEQ, D), 1.0)
    inp["c"] = nrm((BATCH, D), 1.0)
    inp["ctx"] = nrm((BATCH, CTX_LEN, D), 1.0)
    inp["c_ctx"] = nrm((D,), 1.0)
    inp["mod_w"] = nrm((DEPTH, D, MOD_CHUNKS * D), 0.5 * D ** -0.5)
    inp["mod_b"] = nrm((DEPTH, MOD_CHUNKS * D), 0.02)
    inp["norm_pre_mix"] = gain((DEPTH, D))
    inp["norm_post_mix"] = gain((DEPTH, D))
    inp["norm_pre_ffn"] = gain((DEPTH, D))
    inp["norm_post_ffn"] = gain((DEPTH, D))
    inp["ffn_w_up"] = nrm((DEPTH, D, 2 * D_FF), D ** -0.5)
    inp["ffn_conv_w"] = nrm((DEPTH, FFN_CONV_WIDTH, 2 * D_FF), FFN_CONV_WIDTH ** -0.5)
    inp["ffn_conv_b"] = nrm((DEPTH, 2 * D_FF), 0.02)
    inp["ffn_w_down"] = nrm((DEPTH, D_FF, D), D_FF ** -0.5)
    inp["gqa_w_in"] = nrm((nA, D, gqa_cols), D ** -0.5)
    inp["gqa_q_norm"] = gain((nA, HEAD_DIM))
    inp["gqa_k_norm"] = gain((nA, HEAD_DIM))
    inp["gqa_w_out"] = nrm((nA, GQA_HEADS * HEAD_DIM, D), (GQA_HEADS * HEAD_DIM) ** -0.5)
    inp["mlstm_w_in"] = nrm((nB, D, ml_cols), D ** -0.5)
    inp["mlstm_b_gate"] = (nrm((nB, 4, MLSTM_HEADS), 0.1) + gate_base[None]).reshape(nB, 4 * MLSTM_HEADS)
    inp["mlstm_out_norm"] = gain((nB, MLSTM_HEADS * MLSTM_DV))
    inp["mlstm_w_out"] = nrm((nB, MLSTM_HEADS * MLSTM_DV, D), (MLSTM_HEADS * MLSTM_DV) ** -0.5)
    inp["conv_w_pw1"] = nrm((nC, D, 2 * CONV_INNER), D ** -0.5)
    inp["conv_b_pw1"] = nrm((nC, 2 * CONV_INNER), 0.02)
    inp["conv_w_dw"] = nrm((nC, CONV_WIDTH, CONV_INNER), CONV_WIDTH ** -0.5)
    inp["conv_b_dw"] = nrm((nC, CONV_INNER), 0.02)
    inp["conv_ln_w"] = gain((nC, CONV_INNER))
    inp["conv_ln_b"] = nrm((nC, CONV_INNER), 0.02)
    inp["conv_w_pw2"] = nrm((nC, CONV_INNER, D), CONV_INNER ** -0.5)
    inp["conv_b_pw2"] = nrm((nC, D), 0.02)
    inp["nat_w_in"] = nrm((nD, D, 3 * NA_HEADS * NA_HEAD_DIM), D ** -0.5)
    inp["nat_rpb"] = nrm((nD, NA_HEADS, 2 * NA_ROWS - 1, 2 * NA_COLS - 1), 0.1)
    inp["nat_w_out"] = nrm((nD, NA_HEADS * NA_HEAD_DIM, D), (NA_HEADS * NA_HEAD_DIM) ** -0.5)
    return inp


def reference(x, c, ctx, c_ctx, mod_w, mod_b, norm_pre_mix, norm_post_mix, norm_pre_ffn, norm_post_ffn,
              ffn_w_up, ffn_conv_w, ffn_conv_b, ffn_w_down,
              gqa_w_in, gqa_q_norm, gqa_k_norm, gqa_w_out,
              mlstm_w_in, mlstm_b_gate, mlstm_out_norm, mlstm_w_out,
              conv_w_pw1, conv_b_pw1, conv_w_dw, conv_b_dw, conv_ln_w, conv_ln_b, conv_w_pw2, conv_b_pw2,
              nat_w_in, nat_rpb, nat_w_out):
    xc = ctx
    for i in range(DEPTH):
        kind = i % N_MIXERS
        j = i // N_MIXERS
        need_ctx = i < DEPTH - 1
        mod = jax.nn.silu(c) @ mod_w[i] + mod_b[i]
        mod_c = jax.nn.silu(c_ctx) @ mod_w[i] + mod_b[i]
        sh1, sc1, g1, sh2, sc2, g2 = jnp.split(mod[:, None, :], MOD_CHUNKS, axis=-1)
        csh1, csc1, cg1, csh2, csc2, cg2 = jnp.split(mod_c, MOD_CHUNKS)
        h_l = modulate(rms_norm(x, norm_pre_mix[i]), sh1, sc1)
        h_c = modulate(rms_norm(xc, norm_pre_mix[i]), csh1, csc1)
        if kind == 0:
            y_l, y_c = gqa_axial_mixer(h_l, h_c, gqa_w_in[j], gqa_q_norm[j], gqa_k_norm[j], gqa_w_out[j], need_ctx)
        elif kind == 1:
            y_l, y_c = mlstm_mixer(h_l, h_c, mlstm_w_in[j], mlstm_b_gate[j], mlstm_out_norm[j], mlstm_w_out[j], need_ctx)
        elif kind == 2:
            y_l, y_c = conformer_conv_mixer(h_l, h_c, conv_w_pw1[j], conv_b_pw1[j], conv_w_dw[j], conv_b_dw[j],
                                            conv_ln_w[j], conv_ln_b[j], conv_w_pw2[j], conv_b_pw2[j], need_ctx)
        else:
            y_l, y_c = neighbourhood_mixer(h_l, h_c, nat_w_in[j], nat_rpb[j], nat_w_out[j], need_ctx)
        x = x + g1 * rms_norm(y_l, norm_post_mix[i])
        h_l = modulate(rms_norm(x, norm_pre_ffn[i]), sh2, sc2)
        x = x + g2 * rms_norm(conv_ffn(h_l, ffn_w_up[i], ffn_conv_w[i], ffn_conv_b[i], ffn_w_down[i]), norm_post_ffn[i])
        if need_ctx:
            xc = xc + cg1 * rms_norm(y_c, norm_post_mix[i])
            h_c = modulate(rms_norm(xc, norm_pre_ffn[i]), csh2, csc2)
            xc = xc + cg2 * rms_norm(conv_ffn(h_c, ffn_w_up[i], ffn_conv_w[i], ffn_conv_b[i], ffn_w_down[i]),
                                     norm_post_ffn[i])
    return x
```

```python
import numpy as np
from contextlib import ExitStack
import ml_dtypes
import concourse.bass as bass
import concourse.mybir as mybir
from concourse.bass_utils import run_bass_kernel_spmd

F32 = mybir.dt.float32
BF16 = mybir.dt.bfloat16
AF = mybir.ActivationFunctionType
ALU = mybir.AluOpType
AX = mybir.AxisListType
NPBF = ml_dtypes.bfloat16

D = 2048
KC = 16
NCTX = 256
SEQ = 4096
NLAT = 1024
DFF = 5632
EPS = 1e-6
NCORES = 8


class V:
    __slots__ = ("ap", "tile", "lo", "hi")

    def __init__(self, ap, tile, lo, hi):
        self.ap, self.tile, self.lo, self.hi = ap, tile, lo, hi


class Tile:
    def __init__(self, mk, t, shape, name):
        self.mk, self.t, self.shape, self.name = mk, t, list(shape), name
        st = []
        acc = 1
        for s in reversed(self.shape[1:]):
            st.append(acc)
            acc *= s
        self.strides = list(reversed(st))
        self.recs_w = []
        self.recs_r = []

    def __getitem__(self, idx):
        if not isinstance(idx, tuple):
            idx = (idx,)
        ap = self.t[idx]
        lo = 0
        hi = 0
        fidx = list(idx[1:]) + [slice(None)] * (len(self.shape) - len(idx))
        for s, n, stride in zip(fidx, self.shape[1:], self.strides):
            if isinstance(s, slice):
                a = 0 if s.start is None else s.start
                b = n if s.stop is None else s.stop
                step = 1 if s.step is None else s.step
                cnt = (b - a + step - 1) // step
                last = a + (cnt - 1) * step
            else:
                a = s
                last = s
            lo += a * stride
            hi += last * stride
        return V(ap, self, lo, hi + 1)

    def all(self):
        return self[tuple([slice(None)] * len(self.shape))]


class MK:
    SEM_ROLL = 30000

    def __init__(self, nc, n_dma_sems=32):
        self.nc = nc
        self.es = ExitStack()
        self.eng = {"pe": nc.tensor, "act": nc.scalar, "dve": nc.vector, "pool": nc.gpsimd, "sp": nc.sync}
        self.sem = {}
        self.cnt = {}
        self.nsem = 0
        for e in self.eng:
            self._new_sem(e)
        self.waited = {e: {} for e in self.eng}
        self.dma_sems = [self.es.enter_context(nc.semaphore(f"dq{i}")) for i in range(n_dma_sems)]
        self.dma_val = [0] * n_dma_sems
        self.dma_rr = 0
        self.ninst = 0
        self.nwait = 0
        self.rr = 0

    def _new_sem(self, e):
        self.sem[e] = self.es.enter_context(self.nc.semaphore(f"s_{e}_{self.nsem}"))
        self.nsem += 1
        self.cnt[e] = 0

    def sb(self, name, shape, dt=F32):
        t = self.es.enter_context(self.nc.sbuf_tensor("sb_" + name, list(shape), dt))
        return Tile(self, t, shape, name)

    def ps(self, name, shape, dt=F32):
        t = self.es.enter_context(self.nc.psum_tensor("ps_" + name, list(shape), dt))
        return Tile(self, t, shape, name)

    def _wait(self, e, sem, val):
        w = self.waited[e]
        if w.get(sem, 0) >= val:
            return
        w[sem] = val
        self.eng[e].wait_ge(sem, val)
        self.nwait += 1

    def _deps(self, e, reads, writes):
        for v in reads:
            if not isinstance(v, V):
                continue
            for (lo, hi, sem, val, de) in v.tile.recs_w:
                if lo < v.hi and v.lo < hi and not (de == "pe" and e == "pe"):
                    self._wait(e, sem, val)
        for v in writes:
            if not isinstance(v, V):
                continue
            for (lo, hi, sem, val, de) in v.tile.recs_w:
                if lo < v.hi and v.lo < hi and not (de == "pe" and e == "pe"):
                    self._wait(e, sem, val)
            for (lo, hi, sem, val, de) in v.tile.recs_r:
                if lo < v.hi and v.lo < hi and not (de == "pe" and e == "pe"):
                    self._wait(e, sem, val)

    def _record(self, reads, writes, sem, val, e):
        for v in writes:
            if not isinstance(v, V):
                continue
            t = v.tile
            t.recs_w = [r for r in t.recs_w if not (v.lo <= r[0] and r[1] <= v.hi)]
            t.recs_r = [r for r in t.recs_r if not (v.lo <= r[0] and r[1] <= v.hi)]
            t.recs_w.append((v.lo, v.hi, sem, val, e))
        for v in reads:
            if not isinstance(v, V):
                continue
            t = v.tile
            t.recs_r = [r for r in t.recs_r if not (r[2] is sem and v.lo <= r[0] and r[1] <= v.hi)]
            t.recs_r.append((v.lo, v.hi, sem, val, e))

    @staticmethod
    def _ap(v):
        return v.ap if isinstance(v, V) else v

    def op(self, e, fn, reads, writes):
        self._deps(e, reads, writes)
        if self.cnt[e] >= self.SEM_ROLL:
            self._new_sem(e)
        ins = fn()
        self.cnt[e] += 1
        ins.then_inc(self.sem[e], 1)
        self._record(reads, writes, self.sem[e], self.cnt[e], e)
        self.ninst += 1
        return ins

    def dma(self, e, out, in_, **kw):
        self._deps(e, [in_], [out])
        i = self.dma_rr
        self.dma_rr = (self.dma_rr + 1) % len(self.dma_sems)
        s = self.dma_sems[i]
        if self.dma_val[i] > 0:
            self._wait(e, s, self.dma_val[i])
        self.dma_val[i] += 16
        self.eng[e].dma_start(out=self._ap(out), in_=self._ap(in_), **kw).then_inc(s, 16)
        self._record([in_], [out], s, self.dma_val[i], "dma")
        self.ninst += 1

    def finish(self, e="sp"):
        for i, s in enumerate(self.dma_sems):
            if self.dma_val[i] > 0:
                self._wait(e, s, self.dma_val[i])
        for o in self.eng:
            if o != e and self.cnt[o] > 0:
                self._wait(e, self.sem[o], self.cnt[o])

    def close(self):
        self.es.close()

    def matmul(self, out, lhsT, rhs, start=True, stop=True, skip=False):
        return self.op("pe", lambda: self.nc.tensor.matmul(out.ap, lhsT.ap, rhs.ap, start=start, stop=stop,
                                                           skip_group_check=skip),
                       [lhsT, rhs], [out])

    def act(self, out, in_, func, bias=None, scale=None, accum_out=None):
        kw = {}
        reads = [in_]
        writes = [out]
        if bias is not None:
            kw["bias"] = self._ap(bias)
            reads.append(bias)
        if scale is not None:
            kw["scale"] = self._ap(scale)
            reads.append(scale)
        if accum_out is not None:
            kw["accum_out"] = accum_out.ap
            writes.append(accum_out)
        return self.op("act", lambda: self.nc.scalar.activation(out.ap, in_.ap, func, **kw), reads, writes)

    def tt(self, e, out, a, b, op):
        return self.op(e, lambda: self.eng[e].tensor_tensor(out.ap, a.ap, b.ap, op), [a, b], [out])

    def ts(self, e, out, a, s1, op0, s2=None, op1=None):
        reads = [a, s1, s2]
        if op1 is None:
            s2, op1 = 0.0, ALU.add
        return self.op(e, lambda: self.eng[e].tensor_scalar(out.ap, a.ap, self._ap(s1), self._ap(s2), op0, op1),
                       reads, [out])

    def stt(self, out, a, s, b, op0, op1):
        return self.op("dve", lambda: self.nc.vector.scalar_tensor_tensor(out.ap, a.ap, self._ap(s), b.ap, op0, op1),
                       [a, s, b], [out])

    def copy(self, e, out, in_):
        if e == "act":
            return self.op(e, lambda: self.nc.scalar.copy(out.ap, in_.ap), [in_], [out])
        return self.op(e, lambda: self.eng[e].tensor_copy(out.ap, in_.ap), [in_], [out])

    def memset(self, e, out, val):
        return self.op(e, lambda: self.eng[e].memset(out.ap, val), [], [out])

    def recip(self, out, in_):
        return self.op("dve", lambda: self.nc.vector.reciprocal(out.ap, in_.ap), [in_], [out])

    def evac(self, out, in_):
        self.rr ^= 1
        return self.copy("act" if self.rr else "dve", out, in_)


def tiles_of(n, mx=512):
    k = (n + mx - 1) // mx
    base = n // k
    rem = n % k
    out = []
    o = 0
    for i in range(k):
        s = base + (1 if i < rem else 0)
        out.append((o, s))
        o += s
    return out


class Prog:
    def __init__(self, name):
        import time as _t
        self.t_start = _t.time()
        self.name = name
        self.nc = bass.Bass("TRN2", target_bir_lowering=False)
        self.mk = MK(self.nc)
        self.in_names = []
        self.out_names = []
        mk = self.mk
        self.psb = [mk.ps(f"psb{i}", [128, 512], F32) for i in range(8)]
        self.ps_i = 0
        self.ones = mk.sb("ones_bf", [128, 128], BF16)
        mk.memset("dve", self.ones.all(), 1.0)
        self.eps_t = mk.sb("eps_t", [128, 1], F32)
        mk.memset("dve", self.eps_t.all(), EPS)
        self.wq = 0

    def inp(self, name, shape, dt=F32):
        self.in_names.append(name)
        return self.nc.dram_tensor(name, list(shape), dt, kind="ExternalInput").ap()

    def outp(self, name, shape, dt=F32):
        self.out_names.append(name)
        return self.nc.dram_tensor(name, list(shape), dt, kind="ExternalOutput").ap()

    def rstd_of(self, out, ssq, dim, eps=EPS):
        self.mk.act(out, ssq, AF.Sqrt, bias=self.eps_t[0:out.ap.shape[0], 0:1] if eps == EPS else eps, scale=1.0 / dim)
        self.mk.recip(out, out)

    def psum(self):
        p = self.psb[self.ps_i]
        self.ps_i = (self.ps_i + 1) % 8
        return p

    def run(self, in_maps):
        import time as _t
        self.mk.finish()
        t0 = _t.time()
        res = run_bass_kernel_spmd(self.nc, in_maps, core_ids=list(range(NCORES)))
        print(f"[{self.name}] ninst={self.mk.ninst} nwait={self.mk.nwait} build={t0 - self.t_start:.1f}s "
              f"run={_t.time() - t0:.1f}s", flush=True)
        self.mk.close()
        return res.results

    def load_w(self, tile, w_ap, col0, ncols, nk):
        src = w_ap[:, col0:col0 + ncols].rearrange("(kc p) n -> p kc n", p=128)
        step = 4
        for k0 in range(0, nk, step):
            k1 = min(nk, k0 + step)
            self.mk.dma("pool", tile[:, k0:k1, 0:ncols], src[:, k0:k1, :])

    def rstd_fm(self, xT, nk, T, rstd, sqbuf, dim):
        mk = self.mk
        for (t0, tn) in tiles_of(T):
            ps = self.psum()
            for kc in range(nk):
                sq = sqbuf[kc % len(sqbuf)]
                mk.act(sq[:, 0:tn], xT[:, kc, t0:t0 + tn], AF.Square)
                mk.matmul(ps[:, 0:tn], self.ones.all(), sq[:, 0:tn], start=(kc == 0), stop=(kc == nk - 1))
            self.rstd_of(rstd[:, t0:t0 + tn], ps[:, 0:tn], dim)

    def modulate_fm(self, hT, xT, rstd, segs, tmp):
        mk = self.mk
        for kc in range(KC):
            for (t0, tn, a, sh) in segs:
                t = tmp[kc % len(tmp)]
                mk.tt("dve", t[:, 0:tn], xT[:, kc, t0:t0 + tn], rstd[:, t0:t0 + tn], ALU.mult)
                mk.act(hT[:, kc, t0:t0 + tn], t[:, 0:tn], AF.Identity, bias=sh[:, kc:kc + 1], scale=a[:, kc:kc + 1])

    def load_mod(self, modT, nseg):
        t = self.mk.sb("modsb", [128, nseg, 6, KC], F32)
        self.mk.dma("sp", t.all(), modT)
        return t

    def load_vec(self, name, ap_kc):
        t = self.mk.sb(name, [128, KC], F32)
        self.mk.dma("sp", t.all(), ap_kc)
        return t


def vec_pk(v):
    return np.ascontiguousarray(v.reshape(-1, 128).T)


def run_mod(c, c_ctx, mod_w, mod_b):
    P = Prog("mod")
    mk = P.mk
    NCOL = 12288 // NCORES
    cT = P.inp("cT", [128, KC, 3])
    w = P.inp("w", [4, D, NCOL])
    b = P.inp("b", [3, 4, NCOL])
    out = P.outp("out", [3, 4, NCOL])
    cs = mk.sb("cs", [128, KC, 3], F32)
    sT = mk.sb("sT", [128, KC, 3], F32)
    mk.dma("sp", cs.all(), cT)
    mk.act(sT.all(), cs.all(), AF.Silu)
    bs = mk.sb("bs", [3, 4, NCOL], F32)
    mk.dma("sp", bs.all(), b)
    os_ = mk.sb("os", [3, 4, NCOL], F32)
    wb = [mk.sb(f"wb{i}", [128, KC, 512], F32) for i in range(2)]
    it = 0
    for l in range(4):
        for n0 in range(0, NCOL, 512):
            wt = wb[it % 2]
            it += 1
            src = w[l, :, n0:n0 + 512].rearrange("(kc p) n -> p kc n", p=128)
            for k0 in range(0, KC, 4):
                mk.dma("sp" if (k0 // 4) % 2 == 0 else "act", wt[:, k0:k0 + 4, :], src[:, k0:k0 + 4, :])
            ps = P.psum()
            for kc in range(KC):
                mk.matmul(ps[0:3, :], sT[:, kc, :], wt[:, kc, :], start=(kc == 0), stop=(kc == KC - 1))
            mk.tt("dve", os_[:, l, n0:n0 + 512], ps[0:3, :], bs[:, l, n0:n0 + 512], ALU.add)
    mk.dma("sp", out, os_.all())
    cstack = np.stack([c[0], c[1], c_ctx], axis=1)
    cT_np = np.ascontiguousarray(cstack.reshape(KC, 128, 3).transpose(1, 0, 2))
    in_maps = []
    for core in range(NCORES):
        sl = slice(core * NCOL, (core + 1) * NCOL)
        in_maps.append({"cT": cT_np, "w": np.ascontiguousarray(mod_w[:, :, sl]),
                        "b": np.ascontiguousarray(np.broadcast_to(mod_b[None, :, sl], (3, 4, NCOL)))})
    res = P.run(in_maps)
    mod = np.concatenate([r["out"] for r in res], axis=2)
    return mod


def mod_layout(mod, layer, b, with_ctx=True):
    rows = [b, 2] if with_ctx else [b]
    m = mod[rows, layer]
    m = m.reshape(len(rows), 6, KC, 128).transpose(3, 0, 1, 2)
    return np.ascontiguousarray(m)


def norm_mod_stream(P, xT_d, T, segs, normw_t, mod_t, ish, isc, hT):
    mk = P.mk
    nseg = mod_t.shape[1]
    a_t = mk.sb("nm_a", [128, nseg, KC], F32)
    for s in range(nseg):
        mk.stt(a_t[:, s, :], mod_t[:, s, isc, :], 1.0, normw_t.all(), ALU.add, ALU.mult)
    xb = [mk.sb(f"nm_xb{i}", [128, T], F32) for i in range(2)]
    sq = [mk.sb(f"nm_sq{i}", [128, 512], BF16) for i in range(3)]
    rstd = mk.sb("nm_rstd", [128, T], F32)
    tls = tiles_of(T)
    pss = [P.psum() for _ in tls]
    xsrc = xT_d.rearrange("(kc p) t -> p kc t", p=128)
    for kc in range(KC):
        x = xb[kc % 2]
        mk.dma("sp" if kc % 2 == 0 else "act", x.all(), xsrc[:, kc, :])
        for ti, (t0, tn) in enumerate(tls):
            s_ = sq[(kc * len(tls) + ti) % 3]
            mk.act(s_[:, 0:tn], x[:, t0:t0 + tn], AF.Square)
            mk.matmul(pss[ti][:, 0:tn], P.ones.all(), s_[:, 0:tn], start=(kc == 0), stop=(kc == KC - 1))
    for ti, (t0, tn) in enumerate(tls):
        P.rstd_of(rstd[:, t0:t0 + tn], pss[ti][:, 0:tn], D)
    for kc in range(KC):
        x = xb[kc % 2]
        mk.dma("sp" if kc % 2 == 0 else "act", x.all(), xsrc[:, kc, :])
        mk.tt("dve", x.all(), x.all(), rstd.all(), ALU.mult)
        for (t0, tn, s) in segs:
            mk.act(hT[:, kc, t0:t0 + tn], x[:, t0:t0 + tn], AF.Identity,
                   bias=mod_t[:, s, ish, kc:kc + 1], scale=a_t[:, s, kc:kc + 1])


def proj_fm(P, hT, T, w_ap, col0, ncols, out_ap, out_dt, wbufs, stg, nk=KC, post=None):
    mk = P.mk
    tls = tiles_of(T)
    bi = 0
    for b0 in range(0, ncols, 512):
        bn = min(512, ncols - b0)
        wt = wbufs[P.wq % len(wbufs)]
        P.wq += 1
        P.load_w(wt, w_ap, col0 + b0, bn, nk)
        for n0 in range(0, bn, 128):
            st = stg[bi % len(stg)]
            bi += 1
            for (t0, tn) in tls:
                ps = P.psum()
                for kc in range(nk):
                    mk.matmul(ps[:, 0:tn], wt[:, kc, n0:n0 + 128], hT[:, kc, t0:t0 + tn],
                              start=(kc == 0), stop=(kc == nk - 1))
                if post is None:
                    mk.evac(st[:, t0:t0 + tn], ps[:, 0:tn])
                else:
                    post(st, ps, b0 + n0, t0, tn)
            mk.dma("sp", out_ap[b0 + n0:b0 + n0 + 128, :], st[:, 0:T])


def proj_tm(P, hT, T, w_ap, col0, ncols, out_ap, wbufs, stg, nk=KC):
    mk = P.mk
    bi = 0
    for b0 in range(0, ncols, 512):
        bn = min(512, ncols - b0)
        wt = wbufs[P.wq % len(wbufs)]
        P.wq += 1
        P.load_w(wt, w_ap, col0 + b0, bn, nk)
        for t0 in range(0, T, 128):
            st = stg[bi % len(stg)]
            bi += 1
            ps = P.psum()
            for kc in range(nk):
                mk.matmul(ps[:, 0:bn], hT[:, kc, t0:t0 + 128], wt[:, kc, 0:bn], start=(kc == 0), stop=(kc == nk - 1))
            mk.evac(st[:, 0:bn], ps[:, 0:bn])
            mk.dma("sp", out_ap[t0:t0 + 128, b0:b0 + bn], st[:, 0:bn])


def run_A(xT_list, modT_list, normw, w_in, specs, nctx):
    T = NLAT + nctx
    N = w_in.shape[1]
    P = Prog("A")
    mk = P.mk
    xT_d = P.inp("xT", [D, T])
    modT_d = P.inp("modT", [128, 2 if nctx else 1, 6, KC])
    nw_d = P.inp("nw", [128, KC])
    w_d = P.inp("w", [D, N])
    mod_t = P.load_mod(modT_d, 2 if nctx else 1)
    nw_t = P.load_vec("nw_t", nw_d)
    hT = mk.sb("hT", [128, KC, T], BF16)
    segs = [(0, NLAT, 0)] + ([(NLAT, nctx, 1)] if nctx else [])
    norm_mod_stream(P, xT_d, T, segs, nw_t, mod_t, 0, 1, hT)
    wbufs = [mk.sb(f"wbuf{i}", [128, KC, 512], BF16) for i in range(2)]
    stg_fm_b = [mk.sb(f"sfb{i}", [128, T], BF16) for i in range(3)]
    stg_fm_f = [mk.sb(f"sff{i}", [128, T], F32) for i in range(3)]
    stg_tm_b = [mk.sb(f"stb{i}", [128, 512], BF16) for i in range(3)]
    stg_tm_f = [mk.sb(f"stf{i}", [128, 512], F32) for i in range(3)]
    for (name, col0, ncols, lay, dt) in specs:
        bdt = BF16 if dt == "bf16" else F32
        if lay == "fm":
            o = P.outp(name, [ncols, T], bdt)
            proj_fm(P, hT, T, w_d, col0, ncols, o, bdt, wbufs, stg_fm_b if dt == "bf16" else stg_fm_f)
        else:
            o = P.outp(name, [T, ncols], bdt)
            proj_tm(P, hT, T, w_d, col0, ncols, o, wbufs, stg_tm_b if dt == "bf16" else stg_tm_f)
    nwk = vec_pk(normw)
    in_maps = [{"xT": xT_list[c], "modT": modT_list[c], "nw": nwk, "w": w_in} for c in range(NCORES)]
    return P.run(in_maps)


def run_C1(oT_list, xT_list, modT_list, normw, w_out, bias, nctx):
    T = NLAT + nctx
    nseg = 2 if nctx else 1
    P = Prog("C1")
    mk = P.mk
    oT_d = P.inp("oT", [D, T], BF16)
    xT_d = P.inp("xT", [D, T])
    modT_d = P.inp("modT", [128, nseg, 6, KC])
    nw_d = P.inp("nw", [128, KC])
    b_d = P.inp("bias", [128, KC])
    w_d = P.inp("w", [D, D])
    out_d = P.outp("xmid", [D, T])
    mod_t = P.load_mod(modT_d, nseg)
    nw_t = P.load_vec("nw_t", nw_d)
    b_t = P.load_vec("b_t", b_d)
    gw = mk.sb("gw", [128, nseg, KC], F32)
    for s in range(nseg):
        mk.tt("dve", gw[:, s, :], mod_t[:, s, 2, :], nw_t.all(), ALU.mult)
    wres = mk.sb("wres", [128, KC, D], BF16)
    for b0 in range(0, D, 512):
        src = w_d[:, b0:b0 + 512].rearrange("(kc p) n -> p kc n", p=128)
        for k0 in range(0, KC, 4):
            mk.dma("pool", wres[:, k0:k0 + 4, b0:b0 + 512], src[:, k0:k0 + 4, :])
    ob = [mk.sb(f"ob{i}", [128, KC, 512], BF16) for i in range(2)]
    yT = mk.sb("yT", [128, KC, 512], F32)
    sq = [mk.sb(f"sq{i}", [128, 512], BF16) for i in range(2)]
    rstd = mk.sb("rstd", [128, 512], F32)
    xb = [mk.sb(f"xb{i}", [128, 512], F32) for i in range(3)]
    osrc = oT_d.rearrange("(kc p) t -> p kc t", p=128)
    xsrc = xT_d.rearrange("(kc p) t -> p kc t", p=128)
    odst = out_d.rearrange("(kc p) t -> p kc t", p=128)
    tls = [(0, 512, 0), (512, 512, 0)] + ([(NLAT, nctx, 1)] if nctx else [])
    for ti, (t0, tn, seg) in enumerate(tls):
        o = ob[ti % 2]
        for k0 in range(0, KC, 4):
            mk.dma("sp", o[:, k0:k0 + 4, 0:tn], osrc[:, k0:k0 + 4, t0:t0 + tn])
        pss = P.psum()
        for n in range(KC):
            ps = P.psum()
            if ps is pss:
                ps = P.psum()
            for kc in range(KC):
                mk.matmul(ps[:, 0:tn], wres[:, kc, n * 128:(n + 1) * 128], o[:, kc, 0:tn],
                          start=(kc == 0), stop=(kc == KC - 1))
            mk.act(yT[:, n, 0:tn], ps[:, 0:tn], AF.Identity, bias=b_t[:, n:n + 1])
            s_ = sq[n % 2]
            mk.act(s_[:, 0:tn], yT[:, n, 0:tn], AF.Square)
            mk.matmul(pss[:, 0:tn], P.ones.all(), s_[:, 0:tn], start=(n == 0), stop=(n == KC - 1))
        P.rstd_of(rstd[:, 0:tn], pss[:, 0:tn], D)
        for n in range(KC):
            x = xb[n % 3]
            mk.dma("act", x[:, 0:tn], xsrc[:, n, t0:t0 + tn])
            mk.tt("dve", yT[:, n, 0:tn], yT[:, n, 0:tn], rstd[:, 0:tn], ALU.mult)
            mk.stt(x[:, 0:tn], yT[:, n, 0:tn], gw[:, seg, n:n + 1], x[:, 0:tn], ALU.mult, ALU.add)
            mk.dma("sp", odst[:, n, t0:t0 + tn], x[:, 0:tn])
    nwk = vec_pk(normw)
    bk = vec_pk(bias) if bias is not None else np.zeros((128, KC), np.float32)
    in_maps = [{"oT": oT_list[c], "xT": xT_list[c], "modT": modT_list[c], "nw": nwk, "bias": bk, "w": w_out}
               for c in range(NCORES)]
    return [r["xmid"] for r in P.run(in_maps)]


def run_C2(xe_list, mask_list, modT_list, nw_pre, nw_post, w_up, conv_w, conv_b, w_down, groups):
    ng = len(groups)
    nseg = 1 + max(s for _, s in groups)
    Te = sum(g + 2 for g, _ in groups)
    To = sum(g for g, _ in groups)
    NJ = DFF // 128
    P = Prog("C2")
    mk = P.mk
    xe_d = P.inp("xe", [D, Te])
    mask_d = P.inp("mask", [128, KC, 2 * ng])
    modT_d = P.inp("modT", [128, nseg, 6, KC])
    nw1_d = P.inp("nw1", [128, KC])
    nw2_d = P.inp("nw2", [128, KC])
    wu_d = P.inp("wu", [D, 2 * DFF])
    cw_d = P.inp("cw", [128, 2 * NJ, 3])
    cb_d = P.inp("cb", [128, 2 * NJ])
    wd_d = P.inp("wd", [DFF, D])
    out_d = P.outp("xo", [D, To])
    mod_t = P.load_mod(modT_d, nseg)
    nw1 = P.load_vec("nw1_t", nw1_d)
    nw2 = P.load_vec("nw2_t", nw2_d)
    mask_t = mk.sb("mask_t", [128, KC, 2 * ng], F32)
    mk.dma("sp", mask_t.all(), mask_d)
    cw = mk.sb("cw_t", [128, 2 * NJ, 3], F32)
    cb = mk.sb("cb_t", [128, 2 * NJ], F32)
    mk.dma("sp", cw.all(), cw_d)
    mk.dma("sp", cb.all(), cb_d)
    gw = mk.sb("gw", [128, nseg, KC], F32)
    for s in range(nseg):
        mk.tt("dve", gw[:, s, :], mod_t[:, s, 5, :], nw2.all(), ALU.mult)
    hT = mk.sb("hT", [128, KC, Te], BF16)
    segs = []
    o = 0
    for (G, s) in groups:
        segs.append((o, G + 2, s))
        o += G + 2
    norm_mod_stream(P, xe_d, Te, segs, nw1, mod_t, 3, 4, hT)
    o = 0
    for gi, (G, s) in enumerate(groups):
        mk.tt("dve", hT[:, :, o:o + 1], hT[:, :, o:o + 1], mask_t[:, :, 2 * gi:2 * gi + 1], ALU.mult)
        mk.tt("dve", hT[:, :, o + G + 1:o + G + 2], hT[:, :, o + G + 1:o + G + 2],
              mask_t[:, :, 2 * gi + 1:2 * gi + 2], ALU.mult)
        o += G + 2
    GM = max(g for g, _ in groups)
    actT = mk.sb("actT", [128, NJ, GM], BF16)
    wub = [mk.sb(f"wub{i}", [128, KC, 2, 128], BF16) for i in range(2)]
    ub = [mk.sb(f"ub{i}", [128, GM + 2], F32) for i in range(4)]
    cvb = [mk.sb(f"cvb{i}", [128, GM], F32) for i in range(4)]
    wdb = [mk.sb(f"wdb{i}", [128, NJ, 128], BF16) for i in range(2)]
    yT = mk.sb("yT", [128, KC, GM], F32)
    sq = [mk.sb(f"sq{i}", [128, 512], BF16) for i in range(2)]
    rstd = mk.sb("rstd", [128, 512], F32)
    xb = [mk.sb(f"xb{i}", [128, GM], F32) for i in range(3)]
    xsrc = xe_d.rearrange("(kc p) t -> p kc t", p=128)
    odst = out_d.rearrange("(kc p) t -> p kc t", p=128)
    wusrc = wu_d.rearrange("(kc p) n -> p kc n", p=128)
    wdsrc = wd_d.rearrange("(j p) n -> p j n", p=128)
    eo = 0
    oo = 0
    it = 0
    ui = 0
    for gi, (G, s) in enumerate(groups):
        Gx = G + 2
        tls = tiles_of(Gx)
        for j0 in range(0, NJ, 1):
            wt = wub[it % 2]
            it += 1
            for half, cbase in ((0, j0 * 128), (1, DFF + j0 * 128)):
                mk.dma("pool", wt[:, :, half, :], wusrc[:, :, cbase:cbase + 128])
            for jj in range(1):
                j = j0 + jj
                us = []
                for half in range(2):
                    u = ub[ui % 4]
                    ui += 1
                    for (t0, tn) in tls:
                        ps = P.psum()
                        for kc in range(KC):
                            mk.matmul(ps[:, 0:tn], wt[:, kc, half, jj * 128:(jj + 1) * 128],
                                      hT[:, kc, eo + t0:eo + t0 + tn], start=(kc == 0), stop=(kc == KC - 1))
                        mk.copy("act" if half == 0 else "dve", u[:, t0:t0 + tn], ps[:, 0:tn])
                    us.append(u)
                cs = []
                for half in range(2):
                    ch = half * NJ + j
                    u = us[half]
                    cv = cvb[(2 * j + half) % 4]
                    mk.act(cv[:, 0:G], u[:, 1:G + 1], AF.Identity, bias=cb[:, ch:ch + 1], scale=cw[:, ch, 1:2])
                    mk.stt(cv[:, 0:G], u[:, 0:G], cw[:, ch, 0:1], cv[:, 0:G], ALU.mult, ALU.add)
                    mk.stt(cv[:, 0:G], u[:, 2:G + 2], cw[:, ch, 2:3], cv[:, 0:G], ALU.mult, ALU.add)
                    cs.append(cv)
                mk.act(cs[1][:, 0:G], cs[1][:, 0:G], AF.Silu)
                mk.tt("dve", actT[:, j, 0:G], cs[0][:, 0:G], cs[1][:, 0:G], ALU.mult)
        pss = P.psum()
        for n0 in range(0, KC, 1):
            wd = wdb[n0 % 2]
            for j0 in range(0, NJ, 11):
                mk.dma("pool", wd[:, j0:j0 + 11, :], wdsrc[:, j0:j0 + 11, n0 * 128:n0 * 128 + 128])
            for nn in range(1):
                n = n0 + nn
                ps = P.psum()
                if ps is pss:
                    ps = P.psum()
                for j in range(NJ):
                    mk.matmul(ps[:, 0:G], wd[:, j, nn * 128:(nn + 1) * 128], actT[:, j, 0:G],
                              start=(j == 0), stop=(j == NJ - 1))
                mk.copy("act", yT[:, n, 0:G], ps[:, 0:G])
                s_ = sq[n % 2]
                mk.act(s_[:, 0:G], yT[:, n, 0:G], AF.Square)
                mk.matmul(pss[:, 0:G], P.ones.all(), s_[:, 0:G], start=(n == 0), stop=(n == KC - 1))
        P.rstd_of(rstd[:, 0:G], pss[:, 0:G], D)
        for n in range(KC):
            x = xb[n % 3]
            mk.dma("act", x[:, 0:G], xsrc[:, n, eo + 1:eo + 1 + G])
            mk.tt("dve", yT[:, n, 0:G], yT[:, n, 0:G], rstd[:, 0:G], ALU.mult)
            mk.stt(x[:, 0:G], yT[:, n, 0:G], gw[:, s, n:n + 1], x[:, 0:G], ALU.mult, ALU.add)
            mk.dma("sp", odst[:, n, oo:oo + G], x[:, 0:G])
        eo += Gx
        oo += G
    cwk = np.ascontiguousarray(conv_w.T.reshape(2 * NJ, 128, 3).transpose(1, 0, 2))
    cbk = np.ascontiguousarray(conv_b.reshape(2 * NJ, 128).T)
    in_maps = [{"xe": xe_list[c], "mask": mask_list[c], "modT": modT_list[c], "nw1": vec_pk(nw_pre),
                "nw2": vec_pk(nw_post), "wu": w_up, "cw": cwk, "cb": cbk, "wd": w_down} for c in range(NCORES)]
    return [r["xo"] for r in P.run(in_maps)]


def ffn_host_io(xmid_lat, xmid_ctx, nctx):
    xe_list, mask_list = [], []
    groups = [(512, 0), (512, 0)] + ([(nctx, 1)] if nctx else [])
    for c in range(NCORES):
        b, q = c // 4, c % 4
        cols = []
        m = []
        for g in range(2):
            p0 = q * NLAT + g * 512
            blk = np.zeros((514, D), np.float32)
            lo, hi = p0 - 1, p0 + 513
            slo, shi = max(lo, 0), min(hi, SEQ)
            blk[slo - lo:shi - lo] = xmid_lat[b, slo:shi]
            cols.append(blk)
            m += [1.0 if lo >= 0 else 0.0, 1.0 if hi <= SEQ else 0.0]
        if nctx:
            blk = np.zeros((nctx + 2, D), np.float32)
            blk[1:nctx + 1] = xmid_ctx[b]
            cols.append(blk)
            m += [0.0, 0.0]
        xe = np.ascontiguousarray(np.concatenate(cols, axis=0).T)
        xe_list.append(xe)
        mask_list.append(np.ascontiguousarray(np.broadcast_to(np.array(m, np.float32)[None, None, :], (128, KC, len(m)))))
    return xe_list, mask_list, groups


def rope_tables():
    half = 32
    freqs = 10000.0 ** (-np.arange(half, dtype=np.float32) / half)
    pos = np.arange(SEQ)
    row, col = pos // 64, pos % 64
    ang = np.zeros((128, SEQ), np.float32)
    for d in range(128):
        p = row if d < 64 else col
        ang[d] = p.astype(np.float32) * freqs[(d % 64) % 32]
    R = np.zeros((128, 128), np.float32)
    for d in range(128):
        if (d % 64) < 32:
            R[d + 32, d] = -1.0
        else:
            R[d - 32, d] = 1.0
    return np.cos(ang).astype(np.float32), np.sin(ang).astype(np.float32), R.astype(NPBF)


def run_B_gqa(qT_list, kT_list, v_list, q_norm, k_norm):
    T = NLAT + NCTX
    NK = SEQ + NCTX
    NCH = NK // 128
    P = Prog("Bgqa")
    mk = P.mk
    qT_d = P.inp("qT", [D, T], BF16)
    kT_d = P.inp("kT", [512, NK], BF16)
    v_d = P.inp("v", [NK, 512], BF16)
    gn_d = P.inp("gn", [128, 2])
    cos_d = P.inp("cos", [128, SEQ])
    sin_d = P.inp("sin", [128, SEQ])
    cosq_d = P.inp("cosq", [128, NLAT])
    sinq_d = P.inp("sinq", [128, NLAT])
    R_d = P.inp("R", [128, 128], BF16)
    oT_d = P.outp("oT", [D, T], BF16)
    gn = mk.sb("gn", [128, 2], F32)
    mk.dma("sp", gn.all(), gn_d)
    mk.ts("dve", gn[:, 0:1], gn[:, 0:1], 128.0 ** -0.5, ALU.mult)
    Rm = mk.sb("Rm", [128, 128], BF16)
    mk.dma("sp", Rm.all(), R_d)
    cosk = mk.sb("cosk", [128, SEQ], F32)
    sink = mk.sb("sink", [128, SEQ], F32)
    cosq = mk.sb("cosq", [128, NLAT], F32)
    sinq = mk.sb("sinq", [128, NLAT], F32)
    mk.dma("sp", cosk.all(), cos_d)
    mk.dma("act", sink.all(), sin_d)
    mk.dma("sp", cosq.all(), cosq_d)
    mk.dma("act", sinq.all(), sinq_d)
    kT = mk.sb("kT", [128, 4, NK], BF16)
    mk.dma("sp", kT.all(), kT_d.rearrange("(h p) t -> p h t", p=128))
    qT = mk.sb("qT", [128, 16, T], BF16)
    for h0 in range(0, 16, 4):
        mk.dma("act", qT[:, h0:h0 + 4, :], qT_d.rearrange("(h p) t -> p h t", p=128)[:, h0:h0 + 4, :])
    vt = mk.sb("vt", [128, NCH, 512], BF16)
    vsrc = v_d.rearrange("(c p) n -> p c n", p=128)
    for c0 in range(0, NCH, 17):
        mk.dma("sp", vt[:, c0:c0 + 17, :], vsrc[:, c0:c0 + 17, :])
    sqb = [mk.sb(f"sqb{i}", [128, 512], BF16) for i in range(2)]
    rsb = [mk.sb(f"rsb{i}", [128, 512], F32) for i in range(2)]
    knb = [mk.sb(f"knb{i}", [128, 512], BF16) for i in range(2)]
    t1b = [mk.sb(f"t1b{i}", [128, 512], F32) for i in range(2)]
    t2b = [mk.sb(f"t2b{i}", [128, 512], F32) for i in range(2)]
    cnt = [0]

    def normrope(buf, h, t0, tn, gcol, cs, sn, c0):
        i = cnt[0] % 2
        cnt[0] += 1
        x = buf[:, h, t0:t0 + tn]
        mk.act(sqb[i][:, 0:tn], x, AF.Square)
        ps = P.psum()
        mk.matmul(ps[:, 0:tn], P.ones.all(), sqb[i][:, 0:tn])
        P.rstd_of(rsb[i][:, 0:tn], ps[:, 0:tn], 128)
        if cs is None:
            mk.stt(x, x, gcol, rsb[i][:, 0:tn], ALU.mult, ALU.mult)
            return
        mk.stt(knb[i][:, 0:tn], x, gcol, rsb[i][:, 0:tn], ALU.mult, ALU.mult)
        ps2 = P.psum()
        mk.matmul(ps2[:, 0:tn], Rm.all(), knb[i][:, 0:tn])
        mk.tt("pool", t1b[i][:, 0:tn], knb[i][:, 0:tn], cs[:, c0:c0 + tn], ALU.mult)
        mk.tt("dve", t2b[i][:, 0:tn], ps2[:, 0:tn], sn[:, c0:c0 + tn], ALU.mult)
        mk.tt("pool", x, t1b[i][:, 0:tn], t2b[i][:, 0:tn], ALU.add)

    for kv in range(4):
        for t0 in range(0, SEQ, 512):
            normrope(kT, kv, t0, 512, gn[:, 1:2], cosk, sink, t0)
        normrope(kT, kv, SEQ, NCTX, gn[:, 1:2], None, None, 0)
    pT = [mk.sb(f"pT{i}", [128, 512], BF16) for i in range(3)]
    rcp = [mk.sb(f"rcp{i}", [128, 512], F32) for i in range(2)]
    ost = [mk.sb(f"ost{i}", [128, T], BF16) for i in range(2)]
    pi = 0
    acc_i = 0
    for h in range(16):
        kv = h // 4
        for t0 in (0, 512):
            normrope(qT, h, t0, 512, gn[:, 0:1], cosq, sinq, t0)
        normrope(qT, h, NLAT, NCTX, gn[:, 0:1], None, None, 0)
        st = ost[h % 2]
        for (t0, tn, chunks) in ((0, 512, range(NCH)), (512, 512, range(NCH)), (NLAT, NCTX, range(32, NCH))):
            ps_o = P.psb[4 + 2 * (acc_i % 2)]
            ps_s = P.psb[5 + 2 * (acc_i % 2)]
            acc_i += 1
            chunks = list(chunks)
            for ci, c in enumerate(chunks):
                ps = P.psb[pi % 4]
                p_ = pT[pi % 3]
                pi += 1
                mk.matmul(ps[:, 0:tn], kT[:, kv, c * 128:(c + 1) * 128], qT[:, h, t0:t0 + tn])
                mk.act(p_[:, 0:tn], ps[:, 0:tn], AF.Exp)
                mk.matmul(ps_o[:, 0:tn], vt[:, c, kv * 128:(kv + 1) * 128], p_[:, 0:tn],
                          start=(ci == 0), stop=(ci == len(chunks) - 1))
                mk.matmul(ps_s[:, 0:tn], P.ones.all(), p_[:, 0:tn], start=(ci == 0), stop=(ci == len(chunks) - 1))
            r = rcp[acc_i % 2]
            mk.recip(r[:, 0:tn], ps_s[:, 0:tn])
            mk.tt("dve", st[:, t0:t0 + tn], ps_o[:, 0:tn], r[:, 0:tn], ALU.mult)
        mk.dma("sp", oT_d[h * 128:(h + 1) * 128, :], st.all())
    P.ps_i = 0
    cos, sin, R = rope_tables()
    gnk = np.ascontiguousarray(np.stack([q_norm, k_norm], axis=1).astype(np.float32))
    in_maps = []
    for c in range(NCORES):
        q = c % 4
        in_maps.append({"qT": qT_list[c], "kT": kT_list[c], "v": v_list[c], "gn": gnk, "cos": cos, "sin": sin,
                        "cosq": np.ascontiguousarray(cos[:, q * NLAT:(q + 1) * NLAT]),
                        "sinq": np.ascontiguousarray(sin[:, q * NLAT:(q + 1) * NLAT]), "R": R})
    return [r["oT"] for r in P.run(in_maps)]


def to_fm(x_tok):
    return np.ascontiguousarray(x_tok.T)


def core_xT(x_lat, x_ctx, nctx):
    out = []
    for c in range(NCORES):
        b, q = c // 4, c % 4
        parts = [x_lat[b, q * NLAT:(q + 1) * NLAT]]
        if nctx:
            parts.append(x_ctx[b])
        out.append(to_fm(np.concatenate(parts, axis=0)))
    return out


def layer_gqa(x_lat, x_ctx, mod, L, inp):
    modT = [mod_layout(mod, L, c // 4) for c in range(NCORES)]
    xT = core_xT(x_lat, x_ctx, NCTX)
    specs = [("qT", 0, 2048, "fm", "bf16"), ("kT", 2048, 512, "fm", "bf16"), ("v", 2560, 512, "tm", "bf16")]
    ra = run_A(xT, modT, inp["norm_pre_mix"][L], inp["gqa_w_in"][0], specs, NCTX)
    kT_list, v_list = [], []
    for c in range(NCORES):
        b = c // 4
        kT_list.append(np.ascontiguousarray(np.concatenate(
            [ra[4 * b + i]["kT"][:, 0:NLAT] for i in range(4)] + [ra[4 * b]["kT"][:, NLAT:]], axis=1)))
        v_list.append(np.ascontiguousarray(np.concatenate(
            [ra[4 * b + i]["v"][0:NLAT] for i in range(4)] + [ra[4 * b]["v"][NLAT:]], axis=0)))
    oT = run_B_gqa([r["qT"] for r in ra], kT_list, v_list, inp["gqa_q_norm"][0], inp["gqa_k_norm"][0])
    xmid = run_C1(oT, xT, modT, inp["norm_post_mix"][L], inp["gqa_w_out"][0], None, NCTX)
    return xmid


def split_xT(xT_list, nctx):
    x_lat = np.zeros((2, SEQ, D), np.float32)
    x_ctx = np.zeros((2, nctx, D), np.float32) if nctx else None
    for c in range(NCORES):
        b, q = c // 4, c % 4
        x_lat[b, q * NLAT:(q + 1) * NLAT] = xT_list[c][:, 0:NLAT].T
        if nctx and q == 0:
            x_ctx[b] = xT_list[c][:, NLAT:NLAT + nctx].T
    return x_lat, x_ctx


def layer_ffn(xmid_lat, xmid_ctx, mod, L, inp, nctx):
    modT = [mod_layout(mod, L, c // 4, with_ctx=bool(nctx)) for c in range(NCORES)]
    xe_list, mask_list, groups = ffn_host_io(xmid_lat, xmid_ctx, nctx)
    xo = run_C2(xe_list, mask_list, modT, inp["norm_pre_ffn"][L], inp["norm_post_ffn"][L], inp["ffn_w_up"][L],
                inp["ffn_conv_w"][L], inp["ffn_conv_b"][L], inp["ffn_w_down"][L], groups)
    return split_xT(xo, nctx)


def run_B_conv(zT_list, mask_list, b_pw1, w_dw, b_dw, ln_w, ln_b, nctx):
    HW = 15
    Le = NLAT + 2 * HW
    Ce = nctx + 2 * HW
    Te = Le + Ce
    T = NLAT + nctx
    P = Prog("Bconv")
    mk = P.mk
    zT_d = P.inp("zT", [2 * D, Te])
    mask_d = P.inp("mask", [128, Te])
    bp_d = P.inp("bp", [128, 2 * KC])
    wdw_d = P.inp("wdw", [128, KC, 31])
    bdw_d = P.inp("bdw", [128, KC])
    lnw_d = P.inp("lnw", [128, KC])
    lnb_d = P.inp("lnb", [128, KC])
    sT_d = P.outp("sT", [D, T], BF16)
    maskt = mk.sb("maskt", [128, Te], F32)
    mk.dma("sp", maskt.all(), mask_d)
    bp = mk.sb("bp", [128, 2 * KC], F32)
    mk.dma("sp", bp.all(), bp_d)
    wdw = mk.sb("wdw", [128, KC, 31], F32)
    mk.dma("sp", wdw.all(), wdw_d)
    bdw = P.load_vec("bdw", bdw_d)
    lnw = P.load_vec("lnw", lnw_d)
    lnb = P.load_vec("lnb", lnb_d)
    onesf = mk.sb("onesf", [128, 128], F32)
    mk.memset("dve", onesf.all(), 1.0)
    vT = mk.sb("vT", [128, KC, T], F32)
    zb = [mk.sb(f"zb{i}", [128, Te], F32) for i in range(4)]
    ub = [mk.sb(f"ub{i}", [128, Te], F32) for i in range(2)]
    sqf = [mk.sb(f"sqf{i}", [128, 512], F32) for i in range(2)]
    zsrc = zT_d.rearrange("(kc p) t -> p kc t", p=128)
    tls = tiles_of(T)
    ps_sum = [P.psb[i] for i in range(len(tls))]
    ps_sq = [P.psb[3 + i] for i in range(len(tls))]
    for kc in range(KC):
        za = zb[(2 * kc) % 4]
        zg = zb[(2 * kc + 1) % 4]
        mk.dma("sp", za.all(), zsrc[:, kc, :])
        mk.dma("act", zg.all(), zsrc[:, KC + kc, :])
        mk.act(zg.all(), zg.all(), AF.Sigmoid, bias=bp[:, KC + kc:KC + kc + 1])
        mk.tt("pool", zg.all(), zg.all(), maskt.all(), ALU.mult)
        u = ub[kc % 2]
        mk.stt(u.all(), za.all(), bp[:, kc:kc + 1], zg.all(), ALU.add, ALU.mult)
        for (e0, o0, n) in ((0, 0, NLAT), (Le, NLAT, nctx)):
            if n == 0:
                continue
            ov = vT[:, kc, o0:o0 + n]
            mk.ts("dve", ov, u[:, e0:e0 + n], wdw[:, kc, 0:1], ALU.mult, bdw[:, kc:kc + 1], ALU.add)
            for j in range(1, 31):
                mk.stt(ov, u[:, e0 + j:e0 + j + n], wdw[:, kc, j:j + 1], ov, ALU.mult, ALU.add)
        for ti, (t0, tn) in enumerate(tls):
            s_ = sqf[(kc * len(tls) + ti) % 2]
            mk.act(s_[:, 0:tn], vT[:, kc, t0:t0 + tn], AF.Square)
            mk.matmul(ps_sum[ti][:, 0:tn], onesf.all(), vT[:, kc, t0:t0 + tn], start=(kc == 0), stop=(kc == KC - 1))
            mk.matmul(ps_sq[ti][:, 0:tn], onesf.all(), s_[:, 0:tn], start=(kc == 0), stop=(kc == KC - 1))
    mean = mk.sb("mean", [128, T], F32)
    rstd = mk.sb("rstd", [128, T], F32)
    msq = mk.sb("msq", [128, T], F32)
    for ti, (t0, tn) in enumerate(tls):
        mk.ts("dve", mean[:, t0:t0 + tn], ps_sum[ti][:, 0:tn], 1.0 / D, ALU.mult)
        mk.tt("dve", msq[:, t0:t0 + tn], mean[:, t0:t0 + tn], mean[:, t0:t0 + tn], ALU.mult)
        mk.stt(msq[:, t0:t0 + tn], ps_sq[ti][:, 0:tn], 1.0 / D, msq[:, t0:t0 + tn], ALU.mult, ALU.subtract)
        mk.act(rstd[:, t0:t0 + tn], msq[:, t0:t0 + tn], AF.Sqrt, bias=P.eps_t[:, 0:1])
        mk.recip(rstd[:, t0:t0 + tn], rstd[:, t0:t0 + tn])
    sb_ = [mk.sb(f"sbo{i}", [128, T], BF16) for i in range(2)]
    tmp = [mk.sb(f"tmpn{i}", [128, T], F32) for i in range(2)]
    for kc in range(KC):
        t = tmp[kc % 2]
        mk.tt("pool", t.all(), vT[:, kc, :], mean.all(), ALU.subtract)
        mk.tt("dve", t.all(), t.all(), rstd.all(), ALU.mult)
        so = sb_[kc % 2]
        mk.act(so.all(), t.all(), AF.Silu, bias=lnb[:, kc:kc + 1], scale=lnw[:, kc:kc + 1])
        mk.dma("sp", sT_d[kc * 128:(kc + 1) * 128, :], so.all())
    bpk = np.ascontiguousarray(b_pw1.reshape(2 * KC, 128).T)
    wdwk = np.ascontiguousarray(w_dw.T.reshape(KC, 128, 31).transpose(1, 0, 2))
    in_maps = [{"zT": zT_list[c], "mask": mask_list[c], "bp": bpk, "wdw": wdwk, "bdw": vec_pk(b_dw),
                "lnw": vec_pk(ln_w), "lnb": vec_pk(ln_b)} for c in range(NCORES)]
    return [r["sT"] for r in P.run(in_maps)]


def layer_conv(x_lat, x_ctx, mod, L, inp):
    HW = 15
    modT = [mod_layout(mod, L, c // 4) for c in range(NCORES)]
    xT = core_xT(x_lat, x_ctx, NCTX)
    ra = run_A(xT, modT, inp["norm_pre_mix"][L], inp["conv_w_pw1"][0], [("zT", 0, 2 * D, "fm", "f32")], NCTX)
    zT_list, mask_list = [], []
    for b in range(2):
        zl = np.concatenate([ra[4 * b + i]["zT"][:, 0:NLAT] for i in range(4)], axis=1)
        zl = np.pad(zl, ((0, 0), (HW, HW)))
        ml = np.pad(np.ones(SEQ, np.float32), (HW, HW))
        zc = np.pad(ra[4 * b]["zT"][:, NLAT:], ((0, 0), (HW, HW)))
        mc = np.pad(np.ones(NCTX, np.float32), (HW, HW))
        for q in range(4):
            sl = slice(q * NLAT, q * NLAT + NLAT + 2 * HW)
            zT_list.append(np.ascontiguousarray(np.concatenate([zl[:, sl], zc], axis=1)))
            m = np.concatenate([ml[sl], mc])
            mask_list.append(np.ascontiguousarray(np.broadcast_to(m[None, :], (128, m.shape[0]))))
    sT = run_B_conv(zT_list, mask_list, inp["conv_b_pw1"][0], inp["conv_w_dw"][0], inp["conv_b_dw"][0],
                    inp["conv_ln_w"][0], inp["conv_ln_b"][0], NCTX)
    xmid = run_C1(sT, xT, modT, inp["norm_post_mix"][L], inp["conv_w_pw2"][0], inp["conv_b_pw2"][0], NCTX)
    return xmid


def rview(v, pattern, **kw):
    return V(v.ap.rearrange(pattern, **kw), v.tile, v.lo, v.hi)


def nat_tables(rpb):
    p = np.arange(128)
    half, kc = p // 64, p % 64
    i = np.arange(8)
    qc = np.arange(64)
    dr = -8 + 2 * i[None, :] + half[:, None]
    cidx = kc[:, None] - qc[None, :] + 15
    cstart = np.clip(qc - 8, 0, 48)
    cval = (kc[:, None] >= cstart[None, :]) & (kc[:, None] < cstart[None, :] + 16)
    rv = dr >= -7
    B = rpb[:, np.clip(dr + 7, 0, 14)[:, :, None], np.clip(cidx, 0, 30)[:, None, :]]
    B = np.ascontiguousarray(B.transpose(1, 0, 2, 3)).astype(np.float32)
    M = (rv[:, :, None] & cval[:, None, :]).astype(np.float32)
    M = np.ascontiguousarray(np.broadcast_to(M[:, None], (128, 8, 8, 64)))
    return B, M


def nat_rowvalid(q):
    p = np.arange(128)
    half = p // 64
    out = np.zeros((128, 16, 8), np.float32)
    for rl in range(16):
        r = 16 * q + rl
        rs = min(max(r - 4, 0), 56)
        for i in range(8):
            kr = r - 8 + 2 * i + half
            out[:, rl, i] = ((kr >= rs) & (kr < rs + 8)).astype(np.float32)
    return out


def run_B_nat(qT_list, kTw_list, vw_list, kTc_list, vc_list, rpb):
    P = Prog("Bnat")
    mk = P.mk
    NW = 2048
    qT_d = P.inp("qT", [D, NLAT], BF16)
    kT_d = P.inp("kTw", [D, NW], BF16)
    v_d = P.inp("vw", [NW, D], BF16)
    kTc_d = P.inp("kTc", [D, NCTX], BF16)
    vc_d = P.inp("vc", [NCTX, D], BF16)
    B_d = P.inp("B", [128, 16, 8, 64])
    M_d = P.inp("M", [128, 8, 8, 64])
    rv_d = P.inp("rv", [128, 16, 8])
    oT_d = P.outp("oT", [D, NLAT], BF16)
    rv = mk.sb("rv", [128, 16, 8], F32)
    mk.dma("sp", rv.all(), rv_d)
    Mt = mk.sb("Mt", [128, 8, 8, 64], F32)
    mk.dma("sp", Mt.all(), M_d)
    Et = mk.sb("Et", [128, 8, 8, 64], F32)
    kT = mk.sb("kT", [128, 8, NW], BF16)
    qT = mk.sb("qT", [128, 8, NLAT], BF16)
    kTc = mk.sb("kTc", [128, 8, NCTX], BF16)
    vc = mk.sb("vc", [128, 2, 1024], BF16)
    vb = [mk.sb(f"vb{i}", [128, 8, 1024], BF16) for i in range(2)]
    pT = [mk.sb(f"pT{i}", [128, 512], BF16) for i in range(3)]
    rcp = [mk.sb(f"rcp{i}", [128, 512], F32) for i in range(2)]
    ost = mk.sb("ost", [128, 8, NLAT], BF16)
    scale = 128.0 ** -0.5
    pi = 0
    acc_i = 0
    vi = 0
    for g in range(2):
        hs = slice(g * 1024, (g + 1) * 1024)
        mk.dma("sp", Et.all(), B_d[:, g * 8:(g + 1) * 8, :, :])
        mk.act(Et.all(), Et.all(), AF.Exp)
        mk.tt("pool", Et.all(), Et.all(), Mt.all(), ALU.mult)
        for h0 in range(0, 8, 4):
            mk.dma("sp", kT[:, h0:h0 + 4, :], kT_d[hs, :].rearrange("(h p) t -> p h t", p=128)[:, h0:h0 + 4, :])
        mk.dma("act", qT.all(), qT_d[hs, :].rearrange("(h p) t -> p h t", p=128))
        mk.dma("act", kTc.all(), kTc_d[hs, :].rearrange("(h p) t -> p h t", p=128))
        mk.dma("act", vc.all(), vc_d[:, hs].rearrange("(c p) n -> p c n", p=128))
        for rl in range(16):
            vband = vb[vi % 2]
            vi += 1
            mk.dma("sp" if rl % 2 == 0 else "act", vband.all(),
                   v_d[rl * 64:rl * 64 + 1024, hs].rearrange("(c p) n -> p c n", p=128))
            ps_o = P.psb[4 + 2 * (acc_i % 2)]
            ps_s = P.psb[5 + 2 * (acc_i % 2)]
            acc_i += 1
            qs = slice(rl * 64, (rl + 1) * 64)
            for i in range(10):
                ps = P.psb[pi % 4]
                p_ = pT[pi % 3]
                pi += 1
                for h in range(8):
                    if i < 8:
                        lhs = kT[:, h, (rl + 2 * i) * 64:(rl + 2 * i) * 64 + 128]
                    else:
                        lhs = kTc[:, h, (i - 8) * 128:(i - 7) * 128]
                    mk.matmul(ps[:, h * 64:(h + 1) * 64], lhs, qT[:, h, qs])
                mk.act(p_.all(), ps.all(), AF.Exp, scale=scale)
                if i < 8:
                    mk.stt(rview(p_.all(), "p (h q) -> p h q", h=8), rview(p_.all(), "p (h q) -> p h q", h=8),
                           rv[:, rl, i:i + 1], Et[:, :, i, :], ALU.mult, ALU.mult)
                for h in range(8):
                    vv = vband[:, i, h * 128:(h + 1) * 128] if i < 8 else vc[:, i - 8, h * 128:(h + 1) * 128]
                    mk.matmul(ps_o[:, h * 64:(h + 1) * 64], vv, p_[:, h * 64:(h + 1) * 64],
                              start=(i == 0 and h == 0), stop=(i == 9), skip=True)
                mk.matmul(ps_s.all(), P.ones.all(), p_.all(), start=(i == 0), stop=(i == 9))
            r = rcp[acc_i % 2]
            mk.recip(r.all(), ps_s.all())
            mk.tt("dve", ost[:, :, qs], rview(ps_o.all(), "p (h q) -> p h q", h=8),
                  rview(r.all(), "p (h q) -> p h q", h=8), ALU.mult)
        mk.dma("sp", oT_d[hs, :].rearrange("(h p) t -> p h t", p=128), ost.all())
    B, M = nat_tables(rpb)
    in_maps = [{"qT": qT_list[c], "kTw": kTw_list[c], "vw": vw_list[c], "kTc": kTc_list[c], "vc": vc_list[c],
                "B": B, "M": M, "rv": nat_rowvalid(c % 4)} for c in range(NCORES)]
    return [r["oT"] for r in P.run(in_maps)]


def layer_nat(x_lat, x_ctx, mod, L, inp):
    modT = [mod_layout(mod, L, c // 4) for c in range(NCORES)]
    xT = core_xT(x_lat, x_ctx, NCTX)
    specs = [("qT", 0, 2048, "fm", "bf16"), ("kT", 2048, 2048, "fm", "bf16"), ("v", 4096, 2048, "tm", "bf16")]
    ra = run_A(xT, modT, inp["norm_pre_mix"][L], inp["nat_w_in"][0], specs, NCTX)
    qT_list, kTw, vw, kTc, vcl = [], [], [], [], []
    for b in range(2):
        kg = np.concatenate([ra[4 * b + i]["kT"][:, 0:NLAT] for i in range(4)], axis=1)
        kg = np.pad(kg, ((0, 0), (512, 512)))
        vg = np.concatenate([ra[4 * b + i]["v"][0:NLAT] for i in range(4)], axis=0)
        vg = np.pad(vg, ((512, 512), (0, 0)))
        for q in range(4):
            c = 4 * b + q
            qT_list.append(np.ascontiguousarray(ra[c]["qT"][:, 0:NLAT]))
            kTw.append(np.ascontiguousarray(kg[:, q * 1024:q * 1024 + 2048]))
            vw.append(np.ascontiguousarray(vg[q * 1024:q * 1024 + 2048]))
            kTc.append(np.ascontiguousarray(ra[4 * b]["kT"][:, NLAT:]))
            vcl.append(np.ascontiguousarray(ra[4 * b]["v"][NLAT:]))
    oT = run_B_nat(qT_list, kTw, vw, kTc, vcl, inp["nat_rpb"][0])
    modT1 = [mod_layout(mod, L, c // 4, with_ctx=False) for c in range(NCORES)]
    xT1 = [np.ascontiguousarray(x[:, 0:NLAT]) for x in xT]
    xmid = run_C1(oT, xT1, modT1, inp["norm_post_mix"][L], inp["nat_w_out"][0], None, 0)
    return xmid


def mlstm_consts():
    blk = np.arange(128) // 64
    same = blk[:, None] == blk[None, :]
    idx = np.arange(128)
    Uf = (same & (idx[:, None] <= idx[None, :])).astype(np.float32)
    Ub = np.ascontiguousarray(Uf.T)
    I = np.eye(128, dtype=np.float32)
    out = np.zeros((2, 5, 128, 128), np.float32)
    for d, U in enumerate((Uf, Ub)):
        out[d, 0] = U
        out[d, 1] = -U
        out[d, 2] = (U - 1.0) * 30000.0
        out[d, 3] = U.T - I
        out[d, 4] = I
    return np.ascontiguousarray(out.transpose(2, 0, 1, 3))


def run_B_mlstm(qT_list, kT_list, ktm_list, vtm_list, otm_list, gtm_list, bg_list, gain_list):
    NT = NCTX + SEQ
    NSC = NT // 128
    P = Prog("Bmlstm")
    mk = P.mk
    qT_d = P.inp("qT", [2, 128, NT], BF16)
    kT_d = P.inp("kT", [2, 128, NT], BF16)
    ktm_d = P.inp("ktm", [NT, 2, 128], BF16)
    vtm_d = P.inp("vtm", [NT, 2, 256], BF16)
    otm_d = P.inp("otm", [NT, 2, 256])
    gtm_d = P.inp("gtm", [NT, 2, 4])
    bg_d = P.inp("bg", [128, 2, 4])
    gain_d = P.inp("gain", [128, 2, 256])
    cm_d = P.inp("cm", [128, 2, 5, 128])
    out_d = P.outp("hout", [NT, 2, 256], BF16)
    cm = mk.sb("cm", [128, 2, 5, 128], F32)
    mk.dma("sp", cm.all(), cm_d)
    bg = mk.sb("bg", [128, 2, 4], F32)
    mk.dma("sp", bg.all(), bg_d)
    gain = mk.sb("gain", [128, 2, 256], F32)
    mk.dma("sp", gain.all(), gain_d)
    onesf = mk.sb("onesf", [128, 128], F32)
    mk.memset("dve", onesf.all(), 1.0)
    qT = mk.sb("qT", [128, NT], BF16)
    kT = mk.sb("kT", [128, NT], BF16)
    ktm = mk.sb("ktm", [128, NSC, 128], BF16)
    vtm = mk.sb("vtm", [128, NSC, 257], BF16)
    otm = mk.sb("otm", [128, NSC, 256], F32)
    gt = mk.sb("gt", [128, NSC, 4], F32)
    tmpg = mk.sb("tmpg", [128, NSC, 4], F32)
    IG = mk.sb("IG", [128, 2, NSC], F32)
    LF = mk.sb("LF", [128, 2, NSC], F32)
    Hacc = mk.sb("Hacc", [128, NSC, 256], F32)
    Cst = [[mk.sb(f"Cst{d}{i}", [128, 257], F32) for i in range(2)] for d in range(2)]
    Cbf = [[mk.sb(f"Cbf{d}{i}", [128, 257], BF16) for i in range(3)] for d in range(2)]
    Qpad = [[mk.sb(f"Qpad{d}{i}", [128, 256], BF16) for i in range(2)] for d in range(2)]
    for d in range(2):
        for i in range(2):
            mk.memset("pool", Qpad[d][i].all(), 0.0)
    LFbc = [mk.sb(f"LFbc{i}", [128, 128], F32) for i in range(2)]
    Edec = [mk.sb(f"Edec{i}", [128, 128], F32) for i in range(2)]
    DmT = [mk.sb(f"DmT{i}", [128, 128], F32) for i in range(2)]
    wgt = [mk.sb(f"wgt{i}", [128, 1], F32) for i in range(2)]
    Kw = [mk.sb(f"Kw{i}", [128, 128], BF16) for i in range(2)]
    Sm = [mk.sb(f"Sm{i}", [128, 128], BF16) for i in range(2)]
    dn = [mk.sb(f"dn{i}", [128, 1], F32) for i in range(2)]
    ssq = mk.sb("ssq", [128, NSC], F32)
    rst = mk.sb("rst", [128, NSC], F32)
    sqj = mk.sb("sqj", [128, 256], F32)
    sg = [mk.sb(f"sg{i}", [128, 256], F32) for i in range(2)]
    hn = [mk.sb(f"hn{i}", [128, 256], F32) for i in range(2)]
    ob = [mk.sb(f"ob{i}", [128, 256], BF16) for i in range(2)]
    scale = 128.0 ** -0.5
    order_f = list(range(NSC))
    order_b = [1, 0] + list(range(NSC - 1, 1, -1))
    it = 0
    for hd in range(2):
        mk.dma("sp", qT.all(), qT_d[hd])
        mk.dma("act", kT.all(), kT_d[hd])
        mk.dma("sp", ktm.all(), ktm_d[:, hd, :].rearrange("(c p) n -> p c n", p=128))
        mk.dma("act", vtm[:, :, 0:256], vtm_d[:, hd, :].rearrange("(c p) n -> p c n", p=128))
        mk.memset("pool", vtm[:, :, 256:257], 1.0)
        mk.dma("sp", otm.all(), otm_d[:, hd, :].rearrange("(c p) n -> p c n", p=128))
        mk.dma("act", gt.all(), gtm_d[:, hd, :].rearrange("(c p) n -> p c n", p=128))
        for c in range(4):
            mk.act(gt[:, :, c:c + 1], gt[:, :, c:c + 1], AF.Identity, bias=bg[:, hd, c:c + 1])
        mk.act(tmpg.all(), gt.all(), AF.Exp, scale=-1.0)
        mk.act(tmpg.all(), tmpg.all(), AF.Ln, bias=onesf[:, 0:1])
        for d in range(2):
            mk.act(rview(IG[:, d, :], "p (c o) -> p c o", o=1), gt[:, :, 2 * d:2 * d + 1], AF.Copy)
            mk.act(rview(LF[:, d, :], "p (c o) -> p c o", o=1), tmpg[:, :, 2 * d + 1:2 * d + 2], AF.Copy, scale=-1.0)
        mk.memset("pool", Hacc.all(), 0.0)
        cur = [0, 0]
        cbi = [0, 0]
        for d in range(2):
            mk.memset("dve", Cst[d][0].all(), 0.0)
            mk.memset("pool", Cbf[d][0].all(), 0.0)
        for step in range(NSC):
            for d in range(2):
                sc = (order_f if d == 0 else order_b)[step]
                i2 = it % 2
                it += 1
                U, nU, NEG, SL, Id = (cm[:, d, k, :] for k in range(5))
                lfc = LF[:, d, sc:sc + 1]
                igc = IG[:, d, sc:sc + 1]
                tsl = slice(sc * 128, (sc + 1) * 128)
                mk.act(LFbc[i2].all(), onesf.all(), AF.Copy, scale=lfc)
                ps1 = P.psum()
                mk.matmul(ps1[:, 0:128], LFbc[i2].all(), U)
                mk.act(Edec[i2].all(), ps1[:, 0:128], AF.Exp)
                ps2 = P.psum()
                mk.matmul(ps2[:, 0:128], LFbc[i2].all(), U, start=True, stop=False)
                mk.matmul(ps2[:, 0:128], nU, LFbc[i2].all(), start=False, stop=False)
                mk.matmul(ps2[:, 0:128], Id, NEG, start=False, stop=True)
                mk.act(DmT[i2].all(), ps2[:, 0:128], AF.Exp, bias=igc)
                ps3 = P.psum()
                mk.matmul(ps3[:, 0:1], SL, lfc)
                mk.act(wgt[i2].all(), ps3[:, 0:1], AF.Exp, bias=igc)
                mk.ts("dve", Kw[i2].all(), ktm[:, sc, :], wgt[i2][:, 0:1], ALU.mult)
                ps4 = P.psum()
                mk.matmul(ps4[:, 0:128], kT[:, tsl], qT[:, tsl])
                mk.stt(Sm[i2].all(), ps4[:, 0:128], scale, DmT[i2].all(), ALU.mult, ALU.mult)
                qp = Qpad[d][step % 2]
                mk.stt(qp[:, 0:64], qT[:, sc * 128:sc * 128 + 64], scale, Edec[i2][:, 0:64], ALU.mult, ALU.mult)
                mk.stt(qp[:, 192:256], qT[:, sc * 128 + 64:sc * 128 + 128], scale, Edec[i2][:, 64:128],
                       ALU.mult, ALU.mult)
                first, second = ((0, 64), (64, 128)) if d == 0 else ((64, 128), (0, 64))
                gcol = (63, 127) if d == 0 else (64, 0)
                cb_in = Cbf[d][cbi[d] % 3]
                cs_in = Cst[d][cur[d] % 2]
                cs_mid = Cst[d][(cur[d] + 1) % 2]
                cb_mid = Cbf[d][(cbi[d] + 1) % 3]
                cb_out = Cbf[d][(cbi[d] + 2) % 3]
                ps6 = P.psum()
                mk.matmul(ps6[:, 0:257], Kw[i2][first[0]:first[1], :], vtm[first[0]:first[1], sc, :])
                mk.stt(cs_mid.all(), cs_in.all(), Edec[i2][:, gcol[0]:gcol[0] + 1], ps6[:, 0:257], ALU.mult, ALU.add)
                mk.copy("act", cb_mid.all(), cs_mid.all())
                ps7 = P.psum()
                mk.matmul(ps7[:, 0:257], Kw[i2][second[0]:second[1], :], vtm[second[0]:second[1], sc, :])
                mk.stt(cs_in.all(), cs_mid.all(), Edec[i2][:, gcol[1]:gcol[1] + 1], ps7[:, 0:257], ALU.mult, ALU.add)
                mk.copy("act", cb_out.all(), cs_in.all())
                cbi[d] += 2
                cA, cB = (cb_in, cb_mid) if d == 0 else (cb_mid, cb_in)
                ps5 = P.psum()
                mk.matmul(ps5[:, 0:257], Sm[i2].all(), vtm[:, sc, :], start=True, stop=False)
                mk.matmul(ps5[:, 0:257], qp[:, 0:128], cA.all(), start=False, stop=False)
                mk.matmul(ps5[:, 0:257], qp[:, 128:256], cB.all(), start=False, stop=True)
                mk.act(dn[i2].all(), ps5[:, 256:257], AF.Abs)
                mk.ts("dve", dn[i2].all(), dn[i2].all(), 1.0, ALU.max)
                mk.recip(dn[i2].all(), dn[i2].all())
                mk.stt(Hacc[:, sc, :], ps5[:, 0:256], dn[i2][:, 0:1], Hacc[:, sc, :], ALU.mult, ALU.add)
        mk.memset("dve", ssq.all(), 0.0)
        for sc in range(NSC):
            mk.act(sqj.all(), Hacc[:, sc, :], AF.Square, accum_out=ssq[:, sc:sc + 1])
        mk.act(rst.all(), ssq.all(), AF.Sqrt, bias=P.eps_t[:, 0:1], scale=1.0 / 256)
        mk.recip(rst.all(), rst.all())
        for sc in range(NSC):
            j = sc % 2
            mk.act(sg[j].all(), otm[:, sc, :], AF.Sigmoid)
            mk.stt(hn[j].all(), Hacc[:, sc, :], rst[:, sc:sc + 1], gain[:, hd, :], ALU.mult, ALU.mult)
            mk.tt("pool", ob[j].all(), hn[j].all(), sg[j].all(), ALU.mult)
            mk.dma("sp", out_d[sc * 128:(sc + 1) * 128, hd, :], ob[j].all())
    cmk = mlstm_consts()
    in_maps = [{"qT": qT_list[c], "kT": kT_list[c], "ktm": ktm_list[c], "vtm": vtm_list[c], "otm": otm_list[c],
                "gtm": gtm_list[c], "bg": bg_list[c], "gain": gain_list[c], "cm": cmk} for c in range(NCORES)]
    return [r["hout"] for r in P.run(in_maps)]


def layer_mlstm(x_lat, x_ctx, mod, L, inp):
    modT = [mod_layout(mod, L, c // 4) for c in range(NCORES)]
    xT = core_xT(x_lat, x_ctx, NCTX)
    specs = [("qT", 0, 1024, "fm", "bf16"), ("kT", 1024, 1024, "fm", "bf16"), ("ktm", 1024, 1024, "tm", "bf16"),
             ("vtm", 2048, 2048, "tm", "bf16"), ("otm", 4096, 2048, "tm", "f32"), ("gtm", 6144, 32, "tm", "f32")]
    ra = run_A(xT, modT, inp["norm_pre_mix"][L], inp["mlstm_w_in"][0], specs, NCTX)

    def glob_fm(name, b):
        return np.concatenate([ra[4 * b][name][:, NLAT:]] + [ra[4 * b + i][name][:, 0:NLAT] for i in range(4)], axis=1)

    def glob_tm(name, b):
        return np.concatenate([ra[4 * b][name][NLAT:]] + [ra[4 * b + i][name][0:NLAT] for i in range(4)], axis=0)

    lists = [[] for _ in range(8)]
    b_gate = inp["mlstm_b_gate"][0].reshape(4, 8)
    onorm = inp["mlstm_out_norm"][0].reshape(8, 256)
    for b in range(2):
        qg, kg = glob_fm("qT", b), glob_fm("kT", b)
        ktm, vtm, otm, gtm = (glob_tm(n, b) for n in ("ktm", "vtm", "otm", "gtm"))
        NT = qg.shape[1]
        for hp in range(4):
            hs = [2 * hp, 2 * hp + 1]
            lists[0].append(np.ascontiguousarray(qg.reshape(8, 128, NT)[hs]))
            lists[1].append(np.ascontiguousarray(kg.reshape(8, 128, NT)[hs]))
            lists[2].append(np.ascontiguousarray(ktm.reshape(NT, 8, 128)[:, hs]))
            lists[3].append(np.ascontiguousarray(vtm.reshape(NT, 8, 256)[:, hs]))
            lists[4].append(np.ascontiguousarray(otm.reshape(NT, 8, 256)[:, hs]))
            lists[5].append(np.ascontiguousarray(gtm.reshape(NT, 4, 8)[:, :, hs].transpose(0, 2, 1)))
            lists[6].append(np.ascontiguousarray(np.broadcast_to(b_gate[:, hs].T[None], (128, 2, 4))))
            lists[7].append(np.ascontiguousarray(np.broadcast_to(onorm[hs][None], (128, 2, 256))))
    ho = run_B_mlstm(*lists)
    oT = []
    for b in range(2):
        og = np.concatenate([ho[4 * b + hp] for hp in range(4)], axis=1)
        og = og.reshape(NCTX + SEQ, D)
        for q in range(4):
            tok = np.concatenate([og[NCTX + q * NLAT:NCTX + (q + 1) * NLAT], og[0:NCTX]], axis=0)
            oT.append(to_fm(tok))
    xmid = run_C1(oT, xT, modT, inp["norm_post_mix"][L], inp["mlstm_w_out"][0], None, NCTX)
    return xmid


def kernel(**inputs):
    inp = {k: np.asarray(v) for k, v in inputs.items()}
    mod = run_mod(inp["c"], inp["c_ctx"], inp["mod_w"], inp["mod_b"])
    x_lat, x_ctx = inp["x"], inp["ctx"]
    layers = [layer_gqa, layer_mlstm, layer_conv, layer_nat]
    for L in range(4):
        nctx = NCTX if L < 3 else 0
        xmid = layers[L](x_lat, x_ctx, mod, L, inp)
        xl, xc = split_xT(xmid, nctx)
        x_lat, x_ctx = layer_ffn(xl, xc, mod, L, inp, nctx)
    return np.ascontiguousarray(x_lat.astype(np.float32))
```

```python
import numpy as np
from contextlib import ExitStack
import ml_dtypes
import concourse.bass as bass
import concourse.mybir as mybir
from concourse.bass_utils import run_bass_kernel_spmd

F32 = mybir.dt.float32
BF16 = mybir.dt.bfloat16
AF = mybir.ActivationFunctionType
ALU = mybir.AluOpType
AX = mybir.AxisListType
NPBF = ml_dtypes.bfloat16

D = 2048
KC = 16
NCTX = 256
SEQ = 4096
NLAT = 1024
DFF = 5632
EPS = 1e-6
NCORES = 8


class V:
    __slots__ = ("ap", "tile", "lo", "hi")

    def __init__(self, ap, tile, lo, hi):
        self.ap, self.tile, self.lo, self.hi = ap, tile, lo, hi


class Tile:
    def __init__(self, mk, t, shape, name):
        self.mk, self.t, self.shape, self.name = mk, t, list(shape), name
        st = []
        acc = 1
        for s in reversed(self.shape[1:]):
            st.append(acc)
            acc *= s
        self.strides = list(reversed(st))
        self.recs_w = []
        self.recs_r = []

    def __getitem__(self, idx):
        if not isinstance(idx, tuple):
            idx = (idx,)
        ap = self.t[idx]
        lo = 0
        hi = 0
        fidx = list(idx[1:]) + [slice(None)] * (len(self.shape) - len(idx))
        for s, n, stride in zip(fidx, self.shape[1:], self.strides):
            if isinstance(s, slice):
                a = 0 if s.start is None else s.start
                b = n if s.stop is None else s.stop
                step = 1 if s.step is None else s.step
                cnt = (b - a + step - 1) // step
                last = a + (cnt - 1) * step
            else:
                a = s
                last = s
            lo += a * stride
            hi += last * stride
        return V(ap, self, lo, hi + 1)

    def all(self):
        return self[tuple([slice(None)] * len(self.shape))]


class MK:
    SEM_ROLL = 30000

    def __init__(self, nc, n_dma_sems=32):
        self.nc = nc
        self.es = ExitStack()
        self.sem_es = ExitStack()
        self.eng = {"pe": nc.tensor, "act": nc.scalar, "dve": nc.vector, "pool": nc.gpsimd, "sp": nc.sync}
        self.sem = {}
        self.cnt = {}
        self.nsem = 0
        for e in self.eng:
            self._new_sem(e)
        self.waited = {e: {} for e in self.eng}
        self.dma_sems = [self.es.enter_context(nc.semaphore(f"dq{i}")) for i in range(n_dma_sems)]
        self.dma_val = [0] * n_dma_sems
        self.dma_rr = 0
        self.ninst = 0
        self.nwait = 0
        self.rr = 0
        self.stk = []

    def _new_sem(self, e):
        self.sem[e] = self.sem_es.enter_context(self.nc.semaphore(f"s_{e}_{self.nsem}"))
        self.nsem += 1
        self.cnt[e] = 0

    def sb(self, name, shape, dt=F32):
        t = self.es.enter_context(self.nc.sbuf_tensor("sb_" + name, list(shape), dt))
        return Tile(self, t, shape, name)

    def ps(self, name, shape, dt=F32):
        t = self.es.enter_context(self.nc.psum_tensor("ps_" + name, list(shape), dt))
        return Tile(self, t, shape, name)

    def push(self):
        self.stk.append(self.es)
        self.es = ExitStack()

    def pop(self):
        self.es.close()
        self.es = self.stk.pop()

    def barrier(self):
        for e in self.eng:
            for o in self.eng:
                if o != e and self.cnt[o] > 0:
                    self._wait(e, self.sem[o], self.cnt[o])
            for i, s_ in enumerate(self.dma_sems):
                if self.dma_val[i] > 0:
                    self._wait(e, s_, self.dma_val[i])

    def _wait(self, e, sem, val):
        w = self.waited[e]
        if w.get(sem, 0) >= val:
            return
        w[sem] = val
        self.eng[e].wait_ge(sem, val)
        self.nwait += 1

    def _deps(self, e, reads, writes):
        for v in reads:
            if not isinstance(v, V):
                continue
            for (lo, hi, sem, val, de) in v.tile.recs_w:
                if lo < v.hi and v.lo < hi and not (de == "pe" and e == "pe"):
                    self._wait(e, sem, val)
        for v in writes:
            if not isinstance(v, V):
                continue
            for (lo, hi, sem, val, de) in v.tile.recs_w:
                if lo < v.hi and v.lo < hi and not (de == "pe" and e == "pe"):
                    self._wait(e, sem, val)
            for (lo, hi, sem, val, de) in v.tile.recs_r:
                if lo < v.hi and v.lo < hi and not (de == "pe" and e == "pe"):
                    self._wait(e, sem, val)

    def _record(self, reads, writes, sem, val, e):
        for v in writes:
            if not isinstance(v, V):
                continue
            t = v.tile
            t.recs_w = [r for r in t.recs_w if not (v.lo <= r[0] and r[1] <= v.hi)]
            t.recs_r = [r for r in t.recs_r if not (v.lo <= r[0] and r[1] <= v.hi)]
            t.recs_w.append((v.lo, v.hi, sem, val, e))
        for v in reads:
            if not isinstance(v, V):
                continue
            t = v.tile
            t.recs_r = [r for r in t.recs_r if not (r[2] is sem and v.lo <= r[0] and r[1] <= v.hi)]
            t.recs_r.append((v.lo, v.hi, sem, val, e))

    @staticmethod
    def _ap(v):
        return v.ap if isinstance(v, V) else v

    def op(self, e, fn, reads, writes):
        self._deps(e, reads, writes)
        if self.cnt[e] >= self.SEM_ROLL:
            self._new_sem(e)
        ins = fn()
        self.cnt[e] += 1
        ins.then_inc(self.sem[e], 1)
        self._record(reads, writes, self.sem[e], self.cnt[e], e)
        self.ninst += 1
        return ins

    def dma(self, e, out, in_, **kw):
        self._deps(e, [in_], [out])
        i = self.dma_rr
        self.dma_rr = (self.dma_rr + 1) % len(self.dma_sems)
        s = self.dma_sems[i]
        if self.dma_val[i] > 0:
            self._wait(e, s, self.dma_val[i])
        self.dma_val[i] += 16
        self.eng[e].dma_start(out=self._ap(out), in_=self._ap(in_), **kw).then_inc(s, 16)
        self._record([in_], [out], s, self.dma_val[i], "dma")
        self.ninst += 1

    def finish(self, e="sp"):
        for i, s in enumerate(self.dma_sems):
            if self.dma_val[i] > 0:
                self._wait(e, s, self.dma_val[i])
        for o in self.eng:
            if o != e and self.cnt[o] > 0:
                self._wait(e, self.sem[o], self.cnt[o])

    def close(self):
        while self.stk:
            self.pop()
        self.es.close()
        self.sem_es.close()

    def matmul(self, out, lhsT, rhs, start=True, stop=True, skip=False):
        return self.op("pe", lambda: self.nc.tensor.matmul(out.ap, lhsT.ap, rhs.ap, start=start, stop=stop,
                                                           skip_group_check=skip),
                       [lhsT, rhs], [out])

    def act(self, out, in_, func, bias=None, scale=None, accum_out=None):
        kw = {}
        reads = [in_]
        writes = [out]
        if bias is not None:
            kw["bias"] = self._ap(bias)
            reads.append(bias)
        if scale is not None:
            kw["scale"] = self._ap(scale)
            reads.append(scale)
        if accum_out is not None:
            kw["accum_out"] = accum_out.ap
            writes.append(accum_out)
        return self.op("act", lambda: self.nc.scalar.activation(out.ap, in_.ap, func, **kw), reads, writes)

    def tt(self, e, out, a, b, op):
        return self.op(e, lambda: self.eng[e].tensor_tensor(out.ap, a.ap, b.ap, op), [a, b], [out])

    def ts(self, e, out, a, s1, op0, s2=None, op1=None):
        reads = [a, s1, s2]
        if op1 is None:
            s2, op1 = 0.0, ALU.add
        return self.op(e, lambda: self.eng[e].tensor_scalar(out.ap, a.ap, self._ap(s1), self._ap(s2), op0, op1),
                       reads, [out])

    def stt(self, out, a, s, b, op0, op1):
        return self.op("dve", lambda: self.nc.vector.scalar_tensor_tensor(out.ap, a.ap, self._ap(s), b.ap, op0, op1),
                       [a, s, b], [out])

    def copy(self, e, out, in_):
        if e == "act":
            return self.op(e, lambda: self.nc.scalar.copy(out.ap, in_.ap), [in_], [out])
        return self.op(e, lambda: self.eng[e].tensor_copy(out.ap, in_.ap), [in_], [out])

    def memset(self, e, out, val):
        return self.op(e, lambda: self.eng[e].memset(out.ap, val), [], [out])

    def recip(self, out, in_):
        return self.op("dve", lambda: self.nc.vector.reciprocal(out.ap, in_.ap), [in_], [out])

    def evac(self, out, in_):
        self.rr ^= 1
        return self.copy("act" if self.rr else "dve", out, in_)


def tiles_of(n, mx=512):
    k = (n + mx - 1) // mx
    base = n // k
    rem = n % k
    out = []
    o = 0
    for i in range(k):
        s = base + (1 if i < rem else 0)
        out.append((o, s))
        o += s
    return out


class Prog:
    def __init__(self, name):
        import time as _t
        self.t_start = _t.time()
        self.name = name
        self.nc = bass.Bass("TRN2", target_bir_lowering=False)
        self.mk = MK(self.nc)
        self.in_names = []
        self.out_names = []
        mk = self.mk
        self.psb = [mk.ps(f"psb{i}", [128, 512], F32) for i in range(8)]
        self.ps_i = 0
        self.ones = mk.sb("ones_bf", [128, 128], BF16)
        mk.memset("dve", self.ones.all(), 1.0)
        self.eps_t = mk.sb("eps_t", [128, 1], F32)
        mk.memset("dve", self.eps_t.all(), EPS)
        self.wq = 0

    def inp(self, name, shape, dt=F32):
        self.in_names.append(name)
        return self.nc.dram_tensor(name, list(shape), dt, kind="ExternalInput").ap()

    def outp(self, name, shape, dt=F32):
        self.out_names.append(name)
        return self.nc.dram_tensor(name, list(shape), dt, kind="ExternalOutput").ap()

    def rstd_of(self, out, ssq, dim, eps=EPS):
        self.mk.act(out, ssq, AF.Sqrt, bias=self.eps_t[0:out.ap.shape[0], 0:1] if eps == EPS else eps, scale=1.0 / dim)
        self.mk.recip(out, out)

    def psum(self):
        p = self.psb[self.ps_i]
        self.ps_i = (self.ps_i + 1) % 8
        return p

    def run(self, in_maps):
        import time as _t
        self.mk.finish()
        t0 = _t.time()
        import os as _os
        if _os.environ.get("KTRACE"):
            res = run_bass_kernel_spmd(self.nc, in_maps, core_ids=list(range(NCORES)), trace=True)
            print(f"[{self.name}] exec_time_ns={res.exec_time_ns}", flush=True)
        else:
            res = run_bass_kernel_spmd(self.nc, in_maps, core_ids=list(range(NCORES)))
        print(f"[{self.name}] ninst={self.mk.ninst} nwait={self.mk.nwait} build={t0 - self.t_start:.1f}s "
              f"run={_t.time() - t0:.1f}s", flush=True)
        self.mk.close()
        return res.results

    def load_w(self, tile, w_ap, col0, ncols, nk):
        src = w_ap[:, col0:col0 + ncols].rearrange("(kc p) n -> p kc n", p=128)
        step = 4
        for k0 in range(0, nk, step):
            k1 = min(nk, k0 + step)
            self.mk.dma("pool", tile[:, k0:k1, 0:ncols], src[:, k0:k1, :])

    def rstd_fm(self, xT, nk, T, rstd, sqbuf, dim):
        mk = self.mk
        for (t0, tn) in tiles_of(T):
            ps = self.psum()
            for kc in range(nk):
                sq = sqbuf[kc % len(sqbuf)]
                mk.act(sq[:, 0:tn], xT[:, kc, t0:t0 + tn], AF.Square)
                mk.matmul(ps[:, 0:tn], self.ones.all(), sq[:, 0:tn], start=(kc == 0), stop=(kc == nk - 1))
            self.rstd_of(rstd[:, t0:t0 + tn], ps[:, 0:tn], dim)

    def modulate_fm(self, hT, xT, rstd, segs, tmp):
        mk = self.mk
        for kc in range(KC):
            for (t0, tn, a, sh) in segs:
                t = tmp[kc % len(tmp)]
                mk.tt("dve", t[:, 0:tn], xT[:, kc, t0:t0 + tn], rstd[:, t0:t0 + tn], ALU.mult)
                mk.act(hT[:, kc, t0:t0 + tn], t[:, 0:tn], AF.Identity, bias=sh[:, kc:kc + 1], scale=a[:, kc:kc + 1])

    def load_mod(self, modT, nseg):
        t = self.mk.sb("modsb", [128, nseg, 6, KC], F32)
        self.mk.dma("sp", t.all(), modT)
        return t

    def load_vec(self, name, ap_kc):
        t = self.mk.sb(name, [128, KC], F32)
        self.mk.dma("sp", t.all(), ap_kc)
        return t


def vec_pk(v):
    return np.ascontiguousarray(v.reshape(-1, 128).T)


def run_mod(c, c_ctx, mod_w, mod_b):
    P = Prog("mod")
    mk = P.mk
    NCOL = 12288 // NCORES
    cT = P.inp("cT", [128, KC, 3])
    w = P.inp("w", [4, D, NCOL])
    b = P.inp("b", [3, 4, NCOL])
    out = P.outp("out", [3, 4, NCOL])
    cs = mk.sb("cs", [128, KC, 3], F32)
    sT = mk.sb("sT", [128, KC, 3], F32)
    mk.dma("sp", cs.all(), cT)
    mk.act(sT.all(), cs.all(), AF.Silu)
    bs = mk.sb("bs", [3, 4, NCOL], F32)
    mk.dma("sp", bs.all(), b)
    os_ = mk.sb("os", [3, 4, NCOL], F32)
    wb = [mk.sb(f"wb{i}", [128, KC, 512], F32) for i in range(2)]
    it = 0
    for l in range(4):
        for n0 in range(0, NCOL, 512):
            wt = wb[it % 2]
            it += 1
            src = w[l, :, n0:n0 + 512].rearrange("(kc p) n -> p kc n", p=128)
            for k0 in range(0, KC, 4):
                mk.dma("sp" if (k0 // 4) % 2 == 0 else "act", wt[:, k0:k0 + 4, :], src[:, k0:k0 + 4, :])
            ps = P.psum()
            for kc in range(KC):
                mk.matmul(ps[0:3, :], sT[:, kc, :], wt[:, kc, :], start=(kc == 0), stop=(kc == KC - 1))
            mk.tt("dve", os_[:, l, n0:n0 + 512], ps[0:3, :], bs[:, l, n0:n0 + 512], ALU.add)
    mk.dma("sp", out, os_.all())
    cstack = np.stack([c[0], c[1], c_ctx], axis=1)
    cT_np = np.ascontiguousarray(cstack.reshape(KC, 128, 3).transpose(1, 0, 2))
    in_maps = []
    for core in range(NCORES):
        sl = slice(core * NCOL, (core + 1) * NCOL)
        in_maps.append({"cT": cT_np, "w": np.ascontiguousarray(mod_w[:, :, sl]),
                        "b": np.ascontiguousarray(np.broadcast_to(mod_b[None, :, sl], (3, 4, NCOL)))})
    res = P.run(in_maps)
    mod = np.concatenate([r["out"] for r in res], axis=2)
    return mod


def mod_layout(mod, layer, b, with_ctx=True):
    rows = [b, 2] if with_ctx else [b]
    m = mod[rows, layer]
    m = m.reshape(len(rows), 6, KC, 128).transpose(3, 0, 1, 2)
    return np.ascontiguousarray(m)


def norm_mod_stream(P, xT_d, T, segs, normw_t, mod_t, ish, isc, hT):
    mk = P.mk
    nseg = mod_t.shape[1]
    a_t = mk.sb("nm_a", [128, nseg, KC], F32)
    for s in range(nseg):
        mk.stt(a_t[:, s, :], mod_t[:, s, isc, :], 1.0, normw_t.all(), ALU.add, ALU.mult)
    xb = [mk.sb(f"nm_xb{i}", [128, T], F32) for i in range(2)]
    sq = [mk.sb(f"nm_sq{i}", [128, 512], BF16) for i in range(3)]
    rstd = mk.sb("nm_rstd", [128, T], F32)
    tls = tiles_of(T)
    pss = [P.psum() for _ in tls]
    xsrc = xT_d.rearrange("(kc p) t -> p kc t", p=128)
    for kc in range(KC):
        x = xb[kc % 2]
        mk.dma("sp" if kc % 2 == 0 else "act", x.all(), xsrc[:, kc, :])
        for ti, (t0, tn) in enumerate(tls):
            s_ = sq[(kc * len(tls) + ti) % 3]
            mk.act(s_[:, 0:tn], x[:, t0:t0 + tn], AF.Square)
            mk.matmul(pss[ti][:, 0:tn], P.ones.all(), s_[:, 0:tn], start=(kc == 0), stop=(kc == KC - 1))
    for ti, (t0, tn) in enumerate(tls):
        P.rstd_of(rstd[:, t0:t0 + tn], pss[ti][:, 0:tn], D)
    for kc in range(KC):
        x = xb[kc % 2]
        mk.dma("sp" if kc % 2 == 0 else "act", x.all(), xsrc[:, kc, :])
        mk.tt("dve", x.all(), x.all(), rstd.all(), ALU.mult)
        for (t0, tn, s) in segs:
            mk.act(hT[:, kc, t0:t0 + tn], x[:, t0:t0 + tn], AF.Identity,
                   bias=mod_t[:, s, ish, kc:kc + 1], scale=a_t[:, s, kc:kc + 1])


def proj_fm(P, hT, T, w_ap, col0, ncols, out_ap, out_dt, wbufs, stg, nk=KC, post=None):
    mk = P.mk
    tls = tiles_of(T)
    bi = 0
    for b0 in range(0, ncols, 512):
        bn = min(512, ncols - b0)
        wt = wbufs[P.wq % len(wbufs)]
        P.wq += 1
        P.load_w(wt, w_ap, col0 + b0, bn, nk)
        for n0 in range(0, bn, 128):
            st = stg[bi % len(stg)]
            bi += 1
            for (t0, tn) in tls:
                ps = P.psum()
                for kc in range(nk):
                    mk.matmul(ps[:, 0:tn], wt[:, kc, n0:n0 + 128], hT[:, kc, t0:t0 + tn],
                              start=(kc == 0), stop=(kc == nk - 1))
                if post is None:
                    mk.evac(st[:, t0:t0 + tn], ps[:, 0:tn])
                else:
                    post(st, ps, b0 + n0, t0, tn)
            mk.dma("sp", out_ap[b0 + n0:b0 + n0 + 128, :], st[:, 0:T])


def proj_tm(P, hT, T, w_ap, col0, ncols, out_ap, wbufs, stg, nk=KC):
    mk = P.mk
    bi = 0
    for b0 in range(0, ncols, 512):
        bn = min(512, ncols - b0)
        wt = wbufs[P.wq % len(wbufs)]
        P.wq += 1
        P.load_w(wt, w_ap, col0 + b0, bn, nk)
        for t0 in range(0, T, 128):
            st = stg[bi % len(stg)]
            bi += 1
            ps = P.psum()
            for kc in range(nk):
                mk.matmul(ps[:, 0:bn], hT[:, kc, t0:t0 + 128], wt[:, kc, 0:bn], start=(kc == 0), stop=(kc == nk - 1))
            mk.evac(st[:, 0:bn], ps[:, 0:bn])
            mk.dma("sp", out_ap[t0:t0 + 128, b0:b0 + bn], st[:, 0:bn])


def run_A(xT_list, modT_list, normw, w_in, specs, nctx):
    T = NLAT + nctx
    N = w_in.shape[1]
    P = Prog("A")
    mk = P.mk
    xT_d = P.inp("xT", [D, T])
    modT_d = P.inp("modT", [128, 2 if nctx else 1, 6, KC])
    nw_d = P.inp("nw", [128, KC])
    w_d = P.inp("w", [D, N])
    mod_t = P.load_mod(modT_d, 2 if nctx else 1)
    nw_t = P.load_vec("nw_t", nw_d)
    hT = mk.sb("hT", [128, KC, T], BF16)
    segs = [(0, NLAT, 0)] + ([(NLAT, nctx, 1)] if nctx else [])
    norm_mod_stream(P, xT_d, T, segs, nw_t, mod_t, 0, 1, hT)
    wbufs = [mk.sb(f"wbuf{i}", [128, KC, 512], BF16) for i in range(2)]
    stg_fm_b = [mk.sb(f"sfb{i}", [128, T], BF16) for i in range(3)]
    stg_fm_f = [mk.sb(f"sff{i}", [128, T], F32) for i in range(3)]
    stg_tm_b = [mk.sb(f"stb{i}", [128, 512], BF16) for i in range(3)]
    stg_tm_f = [mk.sb(f"stf{i}", [128, 512], F32) for i in range(3)]
    for (name, col0, ncols, lay, dt) in specs:
        bdt = BF16 if dt == "bf16" else F32
        if lay == "fm":
            o = P.outp(name, [ncols, T], bdt)
            proj_fm(P, hT, T, w_d, col0, ncols, o, bdt, wbufs, stg_fm_b if dt == "bf16" else stg_fm_f)
        else:
            o = P.outp(name, [T, ncols], bdt)
            proj_tm(P, hT, T, w_d, col0, ncols, o, wbufs, stg_tm_b if dt == "bf16" else stg_tm_f)
    nwk = vec_pk(normw)
    in_maps = [{"xT": xT_list[c], "modT": modT_list[c], "nw": nwk, "w": w_in} for c in range(NCORES)]
    return P.run(in_maps)


def run_C1(oT_list, xT_list, modT_list, normw, w_out, bias, nctx):
    T = NLAT + nctx
    nseg = 2 if nctx else 1
    P = Prog("C1")
    mk = P.mk
    oT_d = P.inp("oT", [D, T], BF16)
    xT_d = P.inp("xT", [D, T])
    modT_d = P.inp("modT", [128, nseg, 6, KC])
    nw_d = P.inp("nw", [128, KC])
    b_d = P.inp("bias", [128, KC])
    w_d = P.inp("w", [D, D])
    out_d = P.outp("xmid", [D, T])
    mod_t = P.load_mod(modT_d, nseg)
    nw_t = P.load_vec("nw_t", nw_d)
    b_t = P.load_vec("b_t", b_d)
    gw = mk.sb("gw", [128, nseg, KC], F32)
    for s in range(nseg):
        mk.tt("dve", gw[:, s, :], mod_t[:, s, 2, :], nw_t.all(), ALU.mult)
    wres = mk.sb("wres", [128, KC, D], BF16)
    for b0 in range(0, D, 512):
        src = w_d[:, b0:b0 + 512].rearrange("(kc p) n -> p kc n", p=128)
        for k0 in range(0, KC, 4):
            mk.dma("pool", wres[:, k0:k0 + 4, b0:b0 + 512], src[:, k0:k0 + 4, :])
    ob = [mk.sb(f"ob{i}", [128, KC, 512], BF16) for i in range(2)]
    yT = mk.sb("yT", [128, KC, 512], F32)
    sq = [mk.sb(f"sq{i}", [128, 512], BF16) for i in range(2)]
    rstd = mk.sb("rstd", [128, 512], F32)
    xb = [mk.sb(f"xb{i}", [128, 512], F32) for i in range(3)]
    osrc = oT_d.rearrange("(kc p) t -> p kc t", p=128)
    xsrc = xT_d.rearrange("(kc p) t -> p kc t", p=128)
    odst = out_d.rearrange("(kc p) t -> p kc t", p=128)
    tls = [(0, 512, 0), (512, 512, 0)] + ([(NLAT, nctx, 1)] if nctx else [])
    for ti, (t0, tn, seg) in enumerate(tls):
        o = ob[ti % 2]
        for k0 in range(0, KC, 4):
            mk.dma("sp", o[:, k0:k0 + 4, 0:tn], osrc[:, k0:k0 + 4, t0:t0 + tn])
        pss = P.psum()
        for n in range(KC):
            ps = P.psum()
            if ps is pss:
                ps = P.psum()
            for kc in range(KC):
                mk.matmul(ps[:, 0:tn], wres[:, kc, n * 128:(n + 1) * 128], o[:, kc, 0:tn],
                          start=(kc == 0), stop=(kc == KC - 1))
            mk.act(yT[:, n, 0:tn], ps[:, 0:tn], AF.Identity, bias=b_t[:, n:n + 1])
            s_ = sq[n % 2]
            mk.act(s_[:, 0:tn], yT[:, n, 0:tn], AF.Square)
            mk.matmul(pss[:, 0:tn], P.ones.all(), s_[:, 0:tn], start=(n == 0), stop=(n == KC - 1))
        P.rstd_of(rstd[:, 0:tn], pss[:, 0:tn], D)
        for n in range(KC):
            x = xb[n % 3]
            mk.dma("act", x[:, 0:tn], xsrc[:, n, t0:t0 + tn])
            mk.tt("dve", yT[:, n, 0:tn], yT[:, n, 0:tn], rstd[:, 0:tn], ALU.mult)
            mk.stt(x[:, 0:tn], yT[:, n, 0:tn], gw[:, seg, n:n + 1], x[:, 0:tn], ALU.mult, ALU.add)
            mk.dma("sp", odst[:, n, t0:t0 + tn], x[:, 0:tn])
    nwk = vec_pk(normw)
    bk = vec_pk(bias) if bias is not None else np.zeros((128, KC), np.float32)
    in_maps = [{"oT": oT_list[c], "xT": xT_list[c], "modT": modT_list[c], "nw": nwk, "bias": bk, "w": w_out}
               for c in range(NCORES)]
    return [r["xmid"] for r in P.run(in_maps)]


def run_C2_old(xe_list, mask_list, modT_list, nw_pre, nw_post, w_up, conv_w, conv_b, w_down, groups):
    ng = len(groups)
    nseg = 1 + max(s for _, s in groups)
    Te = sum(g + 2 for g, _ in groups)
    To = sum(g for g, _ in groups)
    NJ = DFF // 128
    P = Prog("C2")
    mk = P.mk
    xe_d = P.inp("xe", [D, Te])
    mask_d = P.inp("mask", [128, KC, 2 * ng])
    modT_d = P.inp("modT", [128, nseg, 6, KC])
    nw1_d = P.inp("nw1", [128, KC])
    nw2_d = P.inp("nw2", [128, KC])
    wu_d = P.inp("wu", [D, 2 * DFF])
    cw_d = P.inp("cw", [128, 2 * NJ, 3])
    cb_d = P.inp("cb", [128, 2 * NJ])
    wd_d = P.inp("wd", [DFF, D])
    out_d = P.outp("xo", [D, To])
    mod_t = P.load_mod(modT_d, nseg)
    nw1 = P.load_vec("nw1_t", nw1_d)
    nw2 = P.load_vec("nw2_t", nw2_d)
    mask_t = mk.sb("mask_t", [128, KC, 2 * ng], F32)
    mk.dma("sp", mask_t.all(), mask_d)
    cw = mk.sb("cw_t", [128, 2 * NJ, 3], F32)
    cb = mk.sb("cb_t", [128, 2 * NJ], F32)
    mk.dma("sp", cw.all(), cw_d)
    mk.dma("sp", cb.all(), cb_d)
    gw = mk.sb("gw", [128, nseg, KC], F32)
    for s in range(nseg):
        mk.tt("dve", gw[:, s, :], mod_t[:, s, 5, :], nw2.all(), ALU.mult)
    hT = mk.sb("hT", [128, KC, Te], BF16)
    segs = []
    o = 0
    for (G, s) in groups:
        segs.append((o, G + 2, s))
        o += G + 2
    norm_mod_stream(P, xe_d, Te, segs, nw1, mod_t, 3, 4, hT)
    o = 0
    for gi, (G, s) in enumerate(groups):
        mk.tt("dve", hT[:, :, o:o + 1], hT[:, :, o:o + 1], mask_t[:, :, 2 * gi:2 * gi + 1], ALU.mult)
        mk.tt("dve", hT[:, :, o + G + 1:o + G + 2], hT[:, :, o + G + 1:o + G + 2],
              mask_t[:, :, 2 * gi + 1:2 * gi + 2], ALU.mult)
        o += G + 2
    GM = max(g for g, _ in groups)
    actT = mk.sb("actT", [128, NJ, GM], BF16)
    wub = [mk.sb(f"wub{i}", [128, KC, 2, 128], BF16) for i in range(2)]
    ub = [mk.sb(f"ub{i}", [128, GM + 2], F32) for i in range(4)]
    cvb = [mk.sb(f"cvb{i}", [128, GM], F32) for i in range(4)]
    wdb = [mk.sb(f"wdb{i}", [128, NJ, 128], BF16) for i in range(2)]
    yT = mk.sb("yT", [128, KC, GM], F32)
    sq = [mk.sb(f"sq{i}", [128, 512], BF16) for i in range(2)]
    rstd = mk.sb("rstd", [128, 512], F32)
    xb = [mk.sb(f"xb{i}", [128, GM], F32) for i in range(3)]
    xsrc = xe_d.rearrange("(kc p) t -> p kc t", p=128)
    odst = out_d.rearrange("(kc p) t -> p kc t", p=128)
    wusrc = wu_d.rearrange("(kc p) n -> p kc n", p=128)
    wdsrc = wd_d.rearrange("(j p) n -> p j n", p=128)
    eo = 0
    oo = 0
    it = 0
    ui = 0
    for gi, (G, s) in enumerate(groups):
        Gx = G + 2
        tls = tiles_of(Gx)
        for j0 in range(0, NJ, 1):
            wt = wub[it % 2]
            it += 1
            for half, cbase in ((0, j0 * 128), (1, DFF + j0 * 128)):
                mk.dma("pool", wt[:, :, half, :], wusrc[:, :, cbase:cbase + 128])
            for jj in range(1):
                j = j0 + jj
                us = []
                for half in range(2):
                    u = ub[ui % 4]
                    ui += 1
                    for (t0, tn) in tls:
                        ps = P.psum()
                        for kc in range(KC):
                            mk.matmul(ps[:, 0:tn], wt[:, kc, half, jj * 128:(jj + 1) * 128],
                                      hT[:, kc, eo + t0:eo + t0 + tn], start=(kc == 0), stop=(kc == KC - 1))
                        mk.copy("act" if half == 0 else "dve", u[:, t0:t0 + tn], ps[:, 0:tn])
                    us.append(u)
                cs = []
                for half in range(2):
                    ch = half * NJ + j
                    u = us[half]
                    cv = cvb[(2 * j + half) % 4]
                    mk.act(cv[:, 0:G], u[:, 1:G + 1], AF.Identity, bias=cb[:, ch:ch + 1], scale=cw[:, ch, 1:2])
                    mk.stt(cv[:, 0:G], u[:, 0:G], cw[:, ch, 0:1], cv[:, 0:G], ALU.mult, ALU.add)
                    mk.stt(cv[:, 0:G], u[:, 2:G + 2], cw[:, ch, 2:3], cv[:, 0:G], ALU.mult, ALU.add)
                    cs.append(cv)
                mk.act(cs[1][:, 0:G], cs[1][:, 0:G], AF.Silu)
                mk.tt("dve", actT[:, j, 0:G], cs[0][:, 0:G], cs[1][:, 0:G], ALU.mult)
        pss = P.psum()
        for n0 in range(0, KC, 1):
            wd = wdb[n0 % 2]
            for j0 in range(0, NJ, 11):
                mk.dma("pool", wd[:, j0:j0 + 11, :], wdsrc[:, j0:j0 + 11, n0 * 128:n0 * 128 + 128])
            for nn in range(1):
                n = n0 + nn
                ps = P.psum()
                if ps is pss:
                    ps = P.psum()
                for j in range(NJ):
                    mk.matmul(ps[:, 0:G], wd[:, j, nn * 128:(nn + 1) * 128], actT[:, j, 0:G],
                              start=(j == 0), stop=(j == NJ - 1))
                mk.copy("act", yT[:, n, 0:G], ps[:, 0:G])
                s_ = sq[n % 2]
                mk.act(s_[:, 0:G], yT[:, n, 0:G], AF.Square)
                mk.matmul(pss[:, 0:G], P.ones.all(), s_[:, 0:G], start=(n == 0), stop=(n == KC - 1))
        P.rstd_of(rstd[:, 0:G], pss[:, 0:G], D)
        for n in range(KC):
            x = xb[n % 3]
            mk.dma("act", x[:, 0:G], xsrc[:, n, eo + 1:eo + 1 + G])
            mk.tt("dve", yT[:, n, 0:G], yT[:, n, 0:G], rstd[:, 0:G], ALU.mult)
            mk.stt(x[:, 0:G], yT[:, n, 0:G], gw[:, s, n:n + 1], x[:, 0:G], ALU.mult, ALU.add)
            mk.dma("sp", odst[:, n, oo:oo + G], x[:, 0:G])
        eo += Gx
        oo += G
    cwk = np.ascontiguousarray(conv_w.T.reshape(2 * NJ, 128, 3).transpose(1, 0, 2))
    cbk = np.ascontiguousarray(conv_b.reshape(2 * NJ, 128).T)
    in_maps = [{"xe": xe_list[c], "mask": mask_list[c], "modT": modT_list[c], "nw1": vec_pk(nw_pre),
                "nw2": vec_pk(nw_post), "wu": w_up, "cw": cwk, "cb": cbk, "wd": w_down} for c in range(NCORES)]
    return [r["xo"] for r in P.run(in_maps)]


def run_C2(xe_list, mask_list, modT_list, nw_pre, nw_post, w_up, conv_w, conv_b, w_down, groups):
    ng = len(groups)
    nseg = 1 + max(s for _, s in groups)
    Te = sum(g + 2 for g, _ in groups)
    To = sum(g for g, _ in groups)
    NJ = DFF // 128
    P = Prog("C2")
    mk = P.mk
    xe_d = P.inp("xe", [D, Te])
    mask_d = P.inp("mask", [128, KC, 2 * ng])
    modT_d = P.inp("modT", [128, nseg, 6, KC])
    nw1_d = P.inp("nw1", [128, KC])
    nw2_d = P.inp("nw2", [128, KC])
    wu_d = P.inp("wu", [D, 2 * DFF])
    cw_d = P.inp("cw", [128, 2 * NJ, 3])
    cb_d = P.inp("cb", [128, 2 * NJ])
    wd_d = P.inp("wd", [DFF, D])
    out_d = P.outp("xo", [D, To])
    ysc_d = P.nc.dram_tensor("ysc", [D, To], F32, kind="Internal").ap()
    mod_t = P.load_mod(modT_d, nseg)
    nw1 = P.load_vec("nw1_t", nw1_d)
    nw2 = P.load_vec("nw2_t", nw2_d)
    mask_t = mk.sb("mask_t", [128, KC, 2 * ng], F32)
    mk.dma("sp", mask_t.all(), mask_d)
    cw = mk.sb("cw_t", [128, 2 * NJ, 3], F32)
    cb = mk.sb("cb_t", [128, 2 * NJ], F32)
    mk.dma("sp", cw.all(), cw_d)
    mk.dma("sp", cb.all(), cb_d)
    gw = mk.sb("gw", [128, nseg, KC], F32)
    for s in range(nseg):
        mk.tt("dve", gw[:, s, :], mod_t[:, s, 5, :], nw2.all(), ALU.mult)
    actT = mk.sb("actT", [128, NJ, To], BF16)
    xsrc = xe_d.rearrange("(kc p) t -> p kc t", p=128)
    odst = out_d.rearrange("(kc p) t -> p kc t", p=128)
    ysc = ysc_d.rearrange("(kc p) t -> p kc t", p=128)
    wusrc = wu_d.rearrange("(kc p) n -> p kc n", p=128)
    wdsrc = wd_d.rearrange("(j p) n -> p j n", p=128)
    geo = []
    eo = oo = 0
    for (G, s) in groups:
        geo.append((eo, oo, G, s))
        eo += G + 2
        oo += G
    mk.push()
    hT = mk.sb("hT", [128, KC, Te], BF16)
    mk.push()
    segs = [(e0, G + 2, s) for (e0, o0, G, s) in geo]
    norm_mod_stream(P, xe_d, Te, segs, nw1, mod_t, 3, 4, hT)
    for gi, (e0, o0, G, s) in enumerate(geo):
        mk.tt("dve", hT[:, :, e0:e0 + 1], hT[:, :, e0:e0 + 1], mask_t[:, :, 2 * gi:2 * gi + 1], ALU.mult)
        mk.tt("dve", hT[:, :, e0 + G + 1:e0 + G + 2], hT[:, :, e0 + G + 1:e0 + G + 2],
              mask_t[:, :, 2 * gi + 1:2 * gi + 2], ALU.mult)
    mk.barrier()
    mk.pop()
    mk.push()
    wub = [mk.sb(f"wub{i}", [128, KC, 2, 128], BF16) for i in range(3)]
    ub = [mk.sb(f"ub{i}", [128, Te], F32) for i in range(4)]
    cvb = [mk.sb(f"cvb{i}", [128, To], F32) for i in range(2)]
    tle = tiles_of(Te)
    for j in range(NJ):
        wt = wub[j % 3]
        for half, cbase in ((0, j * 128), (1, DFF + j * 128)):
            mk.dma("pool", wt[:, :, half, :], wusrc[:, :, cbase:cbase + 128])
        us = []
        for half in range(2):
            u = ub[(2 * j + half) % 4]
            for (t0, tn) in tle:
                ps = P.psum()
                for kc in range(KC):
                    mk.matmul(ps[:, 0:tn], wt[:, kc, half, :], hT[:, kc, t0:t0 + tn],
                              start=(kc == 0), stop=(kc == KC - 1))
                mk.copy("act" if half == 0 else "dve", u[:, t0:t0 + tn], ps[:, 0:tn])
            us.append(u)
        for half in range(2):
            ch = half * NJ + j
            u = us[half]
            cv = cvb[half]
            for (e0, o0, G, s) in geo:
                mk.act(cv[:, o0:o0 + G], u[:, e0 + 1:e0 + 1 + G], AF.Identity, bias=cb[:, ch:ch + 1],
                       scale=cw[:, ch, 1:2])
                mk.stt(cv[:, o0:o0 + G], u[:, e0:e0 + G], cw[:, ch, 0:1], cv[:, o0:o0 + G], ALU.mult, ALU.add)
                mk.stt(cv[:, o0:o0 + G], u[:, e0 + 2:e0 + 2 + G], cw[:, ch, 2:3], cv[:, o0:o0 + G], ALU.mult, ALU.add)
        mk.act(cvb[1].all(), cvb[1].all(), AF.Silu)
        mk.tt("dve", actT[:, j, :], cvb[0].all(), cvb[1].all(), ALU.mult)
    mk.barrier()
    mk.pop()
    mk.pop()
    wdb = [mk.sb(f"wdb{i}", [128, NJ, 128], BF16) for i in range(2)]
    yst = [mk.sb(f"yst{i}", [128, To], F32) for i in range(2)]
    sq = [mk.sb(f"sq{i}", [128, 512], BF16) for i in range(2)]
    rstd = mk.sb("rstd", [128, To], F32)
    xb = [mk.sb(f"xb{i}", [128, Te], F32) for i in range(2)]
    tlo = tiles_of(To)
    pss = [P.psb[5 + i] for i in range(len(tlo))]
    pk = 0
    for n in range(KC):
        wd = wdb[n % 2]
        for j0 in range(0, NJ, 11):
            mk.dma("pool", wd[:, j0:j0 + 11, :], wdsrc[:, j0:j0 + 11, n * 128:(n + 1) * 128])
        y = yst[n % 2]
        for ti, (t0, tn) in enumerate(tlo):
            ps = P.psb[pk % 5]
            pk += 1
            for j in range(NJ):
                mk.matmul(ps[:, 0:tn], wd[:, j, :], actT[:, j, t0:t0 + tn], start=(j == 0), stop=(j == NJ - 1))
            mk.copy("act", y[:, t0:t0 + tn], ps[:, 0:tn])
            s_ = sq[(n * len(tlo) + ti) % 2]
            mk.act(s_[:, 0:tn], y[:, t0:t0 + tn], AF.Square)
            mk.matmul(pss[ti][:, 0:tn], P.ones.all(), s_[:, 0:tn], start=(n == 0), stop=(n == KC - 1))
        mk.dma("sp", ysc[:, n, :], y.all())
    for ti, (t0, tn) in enumerate(tlo):
        P.rstd_of(rstd[:, t0:t0 + tn], pss[ti][:, 0:tn], D)
    mk.barrier()
    for n in range(KC):
        y = yst[n % 2]
        x = xb[n % 2]
        mk.dma("sp", y.all(), ysc[:, n, :])
        mk.dma("act", x.all(), xsrc[:, n, :])
        mk.tt("dve", y.all(), y.all(), rstd.all(), ALU.mult)
        for (e0, o0, G, s) in geo:
            mk.stt(y[:, o0:o0 + G], y[:, o0:o0 + G], gw[:, s, n:n + 1], x[:, e0 + 1:e0 + 1 + G], ALU.mult, ALU.add)
        mk.dma("sp", odst[:, n, :], y.all())
    P.ps_i = 0
    cwk = np.ascontiguousarray(conv_w.T.reshape(2 * NJ, 128, 3).transpose(1, 0, 2))
    cbk = np.ascontiguousarray(conv_b.reshape(2 * NJ, 128).T)
    in_maps = [{"xe": xe_list[c], "mask": mask_list[c], "modT": modT_list[c], "nw1": vec_pk(nw_pre),
                "nw2": vec_pk(nw_post), "wu": w_up, "cw": cwk, "cb": cbk, "wd": w_down} for c in range(NCORES)]
    return [r["xo"] for r in P.run(in_maps)]


def ffn_host_io(xmid_lat, xmid_ctx, nctx):
    xe_list, mask_list = [], []
    groups = [(512, 0), (512, 0)] + ([(nctx, 1)] if nctx else [])
    for c in range(NCORES):
        b, q = c // 4, c % 4
        cols = []
        m = []
        for g in range(2):
            p0 = q * NLAT + g * 512
            blk = np.zeros((514, D), np.float32)
            lo, hi = p0 - 1, p0 + 513
            slo, shi = max(lo, 0), min(hi, SEQ)
            blk[slo - lo:shi - lo] = xmid_lat[b, slo:shi]
            cols.append(blk)
            m += [1.0 if lo >= 0 else 0.0, 1.0 if hi <= SEQ else 0.0]
        if nctx:
            blk = np.zeros((nctx + 2, D), np.float32)
            blk[1:nctx + 1] = xmid_ctx[b]
            cols.append(blk)
            m += [0.0, 0.0]
        xe = np.ascontiguousarray(np.concatenate(cols, axis=0).T)
        xe_list.append(xe)
        mask_list.append(np.ascontiguousarray(np.broadcast_to(np.array(m, np.float32)[None, None, :], (128, KC, len(m)))))
    return xe_list, mask_list, groups


def rope_tables():
    half = 32
    freqs = 10000.0 ** (-np.arange(half, dtype=np.float32) / half)
    pos = np.arange(SEQ)
    row, col = pos // 64, pos % 64
    ang = np.zeros((128, SEQ), np.float32)
    for d in range(128):
        p = row if d < 64 else col
        ang[d] = p.astype(np.float32) * freqs[(d % 64) % 32]
    R = np.zeros((128, 128), np.float32)
    for d in range(128):
        if (d % 64) < 32:
            R[d + 32, d] = -1.0
        else:
            R[d - 32, d] = 1.0
    return np.cos(ang).astype(np.float32), np.sin(ang).astype(np.float32), R.astype(NPBF)


def run_B_gqa(qT_list, kT_list, v_list, q_norm, k_norm):
    T = NLAT + NCTX
    NK = SEQ + NCTX
    NCH = NK // 128
    P = Prog("Bgqa")
    mk = P.mk
    qT_d = P.inp("qT", [D, T], BF16)
    kT_d = P.inp("kT", [512, NK], BF16)
    v_d = P.inp("v", [NK, 512], BF16)
    gn_d = P.inp("gn", [128, 2])
    cos_d = P.inp("cos", [128, SEQ])
    sin_d = P.inp("sin", [128, SEQ])
    cosq_d = P.inp("cosq", [128, NLAT])
    sinq_d = P.inp("sinq", [128, NLAT])
    R_d = P.inp("R", [128, 128], BF16)
    oT_d = P.outp("oT", [D, T], BF16)
    gn = mk.sb("gn", [128, 2], F32)
    mk.dma("sp", gn.all(), gn_d)
    mk.ts("dve", gn[:, 0:1], gn[:, 0:1], 128.0 ** -0.5, ALU.mult)
    Rm = mk.sb("Rm", [128, 128], BF16)
    mk.dma("sp", Rm.all(), R_d)
    cosk = mk.sb("cosk", [128, SEQ], F32)
    sink = mk.sb("sink", [128, SEQ], F32)
    cosq = mk.sb("cosq", [128, NLAT], F32)
    sinq = mk.sb("sinq", [128, NLAT], F32)
    mk.dma("sp", cosk.all(), cos_d)
    mk.dma("act", sink.all(), sin_d)
    mk.dma("sp", cosq.all(), cosq_d)
    mk.dma("act", sinq.all(), sinq_d)
    kT = mk.sb("kT", [128, 4, NK], BF16)
    mk.dma("sp", kT.all(), kT_d.rearrange("(h p) t -> p h t", p=128))
    qT = mk.sb("qT", [128, 16, T], BF16)
    for h0 in range(0, 16, 4):
        mk.dma("act", qT[:, h0:h0 + 4, :], qT_d.rearrange("(h p) t -> p h t", p=128)[:, h0:h0 + 4, :])
    vt = mk.sb("vt", [128, NCH, 512], BF16)
    vsrc = v_d.rearrange("(c p) n -> p c n", p=128)
    for c0 in range(0, NCH, 17):
        mk.dma("sp", vt[:, c0:c0 + 17, :], vsrc[:, c0:c0 + 17, :])
    sqb = [mk.sb(f"sqb{i}", [128, 512], BF16) for i in range(2)]
    rsb = [mk.sb(f"rsb{i}", [128, 512], F32) for i in range(2)]
    knb = [mk.sb(f"knb{i}", [128, 512], BF16) for i in range(2)]
    t1b = [mk.sb(f"t1b{i}", [128, 512], F32) for i in range(2)]
    t2b = [mk.sb(f"t2b{i}", [128, 512], F32) for i in range(2)]
    cnt = [0]

    def normrope(buf, h, t0, tn, gcol, cs, sn, c0):
        i = cnt[0] % 2
        cnt[0] += 1
        x = buf[:, h, t0:t0 + tn]
        mk.act(sqb[i][:, 0:tn], x, AF.Square)
        ps = P.psum()
        mk.matmul(ps[:, 0:tn], P.ones.all(), sqb[i][:, 0:tn])
        P.rstd_of(rsb[i][:, 0:tn], ps[:, 0:tn], 128)
        if cs is None:
            mk.stt(x, x, gcol, rsb[i][:, 0:tn], ALU.mult, ALU.mult)
            return
        mk.stt(knb[i][:, 0:tn], x, gcol, rsb[i][:, 0:tn], ALU.mult, ALU.mult)
        ps2 = P.psum()
        mk.matmul(ps2[:, 0:tn], Rm.all(), knb[i][:, 0:tn])
        mk.tt("pool", t1b[i][:, 0:tn], knb[i][:, 0:tn], cs[:, c0:c0 + tn], ALU.mult)
        mk.tt("dve", t2b[i][:, 0:tn], ps2[:, 0:tn], sn[:, c0:c0 + tn], ALU.mult)
        mk.tt("pool", x, t1b[i][:, 0:tn], t2b[i][:, 0:tn], ALU.add)

    for kv in range(4):
        for t0 in range(0, SEQ, 512):
            normrope(kT, kv, t0, 512, gn[:, 1:2], cosk, sink, t0)
        normrope(kT, kv, SEQ, NCTX, gn[:, 1:2], None, None, 0)
    pT = [mk.sb(f"pT{i}", [128, 512], BF16) for i in range(3)]
    rcp = [mk.sb(f"rcp{i}", [128, 512], F32) for i in range(2)]
    ost = [mk.sb(f"ost{i}", [128, T], BF16) for i in range(2)]
    pi = 0
    acc_i = 0
    for h in range(16):
        kv = h // 4
        for t0 in (0, 512):
            normrope(qT, h, t0, 512, gn[:, 0:1], cosq, sinq, t0)
        normrope(qT, h, NLAT, NCTX, gn[:, 0:1], None, None, 0)
        st = ost[h % 2]
        for (t0, tn, chunks) in ((0, 512, range(NCH)), (512, 512, range(NCH)), (NLAT, NCTX, range(32, NCH))):
            ps_o = P.psb[4 + 2 * (acc_i % 2)]
            ps_s = P.psb[5 + 2 * (acc_i % 2)]
            acc_i += 1
            chunks = list(chunks)
            for ci, c in enumerate(chunks):
                ps = P.psb[pi % 4]
                p_ = pT[pi % 3]
                pi += 1
                mk.matmul(ps[:, 0:tn], kT[:, kv, c * 128:(c + 1) * 128], qT[:, h, t0:t0 + tn])
                mk.act(p_[:, 0:tn], ps[:, 0:tn], AF.Exp)
                mk.matmul(ps_o[:, 0:tn], vt[:, c, kv * 128:(kv + 1) * 128], p_[:, 0:tn],
                          start=(ci == 0), stop=(ci == len(chunks) - 1))
                mk.matmul(ps_s[:, 0:tn], P.ones.all(), p_[:, 0:tn], start=(ci == 0), stop=(ci == len(chunks) - 1))
            r = rcp[acc_i % 2]
            mk.recip(r[:, 0:tn], ps_s[:, 0:tn])
            mk.tt("dve", st[:, t0:t0 + tn], ps_o[:, 0:tn], r[:, 0:tn], ALU.mult)
        mk.dma("sp", oT_d[h * 128:(h + 1) * 128, :], st.all())
    P.ps_i = 0
    cos, sin, R = rope_tables()
    gnk = np.ascontiguousarray(np.stack([q_norm, k_norm], axis=1).astype(np.float32))
    in_maps = []
    for c in range(NCORES):
        q = c % 4
        in_maps.append({"qT": qT_list[c], "kT": kT_list[c], "v": v_list[c], "gn": gnk, "cos": cos, "sin": sin,
                        "cosq": np.ascontiguousarray(cos[:, q * NLAT:(q + 1) * NLAT]),
                        "sinq": np.ascontiguousarray(sin[:, q * NLAT:(q + 1) * NLAT]), "R": R})
    return [r["oT"] for r in P.run(in_maps)]


def to_fm(x_tok):
    return np.ascontiguousarray(x_tok.T)


def core_xT(x_lat, x_ctx, nctx):
    out = []
    for c in range(NCORES):
        b, q = c // 4, c % 4
        parts = [x_lat[b, q * NLAT:(q + 1) * NLAT]]
        if nctx:
            parts.append(x_ctx[b])
        out.append(to_fm(np.concatenate(parts, axis=0)))
    return out


def layer_gqa(x_lat, x_ctx, mod, L, inp):
    modT = [mod_layout(mod, L, c // 4) for c in range(NCORES)]
    xT = core_xT(x_lat, x_ctx, NCTX)
    specs = [("qT", 0, 2048, "fm", "bf16"), ("kT", 2048, 512, "fm", "bf16"), ("v", 2560, 512, "tm", "bf16")]
    ra = run_A(xT, modT, inp["norm_pre_mix"][L], inp["gqa_w_in"][0], specs, NCTX)
    kT_list, v_list = [], []
    for c in range(NCORES):
        b = c // 4
        kT_list.append(np.ascontiguousarray(np.concatenate(
            [ra[4 * b + i]["kT"][:, 0:NLAT] for i in range(4)] + [ra[4 * b]["kT"][:, NLAT:]], axis=1)))
        v_list.append(np.ascontiguousarray(np.concatenate(
            [ra[4 * b + i]["v"][0:NLAT] for i in range(4)] + [ra[4 * b]["v"][NLAT:]], axis=0)))
    oT = run_B_gqa([r["qT"] for r in ra], kT_list, v_list, inp["gqa_q_norm"][0], inp["gqa_k_norm"][0])
    xmid = run_C1(oT, xT, modT, inp["norm_post_mix"][L], inp["gqa_w_out"][0], None, NCTX)
    return xmid


def split_xT(xT_list, nctx):
    x_lat = np.zeros((2, SEQ, D), np.float32)
    x_ctx = np.zeros((2, nctx, D), np.float32) if nctx else None
    for c in range(NCORES):
        b, q = c // 4, c % 4
        x_lat[b, q * NLAT:(q + 1) * NLAT] = xT_list[c][:, 0:NLAT].T
        if nctx and q == 0:
            x_ctx[b] = xT_list[c][:, NLAT:NLAT + nctx].T
    return x_lat, x_ctx


def layer_ffn(xmid_lat, xmid_ctx, mod, L, inp, nctx):
    modT = [mod_layout(mod, L, c // 4, with_ctx=bool(nctx)) for c in range(NCORES)]
    xe_list, mask_list, groups = ffn_host_io(xmid_lat, xmid_ctx, nctx)
    xo = run_C2(xe_list, mask_list, modT, inp["norm_pre_ffn"][L], inp["norm_post_ffn"][L], inp["ffn_w_up"][L],
                inp["ffn_conv_w"][L], inp["ffn_conv_b"][L], inp["ffn_w_down"][L], groups)
    return split_xT(xo, nctx)


def run_B_conv(zT_list, mask_list, b_pw1, w_dw, b_dw, ln_w, ln_b, nctx):
    HW = 15
    Le = NLAT + 2 * HW
    Ce = nctx + 2 * HW
    Te = Le + Ce
    T = NLAT + nctx
    P = Prog("Bconv")
    mk = P.mk
    zT_d = P.inp("zT", [2 * D, Te])
    mask_d = P.inp("mask", [128, Te])
    bp_d = P.inp("bp", [128, 2 * KC])
    wdw_d = P.inp("wdw", [128, KC, 31])
    bdw_d = P.inp("bdw", [128, KC])
    lnw_d = P.inp("lnw", [128, KC])
    lnb_d = P.inp("lnb", [128, KC])
    sT_d = P.outp("sT", [D, T], BF16)
    maskt = mk.sb("maskt", [128, Te], F32)
    mk.dma("sp", maskt.all(), mask_d)
    bp = mk.sb("bp", [128, 2 * KC], F32)
    mk.dma("sp", bp.all(), bp_d)
    wdw = mk.sb("wdw", [128, KC, 31], F32)
    mk.dma("sp", wdw.all(), wdw_d)
    bdw = P.load_vec("bdw", bdw_d)
    lnw = P.load_vec("lnw", lnw_d)
    lnb = P.load_vec("lnb", lnb_d)
    onesf = mk.sb("onesf", [128, 128], F32)
    mk.memset("dve", onesf.all(), 1.0)
    vT = mk.sb("vT", [128, KC, T], F32)
    zb = [mk.sb(f"zb{i}", [128, Te], F32) for i in range(4)]
    ub = [mk.sb(f"ub{i}", [128, Te], F32) for i in range(2)]
    sqf = [mk.sb(f"sqf{i}", [128, 512], F32) for i in range(2)]
    zsrc = zT_d.rearrange("(kc p) t -> p kc t", p=128)
    tls = tiles_of(T)
    ps_sum = [P.psb[i] for i in range(len(tls))]
    ps_sq = [P.psb[3 + i] for i in range(len(tls))]
    for kc in range(KC):
        za = zb[(2 * kc) % 4]
        zg = zb[(2 * kc + 1) % 4]
        mk.dma("sp", za.all(), zsrc[:, kc, :])
        mk.dma("act", zg.all(), zsrc[:, KC + kc, :])
        mk.act(zg.all(), zg.all(), AF.Sigmoid, bias=bp[:, KC + kc:KC + kc + 1])
        mk.tt("pool", zg.all(), zg.all(), maskt.all(), ALU.mult)
        u = ub[kc % 2]
        mk.stt(u.all(), za.all(), bp[:, kc:kc + 1], zg.all(), ALU.add, ALU.mult)
        for (e0, o0, n) in ((0, 0, NLAT), (Le, NLAT, nctx)):
            if n == 0:
                continue
            ov = vT[:, kc, o0:o0 + n]
            mk.ts("dve", ov, u[:, e0:e0 + n], wdw[:, kc, 0:1], ALU.mult, bdw[:, kc:kc + 1], ALU.add)
            for j in range(1, 31):
                mk.stt(ov, u[:, e0 + j:e0 + j + n], wdw[:, kc, j:j + 1], ov, ALU.mult, ALU.add)
        for ti, (t0, tn) in enumerate(tls):
            s_ = sqf[(kc * len(tls) + ti) % 2]
            mk.act(s_[:, 0:tn], vT[:, kc, t0:t0 + tn], AF.Square)
            mk.matmul(ps_sum[ti][:, 0:tn], onesf.all(), vT[:, kc, t0:t0 + tn], start=(kc == 0), stop=(kc == KC - 1))
            mk.matmul(ps_sq[ti][:, 0:tn], onesf.all(), s_[:, 0:tn], start=(kc == 0), stop=(kc == KC - 1))
    mean = mk.sb("mean", [128, T], F32)
    rstd = mk.sb("rstd", [128, T], F32)
    msq = mk.sb("msq", [128, T], F32)
    for ti, (t0, tn) in enumerate(tls):
        mk.ts("dve", mean[:, t0:t0 + tn], ps_sum[ti][:, 0:tn], 1.0 / D, ALU.mult)
        mk.tt("dve", msq[:, t0:t0 + tn], mean[:, t0:t0 + tn], mean[:, t0:t0 + tn], ALU.mult)
        mk.stt(msq[:, t0:t0 + tn], ps_sq[ti][:, 0:tn], 1.0 / D, msq[:, t0:t0 + tn], ALU.mult, ALU.subtract)
        mk.act(rstd[:, t0:t0 + tn], msq[:, t0:t0 + tn], AF.Sqrt, bias=P.eps_t[:, 0:1])
        mk.recip(rstd[:, t0:t0 + tn], rstd[:, t0:t0 + tn])
    sb_ = [mk.sb(f"sbo{i}", [128, T], BF16) for i in range(2)]
    tmp = [mk.sb(f"tmpn{i}", [128, T], F32) for i in range(2)]
    for kc in range(KC):
        t = tmp[kc % 2]
        mk.tt("pool", t.all(), vT[:, kc, :], mean.all(), ALU.subtract)
        mk.tt("dve", t.all(), t.all(), rstd.all(), ALU.mult)
        so = sb_[kc % 2]
        mk.act(so.all(), t.all(), AF.Silu, bias=lnb[:, kc:kc + 1], scale=lnw[:, kc:kc + 1])
        mk.dma("sp", sT_d[kc * 128:(kc + 1) * 128, :], so.all())
    bpk = np.ascontiguousarray(b_pw1.reshape(2 * KC, 128).T)
    wdwk = np.ascontiguousarray(w_dw.T.reshape(KC, 128, 31).transpose(1, 0, 2))
    in_maps = [{"zT": zT_list[c], "mask": mask_list[c], "bp": bpk, "wdw": wdwk, "bdw": vec_pk(b_dw),
                "lnw": vec_pk(ln_w), "lnb": vec_pk(ln_b)} for c in range(NCORES)]
    return [r["sT"] for r in P.run(in_maps)]


def layer_conv(x_lat, x_ctx, mod, L, inp):
    HW = 15
    modT = [mod_layout(mod, L, c // 4) for c in range(NCORES)]
    xT = core_xT(x_lat, x_ctx, NCTX)
    ra = run_A(xT, modT, inp["norm_pre_mix"][L], inp["conv_w_pw1"][0], [("zT", 0, 2 * D, "fm", "f32")], NCTX)
    zT_list, mask_list = [], []
    for b in range(2):
        zl = np.concatenate([ra[4 * b + i]["zT"][:, 0:NLAT] for i in range(4)], axis=1)
        zl = np.pad(zl, ((0, 0), (HW, HW)))
        ml = np.pad(np.ones(SEQ, np.float32), (HW, HW))
        zc = np.pad(ra[4 * b]["zT"][:, NLAT:], ((0, 0), (HW, HW)))
        mc = np.pad(np.ones(NCTX, np.float32), (HW, HW))
        for q in range(4):
            sl = slice(q * NLAT, q * NLAT + NLAT + 2 * HW)
            zT_list.append(np.ascontiguousarray(np.concatenate([zl[:, sl], zc], axis=1)))
            m = np.concatenate([ml[sl], mc])
            mask_list.append(np.ascontiguousarray(np.broadcast_to(m[None, :], (128, m.shape[0]))))
    sT = run_B_conv(zT_list, mask_list, inp["conv_b_pw1"][0], inp["conv_w_dw"][0], inp["conv_b_dw"][0],
                    inp["conv_ln_w"][0], inp["conv_ln_b"][0], NCTX)
    xmid = run_C1(sT, xT, modT, inp["norm_post_mix"][L], inp["conv_w_pw2"][0], inp["conv_b_pw2"][0], NCTX)
    return xmid


def rview(v, pattern, **kw):
    return V(v.ap.rearrange(pattern, **kw), v.tile, v.lo, v.hi)


def nat_tables(rpb):
    p = np.arange(128)
    half, kc = p // 64, p % 64
    i = np.arange(8)
    qc = np.arange(64)
    dr = -8 + 2 * i[None, :] + half[:, None]
    cidx = kc[:, None] - qc[None, :] + 15
    cstart = np.clip(qc - 8, 0, 48)
    cval = (kc[:, None] >= cstart[None, :]) & (kc[:, None] < cstart[None, :] + 16)
    rv = dr >= -7
    B = rpb[:, np.clip(dr + 7, 0, 14)[:, :, None], np.clip(cidx, 0, 30)[:, None, :]]
    B = np.ascontiguousarray(B.transpose(1, 0, 2, 3)).astype(np.float32)
    M = (rv[:, :, None] & cval[:, None, :]).astype(np.float32)
    M = np.ascontiguousarray(np.broadcast_to(M[:, None], (128, 8, 8, 64)))
    return B, M


def nat_rowvalid(q):
    p = np.arange(128)
    half = p // 64
    out = np.zeros((128, 16, 8), np.float32)
    for rl in range(16):
        r = 16 * q + rl
        rs = min(max(r - 4, 0), 56)
        for i in range(8):
            kr = r - 8 + 2 * i + half
            out[:, rl, i] = ((kr >= rs) & (kr < rs + 8)).astype(np.float32)
    return out


def run_B_nat(qT_list, kTw_list, vw_list, kTc_list, vc_list, rpb):
    P = Prog("Bnat")
    mk = P.mk
    NW = 2048
    qT_d = P.inp("qT", [D, NLAT], BF16)
    kT_d = P.inp("kTw", [D, NW], BF16)
    v_d = P.inp("vw", [NW, D], BF16)
    kTc_d = P.inp("kTc", [D, NCTX], BF16)
    vc_d = P.inp("vc", [NCTX, D], BF16)
    B_d = P.inp("B", [128, 16, 8, 64])
    M_d = P.inp("M", [128, 8, 8, 64])
    rv_d = P.inp("rv", [128, 16, 8])
    oT_d = P.outp("oT", [D, NLAT], BF16)
    rv = mk.sb("rv", [128, 16, 8], F32)
    mk.dma("sp", rv.all(), rv_d)
    Mt = mk.sb("Mt", [128, 8, 8, 64], F32)
    mk.dma("sp", Mt.all(), M_d)
    Et = mk.sb("Et", [128, 8, 8, 64], F32)
    kT = mk.sb("kT", [128, 8, NW], BF16)
    qT = mk.sb("qT", [128, 8, NLAT], BF16)
    kTc = mk.sb("kTc", [128, 8, NCTX], BF16)
    vc = mk.sb("vc", [128, 2, 1024], BF16)
    vb = [mk.sb(f"vb{i}", [128, 8, 1024], BF16) for i in range(2)]
    pT = [mk.sb(f"pT{i}", [128, 512], BF16) for i in range(3)]
    rcp = [mk.sb(f"rcp{i}", [128, 512], F32) for i in range(2)]
    ost = mk.sb("ost", [128, 8, NLAT], BF16)
    scale = 128.0 ** -0.5
    pi = 0
    acc_i = 0
    vi = 0
    for g in range(2):
        hs = slice(g * 1024, (g + 1) * 1024)
        mk.dma("sp", Et.all(), B_d[:, g * 8:(g + 1) * 8, :, :])
        mk.act(Et.all(), Et.all(), AF.Exp)
        mk.tt("pool", Et.all(), Et.all(), Mt.all(), ALU.mult)
        for h0 in range(0, 8, 4):
            mk.dma("sp", kT[:, h0:h0 + 4, :], kT_d[hs, :].rearrange("(h p) t -> p h t", p=128)[:, h0:h0 + 4, :])
        mk.dma("act", qT.all(), qT_d[hs, :].rearrange("(h p) t -> p h t", p=128))
        mk.dma("act", kTc.all(), kTc_d[hs, :].rearrange("(h p) t -> p h t", p=128))
        mk.dma("act", vc.all(), vc_d[:, hs].rearrange("(c p) n -> p c n", p=128))
        for rl in range(16):
            vband = vb[vi % 2]
            vi += 1
            mk.dma("sp" if rl % 2 == 0 else "act", vband.all(),
                   v_d[rl * 64:rl * 64 + 1024, hs].rearrange("(c p) n -> p c n", p=128))
            ps_o = P.psb[4 + 2 * (acc_i % 2)]
            ps_s = P.psb[5 + 2 * (acc_i % 2)]
            acc_i += 1
            qs = slice(rl * 64, (rl + 1) * 64)
            for i in range(10):
                ps = P.psb[pi % 4]
                p_ = pT[pi % 3]
                pi += 1
                for h in range(8):
                    if i < 8:
                        lhs = kT[:, h, (rl + 2 * i) * 64:(rl + 2 * i) * 64 + 128]
                    else:
                        lhs = kTc[:, h, (i - 8) * 128:(i - 7) * 128]
                    mk.matmul(ps[:, h * 64:(h + 1) * 64], lhs, qT[:, h, qs])
                mk.act(p_.all(), ps.all(), AF.Exp, scale=scale)
                if i < 8:
                    mk.stt(rview(p_.all(), "p (h q) -> p h q", h=8), rview(p_.all(), "p (h q) -> p h q", h=8),
                           rv[:, rl, i:i + 1], Et[:, :, i, :], ALU.mult, ALU.mult)
                for h in range(8):
                    vv = vband[:, i, h * 128:(h + 1) * 128] if i < 8 else vc[:, i - 8, h * 128:(h + 1) * 128]
                    mk.matmul(ps_o[:, h * 64:(h + 1) * 64], vv, p_[:, h * 64:(h + 1) * 64],
                              start=(i == 0 and h == 0), stop=(i == 9), skip=True)
                mk.matmul(ps_s.all(), P.ones.all(), p_.all(), start=(i == 0), stop=(i == 9))
            r = rcp[acc_i % 2]
            mk.recip(r.all(), ps_s.all())
            mk.tt("dve", ost[:, :, qs], rview(ps_o.all(), "p (h q) -> p h q", h=8),
                  rview(r.all(), "p (h q) -> p h q", h=8), ALU.mult)
        mk.dma("sp", oT_d[hs, :].rearrange("(h p) t -> p h t", p=128), ost.all())
    B, M = nat_tables(rpb)
    in_maps = [{"qT": qT_list[c], "kTw": kTw_list[c], "vw": vw_list[c], "kTc": kTc_list[c], "vc": vc_list[c],
                "B": B, "M": M, "rv": nat_rowvalid(c % 4)} for c in range(NCORES)]
    return [r["oT"] for r in P.run(in_maps)]


def layer_nat(x_lat, x_ctx, mod, L, inp):
    modT = [mod_layout(mod, L, c // 4) for c in range(NCORES)]
    xT = core_xT(x_lat, x_ctx, NCTX)
    specs = [("qT", 0, 2048, "fm", "bf16"), ("kT", 2048, 2048, "fm", "bf16"), ("v", 4096, 2048, "tm", "bf16")]
    ra = run_A(xT, modT, inp["norm_pre_mix"][L], inp["nat_w_in"][0], specs, NCTX)
    qT_list, kTw, vw, kTc, vcl = [], [], [], [], []
    for b in range(2):
        kg = np.concatenate([ra[4 * b + i]["kT"][:, 0:NLAT] for i in range(4)], axis=1)
        kg = np.pad(kg, ((0, 0), (512, 512)))
        vg = np.concatenate([ra[4 * b + i]["v"][0:NLAT] for i in range(4)], axis=0)
        vg = np.pad(vg, ((512, 512), (0, 0)))
        for q in range(4):
            c = 4 * b + q
            qT_list.append(np.ascontiguousarray(ra[c]["qT"][:, 0:NLAT]))
            kTw.append(np.ascontiguousarray(kg[:, q * 1024:q * 1024 + 2048]))
            vw.append(np.ascontiguousarray(vg[q * 1024:q * 1024 + 2048]))
            kTc.append(np.ascontiguousarray(ra[4 * b]["kT"][:, NLAT:]))
            vcl.append(np.ascontiguousarray(ra[4 * b]["v"][NLAT:]))
    oT = run_B_nat(qT_list, kTw, vw, kTc, vcl, inp["nat_rpb"][0])
    modT1 = [mod_layout(mod, L, c // 4, with_ctx=False) for c in range(NCORES)]
    xT1 = [np.ascontiguousarray(x[:, 0:NLAT]) for x in xT]
    xmid = run_C1(oT, xT1, modT1, inp["norm_post_mix"][L], inp["nat_w_out"][0], None, 0)
    return xmid


def mlstm_consts():
    blk = np.arange(128) // 64
    same = blk[:, None] == blk[None, :]
    idx = np.arange(128)
    Uf = (same & (idx[:, None] <= idx[None, :])).astype(np.float32)
    Ub = np.ascontiguousarray(Uf.T)
    I = np.eye(128, dtype=np.float32)
    out = np.zeros((2, 5, 128, 128), np.float32)
    for d, U in enumerate((Uf, Ub)):
        out[d, 0] = U
        out[d, 1] = -U
        out[d, 2] = (U - 1.0) * 30000.0
        out[d, 3] = U.T - I
        out[d, 4] = I
    return np.ascontiguousarray(out.transpose(2, 0, 1, 3))


def run_B_mlstm(qT_list, kT_list, ktm_list, vtm_list, otm_list, gtm_list, bg_list, gain_list):
    NT = NCTX + SEQ
    NSC = NT // 128
    P = Prog("Bmlstm")
    mk = P.mk
    qT_d = P.inp("qT", [2, 128, NT], BF16)
    kT_d = P.inp("kT", [2, 128, NT], BF16)
    ktm_d = P.inp("ktm", [NT, 2, 128], BF16)
    vtm_d = P.inp("vtm", [NT, 2, 256], BF16)
    otm_d = P.inp("otm", [NT, 2, 256])
    gtm_d = P.inp("gtm", [NT, 2, 4])
    bg_d = P.inp("bg", [128, 2, 4])
    gain_d = P.inp("gain", [128, 2, 256])
    cm_d = P.inp("cm", [128, 2, 5, 128])
    out_d = P.outp("hout", [NT, 2, 256], BF16)
    cm = mk.sb("cm", [128, 2, 5, 128], F32)
    mk.dma("sp", cm.all(), cm_d)
    bg = mk.sb("bg", [128, 2, 4], F32)
    mk.dma("sp", bg.all(), bg_d)
    gain = mk.sb("gain", [128, 2, 256], F32)
    mk.dma("sp", gain.all(), gain_d)
    onesf = mk.sb("onesf", [128, 128], F32)
    mk.memset("dve", onesf.all(), 1.0)
    qT = mk.sb("qT", [128, NT], BF16)
    kT = mk.sb("kT", [128, NT], BF16)
    ktm = mk.sb("ktm", [128, NSC, 128], BF16)
    vtm = mk.sb("vtm", [128, NSC, 257], BF16)
    otm = mk.sb("otm", [128, NSC, 256], F32)
    gt = mk.sb("gt", [128, NSC, 4], F32)
    tmpg = mk.sb("tmpg", [128, NSC, 4], F32)
    IG = mk.sb("IG", [128, 2, NSC], F32)
    LF = mk.sb("LF", [128, 2, NSC], F32)
    Hacc = mk.sb("Hacc", [128, NSC, 256], F32)
    Cst = [[mk.sb(f"Cst{d}{i}", [128, 257], F32) for i in range(2)] for d in range(2)]
    Cbf = [[mk.sb(f"Cbf{d}{i}", [128, 257], BF16) for i in range(3)] for d in range(2)]
    Qpad = [[mk.sb(f"Qpad{d}{i}", [128, 256], BF16) for i in range(2)] for d in range(2)]
    for d in range(2):
        for i in range(2):
            mk.memset("pool", Qpad[d][i].all(), 0.0)
    LFbc = [mk.sb(f"LFbc{i}", [128, 128], F32) for i in range(2)]
    Edec = [mk.sb(f"Edec{i}", [128, 128], F32) for i in range(2)]
    DmT = [mk.sb(f"DmT{i}", [128, 128], F32) for i in range(2)]
    wgt = [mk.sb(f"wgt{i}", [128, 1], F32) for i in range(2)]
    Kw = [mk.sb(f"Kw{i}", [128, 128], BF16) for i in range(2)]
    Sm = [mk.sb(f"Sm{i}", [128, 128], BF16) for i in range(2)]
    dn = [mk.sb(f"dn{i}", [128, 1], F32) for i in range(2)]
    ssq = mk.sb("ssq", [128, NSC], F32)
    rst = mk.sb("rst", [128, NSC], F32)
    sqj = mk.sb("sqj", [128, 256], F32)
    sg = [mk.sb(f"sg{i}", [128, 256], F32) for i in range(2)]
    hn = [mk.sb(f"hn{i}", [128, 256], F32) for i in range(2)]
    ob = [mk.sb(f"ob{i}", [128, 256], BF16) for i in range(2)]
    scale = 128.0 ** -0.5
    order_f = list(range(NSC))
    order_b = [1, 0] + list(range(NSC - 1, 1, -1))
    it = 0
    for hd in range(2):
        mk.dma("sp", qT.all(), qT_d[hd])
        mk.dma("act", kT.all(), kT_d[hd])
        mk.dma("sp", ktm.all(), ktm_d[:, hd, :].rearrange("(c p) n -> p c n", p=128))
        mk.dma("act", vtm[:, :, 0:256], vtm_d[:, hd, :].rearrange("(c p) n -> p c n", p=128))
        mk.memset("pool", vtm[:, :, 256:257], 1.0)
        mk.dma("sp", otm.all(), otm_d[:, hd, :].rearrange("(c p) n -> p c n", p=128))
        mk.dma("act", gt.all(), gtm_d[:, hd, :].rearrange("(c p) n -> p c n", p=128))
        for c in range(4):
            mk.act(gt[:, :, c:c + 1], gt[:, :, c:c + 1], AF.Identity, bias=bg[:, hd, c:c + 1])
        mk.act(tmpg.all(), gt.all(), AF.Exp, scale=-1.0)
        mk.act(tmpg.all(), tmpg.all(), AF.Ln, bias=onesf[:, 0:1])
        for d in range(2):
            mk.act(rview(IG[:, d, :], "p (c o) -> p c o", o=1), gt[:, :, 2 * d:2 * d + 1], AF.Copy)
            mk.act(rview(LF[:, d, :], "p (c o) -> p c o", o=1), tmpg[:, :, 2 * d + 1:2 * d + 2], AF.Copy, scale=-1.0)
        mk.memset("pool", Hacc.all(), 0.0)
        cur = [0, 0]
        cbi = [0, 0]
        for d in range(2):
            mk.memset("dve", Cst[d][0].all(), 0.0)
            mk.memset("pool", Cbf[d][0].all(), 0.0)
        for step in range(NSC):
            for d in range(2):
                sc = (order_f if d == 0 else order_b)[step]
                i2 = it % 2
                it += 1
                U, nU, NEG, SL, Id = (cm[:, d, k, :] for k in range(5))
                lfc = LF[:, d, sc:sc + 1]
                igc = IG[:, d, sc:sc + 1]
                tsl = slice(sc * 128, (sc + 1) * 128)
                mk.act(LFbc[i2].all(), onesf.all(), AF.Copy, scale=lfc)
                ps1 = P.psum()
                mk.matmul(ps1[:, 0:128], LFbc[i2].all(), U)
                mk.act(Edec[i2].all(), ps1[:, 0:128], AF.Exp)
                ps2 = P.psum()
                mk.matmul(ps2[:, 0:128], LFbc[i2].all(), U, start=True, stop=False)
                mk.matmul(ps2[:, 0:128], nU, LFbc[i2].all(), start=False, stop=False)
                mk.matmul(ps2[:, 0:128], Id, NEG, start=False, stop=True)
                mk.act(DmT[i2].all(), ps2[:, 0:128], AF.Exp, bias=igc)
                ps3 = P.psum()
                mk.matmul(ps3[:, 0:1], SL, lfc)
                mk.act(wgt[i2].all(), ps3[:, 0:1], AF.Exp, bias=igc)
                mk.ts("dve", Kw[i2].all(), ktm[:, sc, :], wgt[i2][:, 0:1], ALU.mult)
                ps4 = P.psum()
                mk.matmul(ps4[:, 0:128], kT[:, tsl], qT[:, tsl])
                mk.stt(Sm[i2].all(), ps4[:, 0:128], scale, DmT[i2].all(), ALU.mult, ALU.mult)
                qp = Qpad[d][step % 2]
                mk.stt(qp[:, 0:64], qT[:, sc * 128:sc * 128 + 64], scale, Edec[i2][:, 0:64], ALU.mult, ALU.mult)
                mk.stt(qp[:, 192:256], qT[:, sc * 128 + 64:sc * 128 + 128], scale, Edec[i2][:, 64:128],
                       ALU.mult, ALU.mult)
                first, second = ((0, 64), (64, 128)) if d == 0 else ((64, 128), (0, 64))
                gcol = (63, 127) if d == 0 else (64, 0)
                cb_in = Cbf[d][cbi[d] % 3]
                cs_in = Cst[d][cur[d] % 2]
                cs_mid = Cst[d][(cur[d] + 1) % 2]
                cb_mid = Cbf[d][(cbi[d] + 1) % 3]
                cb_out = Cbf[d][(cbi[d] + 2) % 3]
                ps6 = P.psum()
                mk.matmul(ps6[:, 0:257], Kw[i2][first[0]:first[1], :], vtm[first[0]:first[1], sc, :])
                mk.stt(cs_mid.all(), cs_in.all(), Edec[i2][:, gcol[0]:gcol[0] + 1], ps6[:, 0:257], ALU.mult, ALU.add)
                mk.copy("act", cb_mid.all(), cs_mid.all())
                ps7 = P.psum()
                mk.matmul(ps7[:, 0:257], Kw[i2][second[0]:second[1], :], vtm[second[0]:second[1], sc, :])
                mk.stt(cs_in.all(), cs_mid.all(), Edec[i2][:, gcol[1]:gcol[1] + 1], ps7[:, 0:257], ALU.mult, ALU.add)
                mk.copy("act", cb_out.all(), cs_in.all())
                cbi[d] += 2
                cA, cB = (cb_in, cb_mid) if d == 0 else (cb_mid, cb_in)
                ps5 = P.psum()
                mk.matmul(ps5[:, 0:257], Sm[i2].all(), vtm[:, sc, :], start=True, stop=False)
                mk.matmul(ps5[:, 0:257], qp[:, 0:128], cA.all(), start=False, stop=False)
                mk.matmul(ps5[:, 0:257], qp[:, 128:256], cB.all(), start=False, stop=True)
                mk.act(dn[i2].all(), ps5[:, 256:257], AF.Abs)
                mk.ts("dve", dn[i2].all(), dn[i2].all(), 1.0, ALU.max)
                mk.recip(dn[i2].all(), dn[i2].all())
                mk.stt(Hacc[:, sc, :], ps5[:, 0:256], dn[i2][:, 0:1], Hacc[:, sc, :], ALU.mult, ALU.add)
        mk.memset("dve", ssq.all(), 0.0)
        for sc in range(NSC):
            mk.act(sqj.all(), Hacc[:, sc, :], AF.Square, accum_out=ssq[:, sc:sc + 1])
        mk.act(rst.all(), ssq.all(), AF.Sqrt, bias=P.eps_t[:, 0:1], scale=1.0 / 256)
        mk.recip(rst.all(), rst.all())
        for sc in range(NSC):
            j = sc % 2
            mk.act(sg[j].all(), otm[:, sc, :], AF.Sigmoid)
            mk.stt(hn[j].all(), Hacc[:, sc, :], rst[:, sc:sc + 1], gain[:, hd, :], ALU.mult, ALU.mult)
            mk.tt("pool", ob[j].all(), hn[j].all(), sg[j].all(), ALU.mult)
            mk.dma("sp", out_d[sc * 128:(sc + 1) * 128, hd, :], ob[j].all())
    cmk = mlstm_consts()
    in_maps = [{"qT": qT_list[c], "kT": kT_list[c], "ktm": ktm_list[c], "vtm": vtm_list[c], "otm": otm_list[c],
                "gtm": gtm_list[c], "bg": bg_list[c], "gain": gain_list[c], "cm": cmk} for c in range(NCORES)]
    return [r["hout"] for r in P.run(in_maps)]


def layer_mlstm(x_lat, x_ctx, mod, L, inp):
    modT = [mod_layout(mod, L, c // 4) for c in range(NCORES)]
    xT = core_xT(x_lat, x_ctx, NCTX)
    specs = [("qT", 0, 1024, "fm", "bf16"), ("kT", 1024, 1024, "fm", "bf16"), ("ktm", 1024, 1024, "tm", "bf16"),
             ("vtm", 2048, 2048, "tm", "bf16"), ("otm", 4096, 2048, "tm", "f32"), ("gtm", 6144, 32, "tm", "f32")]
    ra = run_A(xT, modT, inp["norm_pre_mix"][L], inp["mlstm_w_in"][0], specs, NCTX)

    def glob_fm(name, b):
        return np.concatenate([ra[4 * b][name][:, NLAT:]] + [ra[4 * b + i][name][:, 0:NLAT] for i in range(4)], axis=1)

    def glob_tm(name, b):
        return np.concatenate([ra[4 * b][name][NLAT:]] + [ra[4 * b + i][name][0:NLAT] for i in range(4)], axis=0)

    lists = [[] for _ in range(8)]
    b_gate = inp["mlstm_b_gate"][0].reshape(4, 8)
    onorm = inp["mlstm_out_norm"][0].reshape(8, 256)
    for b in range(2):
        qg, kg = glob_fm("qT", b), glob_fm("kT", b)
        ktm, vtm, otm, gtm = (glob_tm(n, b) for n in ("ktm", "vtm", "otm", "gtm"))
        NT = qg.shape[1]
        for hp in range(4):
            hs = [2 * hp, 2 * hp + 1]
            lists[0].append(np.ascontiguousarray(qg.reshape(8, 128, NT)[hs]))
            lists[1].append(np.ascontiguousarray(kg.reshape(8, 128, NT)[hs]))
            lists[2].append(np.ascontiguousarray(ktm.reshape(NT, 8, 128)[:, hs]))
            lists[3].append(np.ascontiguousarray(vtm.reshape(NT, 8, 256)[:, hs]))
            lists[4].append(np.ascontiguousarray(otm.reshape(NT, 8, 256)[:, hs]))
            lists[5].append(np.ascontiguousarray(gtm.reshape(NT, 4, 8)[:, :, hs].transpose(0, 2, 1)))
            lists[6].append(np.ascontiguousarray(np.broadcast_to(b_gate[:, hs].T[None], (128, 2, 4))))
            lists[7].append(np.ascontiguousarray(np.broadcast_to(onorm[hs][None], (128, 2, 256))))
    ho = run_B_mlstm(*lists)
    oT = []
    for b in range(2):
        og = np.concatenate([ho[4 * b + hp] for hp in range(4)], axis=1)
        og = og.reshape(NCTX + SEQ, D)
        for q in range(4):
            tok = np.concatenate([og[NCTX + q * NLAT:NCTX + (q + 1) * NLAT], og[0:NCTX]], axis=0)
            oT.append(to_fm(tok))
    xmid = run_C1(oT, xT, modT, inp["norm_post_mix"][L], inp["mlstm_w_out"][0], None, NCTX)
    return xmid


def kernel(**inputs):
    inp = {k: np.asarray(v) for k, v in inputs.items()}
    mod = run_mod(inp["c"], inp["c_ctx"], inp["mod_w"], inp["mod_b"])
    x_lat, x_ctx = inp["x"], inp["ctx"]
    layers = [layer_gqa, layer_mlstm, layer_conv, layer_nat]
    for L in range(4):
        nctx = NCTX if L < 3 else 0
        xmid = layers[L](x_lat, x_ctx, mod, L, inp)
        xl, xc = split_xT(xmid, nctx)
        x_lat, x_ctx = layer_ffn(xl, xc, mod, L, inp, nctx)
    return np.ascontiguousarray(x_lat.astype(np.float32))
```

```python
import numpy as np
from contextlib import ExitStack
import ml_dtypes
import concourse.bass as bass
import concourse.mybir as mybir
from concourse.bass_utils import run_bass_kernel_spmd

F32 = mybir.dt.float32
BF16 = mybir.dt.bfloat16
AF = mybir.ActivationFunctionType
ALU = mybir.AluOpType
AX = mybir.AxisListType
NPBF = ml_dtypes.bfloat16

D = 2048
KC = 16
NCTX = 256
SEQ = 4096
NLAT = 1024
NCL = 64
DFF = 5632
EPS = 1e-6
NCORES = 8


class V:
    __slots__ = ("ap", "tile", "lo", "hi")

    def __init__(self, ap, tile, lo, hi):
        self.ap, self.tile, self.lo, self.hi = ap, tile, lo, hi


class Tile:
    def __init__(self, mk, t, shape, name):
        self.mk, self.t, self.shape, self.name = mk, t, list(shape), name
        st = []
        acc = 1
        for s in reversed(self.shape[1:]):
            st.append(acc)
            acc *= s
        self.strides = list(reversed(st))
        self.recs_w = []
        self.recs_r = []

    def __getitem__(self, idx):
        if not isinstance(idx, tuple):
            idx = (idx,)
        ap = self.t[idx]
        lo = 0
        hi = 0
        fidx = list(idx[1:]) + [slice(None)] * (len(self.shape) - len(idx))
        for s, n, stride in zip(fidx, self.shape[1:], self.strides):
            if isinstance(s, slice):
                a = 0 if s.start is None else s.start
                b = n if s.stop is None else s.stop
                step = 1 if s.step is None else s.step
                cnt = (b - a + step - 1) // step
                last = a + (cnt - 1) * step
            else:
                a = s
                last = s
            lo += a * stride
            hi += last * stride
        return V(ap, self, lo, hi + 1)

    def all(self):
        return self[tuple([slice(None)] * len(self.shape))]


class MK:
    SEM_ROLL = 30000

    def __init__(self, nc, n_dma_sems=32):
        self.nc = nc
        self.es = ExitStack()
        self.sem_es = ExitStack()
        self.eng = {"pe": nc.tensor, "act": nc.scalar, "dve": nc.vector, "pool": nc.gpsimd, "sp": nc.sync}
        self.sem = {}
        self.cnt = {}
        self.nsem = 0
        for e in self.eng:
            self._new_sem(e)
        self.waited = {e: {} for e in self.eng}
        self.dma_sems = [self.es.enter_context(nc.semaphore(f"dq{i}")) for i in range(n_dma_sems)]
        self.dma_val = [0] * n_dma_sems
        self.dma_rr = 0
        self.ninst = 0
        self.nwait = 0
        self.rr = 0
        self.stk = []

    def _new_sem(self, e):
        self.sem[e] = self.sem_es.enter_context(self.nc.semaphore(f"s_{e}_{self.nsem}"))
        self.nsem += 1
        self.cnt[e] = 0

    def sb(self, name, shape, dt=F32):
        t = self.es.enter_context(self.nc.sbuf_tensor("sb_" + name, list(shape), dt))
        return Tile(self, t, shape, name)

    def ps(self, name, shape, dt=F32):
        t = self.es.enter_context(self.nc.psum_tensor("ps_" + name, list(shape), dt))
        return Tile(self, t, shape, name)

    def push(self):
        self.stk.append(self.es)
        self.es = ExitStack()

    def pop(self):
        self.es.close()
        self.es = self.stk.pop()

    def barrier(self):
        for e in self.eng:
            for o in self.eng:
                if o != e and self.cnt[o] > 0:
                    self._wait(e, self.sem[o], self.cnt[o])
            for i, s_ in enumerate(self.dma_sems):
                if self.dma_val[i] > 0:
                    self._wait(e, s_, self.dma_val[i])

    def _wait(self, e, sem, val):
        w = self.waited[e]
        if w.get(sem, 0) >= val:
            return
        w[sem] = val
        self.eng[e].wait_ge(sem, val)
        self.nwait += 1

    def _deps(self, e, reads, writes):
        for v in reads:
            if not isinstance(v, V):
                continue
            for (lo, hi, sem, val, de) in v.tile.recs_w:
                if lo < v.hi and v.lo < hi and not (de == "pe" and e == "pe"):
                    self._wait(e, sem, val)
        for v in writes:
            if not isinstance(v, V):
                continue
            for (lo, hi, sem, val, de) in v.tile.recs_w:
                if lo < v.hi and v.lo < hi and not (de == "pe" and e == "pe"):
                    self._wait(e, sem, val)
            for (lo, hi, sem, val, de) in v.tile.recs_r:
                if lo < v.hi and v.lo < hi and not (de == "pe" and e == "pe"):
                    self._wait(e, sem, val)

    def _record(self, reads, writes, sem, val, e):
        for v in writes:
            if not isinstance(v, V):
                continue
            t = v.tile
            t.recs_w = [r for r in t.recs_w if not (v.lo <= r[0] and r[1] <= v.hi)]
            t.recs_r = [r for r in t.recs_r if not (v.lo <= r[0] and r[1] <= v.hi)]
            t.recs_w.append((v.lo, v.hi, sem, val, e))
        for v in reads:
            if not isinstance(v, V):
                continue
            t = v.tile
            t.recs_r = [r for r in t.recs_r if not (r[2] is sem and v.lo <= r[0] and r[1] <= v.hi)]
            t.recs_r.append((v.lo, v.hi, sem, val, e))

    @staticmethod
    def _ap(v):
        return v.ap if isinstance(v, V) else v

    def op(self, e, fn, reads, writes):
        self._deps(e, reads, writes)
        if self.cnt[e] >= self.SEM_ROLL:
            self._new_sem(e)
        ins = fn()
        self.cnt[e] += 1
        ins.then_inc(self.sem[e], 1)
        self._record(reads, writes, self.sem[e], self.cnt[e], e)
        self.ninst += 1
        return ins

    def dma(self, e, out, in_, **kw):
        self._deps(e, [in_], [out])
        i = self.dma_rr
        self.dma_rr = (self.dma_rr + 1) % len(self.dma_sems)
        s = self.dma_sems[i]
        if self.dma_val[i] > 0:
            self._wait(e, s, self.dma_val[i])
        self.dma_val[i] += 16
        self.eng[e].dma_start(out=self._ap(out), in_=self._ap(in_), **kw).then_inc(s, 16)
        self._record([in_], [out], s, self.dma_val[i], "dma")
        self.ninst += 1

    def finish(self, e="sp"):
        for i, s in enumerate(self.dma_sems):
            if self.dma_val[i] > 0:
                self._wait(e, s, self.dma_val[i])
        for o in self.eng:
            if o != e and self.cnt[o] > 0:
                self._wait(e, self.sem[o], self.cnt[o])

    def close(self):
        while self.stk:
            self.pop()
        self.es.close()
        self.sem_es.close()

    def matmul(self, out, lhsT, rhs, start=True, stop=True, skip=False):
        return self.op("pe", lambda: self.nc.tensor.matmul(out.ap, lhsT.ap, rhs.ap, start=start, stop=stop,
                                                           skip_group_check=skip),
                       [lhsT, rhs], [out])

    def act(self, out, in_, func, bias=None, scale=None, accum_out=None):
        kw = {}
        reads = [in_]
        writes = [out]
        if bias is not None:
            kw["bias"] = self._ap(bias)
            reads.append(bias)
        if scale is not None:
            kw["scale"] = self._ap(scale)
            reads.append(scale)
        if accum_out is not None:
            kw["accum_out"] = accum_out.ap
            writes.append(accum_out)
        return self.op("act", lambda: self.nc.scalar.activation(out.ap, in_.ap, func, **kw), reads, writes)

    def tt(self, e, out, a, b, op):
        return self.op(e, lambda: self.eng[e].tensor_tensor(out.ap, a.ap, b.ap, op), [a, b], [out])

    def ts(self, e, out, a, s1, op0, s2=None, op1=None):
        reads = [a, s1, s2]
        if op1 is None:
            s2, op1 = 0.0, ALU.add
        return self.op(e, lambda: self.eng[e].tensor_scalar(out.ap, a.ap, self._ap(s1), self._ap(s2), op0, op1),
                       reads, [out])

    def stt(self, out, a, s, b, op0, op1):
        return self.op("dve", lambda: self.nc.vector.scalar_tensor_tensor(out.ap, a.ap, self._ap(s), b.ap, op0, op1),
                       [a, s, b], [out])

    def copy(self, e, out, in_):
        if e == "act":
            return self.op(e, lambda: self.nc.scalar.copy(out.ap, in_.ap), [in_], [out])
        return self.op(e, lambda: self.eng[e].tensor_copy(out.ap, in_.ap), [in_], [out])

    def memset(self, e, out, val):
        return self.op(e, lambda: self.eng[e].memset(out.ap, val), [], [out])

    def recip(self, out, in_):
        return self.op("dve", lambda: self.nc.vector.reciprocal(out.ap, in_.ap), [in_], [out])

    def evac(self, out, in_):
        self.rr ^= 1
        return self.copy("act" if self.rr else "dve", out, in_)


def tiles_of(n, mx=512):
    k = (n + mx - 1) // mx
    base = n // k
    rem = n % k
    out = []
    o = 0
    for i in range(k):
        s = base + (1 if i < rem else 0)
        out.append((o, s))
        o += s
    return out


class Prog:
    def __init__(self, name):
        import time as _t
        self.t_start = _t.time()
        self.name = name
        self.nc = bass.Bass("TRN2", target_bir_lowering=False)
        self.mk = MK(self.nc)
        self.in_names = []
        self.out_names = []
        mk = self.mk
        self.psb = [mk.ps(f"psb{i}", [128, 512], F32) for i in range(8)]
        self.ps_i = 0
        self.ones = mk.sb("ones_bf", [128, 128], BF16)
        mk.memset("dve", self.ones.all(), 1.0)
        self.eps_t = mk.sb("eps_t", [128, 1], F32)
        mk.memset("dve", self.eps_t.all(), EPS)
        self.wq = 0

    def inp(self, name, shape, dt=F32):
        self.in_names.append(name)
        return self.nc.dram_tensor(name, list(shape), dt, kind="ExternalInput").ap()

    def outp(self, name, shape, dt=F32):
        self.out_names.append(name)
        return self.nc.dram_tensor(name, list(shape), dt, kind="ExternalOutput").ap()

    def rstd_of(self, out, ssq, dim, eps=EPS):
        self.mk.act(out, ssq, AF.Sqrt, bias=self.eps_t[0:out.ap.shape[0], 0:1] if eps == EPS else eps, scale=1.0 / dim)
        self.mk.recip(out, out)

    def psum(self):
        p = self.psb[self.ps_i]
        self.ps_i = (self.ps_i + 1) % 8
        return p

    def run(self, in_maps):
        import time as _t
        self.mk.finish()
        t0 = _t.time()
        import os as _os
        if _os.environ.get("KTRACE"):
            res = run_bass_kernel_spmd(self.nc, in_maps, core_ids=list(range(NCORES)), trace=True)
            print(f"[{self.name}] exec_time_ns={res.exec_time_ns}", flush=True)
        else:
            res = run_bass_kernel_spmd(self.nc, in_maps, core_ids=list(range(NCORES)))
        print(f"[{self.name}] ninst={self.mk.ninst} nwait={self.mk.nwait} build={t0 - self.t_start:.1f}s "
              f"run={_t.time() - t0:.1f}s", flush=True)
        self.mk.close()
        return res.results

    def load_w(self, tile, w_ap, col0, ncols, nk):
        src = w_ap[:, col0:col0 + ncols].rearrange("(kc p) n -> p kc n", p=128)
        step = 4
        for k0 in range(0, nk, step):
            k1 = min(nk, k0 + step)
            self.mk.dma("pool", tile[:, k0:k1, 0:ncols], src[:, k0:k1, :])

    def rstd_fm(self, xT, nk, T, rstd, sqbuf, dim):
        mk = self.mk
        for (t0, tn) in tiles_of(T):
            ps = self.psum()
            for kc in range(nk):
                sq = sqbuf[kc % len(sqbuf)]
                mk.act(sq[:, 0:tn], xT[:, kc, t0:t0 + tn], AF.Square)
                mk.matmul(ps[:, 0:tn], self.ones.all(), sq[:, 0:tn], start=(kc == 0), stop=(kc == nk - 1))
            self.rstd_of(rstd[:, t0:t0 + tn], ps[:, 0:tn], dim)

    def modulate_fm(self, hT, xT, rstd, segs, tmp):
        mk = self.mk
        for kc in range(KC):
            for (t0, tn, a, sh) in segs:
                t = tmp[kc % len(tmp)]
                mk.tt("dve", t[:, 0:tn], xT[:, kc, t0:t0 + tn], rstd[:, t0:t0 + tn], ALU.mult)
                mk.act(hT[:, kc, t0:t0 + tn], t[:, 0:tn], AF.Identity, bias=sh[:, kc:kc + 1], scale=a[:, kc:kc + 1])

    def load_mod(self, modT, nseg):
        t = self.mk.sb("modsb", [128, nseg, 6, KC], F32)
        self.mk.dma("sp", t.all(), modT)
        return t

    def load_vec(self, name, ap_kc):
        t = self.mk.sb(name, [128, KC], F32)
        self.mk.dma("sp", t.all(), ap_kc)
        return t


def vec_pk(v):
    return np.ascontiguousarray(v.reshape(-1, 128).T)


def run_mod(c, c_ctx, mod_w, mod_b):
    P = Prog("mod")
    mk = P.mk
    NCOL = 12288 // NCORES
    cT = P.inp("cT", [128, KC, 3])
    w = P.inp("w", [4, D, NCOL])
    b = P.inp("b", [3, 4, NCOL])
    out = P.outp("out", [3, 4, NCOL])
    cs = mk.sb("cs", [128, KC, 3], F32)
    sT = mk.sb("sT", [128, KC, 3], F32)
    mk.dma("sp", cs.all(), cT)
    mk.act(sT.all(), cs.all(), AF.Silu)
    bs = mk.sb("bs", [3, 4, NCOL], F32)
    mk.dma("sp", bs.all(), b)
    os_ = mk.sb("os", [3, 4, NCOL], F32)
    wb = [mk.sb(f"wb{i}", [128, KC, 512], F32) for i in range(2)]
    it = 0
    for l in range(4):
        for n0 in range(0, NCOL, 512):
            wt = wb[it % 2]
            it += 1
            src = w[l, :, n0:n0 + 512].rearrange("(kc p) n -> p kc n", p=128)
            for k0 in range(0, KC, 4):
                mk.dma("sp" if (k0 // 4) % 2 == 0 else "act", wt[:, k0:k0 + 4, :], src[:, k0:k0 + 4, :])
            ps = P.psum()
            for kc in range(KC):
                mk.matmul(ps[0:3, :], sT[:, kc, :], wt[:, kc, :], start=(kc == 0), stop=(kc == KC - 1))
            mk.tt("dve", os_[:, l, n0:n0 + 512], ps[0:3, :], bs[:, l, n0:n0 + 512], ALU.add)
    mk.dma("sp", out, os_.all())
    cstack = np.stack([c[0], c[1], c_ctx], axis=1)
    cT_np = np.ascontiguousarray(cstack.reshape(KC, 128, 3).transpose(1, 0, 2))
    in_maps = []
    for core in range(NCORES):
        sl = slice(core * NCOL, (core + 1) * NCOL)
        in_maps.append({"cT": cT_np, "w": np.ascontiguousarray(mod_w[:, :, sl]),
                        "b": np.ascontiguousarray(np.broadcast_to(mod_b[None, :, sl], (3, 4, NCOL)))})
    res = P.run(in_maps)
    mod = np.concatenate([r["out"] for r in res], axis=2)
    return mod


def mod_layout(mod, layer, b, with_ctx=True):
    rows = [b, 2] if with_ctx else [b]
    m = mod[rows, layer]
    m = m.reshape(len(rows), 6, KC, 128).transpose(3, 0, 1, 2)
    return np.ascontiguousarray(m)


def norm_mod_stream(P, xT_d, T, segs, normw_t, mod_t, ish, isc, hT):
    mk = P.mk
    nseg = mod_t.shape[1]
    a_t = mk.sb("nm_a", [128, nseg, KC], F32)
    for s in range(nseg):
        mk.stt(a_t[:, s, :], mod_t[:, s, isc, :], 1.0, normw_t.all(), ALU.add, ALU.mult)
    xb = [mk.sb(f"nm_xb{i}", [128, T], F32) for i in range(2)]
    sq = [mk.sb(f"nm_sq{i}", [128, 512], BF16) for i in range(3)]
    rstd = mk.sb("nm_rstd", [128, T], F32)
    tls = tiles_of(T)
    pss = [P.psum() for _ in tls]
    xsrc = xT_d.rearrange("(kc p) t -> p kc t", p=128)
    for kc in range(KC):
        x = xb[kc % 2]
        mk.dma("sp" if kc % 2 == 0 else "act", x.all(), xsrc[:, kc, :])
        for ti, (t0, tn) in enumerate(tls):
            s_ = sq[(kc * len(tls) + ti) % 3]
            mk.act(s_[:, 0:tn], x[:, t0:t0 + tn], AF.Square)
            mk.matmul(pss[ti][:, 0:tn], P.ones.all(), s_[:, 0:tn], start=(kc == 0), stop=(kc == KC - 1))
    for ti, (t0, tn) in enumerate(tls):
        P.rstd_of(rstd[:, t0:t0 + tn], pss[ti][:, 0:tn], D)
    for kc in range(KC):
        x = xb[kc % 2]
        mk.dma("sp" if kc % 2 == 0 else "act", x.all(), xsrc[:, kc, :])
        mk.tt("dve", x.all(), x.all(), rstd.all(), ALU.mult)
        for (t0, tn, s) in segs:
            mk.act(hT[:, kc, t0:t0 + tn], x[:, t0:t0 + tn], AF.Identity,
                   bias=mod_t[:, s, ish, kc:kc + 1], scale=a_t[:, s, kc:kc + 1])


def proj_fm(P, hT, T, w_ap, col0, ncols, out_ap, out_dt, wbufs, stg, nk=KC, post=None):
    mk = P.mk
    tls = tiles_of(T)
    bi = 0
    for b0 in range(0, ncols, 512):
        bn = min(512, ncols - b0)
        wt = wbufs[P.wq % len(wbufs)]
        P.wq += 1
        P.load_w(wt, w_ap, col0 + b0, bn, nk)
        for n0 in range(0, bn, 128):
            st = stg[bi % len(stg)]
            bi += 1
            for (t0, tn) in tls:
                ps = P.psum()
                for kc in range(nk):
                    mk.matmul(ps[:, 0:tn], wt[:, kc, n0:n0 + 128], hT[:, kc, t0:t0 + tn],
                              start=(kc == 0), stop=(kc == nk - 1))
                if post is None:
                    mk.evac(st[:, t0:t0 + tn], ps[:, 0:tn])
                else:
                    post(st, ps, b0 + n0, t0, tn)
            mk.dma("sp", out_ap[b0 + n0:b0 + n0 + 128, :], st[:, 0:T])


def proj_tm(P, hT, T, w_ap, col0, ncols, out_ap, wbufs, stg, nk=KC):
    mk = P.mk
    bi = 0
    for b0 in range(0, ncols, 512):
        bn = min(512, ncols - b0)
        wt = wbufs[P.wq % len(wbufs)]
        P.wq += 1
        P.load_w(wt, w_ap, col0 + b0, bn, nk)
        for t0 in range(0, T, 128):
            tn = min(128, T - t0)
            st = stg[bi % len(stg)]
            bi += 1
            ps = P.psum()
            for kc in range(nk):
                mk.matmul(ps[0:tn, 0:bn], hT[:, kc, t0:t0 + tn], wt[:, kc, 0:bn], start=(kc == 0), stop=(kc == nk - 1))
            mk.evac(st[0:tn, 0:bn], ps[0:tn, 0:bn])
            mk.dma("sp", out_ap[t0:t0 + tn, b0:b0 + bn], st[0:tn, 0:bn])


def run_A(xT_list, modT_list, normw, w_in, specs, nctx):
    T = NLAT + nctx
    N = w_in.shape[1]
    P = Prog("A")
    mk = P.mk
    xT_d = P.inp("xT", [D, T])
    modT_d = P.inp("modT", [128, 2 if nctx else 1, 6, KC])
    nw_d = P.inp("nw", [128, KC])
    w_d = P.inp("w", [D, N])
    mod_t = P.load_mod(modT_d, 2 if nctx else 1)
    nw_t = P.load_vec("nw_t", nw_d)
    hT = mk.sb("hT", [128, KC, T], BF16)
    segs = [(0, NLAT, 0)] + ([(NLAT, nctx, 1)] if nctx else [])
    norm_mod_stream(P, xT_d, T, segs, nw_t, mod_t, 0, 1, hT)
    wbufs = [mk.sb(f"wbuf{i}", [128, KC, 512], BF16) for i in range(2)]
    stg_fm_b = [mk.sb(f"sfb{i}", [128, T], BF16) for i in range(3)]
    stg_fm_f = [mk.sb(f"sff{i}", [128, T], F32) for i in range(3)]
    stg_tm_b = [mk.sb(f"stb{i}", [128, 512], BF16) for i in range(3)]
    stg_tm_f = [mk.sb(f"stf{i}", [128, 512], F32) for i in range(3)]
    for (name, col0, ncols, lay, dt) in specs:
        bdt = BF16 if dt == "bf16" else F32
        if lay == "fm":
            o = P.outp(name, [ncols, T], bdt)
            proj_fm(P, hT, T, w_d, col0, ncols, o, bdt, wbufs, stg_fm_b if dt == "bf16" else stg_fm_f)
        else:
            o = P.outp(name, [T, ncols], bdt)
            proj_tm(P, hT, T, w_d, col0, ncols, o, wbufs, stg_tm_b if dt == "bf16" else stg_tm_f)
    nwk = vec_pk(normw)
    in_maps = [{"xT": xT_list[c], "modT": modT_list[c], "nw": nwk, "w": w_in} for c in range(NCORES)]
    return P.run(in_maps)


def run_C1(oT_list, xT_list, modT_list, normw, w_out, bias, nctx):
    T = NLAT + nctx
    nseg = 2 if nctx else 1
    P = Prog("C1")
    mk = P.mk
    oT_d = P.inp("oT", [D, T], BF16)
    xT_d = P.inp("xT", [D, T])
    modT_d = P.inp("modT", [128, nseg, 6, KC])
    nw_d = P.inp("nw", [128, KC])
    b_d = P.inp("bias", [128, KC])
    w_d = P.inp("w", [D, D])
    out_d = P.outp("xmid", [D, T])
    mod_t = P.load_mod(modT_d, nseg)
    nw_t = P.load_vec("nw_t", nw_d)
    b_t = P.load_vec("b_t", b_d)
    gw = mk.sb("gw", [128, nseg, KC], F32)
    for s in range(nseg):
        mk.tt("dve", gw[:, s, :], mod_t[:, s, 2, :], nw_t.all(), ALU.mult)
    wres = mk.sb("wres", [128, KC, D], BF16)
    for b0 in range(0, D, 512):
        src = w_d[:, b0:b0 + 512].rearrange("(kc p) n -> p kc n", p=128)
        for k0 in range(0, KC, 4):
            mk.dma("pool", wres[:, k0:k0 + 4, b0:b0 + 512], src[:, k0:k0 + 4, :])
    ob = [mk.sb(f"ob{i}", [128, KC, 512], BF16) for i in range(2)]
    yTb = [mk.sb(f"yT{i}", [128, KC, 512], F32) for i in range(2)]
    sq = [mk.sb(f"sq{i}", [128, 512], BF16) for i in range(2)]
    rstd = mk.sb("rstd", [128, 512], F32)
    xb = [mk.sb(f"xb{i}", [128, 512], F32) for i in range(3)]
    osrc = oT_d.rearrange("(kc p) t -> p kc t", p=128)
    xsrc = xT_d.rearrange("(kc p) t -> p kc t", p=128)
    odst = out_d.rearrange("(kc p) t -> p kc t", p=128)
    tls = [(0, 512, 0), (512, 512, 0)] + ([(NLAT, nctx, 1)] if nctx else [])
    for ti, (t0, tn, seg) in enumerate(tls):
        o = ob[ti % 2]
        yT = yTb[ti % 2]
        for k0 in range(0, KC, 4):
            mk.dma("sp", o[:, k0:k0 + 4, 0:tn], osrc[:, k0:k0 + 4, t0:t0 + tn])
        pss = P.psum()
        for n in range(KC):
            ps = P.psum()
            if ps is pss:
                ps = P.psum()
            for kc in range(KC):
                mk.matmul(ps[:, 0:tn], wres[:, kc, n * 128:(n + 1) * 128], o[:, kc, 0:tn],
                          start=(kc == 0), stop=(kc == KC - 1))
            mk.act(yT[:, n, 0:tn], ps[:, 0:tn], AF.Identity, bias=b_t[:, n:n + 1])
            s_ = sq[n % 2]
            mk.act(s_[:, 0:tn], yT[:, n, 0:tn], AF.Square)
            mk.matmul(pss[:, 0:tn], P.ones.all(), s_[:, 0:tn], start=(n == 0), stop=(n == KC - 1))
        P.rstd_of(rstd[:, 0:tn], pss[:, 0:tn], D)
        for n in range(KC):
            x = xb[n % 3]
            mk.dma("act", x[:, 0:tn], xsrc[:, n, t0:t0 + tn])
            mk.tt("dve", yT[:, n, 0:tn], yT[:, n, 0:tn], rstd[:, 0:tn], ALU.mult)
            mk.stt(x[:, 0:tn], yT[:, n, 0:tn], gw[:, seg, n:n + 1], x[:, 0:tn], ALU.mult, ALU.add)
            mk.dma("sp", odst[:, n, t0:t0 + tn], x[:, 0:tn])
    nwk = vec_pk(normw)
    bk = vec_pk(bias) if bias is not None else np.zeros((128, KC), np.float32)
    in_maps = [{"oT": oT_list[c], "xT": xT_list[c], "modT": modT_list[c], "nw": nwk, "bias": bk, "w": w_out}
               for c in range(NCORES)]
    return [r["xmid"] for r in P.run(in_maps)]


def run_C2_old(xe_list, mask_list, modT_list, nw_pre, nw_post, w_up, conv_w, conv_b, w_down, groups):
    ng = len(groups)
    nseg = 1 + max(s for _, s in groups)
    Te = sum(g + 2 for g, _ in groups)
    To = sum(g for g, _ in groups)
    NJ = DFF // 128
    P = Prog("C2")
    mk = P.mk
    xe_d = P.inp("xe", [D, Te])
    mask_d = P.inp("mask", [128, KC, 2 * ng])
    modT_d = P.inp("modT", [128, nseg, 6, KC])
    nw1_d = P.inp("nw1", [128, KC])
    nw2_d = P.inp("nw2", [128, KC])
    wu_d = P.inp("wu", [D, 2 * DFF])
    cw_d = P.inp("cw", [128, 2 * NJ, 3])
    cb_d = P.inp("cb", [128, 2 * NJ])
    wd_d = P.inp("wd", [DFF, D])
    out_d = P.outp("xo", [D, To])
    mod_t = P.load_mod(modT_d, nseg)
    nw1 = P.load_vec("nw1_t", nw1_d)
    nw2 = P.load_vec("nw2_t", nw2_d)
    mask_t = mk.sb("mask_t", [128, KC, 2 * ng], F32)
    mk.dma("sp", mask_t.all(), mask_d)
    cw = mk.sb("cw_t", [128, 2 * NJ, 3], F32)
    cb = mk.sb("cb_t", [128, 2 * NJ], F32)
    mk.dma("sp", cw.all(), cw_d)
    mk.dma("sp", cb.all(), cb_d)
    gw = mk.sb("gw", [128, nseg, KC], F32)
    for s in range(nseg):
        mk.tt("dve", gw[:, s, :], mod_t[:, s, 5, :], nw2.all(), ALU.mult)
    hT = mk.sb("hT", [128, KC, Te], BF16)
    segs = []
    o = 0
    for (G, s) in groups:
        segs.append((o, G + 2, s))
        o += G + 2
    norm_mod_stream(P, xe_d, Te, segs, nw1, mod_t, 3, 4, hT)
    o = 0
    for gi, (G, s) in enumerate(groups):
        mk.tt("dve", hT[:, :, o:o + 1], hT[:, :, o:o + 1], mask_t[:, :, 2 * gi:2 * gi + 1], ALU.mult)
        mk.tt("dve", hT[:, :, o + G + 1:o + G + 2], hT[:, :, o + G + 1:o + G + 2],
              mask_t[:, :, 2 * gi + 1:2 * gi + 2], ALU.mult)
        o += G + 2
    GM = max(g for g, _ in groups)
    actT = mk.sb("actT", [128, NJ, GM], BF16)
    wub = [mk.sb(f"wub{i}", [128, KC, 2, 128], BF16) for i in range(2)]
    ub = [mk.sb(f"ub{i}", [128, GM + 2], F32) for i in range(4)]
    cvb = [mk.sb(f"cvb{i}", [128, GM], F32) for i in range(4)]
    wdb = [mk.sb(f"wdb{i}", [128, NJ, 128], BF16) for i in range(2)]
    yT = mk.sb("yT", [128, KC, GM], F32)
    sq = [mk.sb(f"sq{i}", [128, 512], BF16) for i in range(2)]
    rstd = mk.sb("rstd", [128, 512], F32)
    xb = [mk.sb(f"xb{i}", [128, GM], F32) for i in range(3)]
    xsrc = xe_d.rearrange("(kc p) t -> p kc t", p=128)
    odst = out_d.rearrange("(kc p) t -> p kc t", p=128)
    wusrc = wu_d.rearrange("(kc p) n -> p kc n", p=128)
    wdsrc = wd_d.rearrange("(j p) n -> p j n", p=128)
    eo = 0
    oo = 0
    it = 0
    ui = 0
    for gi, (G, s) in enumerate(groups):
        Gx = G + 2
        tls = tiles_of(Gx)
        for j0 in range(0, NJ, 1):
            wt = wub[it % 2]
            it += 1
            for half, cbase in ((0, j0 * 128), (1, DFF + j0 * 128)):
                mk.dma("pool", wt[:, :, half, :], wusrc[:, :, cbase:cbase + 128])
            for jj in range(1):
                j = j0 + jj
                us = []
                for half in range(2):
                    u = ub[ui % 4]
                    ui += 1
                    for (t0, tn) in tls:
                        ps = P.psum()
                        for kc in range(KC):
                            mk.matmul(ps[:, 0:tn], wt[:, kc, half, jj * 128:(jj + 1) * 128],
                                      hT[:, kc, eo + t0:eo + t0 + tn], start=(kc == 0), stop=(kc == KC - 1))
                        mk.copy("act" if half == 0 else "dve", u[:, t0:t0 + tn], ps[:, 0:tn])
                    us.append(u)
                cs = []
                for half in range(2):
                    ch = half * NJ + j
                    u = us[half]
                    cv = cvb[(2 * j + half) % 4]
                    mk.act(cv[:, 0:G], u[:, 1:G + 1], AF.Identity, bias=cb[:, ch:ch + 1], scale=cw[:, ch, 1:2])
                    mk.stt(cv[:, 0:G], u[:, 0:G], cw[:, ch, 0:1], cv[:, 0:G], ALU.mult, ALU.add)
                    mk.stt(cv[:, 0:G], u[:, 2:G + 2], cw[:, ch, 2:3], cv[:, 0:G], ALU.mult, ALU.add)
                    cs.append(cv)
                mk.act(cs[1][:, 0:G], cs[1][:, 0:G], AF.Silu)
                mk.tt("dve", actT[:, j, 0:G], cs[0][:, 0:G], cs[1][:, 0:G], ALU.mult)
        pss = P.psum()
        for n0 in range(0, KC, 1):
            wd = wdb[n0 % 2]
            for j0 in range(0, NJ, 11):
                mk.dma("pool", wd[:, j0:j0 + 11, :], wdsrc[:, j0:j0 + 11, n0 * 128:n0 * 128 + 128])
            for nn in range(1):
                n = n0 + nn
                ps = P.psum()
                if ps is pss:
                    ps = P.psum()
                for j in range(NJ):
                    mk.matmul(ps[:, 0:G], wd[:, j, nn * 128:(nn + 1) * 128], actT[:, j, 0:G],
                              start=(j == 0), stop=(j == NJ - 1))
                mk.copy("act", yT[:, n, 0:G], ps[:, 0:G])
                s_ = sq[n % 2]
                mk.act(s_[:, 0:G], yT[:, n, 0:G], AF.Square)
                mk.matmul(pss[:, 0:G], P.ones.all(), s_[:, 0:G], start=(n == 0), stop=(n == KC - 1))
        P.rstd_of(rstd[:, 0:G], pss[:, 0:G], D)
        for n in range(KC):
            x = xb[n % 3]
            mk.dma("act", x[:, 0:G], xsrc[:, n, eo + 1:eo + 1 + G])
            mk.tt("dve", yT[:, n, 0:G], yT[:, n, 0:G], rstd[:, 0:G], ALU.mult)
            mk.stt(x[:, 0:G], yT[:, n, 0:G], gw[:, s, n:n + 1], x[:, 0:G], ALU.mult, ALU.add)
            mk.dma("sp", odst[:, n, oo:oo + G], x[:, 0:G])
        eo += Gx
        oo += G
    cwk = np.ascontiguousarray(conv_w.T.reshape(2 * NJ, 128, 3).transpose(1, 0, 2))
    cbk = np.ascontiguousarray(conv_b.reshape(2 * NJ, 128).T)
    in_maps = [{"xe": xe_list[c], "mask": mask_list[c], "modT": modT_list[c], "nw1": vec_pk(nw_pre),
                "nw2": vec_pk(nw_post), "wu": w_up, "cw": cwk, "cb": cbk, "wd": w_down} for c in range(NCORES)]
    return [r["xo"] for r in P.run(in_maps)]


def run_C2(xe_list, mask_list, modT_list, nw_pre, nw_post, w_up, conv_w, conv_b, w_down, groups):
    ng = len(groups)
    nseg = 1 + max(s for _, s in groups)
    Te = sum(g + 2 for g, _ in groups)
    To = sum(g for g, _ in groups)
    NJ = DFF // 128
    P = Prog("C2")
    mk = P.mk
    xe_d = P.inp("xe", [D, Te])
    mask_d = P.inp("mask", [128, KC, 2 * ng])
    modT_d = P.inp("modT", [128, nseg, 6, KC])
    nw1_d = P.inp("nw1", [128, KC])
    nw2_d = P.inp("nw2", [128, KC])
    wu_d = P.inp("wu", [D, 2 * DFF])
    cw_d = P.inp("cw", [128, 2 * NJ, 3])
    cb_d = P.inp("cb", [128, 2 * NJ])
    wd_d = P.inp("wd", [DFF, D])
    out_d = P.outp("xo", [D, To])
    ysc_d = P.nc.dram_tensor("ysc", [D, To], F32, kind="Internal").ap()
    mod_t = P.load_mod(modT_d, nseg)
    nw1 = P.load_vec("nw1_t", nw1_d)
    nw2 = P.load_vec("nw2_t", nw2_d)
    mask_t = mk.sb("mask_t", [128, KC, 2 * ng], F32)
    mk.dma("sp", mask_t.all(), mask_d)
    cw = mk.sb("cw_t", [128, 2 * NJ, 3], F32)
    cb = mk.sb("cb_t", [128, 2 * NJ], F32)
    mk.dma("sp", cw.all(), cw_d)
    mk.dma("sp", cb.all(), cb_d)
    gw = mk.sb("gw", [128, nseg, KC], F32)
    for s in range(nseg):
        mk.tt("dve", gw[:, s, :], mod_t[:, s, 5, :], nw2.all(), ALU.mult)
    actT = mk.sb("actT", [128, NJ, To], BF16)
    xsrc = xe_d.rearrange("(kc p) t -> p kc t", p=128)
    odst = out_d.rearrange("(kc p) t -> p kc t", p=128)
    ysc = ysc_d.rearrange("(kc p) t -> p kc t", p=128)
    wusrc = wu_d.rearrange("(kc p) n -> p kc n", p=128)
    wdsrc = wd_d.rearrange("(j p) n -> p j n", p=128)
    geo = []
    eo = oo = 0
    for (G, s) in groups:
        geo.append((eo, oo, G, s))
        eo += G + 2
        oo += G
    mk.push()
    hT = mk.sb("hT", [128, KC, Te], BF16)
    mk.push()
    segs = [(e0, G + 2, s) for (e0, o0, G, s) in geo]
    norm_mod_stream(P, xe_d, Te, segs, nw1, mod_t, 3, 4, hT)
    for gi, (e0, o0, G, s) in enumerate(geo):
        mk.tt("dve", hT[:, :, e0:e0 + 1], hT[:, :, e0:e0 + 1], mask_t[:, :, 2 * gi:2 * gi + 1], ALU.mult)
        mk.tt("dve", hT[:, :, e0 + G + 1:e0 + G + 2], hT[:, :, e0 + G + 1:e0 + G + 2],
              mask_t[:, :, 2 * gi + 1:2 * gi + 2], ALU.mult)
    mk.barrier()
    mk.pop()
    mk.push()
    wub = [mk.sb(f"wub{i}", [128, KC, 2, 128], BF16) for i in range(3)]
    ub = [mk.sb(f"ub{i}", [128, Te], F32) for i in range(4)]
    cvb = [mk.sb(f"cvb{i}", [128, To], F32) for i in range(2)]
    tle = tiles_of(Te)
    for j in range(NJ):
        wt = wub[j % 3]
        for half, cbase in ((0, j * 128), (1, DFF + j * 128)):
            mk.dma("pool", wt[:, :, half, :], wusrc[:, :, cbase:cbase + 128])
        us = []
        for half in range(2):
            u = ub[(2 * j + half) % 4]
            for (t0, tn) in tle:
                ps = P.psum()
                for kc in range(KC):
                    mk.matmul(ps[:, 0:tn], wt[:, kc, half, :], hT[:, kc, t0:t0 + tn],
                              start=(kc == 0), stop=(kc == KC - 1))
                mk.copy("act" if half == 0 else "dve", u[:, t0:t0 + tn], ps[:, 0:tn])
            us.append(u)
        for half in range(2):
            ch = half * NJ + j
            u = us[half]
            cv = cvb[half]
            for (e0, o0, G, s) in geo:
                mk.act(cv[:, o0:o0 + G], u[:, e0 + 1:e0 + 1 + G], AF.Identity, bias=cb[:, ch:ch + 1],
                       scale=cw[:, ch, 1:2])
                mk.stt(cv[:, o0:o0 + G], u[:, e0:e0 + G], cw[:, ch, 0:1], cv[:, o0:o0 + G], ALU.mult, ALU.add)
                mk.stt(cv[:, o0:o0 + G], u[:, e0 + 2:e0 + 2 + G], cw[:, ch, 2:3], cv[:, o0:o0 + G], ALU.mult, ALU.add)
        mk.act(cvb[1].all(), cvb[1].all(), AF.Silu)
        mk.tt("dve", actT[:, j, :], cvb[0].all(), cvb[1].all(), ALU.mult)
    mk.barrier()
    mk.pop()
    mk.pop()
    wdb = [mk.sb(f"wdb{i}", [128, NJ, 128], BF16) for i in range(2)]
    yst = [mk.sb(f"yst{i}", [128, To], F32) for i in range(2)]
    sq = [mk.sb(f"sq{i}", [128, 512], BF16) for i in range(2)]
    rstd = mk.sb("rstd", [128, To], F32)
    xb = [mk.sb(f"xb{i}", [128, Te], F32) for i in range(2)]
    tlo = tiles_of(To)
    pss = [P.psb[5 + i] for i in range(len(tlo))]
    pk = 0
    for n in range(KC):
        wd = wdb[n % 2]
        for j0 in range(0, NJ, 11):
            mk.dma("pool", wd[:, j0:j0 + 11, :], wdsrc[:, j0:j0 + 11, n * 128:(n + 1) * 128])
        y = yst[n % 2]
        for ti, (t0, tn) in enumerate(tlo):
            ps = P.psb[pk % 5]
            pk += 1
            for j in range(NJ):
                mk.matmul(ps[:, 0:tn], wd[:, j, :], actT[:, j, t0:t0 + tn], start=(j == 0), stop=(j == NJ - 1))
            mk.copy("act", y[:, t0:t0 + tn], ps[:, 0:tn])
            s_ = sq[(n * len(tlo) + ti) % 2]
            mk.act(s_[:, 0:tn], y[:, t0:t0 + tn], AF.Square)
            mk.matmul(pss[ti][:, 0:tn], P.ones.all(), s_[:, 0:tn], start=(n == 0), stop=(n == KC - 1))
        mk.dma("sp", ysc[:, n, :], y.all())
    for ti, (t0, tn) in enumerate(tlo):
        P.rstd_of(rstd[:, t0:t0 + tn], pss[ti][:, 0:tn], D)
    mk.barrier()
    for n in range(KC):
        y = yst[n % 2]
        x = xb[n % 2]
        mk.dma("sp", y.all(), ysc[:, n, :])
        mk.dma("act", x.all(), xsrc[:, n, :])
        mk.tt("dve", y.all(), y.all(), rstd.all(), ALU.mult)
        for (e0, o0, G, s) in geo:
            mk.stt(y[:, o0:o0 + G], y[:, o0:o0 + G], gw[:, s, n:n + 1], x[:, e0 + 1:e0 + 1 + G], ALU.mult, ALU.add)
        mk.dma("sp", odst[:, n, :], y.all())
    P.ps_i = 0
    cwk = np.ascontiguousarray(conv_w.T.reshape(2 * NJ, 128, 3).transpose(1, 0, 2))
    cbk = np.ascontiguousarray(conv_b.reshape(2 * NJ, 128).T)
    in_maps = [{"xe": xe_list[c], "mask": mask_list[c], "modT": modT_list[c], "nw1": vec_pk(nw_pre),
                "nw2": vec_pk(nw_post), "wu": w_up, "cw": cwk, "cb": cbk, "wd": w_down} for c in range(NCORES)]
    return [r["xo"] for r in P.run(in_maps)]


def ffn_host_io(xmid_lat, xmid_ctx, nctx):
    xe_list, mask_list = [], []
    groups = [(512, 0), (512, 0)] + ([(nctx, 1)] if nctx else [])
    for c in range(NCORES):
        b, q = c // 4, c % 4
        cols = []
        m = []
        for g in range(2):
            p0 = q * NLAT + g * 512
            blk = np.zeros((514, D), np.float32)
            lo, hi = p0 - 1, p0 + 513
            slo, shi = max(lo, 0), min(hi, SEQ)
            blk[slo - lo:shi - lo] = xmid_lat[b, slo:shi]
            cols.append(blk)
            m += [1.0 if lo >= 0 else 0.0, 1.0 if hi <= SEQ else 0.0]
        if nctx:
            blk = np.zeros((nctx + 2, D), np.float32)
            lo, hi = q * nctx - 1, q * nctx + nctx + 1
            slo, shi = max(lo, 0), min(hi, NCTX)
            blk[slo - lo:shi - lo] = xmid_ctx[b, slo:shi]
            cols.append(blk)
            m += [1.0 if lo >= 0 else 0.0, 1.0 if hi <= NCTX else 0.0]
        xe = np.ascontiguousarray(np.concatenate(cols, axis=0).T)
        xe_list.append(xe)
        mask_list.append(np.ascontiguousarray(np.broadcast_to(np.array(m, np.float32)[None, None, :], (128, KC, len(m)))))
    return xe_list, mask_list, groups


def rope_tables():
    half = 32
    freqs = 10000.0 ** (-np.arange(half, dtype=np.float32) / half)
    pos = np.arange(SEQ)
    row, col = pos // 64, pos % 64
    ang = np.zeros((128, SEQ), np.float32)
    for d in range(128):
        p = row if d < 64 else col
        ang[d] = p.astype(np.float32) * freqs[(d % 64) % 32]
    R = np.zeros((128, 128), np.float32)
    for d in range(128):
        if (d % 64) < 32:
            R[d + 32, d] = -1.0
        else:
            R[d - 32, d] = 1.0
    return np.cos(ang).astype(np.float32), np.sin(ang).astype(np.float32), R.astype(NPBF)


def run_B_gqa(qT_list, kT_list, v_list, q_norm, k_norm):
    T = NLAT + NCL
    NK = SEQ + NCTX
    NCH = NK // 128
    P = Prog("Bgqa")
    mk = P.mk
    qT_d = P.inp("qT", [D, T], BF16)
    kT_d = P.inp("kT", [512, NK], BF16)
    v_d = P.inp("v", [NK, 512], BF16)
    gn_d = P.inp("gn", [128, 2])
    cos_d = P.inp("cos", [128, SEQ])
    sin_d = P.inp("sin", [128, SEQ])
    cosq_d = P.inp("cosq", [128, NLAT])
    sinq_d = P.inp("sinq", [128, NLAT])
    R_d = P.inp("R", [128, 128], BF16)
    oT_d = P.outp("oT", [D, T], BF16)
    gn = mk.sb("gn", [128, 2], F32)
    mk.dma("sp", gn.all(), gn_d)
    mk.ts("dve", gn[:, 0:1], gn[:, 0:1], 128.0 ** -0.5, ALU.mult)
    Rm = mk.sb("Rm", [128, 128], BF16)
    mk.dma("sp", Rm.all(), R_d)
    cosk = mk.sb("cosk", [128, SEQ], F32)
    sink = mk.sb("sink", [128, SEQ], F32)
    cosq = mk.sb("cosq", [128, NLAT], F32)
    sinq = mk.sb("sinq", [128, NLAT], F32)
    mk.dma("sp", cosk.all(), cos_d)
    mk.dma("act", sink.all(), sin_d)
    mk.dma("sp", cosq.all(), cosq_d)
    mk.dma("act", sinq.all(), sinq_d)
    kT = mk.sb("kT", [128, 4, NK], BF16)
    mk.dma("sp", kT.all(), kT_d.rearrange("(h p) t -> p h t", p=128))
    qT = mk.sb("qT", [128, 16, T], BF16)
    for h0 in range(0, 16, 4):
        mk.dma("act", qT[:, h0:h0 + 4, :], qT_d.rearrange("(h p) t -> p h t", p=128)[:, h0:h0 + 4, :])
    vt = mk.sb("vt", [128, NCH, 512], BF16)
    vsrc = v_d.rearrange("(c p) n -> p c n", p=128)
    for c0 in range(0, NCH, 17):
        mk.dma("sp", vt[:, c0:c0 + 17, :], vsrc[:, c0:c0 + 17, :])
    sqb = [mk.sb(f"sqb{i}", [128, 512], BF16) for i in range(2)]
    rsb = [mk.sb(f"rsb{i}", [128, 512], F32) for i in range(2)]
    knb = [mk.sb(f"knb{i}", [128, 512], BF16) for i in range(2)]
    t1b = [mk.sb(f"t1b{i}", [128, 512], F32) for i in range(2)]
    t2b = [mk.sb(f"t2b{i}", [128, 512], F32) for i in range(2)]
    cnt = [0]

    def normrope(buf, h, t0, tn, gcol, cs, sn, c0):
        i = cnt[0] % 2
        cnt[0] += 1
        x = buf[:, h, t0:t0 + tn]
        mk.act(sqb[i][:, 0:tn], x, AF.Square)
        ps = P.psum()
        mk.matmul(ps[:, 0:tn], P.ones.all(), sqb[i][:, 0:tn])
        P.rstd_of(rsb[i][:, 0:tn], ps[:, 0:tn], 128)
        if cs is None:
            mk.stt(x, x, gcol, rsb[i][:, 0:tn], ALU.mult, ALU.mult)
            return
        mk.stt(knb[i][:, 0:tn], x, gcol, rsb[i][:, 0:tn], ALU.mult, ALU.mult)
        ps2 = P.psum()
        mk.matmul(ps2[:, 0:tn], Rm.all(), knb[i][:, 0:tn])
        mk.tt("pool", t1b[i][:, 0:tn], knb[i][:, 0:tn], cs[:, c0:c0 + tn], ALU.mult)
        mk.tt("dve", t2b[i][:, 0:tn], ps2[:, 0:tn], sn[:, c0:c0 + tn], ALU.mult)
        mk.tt("pool", x, t1b[i][:, 0:tn], t2b[i][:, 0:tn], ALU.add)

    for kv in range(4):
        for t0 in range(0, SEQ, 512):
            normrope(kT, kv, t0, 512, gn[:, 1:2], cosk, sink, t0)
        normrope(kT, kv, SEQ, NCTX, gn[:, 1:2], None, None, 0)
    pT = [mk.sb(f"pT{i}", [128, 512], BF16) for i in range(4)]
    rcp = [mk.sb(f"rcp{i}", [128, 512], F32) for i in range(2)]
    ost = [mk.sb(f"ost{i}", [128, T], BF16) for i in range(2)]
    pi = 0
    acc_i = 0
    for h in range(16):
        kv = h // 4
        for t0 in (0, 512):
            normrope(qT, h, t0, 512, gn[:, 0:1], cosq, sinq, t0)
        normrope(qT, h, NLAT, NCL, gn[:, 0:1], None, None, 0)
        st = ost[h % 2]
        for (t0, tn, chunks) in ((0, 512, range(NCH)), (512, 512, range(NCH)), (NLAT, NCL, range(32, NCH))):
            ps_o = P.psb[4 + 2 * (acc_i % 2)]
            ps_s = P.psb[5 + 2 * (acc_i % 2)]
            acc_i += 1
            chunks = list(chunks)
            LOOK = 2
            pend = []
            for ci in range(len(chunks) + LOOK):
                if ci < len(chunks):
                    c = chunks[ci]
                    ps = P.psb[pi % 4]
                    p_ = pT[pi % 4]
                    pi += 1
                    mk.matmul(ps[:, 0:tn], kT[:, kv, c * 128:(c + 1) * 128], qT[:, h, t0:t0 + tn])
                    mk.act(p_[:, 0:tn], ps[:, 0:tn], AF.Exp)
                    pend.append((ci, c, p_))
                if ci >= LOOK:
                    cj, c, p_ = pend.pop(0)
                    mk.matmul(ps_o[:, 0:tn], vt[:, c, kv * 128:(kv + 1) * 128], p_[:, 0:tn],
                              start=(cj == 0), stop=(cj == len(chunks) - 1))
                    mk.matmul(ps_s[:, 0:tn], P.ones.all(), p_[:, 0:tn], start=(cj == 0),
                              stop=(cj == len(chunks) - 1))
            r = rcp[acc_i % 2]
            mk.recip(r[:, 0:tn], ps_s[:, 0:tn])
            mk.tt("dve", st[:, t0:t0 + tn], ps_o[:, 0:tn], r[:, 0:tn], ALU.mult)
        mk.dma("sp", oT_d[h * 128:(h + 1) * 128, :], st.all())
    P.ps_i = 0
    cos, sin, R = rope_tables()
    gnk = np.ascontiguousarray(np.stack([q_norm, k_norm], axis=1).astype(np.float32))
    in_maps = []
    for c in range(NCORES):
        q = c % 4
        in_maps.append({"qT": qT_list[c], "kT": kT_list[c], "v": v_list[c], "gn": gnk, "cos": cos, "sin": sin,
                        "cosq": np.ascontiguousarray(cos[:, q * NLAT:(q + 1) * NLAT]),
                        "sinq": np.ascontiguousarray(sin[:, q * NLAT:(q + 1) * NLAT]), "R": R})
    return [r["oT"] for r in P.run(in_maps)]


def to_fm(x_tok):
    return np.ascontiguousarray(x_tok.T)


def core_xT(x_lat, x_ctx, nctx):
    out = []
    for c in range(NCORES):
        b, q = c // 4, c % 4
        parts = [x_lat[b, q * NLAT:(q + 1) * NLAT]]
        if nctx:
            parts.append(x_ctx[b, q * nctx:(q + 1) * nctx])
        out.append(to_fm(np.concatenate(parts, axis=0)))
    return out


def layer_gqa(x_lat, x_ctx, mod, L, inp):
    modT = [mod_layout(mod, L, c // 4) for c in range(NCORES)]
    xT = core_xT(x_lat, x_ctx, NCL)
    specs = [("qT", 0, 2048, "fm", "bf16"), ("kT", 2048, 512, "fm", "bf16"), ("v", 2560, 512, "tm", "bf16")]
    ra = run_A(xT, modT, inp["norm_pre_mix"][L], inp["gqa_w_in"][0], specs, NCL)
    kT_list, v_list = [], []
    for c in range(NCORES):
        b = c // 4
        kT_list.append(np.ascontiguousarray(np.concatenate(
            [ra[4 * b + i]["kT"][:, 0:NLAT] for i in range(4)] + [ra[4 * b + i]["kT"][:, NLAT:] for i in range(4)],
            axis=1)))
        v_list.append(np.ascontiguousarray(np.concatenate(
            [ra[4 * b + i]["v"][0:NLAT] for i in range(4)] + [ra[4 * b + i]["v"][NLAT:] for i in range(4)], axis=0)))
    oT = run_B_gqa([r["qT"] for r in ra], kT_list, v_list, inp["gqa_q_norm"][0], inp["gqa_k_norm"][0])
    xmid = run_C1(oT, xT, modT, inp["norm_post_mix"][L], inp["gqa_w_out"][0], None, NCL)
    return xmid


def split_xT(xT_list, nctx):
    x_lat = np.zeros((2, SEQ, D), np.float32)
    x_ctx = np.zeros((2, 4 * nctx, D), np.float32) if nctx else None
    for c in range(NCORES):
        b, q = c // 4, c % 4
        x_lat[b, q * NLAT:(q + 1) * NLAT] = xT_list[c][:, 0:NLAT].T
        if nctx:
            x_ctx[b, q * nctx:(q + 1) * nctx] = xT_list[c][:, NLAT:NLAT + nctx].T
    return x_lat, x_ctx


def layer_ffn(xmid_lat, xmid_ctx, mod, L, inp, nctx):
    modT = [mod_layout(mod, L, c // 4, with_ctx=bool(nctx)) for c in range(NCORES)]
    xe_list, mask_list, groups = ffn_host_io(xmid_lat, xmid_ctx, nctx)
    xo = run_C2(xe_list, mask_list, modT, inp["norm_pre_ffn"][L], inp["norm_post_ffn"][L], inp["ffn_w_up"][L],
                inp["ffn_conv_w"][L], inp["ffn_conv_b"][L], inp["ffn_w_down"][L], groups)
    return split_xT(xo, nctx)


def run_B_conv(zT_list, mask_list, b_pw1, w_dw, b_dw, ln_w, ln_b, nctx):
    HW = 15
    Le = NLAT + 2 * HW
    Ce = nctx + 2 * HW
    Te = Le + Ce
    T = NLAT + nctx
    P = Prog("Bconv")
    mk = P.mk
    zT_d = P.inp("zT", [2 * D, Te])
    mask_d = P.inp("mask", [128, Te])
    bp_d = P.inp("bp", [128, 2 * KC])
    wdw_d = P.inp("wdw", [128, KC, 31])
    bdw_d = P.inp("bdw", [128, KC])
    lnw_d = P.inp("lnw", [128, KC])
    lnb_d = P.inp("lnb", [128, KC])
    sT_d = P.outp("sT", [D, T], BF16)
    id_d = P.inp("ident", [128, 128], BF16)
    maskt = mk.sb("maskt", [128, Te], F32)
    mk.dma("sp", maskt.all(), mask_d)
    bp = mk.sb("bp", [128, 2 * KC], F32)
    mk.dma("sp", bp.all(), bp_d)
    wdw = mk.sb("wdw", [128, KC, 31], F32)
    mk.dma("sp", wdw.all(), wdw_d)
    bdw = P.load_vec("bdw", bdw_d)
    lnw = P.load_vec("lnw", lnw_d)
    lnb = P.load_vec("lnb", lnb_d)
    onesf = mk.sb("onesf", [128, 128], F32)
    mk.memset("dve", onesf.all(), 1.0)
    vT = mk.sb("vT", [128, KC, T], F32)
    zb = [mk.sb(f"zb{i}", [128, Te], F32) for i in range(4)]
    ub = [mk.sb(f"ub{i}", [128, Te], BF16) for i in range(2)]
    sqf = [mk.sb(f"sqf{i}", [128, 512], F32) for i in range(2)]
    dgb = [mk.sb(f"dgb{i}", [128, 31, 128], BF16) for i in range(2)]
    identb = mk.sb("identb", [128, 128], BF16)
    mk.dma("sp", identb.all(), id_d)
    pcv = [0]
    zsrc = zT_d.rearrange("(kc p) t -> p kc t", p=128)
    tls = tiles_of(T)
    ps_sum = [P.psb[i] for i in range(len(tls))]
    ps_sq = [P.psb[3 + i] for i in range(len(tls))]
    for kc in range(KC):
        za = zb[(2 * kc) % 4]
        zg = zb[(2 * kc + 1) % 4]
        mk.dma("sp", za.all(), zsrc[:, kc, :])
        mk.dma("act", zg.all(), zsrc[:, KC + kc, :])
        mk.act(zg.all(), zg.all(), AF.Sigmoid, bias=bp[:, KC + kc:KC + kc + 1])
        mk.tt("pool", zg.all(), zg.all(), maskt.all(), ALU.mult)
        u = ub[kc % 2]
        mk.stt(u.all(), za.all(), bp[:, kc:kc + 1], zg.all(), ALU.add, ALU.mult)
        dg = dgb[kc % 2]
        for j in range(31):
            mk.act(dg[:, j, :], identb.all(), AF.Copy, scale=wdw[:, kc, j:j + 1])
        for (e0, o0, n) in ((0, 0, NLAT), (Le, NLAT, nctx)):
            if n == 0:
                continue
            for (t0, tn) in tiles_of(n):
                ps = P.psb[6 + (pcv[0] % 2)]
                pcv[0] += 1
                for j in range(31):
                    mk.matmul(ps[:, 0:tn], dg[:, j, :], u[:, e0 + j + t0:e0 + j + t0 + tn],
                              start=(j == 0), stop=(j == 30))
                mk.act(vT[:, kc, o0 + t0:o0 + t0 + tn], ps[:, 0:tn], AF.Identity, bias=bdw[:, kc:kc + 1])
        for ti, (t0, tn) in enumerate(tls):
            s_ = sqf[(kc * len(tls) + ti) % 2]
            mk.act(s_[:, 0:tn], vT[:, kc, t0:t0 + tn], AF.Square)
            mk.matmul(ps_sum[ti][:, 0:tn], onesf.all(), vT[:, kc, t0:t0 + tn], start=(kc == 0), stop=(kc == KC - 1))
            mk.matmul(ps_sq[ti][:, 0:tn], onesf.all(), s_[:, 0:tn], start=(kc == 0), stop=(kc == KC - 1))
    mean = mk.sb("mean", [128, T], F32)
    rstd = mk.sb("rstd", [128, T], F32)
    msq = mk.sb("msq", [128, T], F32)
    for ti, (t0, tn) in enumerate(tls):
        mk.ts("dve", mean[:, t0:t0 + tn], ps_sum[ti][:, 0:tn], 1.0 / D, ALU.mult)
        mk.tt("dve", msq[:, t0:t0 + tn], mean[:, t0:t0 + tn], mean[:, t0:t0 + tn], ALU.mult)
        mk.stt(msq[:, t0:t0 + tn], ps_sq[ti][:, 0:tn], 1.0 / D, msq[:, t0:t0 + tn], ALU.mult, ALU.subtract)
        mk.act(rstd[:, t0:t0 + tn], msq[:, t0:t0 + tn], AF.Sqrt, bias=P.eps_t[:, 0:1])
        mk.recip(rstd[:, t0:t0 + tn], rstd[:, t0:t0 + tn])
    sb_ = [mk.sb(f"sbo{i}", [128, T], BF16) for i in range(2)]
    tmp = [mk.sb(f"tmpn{i}", [128, T], F32) for i in range(2)]
    for kc in range(KC):
        t = tmp[kc % 2]
        mk.tt("pool", t.all(), vT[:, kc, :], mean.all(), ALU.subtract)
        mk.tt("dve", t.all(), t.all(), rstd.all(), ALU.mult)
        so = sb_[kc % 2]
        mk.act(so.all(), t.all(), AF.Silu, bias=lnb[:, kc:kc + 1], scale=lnw[:, kc:kc + 1])
        mk.dma("sp", sT_d[kc * 128:(kc + 1) * 128, :], so.all())
    bpk = np.ascontiguousarray(b_pw1.reshape(2 * KC, 128).T)
    wdwk = np.ascontiguousarray(w_dw.T.reshape(KC, 128, 31).transpose(1, 0, 2))
    in_maps = [{"zT": zT_list[c], "mask": mask_list[c], "bp": bpk, "wdw": wdwk, "bdw": vec_pk(b_dw),
                "lnw": vec_pk(ln_w), "lnb": vec_pk(ln_b), "ident": np.eye(128, dtype=np.float32).astype(NPBF)}
               for c in range(NCORES)]
    return [r["sT"] for r in P.run(in_maps)]


def layer_conv(x_lat, x_ctx, mod, L, inp):
    HW = 15
    modT = [mod_layout(mod, L, c // 4) for c in range(NCORES)]
    xT = core_xT(x_lat, x_ctx, NCL)
    ra = run_A(xT, modT, inp["norm_pre_mix"][L], inp["conv_w_pw1"][0], [("zT", 0, 2 * D, "fm", "f32")], NCL)
    zT_list, mask_list = [], []
    for b in range(2):
        zl = np.concatenate([ra[4 * b + i]["zT"][:, 0:NLAT] for i in range(4)], axis=1)
        zl = np.pad(zl, ((0, 0), (HW, HW)))
        ml = np.pad(np.ones(SEQ, np.float32), (HW, HW))
        zc = np.pad(np.concatenate([ra[4 * b + i]["zT"][:, NLAT:] for i in range(4)], axis=1), ((0, 0), (HW, HW)))
        mc = np.pad(np.ones(NCTX, np.float32), (HW, HW))
        for q in range(4):
            sl = slice(q * NLAT, q * NLAT + NLAT + 2 * HW)
            slc = slice(q * NCL, q * NCL + NCL + 2 * HW)
            zT_list.append(np.ascontiguousarray(np.concatenate([zl[:, sl], zc[:, slc]], axis=1)))
            m = np.concatenate([ml[sl], mc[slc]])
            mask_list.append(np.ascontiguousarray(np.broadcast_to(m[None, :], (128, m.shape[0]))))
    sT = run_B_conv(zT_list, mask_list, inp["conv_b_pw1"][0], inp["conv_w_dw"][0], inp["conv_b_dw"][0],
                    inp["conv_ln_w"][0], inp["conv_ln_b"][0], NCL)
    xmid = run_C1(sT, xT, modT, inp["norm_post_mix"][L], inp["conv_w_pw2"][0], inp["conv_b_pw2"][0], NCL)
    return xmid


def rview(v, pattern, **kw):
    return V(v.ap.rearrange(pattern, **kw), v.tile, v.lo, v.hi)


def nat_tables(rpb):
    p = np.arange(128)
    half, kc = p // 64, p % 64
    i = np.arange(8)
    qc = np.arange(64)
    dr = -8 + 2 * i[None, :] + half[:, None]
    cidx = kc[:, None] - qc[None, :] + 15
    cstart = np.clip(qc - 8, 0, 48)
    cval = (kc[:, None] >= cstart[None, :]) & (kc[:, None] < cstart[None, :] + 16)
    rv = dr >= -7
    B = rpb[:, np.clip(dr + 7, 0, 14)[:, :, None], np.clip(cidx, 0, 30)[:, None, :]]
    B = np.ascontiguousarray(B.transpose(1, 0, 2, 3)).astype(np.float32)
    M = (rv[:, :, None] & cval[:, None, :]).astype(np.float32)
    M = np.ascontiguousarray(np.broadcast_to(M[:, None], (128, 8, 8, 64)))
    return B, M


def nat_rowvalid(q):
    p = np.arange(128)
    half = p // 64
    out = np.zeros((128, 16, 8), np.float32)
    for rl in range(16):
        r = 16 * q + rl
        rs = min(max(r - 4, 0), 56)
        for i in range(8):
            kr = r - 8 + 2 * i + half
            out[:, rl, i] = ((kr >= rs) & (kr < rs + 8)).astype(np.float32)
    return out


def run_B_nat(qT_list, kTw_list, vw_list, kTc_list, vc_list, rpb):
    P = Prog("Bnat")
    mk = P.mk
    NW = 2048
    qT_d = P.inp("qT", [D, NLAT], BF16)
    kT_d = P.inp("kTw", [D, NW], BF16)
    v_d = P.inp("vw", [NW, D], BF16)
    kTc_d = P.inp("kTc", [D, NCTX], BF16)
    vc_d = P.inp("vc", [NCTX, D], BF16)
    B_d = P.inp("B", [128, 16, 8, 64])
    M_d = P.inp("M", [128, 8, 8, 64])
    rv_d = P.inp("rv", [128, 16, 8])
    oT_d = P.outp("oT", [D, NLAT], BF16)
    rv = mk.sb("rv", [128, 16, 8], F32)
    mk.dma("sp", rv.all(), rv_d)
    Mt = mk.sb("Mt", [128, 8, 8, 64], F32)
    mk.dma("sp", Mt.all(), M_d)
    Et = mk.sb("Et", [128, 8, 8, 64], F32)
    kT = mk.sb("kT", [128, 8, NW], BF16)
    qT = mk.sb("qT", [128, 8, NLAT], BF16)
    kTc = mk.sb("kTc", [128, 8, NCTX], BF16)
    vc = mk.sb("vc", [128, 2, 1024], BF16)
    vb = [mk.sb(f"vb{i}", [128, 8, 1024], BF16) for i in range(2)]
    pT = [mk.sb(f"pT{i}", [128, 512], BF16) for i in range(4)]
    rcp = [mk.sb(f"rcp{i}", [128, 512], F32) for i in range(2)]
    ost = mk.sb("ost", [128, 8, NLAT], BF16)
    scale = 128.0 ** -0.5
    pi = 0
    acc_i = 0
    vi = 0
    for g in range(2):
        hs = slice(g * 1024, (g + 1) * 1024)
        mk.dma("sp", Et.all(), B_d[:, g * 8:(g + 1) * 8, :, :])
        mk.act(Et.all(), Et.all(), AF.Exp)
        mk.tt("pool", Et.all(), Et.all(), Mt.all(), ALU.mult)
        for h0 in range(0, 8, 4):
            mk.dma("sp", kT[:, h0:h0 + 4, :], kT_d[hs, :].rearrange("(h p) t -> p h t", p=128)[:, h0:h0 + 4, :])
        mk.dma("act", qT.all(), qT_d[hs, :].rearrange("(h p) t -> p h t", p=128))
        mk.dma("act", kTc.all(), kTc_d[hs, :].rearrange("(h p) t -> p h t", p=128))
        mk.dma("act", vc.all(), vc_d[:, hs].rearrange("(c p) n -> p c n", p=128))
        for rl in range(16):
            vband = vb[vi % 2]
            vi += 1
            mk.dma("sp" if rl % 2 == 0 else "act", vband.all(),
                   v_d[rl * 64:rl * 64 + 1024, hs].rearrange("(c p) n -> p c n", p=128))
            ps_o = P.psb[4 + 2 * (acc_i % 2)]
            ps_s = P.psb[5 + 2 * (acc_i % 2)]
            acc_i += 1
            qs = slice(rl * 64, (rl + 1) * 64)
            LOOK = 2
            pend = []
            for ii in range(10 + LOOK):
                if ii < 10:
                    i = ii
                    ps = P.psb[pi % 4]
                    p_ = pT[pi % 4]
                    pi += 1
                    for h in range(8):
                        if i < 8:
                            lhs = kT[:, h, (rl + 2 * i) * 64:(rl + 2 * i) * 64 + 128]
                        else:
                            lhs = kTc[:, h, (i - 8) * 128:(i - 7) * 128]
                        mk.matmul(ps[:, h * 64:(h + 1) * 64], lhs, qT[:, h, qs])
                    mk.act(p_.all(), ps.all(), AF.Exp, scale=scale)
                    if i < 8:
                        mk.stt(rview(p_.all(), "p (h q) -> p h q", h=8), rview(p_.all(), "p (h q) -> p h q", h=8),
                               rv[:, rl, i:i + 1], Et[:, :, i, :], ALU.mult, ALU.mult)
                    pend.append((i, p_))
                if ii < LOOK:
                    continue
                i, p_ = pend.pop(0)
                for h in range(8):
                    vv = vband[:, i, h * 128:(h + 1) * 128] if i < 8 else vc[:, i - 8, h * 128:(h + 1) * 128]
                    mk.matmul(ps_o[:, h * 64:(h + 1) * 64], vv, p_[:, h * 64:(h + 1) * 64],
                              start=(i == 0 and h == 0), stop=(i == 9), skip=True)
                mk.matmul(ps_s.all(), P.ones.all(), p_.all(), start=(i == 0), stop=(i == 9))
            r = rcp[acc_i % 2]
            mk.recip(r.all(), ps_s.all())
            mk.tt("dve", ost[:, :, qs], rview(ps_o.all(), "p (h q) -> p h q", h=8),
                  rview(r.all(), "p (h q) -> p h q", h=8), ALU.mult)
        mk.dma("sp", oT_d[hs, :].rearrange("(h p) t -> p h t", p=128), ost.all())
    B, M = nat_tables(rpb)
    in_maps = [{"qT": qT_list[c], "kTw": kTw_list[c], "vw": vw_list[c], "kTc": kTc_list[c], "vc": vc_list[c],
                "B": B, "M": M, "rv": nat_rowvalid(c % 4)} for c in range(NCORES)]
    return [r["oT"] for r in P.run(in_maps)]


def layer_nat(x_lat, x_ctx, mod, L, inp):
    modT = [mod_layout(mod, L, c // 4) for c in range(NCORES)]
    xT = core_xT(x_lat, x_ctx, NCL)
    specs = [("qT", 0, 2048, "fm", "bf16"), ("kT", 2048, 2048, "fm", "bf16"), ("v", 4096, 2048, "tm", "bf16")]
    ra = run_A(xT, modT, inp["norm_pre_mix"][L], inp["nat_w_in"][0], specs, NCL)
    qT_list, kTw, vw, kTc, vcl = [], [], [], [], []
    for b in range(2):
        kg = np.concatenate([ra[4 * b + i]["kT"][:, 0:NLAT] for i in range(4)], axis=1)
        kg = np.pad(kg, ((0, 0), (512, 512)))
        vg = np.concatenate([ra[4 * b + i]["v"][0:NLAT] for i in range(4)], axis=0)
        vg = np.pad(vg, ((512, 512), (0, 0)))
        for q in range(4):
            c = 4 * b + q
            qT_list.append(np.ascontiguousarray(ra[c]["qT"][:, 0:NLAT]))
            kTw.append(np.ascontiguousarray(kg[:, q * 1024:q * 1024 + 2048]))
            vw.append(np.ascontiguousarray(vg[q * 1024:q * 1024 + 2048]))
            kTc.append(np.ascontiguousarray(np.concatenate([ra[4 * b + i]["kT"][:, NLAT:] for i in range(4)], axis=1)))
            vcl.append(np.ascontiguousarray(np.concatenate([ra[4 * b + i]["v"][NLAT:] for i in range(4)], axis=0)))
    oT = run_B_nat(qT_list, kTw, vw, kTc, vcl, inp["nat_rpb"][0])
    modT1 = [mod_layout(mod, L, c // 4, with_ctx=False) for c in range(NCORES)]
    xT1 = [np.ascontiguousarray(x[:, 0:NLAT]) for x in xT]
    xmid = run_C1(oT, xT1, modT1, inp["norm_post_mix"][L], inp["nat_w_out"][0], None, 0)
    return xmid


def mlstm_consts():
    blk = np.arange(128) // 64
    same = blk[:, None] == blk[None, :]
    idx = np.arange(128)
    Uf = (same & (idx[:, None] <= idx[None, :])).astype(np.float32)
    Ub = np.ascontiguousarray(Uf.T)
    I = np.eye(128, dtype=np.float32)
    out = np.zeros((2, 5, 128, 128), np.float32)
    for d, U in enumerate((Uf, Ub)):
        out[d, 0] = U
        out[d, 1] = -U
        out[d, 2] = (U - 1.0) * 30000.0
        out[d, 3] = U.T - I
        out[d, 4] = I
    return np.ascontiguousarray(out.transpose(2, 0, 1, 3))


def run_B_mlstm(qT_list, kT_list, ktm_list, vtm_list, otm_list, gtm_list, bg_list, gain_list):
    NT = NCTX + SEQ
    NSC = NT // 128
    P = Prog("Bmlstm")
    mk = P.mk
    qT_d = P.inp("qT", [2, 128, NT], BF16)
    kT_d = P.inp("kT", [2, 128, NT], BF16)
    ktm_d = P.inp("ktm", [NT, 2, 128], BF16)
    vtm_d = P.inp("vtm", [NT, 2, 256], BF16)
    otm_d = P.inp("otm", [NT, 2, 256])
    gtm_d = P.inp("gtm", [NT, 2, 4])
    bg_d = P.inp("bg", [128, 2, 4])
    gain_d = P.inp("gain", [128, 2, 256])
    cm_d = P.inp("cm", [128, 2, 5, 128])
    out_d = P.outp("hout", [NT, 2, 256], BF16)
    cm = mk.sb("cm", [128, 2, 5, 128], F32)
    mk.dma("sp", cm.all(), cm_d)
    bg = mk.sb("bg", [128, 2, 4], F32)
    mk.dma("sp", bg.all(), bg_d)
    gain = mk.sb("gain", [128, 2, 256], F32)
    mk.dma("sp", gain.all(), gain_d)
    onesf = mk.sb("onesf", [128, 128], F32)
    mk.memset("dve", onesf.all(), 1.0)
    qT = mk.sb("qT", [128, NT], BF16)
    kT = mk.sb("kT", [128, NT], BF16)
    ktm = mk.sb("ktm", [128, NSC, 128], BF16)
    vtm = mk.sb("vtm", [128, NSC, 257], BF16)
    otm = mk.sb("otm", [128, NSC, 256], F32)
    gt = mk.sb("gt", [128, NSC, 4], F32)
    tmpg = mk.sb("tmpg", [128, NSC, 4], F32)
    IG = mk.sb("IG", [128, 2, NSC], F32)
    LF = mk.sb("LF", [128, 2, NSC], F32)
    Hacc = mk.sb("Hacc", [128, NSC, 256], F32)
    Cst = [[mk.sb(f"Cst{d}{i}", [128, 257], F32) for i in range(2)] for d in range(2)]
    Cbf = [[mk.sb(f"Cbf{d}{i}", [128, 257], BF16) for i in range(3)] for d in range(2)]
    Qpad = [[mk.sb(f"Qpad{d}{i}", [128, 256], BF16) for i in range(2)] for d in range(2)]
    for d in range(2):
        for i in range(2):
            mk.memset("pool", Qpad[d][i].all(), 0.0)
    LFbc = [mk.sb(f"LFbc{i}", [128, 128], F32) for i in range(2)]
    Edec = [mk.sb(f"Edec{i}", [128, 128], F32) for i in range(2)]
    DmT = [mk.sb(f"DmT{i}", [128, 128], F32) for i in range(2)]
    wgt = [mk.sb(f"wgt{i}", [128, 1], F32) for i in range(2)]
    Kw = [mk.sb(f"Kw{i}", [128, 128], BF16) for i in range(2)]
    Sm = [mk.sb(f"Sm{i}", [128, 128], BF16) for i in range(2)]
    dn = [mk.sb(f"dn{i}", [128, 1], F32) for i in range(2)]
    ssq = mk.sb("ssq", [128, NSC], F32)
    rst = mk.sb("rst", [128, NSC], F32)
    sqj = mk.sb("sqj", [128, 256], F32)
    sg = [mk.sb(f"sg{i}", [128, 256], F32) for i in range(2)]
    hn = [mk.sb(f"hn{i}", [128, 256], F32) for i in range(2)]
    ob = [mk.sb(f"ob{i}", [128, 256], BF16) for i in range(2)]
    scale = 128.0 ** -0.5
    order_f = list(range(NSC))
    order_b = [1, 0] + list(range(NSC - 1, 1, -1))
    it = 0
    for hd in range(2):
        mk.dma("sp", qT.all(), qT_d[hd])
        mk.dma("act", kT.all(), kT_d[hd])
        mk.dma("sp", ktm.all(), ktm_d[:, hd, :].rearrange("(c p) n -> p c n", p=128))
        mk.dma("act", vtm[:, :, 0:256], vtm_d[:, hd, :].rearrange("(c p) n -> p c n", p=128))
        mk.memset("pool", vtm[:, :, 256:257], 1.0)
        mk.dma("sp", otm.all(), otm_d[:, hd, :].rearrange("(c p) n -> p c n", p=128))
        mk.dma("act", gt.all(), gtm_d[:, hd, :].rearrange("(c p) n -> p c n", p=128))
        for c in range(4):
            mk.act(gt[:, :, c:c + 1], gt[:, :, c:c + 1], AF.Identity, bias=bg[:, hd, c:c + 1])
        mk.act(tmpg.all(), gt.all(), AF.Exp, scale=-1.0)
        mk.act(tmpg.all(), tmpg.all(), AF.Ln, bias=onesf[:, 0:1])
        for d in range(2):
            mk.act(rview(IG[:, d, :], "p (c o) -> p c o", o=1), gt[:, :, 2 * d:2 * d + 1], AF.Copy)
            mk.act(rview(LF[:, d, :], "p (c o) -> p c o", o=1), tmpg[:, :, 2 * d + 1:2 * d + 2], AF.Copy, scale=-1.0)
        mk.memset("pool", Hacc.all(), 0.0)
        cur = [0, 0]
        cbi = [0, 0]
        for d in range(2):
            mk.memset("dve", Cst[d][0].all(), 0.0)
            mk.memset("pool", Cbf[d][0].all(), 0.0)
        for step in range(NSC):
            for d in range(2):
                sc = (order_f if d == 0 else order_b)[step]
                i2 = it % 2
                it += 1
                U, nU, NEG, SL, Id = (cm[:, d, k, :] for k in range(5))
                lfc = LF[:, d, sc:sc + 1]
                igc = IG[:, d, sc:sc + 1]
                tsl = slice(sc * 128, (sc + 1) * 128)
                mk.act(LFbc[i2].all(), onesf.all(), AF.Copy, scale=lfc)
                ps1 = P.psum()
                mk.matmul(ps1[:, 0:128], LFbc[i2].all(), U)
                mk.act(Edec[i2].all(), ps1[:, 0:128], AF.Exp)
                ps2 = P.psum()
                mk.matmul(ps2[:, 0:128], LFbc[i2].all(), U, start=True, stop=False)
                mk.matmul(ps2[:, 0:128], nU, LFbc[i2].all(), start=False, stop=False)
                mk.matmul(ps2[:, 0:128], Id, NEG, start=False, stop=True)
                mk.act(DmT[i2].all(), ps2[:, 0:128], AF.Exp, bias=igc)
                ps3 = P.psum()
                mk.matmul(ps3[:, 0:1], SL, lfc)
                mk.act(wgt[i2].all(), ps3[:, 0:1], AF.Exp, bias=igc)
                mk.ts("dve", Kw[i2].all(), ktm[:, sc, :], wgt[i2][:, 0:1], ALU.mult)
                ps4 = P.psum()
                mk.matmul(ps4[:, 0:128], kT[:, tsl], qT[:, tsl])
                mk.stt(Sm[i2].all(), ps4[:, 0:128], scale, DmT[i2].all(), ALU.mult, ALU.mult)
                qp = Qpad[d][step % 2]
                mk.stt(qp[:, 0:64], qT[:, sc * 128:sc * 128 + 64], scale, Edec[i2][:, 0:64], ALU.mult, ALU.mult)
                mk.stt(qp[:, 192:256], qT[:, sc * 128 + 64:sc * 128 + 128], scale, Edec[i2][:, 64:128],
                       ALU.mult, ALU.mult)
                first, second = ((0, 64), (64, 128)) if d == 0 else ((64, 128), (0, 64))
                gcol = (63, 127) if d == 0 else (64, 0)
                cb_in = Cbf[d][cbi[d] % 3]
                cs_in = Cst[d][cur[d] % 2]
                cs_mid = Cst[d][(cur[d] + 1) % 2]
                cb_mid = Cbf[d][(cbi[d] + 1) % 3]
                cb_out = Cbf[d][(cbi[d] + 2) % 3]
                ps6 = P.psum()
                mk.matmul(ps6[:, 0:257], Kw[i2][first[0]:first[1], :], vtm[first[0]:first[1], sc, :])
                mk.stt(cs_mid.all(), cs_in.all(), Edec[i2][:, gcol[0]:gcol[0] + 1], ps6[:, 0:257], ALU.mult, ALU.add)
                mk.copy("act", cb_mid.all(), cs_mid.all())
                ps7 = P.psum()
                mk.matmul(ps7[:, 0:257], Kw[i2][second[0]:second[1], :], vtm[second[0]:second[1], sc, :])
                mk.stt(cs_in.all(), cs_mid.all(), Edec[i2][:, gcol[1]:gcol[1] + 1], ps7[:, 0:257], ALU.mult, ALU.add)
                mk.copy("act", cb_out.all(), cs_in.all())
                cbi[d] += 2
                cA, cB = (cb_in, cb_mid) if d == 0 else (cb_mid, cb_in)
                ps5 = P.psum()
                mk.matmul(ps5[:, 0:257], Sm[i2].all(), vtm[:, sc, :], start=True, stop=False)
                mk.matmul(ps5[:, 0:257], qp[:, 0:128], cA.all(), start=False, stop=False)
                mk.matmul(ps5[:, 0:257], qp[:, 128:256], cB.all(), start=False, stop=True)
                mk.act(dn[i2].all(), ps5[:, 256:257], AF.Abs)
                mk.ts("dve", dn[i2].all(), dn[i2].all(), 1.0, ALU.max)
                mk.recip(dn[i2].all(), dn[i2].all())
                mk.stt(Hacc[:, sc, :], ps5[:, 0:256], dn[i2][:, 0:1], Hacc[:, sc, :], ALU.mult, ALU.add)
        mk.memset("dve", ssq.all(), 0.0)
        for sc in range(NSC):
            mk.act(sqj.all(), Hacc[:, sc, :], AF.Square, accum_out=ssq[:, sc:sc + 1])
        mk.act(rst.all(), ssq.all(), AF.Sqrt, bias=P.eps_t[:, 0:1], scale=1.0 / 256)
        mk.recip(rst.all(), rst.all())
        for sc in range(NSC):
            j = sc % 2
            mk.act(sg[j].all(), otm[:, sc, :], AF.Sigmoid)
            mk.stt(hn[j].all(), Hacc[:, sc, :], rst[:, sc:sc + 1], gain[:, hd, :], ALU.mult, ALU.mult)
            mk.tt("pool", ob[j].all(), hn[j].all(), sg[j].all(), ALU.mult)
            mk.dma("sp", out_d[sc * 128:(sc + 1) * 128, hd, :], ob[j].all())
    cmk = mlstm_consts()
    in_maps = [{"qT": qT_list[c], "kT": kT_list[c], "ktm": ktm_list[c], "vtm": vtm_list[c], "otm": otm_list[c],
                "gtm": gtm_list[c], "bg": bg_list[c], "gain": gain_list[c], "cm": cmk} for c in range(NCORES)]
    return [r["hout"] for r in P.run(in_maps)]


def layer_mlstm(x_lat, x_ctx, mod, L, inp):
    modT = [mod_layout(mod, L, c // 4) for c in range(NCORES)]
    xT = core_xT(x_lat, x_ctx, NCL)
    specs = [("qT", 0, 1024, "fm", "bf16"), ("kT", 1024, 1024, "fm", "bf16"), ("ktm", 1024, 1024, "tm", "bf16"),
             ("vtm", 2048, 2048, "tm", "bf16"), ("otm", 4096, 2048, "tm", "f32"), ("gtm", 6144, 32, "tm", "f32")]
    ra = run_A(xT, modT, inp["norm_pre_mix"][L], inp["mlstm_w_in"][0], specs, NCL)

    def glob_fm(name, b):
        return np.concatenate([ra[4 * b + i][name][:, NLAT:] for i in range(4)]
                              + [ra[4 * b + i][name][:, 0:NLAT] for i in range(4)], axis=1)

    def glob_tm(name, b):
        return np.concatenate([ra[4 * b + i][name][NLAT:] for i in range(4)]
                              + [ra[4 * b + i][name][0:NLAT] for i in range(4)], axis=0)

    lists = [[] for _ in range(8)]
    b_gate = inp["mlstm_b_gate"][0].reshape(4, 8)
    onorm = inp["mlstm_out_norm"][0].reshape(8, 256)
    for b in range(2):
        qg, kg = glob_fm("qT", b), glob_fm("kT", b)
        ktm, vtm, otm, gtm = (glob_tm(n, b) for n in ("ktm", "vtm", "otm", "gtm"))
        NT = qg.shape[1]
        for hp in range(4):
            hs = [2 * hp, 2 * hp + 1]
            lists[0].append(np.ascontiguousarray(qg.reshape(8, 128, NT)[hs]))
            lists[1].append(np.ascontiguousarray(kg.reshape(8, 128, NT)[hs]))
            lists[2].append(np.ascontiguousarray(ktm.reshape(NT, 8, 128)[:, hs]))
            lists[3].append(np.ascontiguousarray(vtm.reshape(NT, 8, 256)[:, hs]))
            lists[4].append(np.ascontiguousarray(otm.reshape(NT, 8, 256)[:, hs]))
            lists[5].append(np.ascontiguousarray(gtm.reshape(NT, 4, 8)[:, :, hs].transpose(0, 2, 1)))
            lists[6].append(np.ascontiguousarray(np.broadcast_to(b_gate[:, hs].T[None], (128, 2, 4))))
            lists[7].append(np.ascontiguousarray(np.broadcast_to(onorm[hs][None], (128, 2, 256))))
    ho = run_B_mlstm(*lists)
    oT = []
    for b in range(2):
        og = np.concatenate([ho[4 * b + hp] for hp in range(4)], axis=1)
        og = og.reshape(NCTX + SEQ, D)
        for q in range(4):
            tok = np.concatenate([og[NCTX + q * NLAT:NCTX + (q + 1) * NLAT], og[q * NCL:(q + 1) * NCL]], axis=0)
            oT.append(to_fm(tok))
    xmid = run_C1(oT, xT, modT, inp["norm_post_mix"][L], inp["mlstm_w_out"][0], None, NCL)
    return xmid


def kernel(**inputs):
    inp = {k: np.asarray(v) for k, v in inputs.items()}
    mod = run_mod(inp["c"], inp["c_ctx"], inp["mod_w"], inp["mod_b"])
    x_lat, x_ctx = inp["x"], inp["ctx"]
    layers = [layer_gqa, layer_mlstm, layer_conv, layer_nat]
    for L in range(4):
        nctx = NCL if L < 3 else 0
        xmid = layers[L](x_lat, x_ctx, mod, L, inp)
        xl, xc = split_xT(xmid, nctx)
        x_lat, x_ctx = layer_ffn(xl, xc, mod, L, inp, nctx)
    return np.ascontiguousarray(x_lat.astype(np.float32))
```

```python
import numpy as np
from contextlib import ExitStack
import ml_dtypes
import concourse.bass as bass
import concourse.mybir as mybir
from concourse.bass_utils import run_bass_kernel_spmd

F32 = mybir.dt.float32
BF16 = mybir.dt.bfloat16
AF = mybir.ActivationFunctionType
ALU = mybir.AluOpType
AX = mybir.AxisListType
NPBF = ml_dtypes.bfloat16

D = 2048
KC = 16
NCTX = 256
SEQ = 4096
NLAT = 1024
NCL = 64
DFF = 5632
EPS = 1e-6
NCORES = 8


class V:
    __slots__ = ("ap", "tile", "lo", "hi")

    def __init__(self, ap, tile, lo, hi):
        self.ap, self.tile, self.lo, self.hi = ap, tile, lo, hi


class Tile:
    def __init__(self, mk, t, shape, name):
        self.mk, self.t, self.shape, self.name = mk, t, list(shape), name
        st = []
        acc = 1
        for s in reversed(self.shape[1:]):
            st.append(acc)
            acc *= s
        self.strides = list(reversed(st))
        self.recs_w = []
        self.recs_r = []

    def __getitem__(self, idx):
        if not isinstance(idx, tuple):
            idx = (idx,)
        ap = self.t[idx]
        lo = 0
        hi = 0
        fidx = list(idx[1:]) + [slice(None)] * (len(self.shape) - len(idx))
        for s, n, stride in zip(fidx, self.shape[1:], self.strides):
            if isinstance(s, slice):
                a = 0 if s.start is None else s.start
                b = n if s.stop is None else s.stop
                step = 1 if s.step is None else s.step
                cnt = (b - a + step - 1) // step
                last = a + (cnt - 1) * step
            else:
                a = s
                last = s
            lo += a * stride
            hi += last * stride
        return V(ap, self, lo, hi + 1)

    def all(self):
        return self[tuple([slice(None)] * len(self.shape))]


class MK:
    SEM_ROLL = 30000

    def __init__(self, nc, n_dma_sems=32):
        self.nc = nc
        self.es = ExitStack()
        self.sem_es = ExitStack()
        self.eng = {"pe": nc.tensor, "act": nc.scalar, "dve": nc.vector, "pool": nc.gpsimd, "sp": nc.sync}
        self.sem = {}
        self.cnt = {}
        self.nsem = 0
        for e in self.eng:
            self._new_sem(e)
        self.waited = {e: {} for e in self.eng}
        self.dma_sems = [self.es.enter_context(nc.semaphore(f"dq{i}")) for i in range(n_dma_sems)]
        self.dma_val = [0] * n_dma_sems
        self.dma_rr = 0
        self.ninst = 0
        self.nwait = 0
        self.rr = 0
        self.stk = []

    def _new_sem(self, e):
        self.sem[e] = self.sem_es.enter_context(self.nc.semaphore(f"s_{e}_{self.nsem}"))
        self.nsem += 1
        self.cnt[e] = 0

    def sb(self, name, shape, dt=F32):
        t = self.es.enter_context(self.nc.sbuf_tensor("sb_" + name, list(shape), dt))
        return Tile(self, t, shape, name)

    def ps(self, name, shape, dt=F32):
        t = self.es.enter_context(self.nc.psum_tensor("ps_" + name, list(shape), dt))
        return Tile(self, t, shape, name)

    def push(self):
        self.stk.append(self.es)
        self.es = ExitStack()

    def pop(self):
        self.es.close()
        self.es = self.stk.pop()

    def barrier(self):
        for e in self.eng:
            for o in self.eng:
                if o != e and self.cnt[o] > 0:
                    self._wait(e, self.sem[o], self.cnt[o])
            for i, s_ in enumerate(self.dma_sems):
                if self.dma_val[i] > 0:
                    self._wait(e, s_, self.dma_val[i])

    def _wait(self, e, sem, val):
        w = self.waited[e]
        if w.get(sem, 0) >= val:
            return
        w[sem] = val
        self.eng[e].wait_ge(sem, val)
        self.nwait += 1

    def _deps(self, e, reads, writes):
        for v in reads:
            if not isinstance(v, V):
                continue
            for (lo, hi, sem, val, de) in v.tile.recs_w:
                if lo < v.hi and v.lo < hi and not (de == "pe" and e == "pe"):
                    self._wait(e, sem, val)
        for v in writes:
            if not isinstance(v, V):
                continue
            for (lo, hi, sem, val, de) in v.tile.recs_w:
                if lo < v.hi and v.lo < hi and not (de == "pe" and e == "pe"):
                    self._wait(e, sem, val)
            for (lo, hi, sem, val, de) in v.tile.recs_r:
                if lo < v.hi and v.lo < hi and not (de == "pe" and e == "pe"):
                    self._wait(e, sem, val)

    def _record(self, reads, writes, sem, val, e):
        for v in writes:
            if not isinstance(v, V):
                continue
            t = v.tile
            t.recs_w = [r for r in t.recs_w if not (v.lo <= r[0] and r[1] <= v.hi)]
            t.recs_r = [r for r in t.recs_r if not (v.lo <= r[0] and r[1] <= v.hi)]
            t.recs_w.append((v.lo, v.hi, sem, val, e))
        for v in reads:
            if not isinstance(v, V):
                continue
            t = v.tile
            t.recs_r = [r for r in t.recs_r if not (r[2] is sem and v.lo <= r[0] and r[1] <= v.hi)]
            t.recs_r.append((v.lo, v.hi, sem, val, e))

    @staticmethod
    def _ap(v):
        return v.ap if isinstance(v, V) else v

    def op(self, e, fn, reads, writes):
        self._deps(e, reads, writes)
        if self.cnt[e] >= self.SEM_ROLL:
            self._new_sem(e)
        ins = fn()
        self.cnt[e] += 1
        ins.then_inc(self.sem[e], 1)
        self._record(reads, writes, self.sem[e], self.cnt[e], e)
        self.ninst += 1
        return ins

    def dma(self, e, out, in_, **kw):
        self._deps(e, [in_], [out])
        i = self.dma_rr
        self.dma_rr = (self.dma_rr + 1) % len(self.dma_sems)
        s = self.dma_sems[i]
        if self.dma_val[i] > 0:
            self._wait(e, s, self.dma_val[i])
        self.dma_val[i] += 16
        self.eng[e].dma_start(out=self._ap(out), in_=self._ap(in_), **kw).then_inc(s, 16)
        self._record([in_], [out], s, self.dma_val[i], "dma")
        self.ninst += 1

    def finish(self, e="sp"):
        for i, s in enumerate(self.dma_sems):
            if self.dma_val[i] > 0:
                self._wait(e, s, self.dma_val[i])
        for o in self.eng:
            if o != e and self.cnt[o] > 0:
                self._wait(e, self.sem[o], self.cnt[o])

    def close(self):
        while self.stk:
            self.pop()
        self.es.close()
        self.sem_es.close()

    def matmul(self, out, lhsT, rhs, start=True, stop=True, skip=False):
        return self.op("pe", lambda: self.nc.tensor.matmul(out.ap, lhsT.ap, rhs.ap, start=start, stop=stop,
                                                           skip_group_check=skip),
                       [lhsT, rhs], [out])

    def act(self, out, in_, func, bias=None, scale=None, accum_out=None):
        kw = {}
        reads = [in_]
        writes = [out]
        if bias is not None:
            kw["bias"] = self._ap(bias)
            reads.append(bias)
        if scale is not None:
            kw["scale"] = self._ap(scale)
            reads.append(scale)
        if accum_out is not None:
            kw["accum_out"] = accum_out.ap
            writes.append(accum_out)
        return self.op("act", lambda: self.nc.scalar.activation(out.ap, in_.ap, func, **kw), reads, writes)

    def tt(self, e, out, a, b, op):
        return self.op(e, lambda: self.eng[e].tensor_tensor(out.ap, a.ap, b.ap, op), [a, b], [out])

    def ts(self, e, out, a, s1, op0, s2=None, op1=None):
        reads = [a, s1, s2]
        if op1 is None:
            s2, op1 = 0.0, ALU.add
        return self.op(e, lambda: self.eng[e].tensor_scalar(out.ap, a.ap, self._ap(s1), self._ap(s2), op0, op1),
                       reads, [out])

    def stt(self, out, a, s, b, op0, op1):
        return self.op("dve", lambda: self.nc.vector.scalar_tensor_tensor(out.ap, a.ap, self._ap(s), b.ap, op0, op1),
                       [a, s, b], [out])

    def copy(self, e, out, in_):
        if e == "act":
            return self.op(e, lambda: self.nc.scalar.copy(out.ap, in_.ap), [in_], [out])
        return self.op(e, lambda: self.eng[e].tensor_copy(out.ap, in_.ap), [in_], [out])

    def memset(self, e, out, val):
        return self.op(e, lambda: self.eng[e].memset(out.ap, val), [], [out])

    def recip(self, out, in_):
        return self.op("dve", lambda: self.nc.vector.reciprocal(out.ap, in_.ap), [in_], [out])

    def evac(self, out, in_):
        self.rr ^= 1
        return self.copy("act" if self.rr else "dve", out, in_)


def tiles_of(n, mx=512):
    k = (n + mx - 1) // mx
    base = n // k
    rem = n % k
    out = []
    o = 0
    for i in range(k):
        s = base + (1 if i < rem else 0)
        out.append((o, s))
        o += s
    return out


class Prog:
    def __init__(self, name):
        import time as _t
        self.t_start = _t.time()
        self.name = name
        self.nc = bass.Bass("TRN2", target_bir_lowering=False)
        self.mk = MK(self.nc)
        self.in_names = []
        self.out_names = []
        mk = self.mk
        self.psb = [mk.ps(f"psb{i}", [128, 512], F32) for i in range(8)]
        self.ps_i = 0
        self.ones = mk.sb("ones_bf", [128, 128], BF16)
        mk.memset("dve", self.ones.all(), 1.0)
        self.eps_t = mk.sb("eps_t", [128, 1], F32)
        mk.memset("dve", self.eps_t.all(), EPS)
        self.wq = 0

    def inp(self, name, shape, dt=F32):
        self.in_names.append(name)
        return self.nc.dram_tensor(name, list(shape), dt, kind="ExternalInput").ap()

    def outp(self, name, shape, dt=F32):
        self.out_names.append(name)
        return self.nc.dram_tensor(name, list(shape), dt, kind="ExternalOutput").ap()

    def rstd_of(self, out, ssq, dim, eps=EPS):
        self.mk.act(out, ssq, AF.Sqrt, bias=self.eps_t[0:out.ap.shape[0], 0:1] if eps == EPS else eps, scale=1.0 / dim)
        self.mk.recip(out, out)

    def psum(self):
        p = self.psb[self.ps_i]
        self.ps_i = (self.ps_i + 1) % 8
        return p

    def run(self, in_maps):
        import time as _t
        self.mk.finish()
        t0 = _t.time()
        import os as _os
        if _os.environ.get("KTRACE"):
            res = run_bass_kernel_spmd(self.nc, in_maps, core_ids=list(range(NCORES)), trace=True)
            print(f"[{self.name}] exec_time_ns={res.exec_time_ns}", flush=True)
        else:
            res = run_bass_kernel_spmd(self.nc, in_maps, core_ids=list(range(NCORES)))
        print(f"[{self.name}] ninst={self.mk.ninst} nwait={self.mk.nwait} build={t0 - self.t_start:.1f}s "
              f"run={_t.time() - t0:.1f}s", flush=True)
        self.mk.close()
        return res.results

    def load_w(self, tile, w_ap, col0, ncols, nk):
        src = w_ap[:, col0:col0 + ncols].rearrange("(kc p) n -> p kc n", p=128)
        step = 4
        for k0 in range(0, nk, step):
            k1 = min(nk, k0 + step)
            self.mk.dma("pool", tile[:, k0:k1, 0:ncols], src[:, k0:k1, :])

    def rstd_fm(self, xT, nk, T, rstd, sqbuf, dim):
        mk = self.mk
        for (t0, tn) in tiles_of(T):
            ps = self.psum()
            for kc in range(nk):
                sq = sqbuf[kc % len(sqbuf)]
                mk.act(sq[:, 0:tn], xT[:, kc, t0:t0 + tn], AF.Square)
                mk.matmul(ps[:, 0:tn], self.ones.all(), sq[:, 0:tn], start=(kc == 0), stop=(kc == nk - 1))
            self.rstd_of(rstd[:, t0:t0 + tn], ps[:, 0:tn], dim)

    def modulate_fm(self, hT, xT, rstd, segs, tmp):
        mk = self.mk
        for kc in range(KC):
            for (t0, tn, a, sh) in segs:
                t = tmp[kc % len(tmp)]
                mk.tt("dve", t[:, 0:tn], xT[:, kc, t0:t0 + tn], rstd[:, t0:t0 + tn], ALU.mult)
                mk.act(hT[:, kc, t0:t0 + tn], t[:, 0:tn], AF.Identity, bias=sh[:, kc:kc + 1], scale=a[:, kc:kc + 1])

    def load_mod(self, modT, nseg):
        t = self.mk.sb("modsb", [128, nseg, 6, KC], F32)
        self.mk.dma("sp", t.all(), modT)
        return t

    def load_vec(self, name, ap_kc):
        t = self.mk.sb(name, [128, KC], F32)
        self.mk.dma("sp", t.all(), ap_kc)
        return t


def vec_pk(v):
    return np.ascontiguousarray(v.reshape(-1, 128).T)


def run_mod(c, c_ctx, mod_w, mod_b):
    P = Prog("mod")
    mk = P.mk
    NCOL = 12288 // NCORES
    cT = P.inp("cT", [128, KC, 3])
    w = P.inp("w", [4, D, NCOL])
    b = P.inp("b", [3, 4, NCOL])
    out = P.outp("out", [3, 4, NCOL])
    cs = mk.sb("cs", [128, KC, 3], F32)
    sT = mk.sb("sT", [128, KC, 3], F32)
    mk.dma("sp", cs.all(), cT)
    mk.act(sT.all(), cs.all(), AF.Silu)
    bs = mk.sb("bs", [3, 4, NCOL], F32)
    mk.dma("sp", bs.all(), b)
    os_ = mk.sb("os", [3, 4, NCOL], F32)
    wb = [mk.sb(f"wb{i}", [128, KC, 512], F32) for i in range(2)]
    it = 0
    for l in range(4):
        for n0 in range(0, NCOL, 512):
            wt = wb[it % 2]
            it += 1
            src = w[l, :, n0:n0 + 512].rearrange("(kc p) n -> p kc n", p=128)
            for k0 in range(0, KC, 4):
                mk.dma("sp" if (k0 // 4) % 2 == 0 else "act", wt[:, k0:k0 + 4, :], src[:, k0:k0 + 4, :])
            ps = P.psum()
            for kc in range(KC):
                mk.matmul(ps[0:3, :], sT[:, kc, :], wt[:, kc, :], start=(kc == 0), stop=(kc == KC - 1))
            mk.tt("dve", os_[:, l, n0:n0 + 512], ps[0:3, :], bs[:, l, n0:n0 + 512], ALU.add)
    mk.dma("sp", out, os_.all())
    cstack = np.stack([c[0], c[1], c_ctx], axis=1)
    cT_np = np.ascontiguousarray(cstack.reshape(KC, 128, 3).transpose(1, 0, 2))
    in_maps = []
    for core in range(NCORES):
        sl = slice(core * NCOL, (core + 1) * NCOL)
        in_maps.append({"cT": cT_np, "w": np.ascontiguousarray(mod_w[:, :, sl]),
                        "b": np.ascontiguousarray(np.broadcast_to(mod_b[None, :, sl], (3, 4, NCOL)))})
    res = P.run(in_maps)
    mod = np.concatenate([r["out"] for r in res], axis=2)
    return mod


def mod_layout(mod, layer, b, with_ctx=True):
    rows = [b, 2] if with_ctx else [b]
    m = mod[rows, layer]
    m = m.reshape(len(rows), 6, KC, 128).transpose(3, 0, 1, 2)
    return np.ascontiguousarray(m)


def norm_mod_stream(P, xT_d, T, segs, normw_t, mod_t, ish, isc, hT):
    mk = P.mk
    nseg = mod_t.shape[1]
    a_t = mk.sb("nm_a", [128, nseg, KC], F32)
    for s in range(nseg):
        mk.stt(a_t[:, s, :], mod_t[:, s, isc, :], 1.0, normw_t.all(), ALU.add, ALU.mult)
    NXB = 4
    xb = [mk.sb(f"nm_xb{i}", [128, T], F32) for i in range(NXB)]
    sq = [mk.sb(f"nm_sq{i}", [128, 512], BF16) for i in range(3)]
    rstd = mk.sb("nm_rstd", [128, T], F32)
    tls = tiles_of(T)
    pss = [P.psum() for _ in tls]
    xsrc = xT_d.rearrange("(kc p) t -> p kc t", p=128)
    for kc in range(KC):
        x = xb[kc % NXB]
        mk.dma("sp", x.all(), xsrc[:, kc, :])
        for ti, (t0, tn) in enumerate(tls):
            s_ = sq[(kc * len(tls) + ti) % 3]
            mk.act(s_[:, 0:tn], x[:, t0:t0 + tn], AF.Square)
            mk.matmul(pss[ti][:, 0:tn], P.ones.all(), s_[:, 0:tn], start=(kc == 0), stop=(kc == KC - 1))
    for ti, (t0, tn) in enumerate(tls):
        P.rstd_of(rstd[:, t0:t0 + tn], pss[ti][:, 0:tn], D)
    for kc in range(KC):
        x = xb[kc % NXB]
        mk.dma("sp", x.all(), xsrc[:, kc, :])
        mk.tt("dve", x.all(), x.all(), rstd.all(), ALU.mult)
        for (t0, tn, s) in segs:
            mk.act(hT[:, kc, t0:t0 + tn], x[:, t0:t0 + tn], AF.Identity,
                   bias=mod_t[:, s, ish, kc:kc + 1], scale=a_t[:, s, kc:kc + 1])


def proj_fm(P, hT, T, w_ap, col0, ncols, out_ap, out_dt, wbufs, stg, nk=KC, post=None):
    mk = P.mk
    tls = tiles_of(T)
    bi = 0
    for b0 in range(0, ncols, 512):
        bn = min(512, ncols - b0)
        wt = wbufs[P.wq % len(wbufs)]
        P.wq += 1
        P.load_w(wt, w_ap, col0 + b0, bn, nk)
        for n0 in range(0, bn, 128):
            st = stg[bi % len(stg)]
            bi += 1
            for (t0, tn) in tls:
                ps = P.psum()
                for kc in range(nk):
                    mk.matmul(ps[:, 0:tn], wt[:, kc, n0:n0 + 128], hT[:, kc, t0:t0 + tn],
                              start=(kc == 0), stop=(kc == nk - 1))
                if post is None:
                    mk.evac(st[:, t0:t0 + tn], ps[:, 0:tn])
                else:
                    post(st, ps, b0 + n0, t0, tn)
            mk.dma("sp", out_ap[b0 + n0:b0 + n0 + 128, :], st[:, 0:T])


def proj_tm(P, hT, T, w_ap, col0, ncols, out_ap, wbufs, stg, nk=KC):
    mk = P.mk
    bi = 0
    for b0 in range(0, ncols, 512):
        bn = min(512, ncols - b0)
        wt = wbufs[P.wq % len(wbufs)]
        P.wq += 1
        P.load_w(wt, w_ap, col0 + b0, bn, nk)
        for t0 in range(0, T, 128):
            tn = min(128, T - t0)
            st = stg[bi % len(stg)]
            bi += 1
            ps = P.psum()
            for kc in range(nk):
                mk.matmul(ps[0:tn, 0:bn], hT[:, kc, t0:t0 + tn], wt[:, kc, 0:bn], start=(kc == 0), stop=(kc == nk - 1))
            mk.evac(st[0:tn, 0:bn], ps[0:tn, 0:bn])
            mk.dma("sp", out_ap[t0:t0 + tn, b0:b0 + bn], st[0:tn, 0:bn])


def run_A(xT_list, modT_list, normw, w_in, specs, nctx):
    T = NLAT + nctx
    N = w_in.shape[1]
    P = Prog("A")
    mk = P.mk
    xT_d = P.inp("xT", [D, T])
    modT_d = P.inp("modT", [128, 2 if nctx else 1, 6, KC])
    nw_d = P.inp("nw", [128, KC])
    w_d = P.inp("w", [D, N])
    mod_t = P.load_mod(modT_d, 2 if nctx else 1)
    nw_t = P.load_vec("nw_t", nw_d)
    hT = mk.sb("hT", [128, KC, T], BF16)
    segs = [(0, NLAT, 0)] + ([(NLAT, nctx, 1)] if nctx else [])
    norm_mod_stream(P, xT_d, T, segs, nw_t, mod_t, 0, 1, hT)
    wbufs = [mk.sb(f"wbuf{i}", [128, KC, 512], BF16) for i in range(2)]
    stg_fm_b = [mk.sb(f"sfb{i}", [128, T], BF16) for i in range(3)]
    stg_fm_f = [mk.sb(f"sff{i}", [128, T], F32) for i in range(3)]
    stg_tm_b = [mk.sb(f"stb{i}", [128, 512], BF16) for i in range(3)]
    stg_tm_f = [mk.sb(f"stf{i}", [128, 512], F32) for i in range(3)]
    for (name, col0, ncols, lay, dt) in specs:
        bdt = BF16 if dt == "bf16" else F32
        if lay == "fm":
            o = P.outp(name, [ncols, T], bdt)
            proj_fm(P, hT, T, w_d, col0, ncols, o, bdt, wbufs, stg_fm_b if dt == "bf16" else stg_fm_f)
        else:
            o = P.outp(name, [T, ncols], bdt)
            proj_tm(P, hT, T, w_d, col0, ncols, o, wbufs, stg_tm_b if dt == "bf16" else stg_tm_f)
    nwk = vec_pk(normw)
    in_maps = [{"xT": xT_list[c], "modT": modT_list[c], "nw": nwk, "w": w_in} for c in range(NCORES)]
    return P.run(in_maps)


def run_C1(oT_list, xT_list, modT_list, normw, w_out, bias, nctx):
    T = NLAT + nctx
    nseg = 2 if nctx else 1
    P = Prog("C1")
    mk = P.mk
    oT_d = P.inp("oT", [D, T], BF16)
    xT_d = P.inp("xT", [D, T])
    modT_d = P.inp("modT", [128, nseg, 6, KC])
    nw_d = P.inp("nw", [128, KC])
    b_d = P.inp("bias", [128, KC])
    w_d = P.inp("w", [D, D])
    out_d = P.outp("xmid", [D, T])
    mod_t = P.load_mod(modT_d, nseg)
    nw_t = P.load_vec("nw_t", nw_d)
    b_t = P.load_vec("b_t", b_d)
    gw = mk.sb("gw", [128, nseg, KC], F32)
    for s in range(nseg):
        mk.tt("dve", gw[:, s, :], mod_t[:, s, 2, :], nw_t.all(), ALU.mult)
    wres = mk.sb("wres", [128, KC, D], BF16)
    for b0 in range(0, D, 512):
        src = w_d[:, b0:b0 + 512].rearrange("(kc p) n -> p kc n", p=128)
        for k0 in range(0, KC, 4):
            mk.dma("pool", wres[:, k0:k0 + 4, b0:b0 + 512], src[:, k0:k0 + 4, :])
    ob = [mk.sb(f"ob{i}", [128, KC, 512], BF16) for i in range(2)]
    yTb = [mk.sb(f"yT{i}", [128, KC, 512], F32) for i in range(2)]
    sq = [mk.sb(f"sq{i}", [128, 512], BF16) for i in range(4)]
    rstd = mk.sb("rstd", [128, 512], F32)
    xb = [mk.sb(f"xb{i}", [128, 512], F32) for i in range(4)]
    osrc = oT_d.rearrange("(kc p) t -> p kc t", p=128)
    xsrc = xT_d.rearrange("(kc p) t -> p kc t", p=128)
    odst = out_d.rearrange("(kc p) t -> p kc t", p=128)
    tls = [(0, 512, 0), (512, 512, 0)] + ([(NLAT, nctx, 1)] if nctx else [])
    def load_o(ti):
        t0_, tn_, _ = tls[ti]
        for k0 in range(0, KC, 4):
            mk.dma("sp", ob[ti % 2][:, k0:k0 + 4, 0:tn_], osrc[:, k0:k0 + 4, t0_:t0_ + tn_])

    load_o(0)
    for ti, (t0, tn, seg) in enumerate(tls):
        o = ob[ti % 2]
        yT = yTb[ti % 2]
        if ti + 1 < len(tls):
            load_o(ti + 1)
        pss = P.psb[7]
        pend = []
        for n in range(KC + 2):
            if n < KC:
                ps = P.psb[(ti * KC + n) % 6]
                for kc in range(KC):
                    mk.matmul(ps[:, 0:tn], wres[:, kc, n * 128:(n + 1) * 128], o[:, kc, 0:tn],
                              start=(kc == 0), stop=(kc == KC - 1))
                mk.act(yT[:, n, 0:tn], ps[:, 0:tn], AF.Identity, bias=b_t[:, n:n + 1])
                s_ = sq[n % 4]
                mk.act(s_[:, 0:tn], yT[:, n, 0:tn], AF.Square)
                pend.append((n, s_))
            if n >= 2:
                m, s_ = pend.pop(0)
                mk.matmul(pss[:, 0:tn], P.ones.all(), s_[:, 0:tn], start=(m == 0), stop=(m == KC - 1))
        P.rstd_of(rstd[:, 0:tn], pss[:, 0:tn], D)
        for n in range(KC):
            x = xb[n % 4]
            mk.dma("sp", x[:, 0:tn], xsrc[:, n, t0:t0 + tn])
            mk.tt("dve", yT[:, n, 0:tn], yT[:, n, 0:tn], rstd[:, 0:tn], ALU.mult)
            mk.stt(x[:, 0:tn], yT[:, n, 0:tn], gw[:, seg, n:n + 1], x[:, 0:tn], ALU.mult, ALU.add)
            mk.dma("pool", odst[:, n, t0:t0 + tn], x[:, 0:tn])
    nwk = vec_pk(normw)
    bk = vec_pk(bias) if bias is not None else np.zeros((128, KC), np.float32)
    in_maps = [{"oT": oT_list[c], "xT": xT_list[c], "modT": modT_list[c], "nw": nwk, "bias": bk, "w": w_out}
               for c in range(NCORES)]
    return [r["xmid"] for r in P.run(in_maps)]


def run_C2_old(xe_list, mask_list, modT_list, nw_pre, nw_post, w_up, conv_w, conv_b, w_down, groups):
    ng = len(groups)
    nseg = 1 + max(s for _, s in groups)
    Te = sum(g + 2 for g, _ in groups)
    To = sum(g for g, _ in groups)
    NJ = DFF // 128
    P = Prog("C2")
    mk = P.mk
    xe_d = P.inp("xe", [D, Te])
    mask_d = P.inp("mask", [128, KC, 2 * ng])
    modT_d = P.inp("modT", [128, nseg, 6, KC])
    nw1_d = P.inp("nw1", [128, KC])
    nw2_d = P.inp("nw2", [128, KC])
    wu_d = P.inp("wu", [D, 2 * DFF])
    cw_d = P.inp("cw", [128, 2 * NJ, 3])
    cb_d = P.inp("cb", [128, 2 * NJ])
    wd_d = P.inp("wd", [DFF, D])
    out_d = P.outp("xo", [D, To])
    mod_t = P.load_mod(modT_d, nseg)
    nw1 = P.load_vec("nw1_t", nw1_d)
    nw2 = P.load_vec("nw2_t", nw2_d)
    mask_t = mk.sb("mask_t", [128, KC, 2 * ng], F32)
    mk.dma("sp", mask_t.all(), mask_d)
    cw = mk.sb("cw_t", [128, 2 * NJ, 3], F32)
    cb = mk.sb("cb_t", [128, 2 * NJ], F32)
    mk.dma("sp", cw.all(), cw_d)
    mk.dma("sp", cb.all(), cb_d)
    gw = mk.sb("gw", [128, nseg, KC], F32)
    for s in range(nseg):
        mk.tt("dve", gw[:, s, :], mod_t[:, s, 5, :], nw2.all(), ALU.mult)
    hT = mk.sb("hT", [128, KC, Te], BF16)
    segs = []
    o = 0
    for (G, s) in groups:
        segs.append((o, G + 2, s))
        o += G + 2
    norm_mod_stream(P, xe_d, Te, segs, nw1, mod_t, 3, 4, hT)
    o = 0
    for gi, (G, s) in enumerate(groups):
        mk.tt("dve", hT[:, :, o:o + 1], hT[:, :, o:o + 1], mask_t[:, :, 2 * gi:2 * gi + 1], ALU.mult)
        mk.tt("dve", hT[:, :, o + G + 1:o + G + 2], hT[:, :, o + G + 1:o + G + 2],
              mask_t[:, :, 2 * gi + 1:2 * gi + 2], ALU.mult)
        o += G + 2
    GM = max(g for g, _ in groups)
    actT = mk.sb("actT", [128, NJ, GM], BF16)
    wub = [mk.sb(f"wub{i}", [128, KC, 2, 128], BF16) for i in range(2)]
    ub = [mk.sb(f"ub{i}", [128, GM + 2], F32) for i in range(4)]
    cvb = [mk.sb(f"cvb{i}", [128, GM], F32) for i in range(4)]
    wdb = [mk.sb(f"wdb{i}", [128, NJ, 128], BF16) for i in range(2)]
    yT = mk.sb("yT", [128, KC, GM], F32)
    sq = [mk.sb(f"sq{i}", [128, 512], BF16) for i in range(2)]
    rstd = mk.sb("rstd", [128, 512], F32)
    xb = [mk.sb(f"xb{i}", [128, GM], F32) for i in range(3)]
    xsrc = xe_d.rearrange("(kc p) t -> p kc t", p=128)
    odst = out_d.rearrange("(kc p) t -> p kc t", p=128)
    wusrc = wu_d.rearrange("(kc p) n -> p kc n", p=128)
    wdsrc = wd_d.rearrange("(j p) n -> p j n", p=128)
    eo = 0
    oo = 0
    it = 0
    ui = 0
    for gi, (G, s) in enumerate(groups):
        Gx = G + 2
        tls = tiles_of(Gx)
        for j0 in range(0, NJ, 1):
            wt = wub[it % 2]
            it += 1
            for half, cbase in ((0, j0 * 128), (1, DFF + j0 * 128)):
                mk.dma("pool", wt[:, :, half, :], wusrc[:, :, cbase:cbase + 128])
            for jj in range(1):
                j = j0 + jj
                us = []
                for half in range(2):
                    u = ub[ui % 4]
                    ui += 1
                    for (t0, tn) in tls:
                        ps = P.psum()
                        for kc in range(KC):
                            mk.matmul(ps[:, 0:tn], wt[:, kc, half, jj * 128:(jj + 1) * 128],
                                      hT[:, kc, eo + t0:eo + t0 + tn], start=(kc == 0), stop=(kc == KC - 1))
                        mk.copy("act" if half == 0 else "dve", u[:, t0:t0 + tn], ps[:, 0:tn])
                    us.append(u)
                cs = []
                for half in range(2):
                    ch = half * NJ + j
                    u = us[half]
                    cv = cvb[(2 * j + half) % 4]
                    mk.act(cv[:, 0:G], u[:, 1:G + 1], AF.Identity, bias=cb[:, ch:ch + 1], scale=cw[:, ch, 1:2])
                    mk.stt(cv[:, 0:G], u[:, 0:G], cw[:, ch, 0:1], cv[:, 0:G], ALU.mult, ALU.add)
                    mk.stt(cv[:, 0:G], u[:, 2:G + 2], cw[:, ch, 2:3], cv[:, 0:G], ALU.mult, ALU.add)
                    cs.append(cv)
                mk.act(cs[1][:, 0:G], cs[1][:, 0:G], AF.Silu)
                mk.tt("dve", actT[:, j, 0:G], cs[0][:, 0:G], cs[1][:, 0:G], ALU.mult)
        pss = P.psum()
        for n0 in range(0, KC, 1):
            wd = wdb[n0 % 2]
            for j0 in range(0, NJ, 11):
                mk.dma("pool", wd[:, j0:j0 + 11, :], wdsrc[:, j0:j0 + 11, n0 * 128:n0 * 128 + 128])
            for nn in range(1):
                n = n0 + nn
                ps = P.psum()
                if ps is pss:
                    ps = P.psum()
                for j in range(NJ):
                    mk.matmul(ps[:, 0:G], wd[:, j, nn * 128:(nn + 1) * 128], actT[:, j, 0:G],
                              start=(j == 0), stop=(j == NJ - 1))
                mk.copy("act", yT[:, n, 0:G], ps[:, 0:G])
                s_ = sq[n % 2]
                mk.act(s_[:, 0:G], yT[:, n, 0:G], AF.Square)
                mk.matmul(pss[:, 0:G], P.ones.all(), s_[:, 0:G], start=(n == 0), stop=(n == KC - 1))
        P.rstd_of(rstd[:, 0:G], pss[:, 0:G], D)
        for n in range(KC):
            x = xb[n % 3]
            mk.dma("act", x[:, 0:G], xsrc[:, n, eo + 1:eo + 1 + G])
            mk.tt("dve", yT[:, n, 0:G], yT[:, n, 0:G], rstd[:, 0:G], ALU.mult)
            mk.stt(x[:, 0:G], yT[:, n, 0:G], gw[:, s, n:n + 1], x[:, 0:G], ALU.mult, ALU.add)
            mk.dma("sp", odst[:, n, oo:oo + G], x[:, 0:G])
        eo += Gx
        oo += G
    cwk = np.ascontiguousarray(conv_w.T.reshape(2 * NJ, 128, 3).transpose(1, 0, 2))
    cbk = np.ascontiguousarray(conv_b.reshape(2 * NJ, 128).T)
    in_maps = [{"xe": xe_list[c], "mask": mask_list[c], "modT": modT_list[c], "nw1": vec_pk(nw_pre),
                "nw2": vec_pk(nw_post), "wu": w_up, "cw": cwk, "cb": cbk, "wd": w_down} for c in range(NCORES)]
    return [r["xo"] for r in P.run(in_maps)]


def run_C2(xe_list, mask_list, modT_list, nw_pre, nw_post, w_up, conv_w, conv_b, w_down, groups):
    ng = len(groups)
    nseg = 1 + max(s for _, s in groups)
    Te = sum(g + 2 for g, _ in groups)
    To = sum(g for g, _ in groups)
    NJ = DFF // 128
    P = Prog("C2")
    mk = P.mk
    xe_d = P.inp("xe", [D, Te])
    mask_d = P.inp("mask", [128, KC, 2 * ng])
    modT_d = P.inp("modT", [128, nseg, 6, KC])
    nw1_d = P.inp("nw1", [128, KC])
    nw2_d = P.inp("nw2", [128, KC])
    wu_d = P.inp("wu", [D, 2 * DFF])
    cw_d = P.inp("cw", [128, 2 * NJ, 3])
    cb_d = P.inp("cb", [128, 2 * NJ])
    wd_d = P.inp("wd", [DFF, D])
    out_d = P.outp("xo", [D, To])
    ysc_d = P.nc.dram_tensor("ysc", [D, To], F32, kind="Internal").ap()
    mod_t = P.load_mod(modT_d, nseg)
    nw1 = P.load_vec("nw1_t", nw1_d)
    nw2 = P.load_vec("nw2_t", nw2_d)
    mask_t = mk.sb("mask_t", [128, KC, 2 * ng], F32)
    mk.dma("sp", mask_t.all(), mask_d)
    cw = mk.sb("cw_t", [128, 2 * NJ, 3], F32)
    cb = mk.sb("cb_t", [128, 2 * NJ], F32)
    mk.dma("sp", cw.all(), cw_d)
    mk.dma("sp", cb.all(), cb_d)
    gw = mk.sb("gw", [128, nseg, KC], F32)
    for s in range(nseg):
        mk.tt("dve", gw[:, s, :], mod_t[:, s, 5, :], nw2.all(), ALU.mult)
    actT = mk.sb("actT", [128, NJ, To], BF16)
    xsrc = xe_d.rearrange("(kc p) t -> p kc t", p=128)
    odst = out_d.rearrange("(kc p) t -> p kc t", p=128)
    ysc = ysc_d.rearrange("(kc p) t -> p kc t", p=128)
    wusrc = wu_d.rearrange("(kc p) n -> p kc n", p=128)
    wdsrc = wd_d.rearrange("(j p) n -> p j n", p=128)
    geo = []
    eo = oo = 0
    for (G, s) in groups:
        geo.append((eo, oo, G, s))
        eo += G + 2
        oo += G
    mk.push()
    hT = mk.sb("hT", [128, KC, Te], BF16)
    mk.push()
    segs = [(e0, G + 2, s) for (e0, o0, G, s) in geo]
    norm_mod_stream(P, xe_d, Te, segs, nw1, mod_t, 3, 4, hT)
    for gi, (e0, o0, G, s) in enumerate(geo):
        mk.tt("dve", hT[:, :, e0:e0 + 1], hT[:, :, e0:e0 + 1], mask_t[:, :, 2 * gi:2 * gi + 1], ALU.mult)
        mk.tt("dve", hT[:, :, e0 + G + 1:e0 + G + 2], hT[:, :, e0 + G + 1:e0 + G + 2],
              mask_t[:, :, 2 * gi + 1:2 * gi + 2], ALU.mult)
    mk.barrier()
    mk.pop()
    mk.push()
    wub = [mk.sb(f"wub{i}", [128, KC, 2, 128], BF16) for i in range(3)]
    ub = [mk.sb(f"ub{i}", [128, Te], F32) for i in range(4)]
    cvb = [mk.sb(f"cvb{i}", [128, To], F32) for i in range(2)]
    tle = tiles_of(Te)
    for j in range(NJ):
        wt = wub[j % 3]
        for half, cbase in ((0, j * 128), (1, DFF + j * 128)):
            mk.dma("pool", wt[:, :, half, :], wusrc[:, :, cbase:cbase + 128])
        us = []
        for half in range(2):
            u = ub[(2 * j + half) % 4]
            for (t0, tn) in tle:
                ps = P.psum()
                for kc in range(KC):
                    mk.matmul(ps[:, 0:tn], wt[:, kc, half, :], hT[:, kc, t0:t0 + tn],
                              start=(kc == 0), stop=(kc == KC - 1))
                mk.copy("act" if half == 0 else "dve", u[:, t0:t0 + tn], ps[:, 0:tn])
            us.append(u)
        for half in range(2):
            ch = half * NJ + j
            u = us[half]
            cv = cvb[half]
            for (e0, o0, G, s) in geo:
                mk.act(cv[:, o0:o0 + G], u[:, e0 + 1:e0 + 1 + G], AF.Identity, bias=cb[:, ch:ch + 1],
                       scale=cw[:, ch, 1:2])
                mk.stt(cv[:, o0:o0 + G], u[:, e0:e0 + G], cw[:, ch, 0:1], cv[:, o0:o0 + G], ALU.mult, ALU.add)
                mk.stt(cv[:, o0:o0 + G], u[:, e0 + 2:e0 + 2 + G], cw[:, ch, 2:3], cv[:, o0:o0 + G], ALU.mult, ALU.add)
        mk.act(cvb[1].all(), cvb[1].all(), AF.Silu)
        mk.tt("dve", actT[:, j, :], cvb[0].all(), cvb[1].all(), ALU.mult)
    mk.barrier()
    mk.pop()
    mk.pop()
    wdb = [mk.sb(f"wdb{i}", [128, NJ, 128], BF16) for i in range(2)]
    yst = [mk.sb(f"yst{i}", [128, To], F32) for i in range(2)]
    sq = [mk.sb(f"sq{i}", [128, 512], BF16) for i in range(4)]
    rstd = mk.sb("rstd", [128, To], F32)
    xb = [mk.sb(f"xb{i}", [128, Te], F32) for i in range(2)]
    tlo = tiles_of(To)
    pss = [P.psb[5 + i] for i in range(len(tlo))]
    pk = 0
    pend2 = []
    for n in range(KC):
        wd = wdb[n % 2]
        for j0 in range(0, NJ, 11):
            mk.dma("pool", wd[:, j0:j0 + 11, :], wdsrc[:, j0:j0 + 11, n * 128:(n + 1) * 128])
        y = yst[n % 2]
        for ti, (t0, tn) in enumerate(tlo):
            ps = P.psb[pk % 5]
            pk += 1
            for j in range(NJ):
                mk.matmul(ps[:, 0:tn], wd[:, j, :], actT[:, j, t0:t0 + tn], start=(j == 0), stop=(j == NJ - 1))
            mk.copy("act", y[:, t0:t0 + tn], ps[:, 0:tn])
            s_ = sq[(n * len(tlo) + ti) % 4]
            mk.act(s_[:, 0:tn], y[:, t0:t0 + tn], AF.Square)
            pend2.append((n, ti, tn, s_))
            if len(pend2) > 2:
                m_, ti_, tn_, sq_ = pend2.pop(0)
                mk.matmul(pss[ti_][:, 0:tn_], P.ones.all(), sq_[:, 0:tn_], start=(m_ == 0), stop=(m_ == KC - 1))
        mk.dma("sp", ysc[:, n, :], y.all())
    for (m_, ti_, tn_, sq_) in pend2:
        mk.matmul(pss[ti_][:, 0:tn_], P.ones.all(), sq_[:, 0:tn_], start=(m_ == 0), stop=(m_ == KC - 1))
    for ti, (t0, tn) in enumerate(tlo):
        P.rstd_of(rstd[:, t0:t0 + tn], pss[ti][:, 0:tn], D)
    mk.barrier()
    y3 = yst + [mk.sb(f"yst3{i}", [128, To], F32) for i in range(2)]
    x3 = xb + [mk.sb(f"xb3{i}", [128, Te], F32) for i in range(2)]
    for n in range(KC):
        y = y3[n % 4]
        x = x3[n % 4]
        mk.dma("sp", y.all(), ysc[:, n, :])
        mk.dma("sp", x.all(), xsrc[:, n, :])
        mk.tt("dve", y.all(), y.all(), rstd.all(), ALU.mult)
        for (e0, o0, G, s) in geo:
            mk.stt(y[:, o0:o0 + G], y[:, o0:o0 + G], gw[:, s, n:n + 1], x[:, e0 + 1:e0 + 1 + G], ALU.mult, ALU.add)
        mk.dma("pool", odst[:, n, :], y.all())
    P.ps_i = 0
    cwk = np.ascontiguousarray(conv_w.T.reshape(2 * NJ, 128, 3).transpose(1, 0, 2))
    cbk = np.ascontiguousarray(conv_b.reshape(2 * NJ, 128).T)
    in_maps = [{"xe": xe_list[c], "mask": mask_list[c], "modT": modT_list[c], "nw1": vec_pk(nw_pre),
                "nw2": vec_pk(nw_post), "wu": w_up, "cw": cwk, "cb": cbk, "wd": w_down} for c in range(NCORES)]
    return [r["xo"] for r in P.run(in_maps)]


def ffn_host_io(xmid_lat, xmid_ctx, nctx):
    xe_list, mask_list = [], []
    groups = [(512, 0), (512, 0)] + ([(nctx, 1)] if nctx else [])
    for c in range(NCORES):
        b, q = c // 4, c % 4
        cols = []
        m = []
        for g in range(2):
            p0 = q * NLAT + g * 512
            blk = np.zeros((514, D), np.float32)
            lo, hi = p0 - 1, p0 + 513
            slo, shi = max(lo, 0), min(hi, SEQ)
            blk[slo - lo:shi - lo] = xmid_lat[b, slo:shi]
            cols.append(blk)
            m += [1.0 if lo >= 0 else 0.0, 1.0 if hi <= SEQ else 0.0]
        if nctx:
            blk = np.zeros((nctx + 2, D), np.float32)
            lo, hi = q * nctx - 1, q * nctx + nctx + 1
            slo, shi = max(lo, 0), min(hi, NCTX)
            blk[slo - lo:shi - lo] = xmid_ctx[b, slo:shi]
            cols.append(blk)
            m += [1.0 if lo >= 0 else 0.0, 1.0 if hi <= NCTX else 0.0]
        xe = np.ascontiguousarray(np.concatenate(cols, axis=0).T)
        xe_list.append(xe)
        mask_list.append(np.ascontiguousarray(np.broadcast_to(np.array(m, np.float32)[None, None, :], (128, KC, len(m)))))
    return xe_list, mask_list, groups


def rope_tables():
    half = 32
    freqs = 10000.0 ** (-np.arange(half, dtype=np.float32) / half)
    pos = np.arange(SEQ)
    row, col = pos // 64, pos % 64
    ang = np.zeros((128, SEQ), np.float32)
    for d in range(128):
        p = row if d < 64 else col
        ang[d] = p.astype(np.float32) * freqs[(d % 64) % 32]
    R = np.zeros((128, 128), np.float32)
    for d in range(128):
        if (d % 64) < 32:
            R[d + 32, d] = -1.0
        else:
            R[d - 32, d] = 1.0
    return np.cos(ang).astype(np.float32), np.sin(ang).astype(np.float32), R.astype(NPBF)


def run_B_gqa(qT_list, kT_list, v_list, q_norm, k_norm):
    T = NLAT + NCL
    NK = SEQ + NCTX
    NCH = NK // 128
    P = Prog("Bgqa")
    mk = P.mk
    qT_d = P.inp("qT", [D, T], BF16)
    kT_d = P.inp("kT", [512, NK], BF16)
    v_d = P.inp("v", [NK, 512], BF16)
    gn_d = P.inp("gn", [128, 2])
    cos_d = P.inp("cos", [128, SEQ])
    sin_d = P.inp("sin", [128, SEQ])
    cosq_d = P.inp("cosq", [128, NLAT])
    sinq_d = P.inp("sinq", [128, NLAT])
    R_d = P.inp("R", [128, 128], BF16)
    oT_d = P.outp("oT", [D, T], BF16)
    gn = mk.sb("gn", [128, 2], F32)
    mk.dma("sp", gn.all(), gn_d)
    mk.ts("dve", gn[:, 0:1], gn[:, 0:1], 128.0 ** -0.5, ALU.mult)
    Rm = mk.sb("Rm", [128, 128], BF16)
    mk.dma("sp", Rm.all(), R_d)
    cosk = mk.sb("cosk", [128, SEQ], F32)
    sink = mk.sb("sink", [128, SEQ], F32)
    cosq = mk.sb("cosq", [128, NLAT], F32)
    sinq = mk.sb("sinq", [128, NLAT], F32)
    mk.dma("sp", cosk.all(), cos_d)
    mk.dma("act", sink.all(), sin_d)
    mk.dma("sp", cosq.all(), cosq_d)
    mk.dma("act", sinq.all(), sinq_d)
    kT = mk.sb("kT", [128, 4, NK], BF16)
    mk.dma("sp", kT.all(), kT_d.rearrange("(h p) t -> p h t", p=128))
    qT = mk.sb("qT", [128, 16, T], BF16)
    for h0 in range(0, 16, 4):
        mk.dma("act", qT[:, h0:h0 + 4, :], qT_d.rearrange("(h p) t -> p h t", p=128)[:, h0:h0 + 4, :])
    vt = mk.sb("vt", [128, NCH, 512], BF16)
    vsrc = v_d.rearrange("(c p) n -> p c n", p=128)
    for c0 in range(0, NCH, 17):
        mk.dma("sp", vt[:, c0:c0 + 17, :], vsrc[:, c0:c0 + 17, :])
    sqb = [mk.sb(f"sqb{i}", [128, 512], BF16) for i in range(2)]
    rsb = [mk.sb(f"rsb{i}", [128, 512], F32) for i in range(2)]
    knb = [mk.sb(f"knb{i}", [128, 512], BF16) for i in range(2)]
    t1b = [mk.sb(f"t1b{i}", [128, 512], F32) for i in range(2)]
    t2b = [mk.sb(f"t2b{i}", [128, 512], F32) for i in range(2)]
    cnt = [0]

    def normrope(buf, h, t0, tn, gcol, cs, sn, c0):
        i = cnt[0] % 2
        cnt[0] += 1
        x = buf[:, h, t0:t0 + tn]
        mk.act(sqb[i][:, 0:tn], x, AF.Square)
        ps = P.psum()
        mk.matmul(ps[:, 0:tn], P.ones.all(), sqb[i][:, 0:tn])
        P.rstd_of(rsb[i][:, 0:tn], ps[:, 0:tn], 128)
        if cs is None:
            mk.stt(x, x, gcol, rsb[i][:, 0:tn], ALU.mult, ALU.mult)
            return
        mk.stt(knb[i][:, 0:tn], x, gcol, rsb[i][:, 0:tn], ALU.mult, ALU.mult)
        ps2 = P.psum()
        mk.matmul(ps2[:, 0:tn], Rm.all(), knb[i][:, 0:tn])
        mk.tt("pool", t1b[i][:, 0:tn], knb[i][:, 0:tn], cs[:, c0:c0 + tn], ALU.mult)
        mk.tt("dve", t2b[i][:, 0:tn], ps2[:, 0:tn], sn[:, c0:c0 + tn], ALU.mult)
        mk.tt("pool", x, t1b[i][:, 0:tn], t2b[i][:, 0:tn], ALU.add)

    for kv in range(4):
        for t0 in range(0, SEQ, 512):
            normrope(kT, kv, t0, 512, gn[:, 1:2], cosk, sink, t0)
        normrope(kT, kv, SEQ, NCTX, gn[:, 1:2], None, None, 0)
    pT = [mk.sb(f"pT{i}", [128, 512], BF16) for i in range(4)]
    rcp = [mk.sb(f"rcp{i}", [128, 512], F32) for i in range(2)]
    ost = [mk.sb(f"ost{i}", [128, T], BF16) for i in range(2)]
    pi = 0
    acc_i = 0
    for h in range(16):
        kv = h // 4
        for t0 in (0, 512):
            normrope(qT, h, t0, 512, gn[:, 0:1], cosq, sinq, t0)
        normrope(qT, h, NLAT, NCL, gn[:, 0:1], None, None, 0)
        st = ost[h % 2]
        for (t0, tn, chunks) in ((0, 512, range(NCH)), (512, 512, range(NCH)), (NLAT, NCL, range(32, NCH))):
            ps_o = P.psb[4 + 2 * (acc_i % 2)]
            ps_s = P.psb[5 + 2 * (acc_i % 2)]
            acc_i += 1
            chunks = list(chunks)
            LOOK = 2
            pend = []
            for ci in range(len(chunks) + LOOK):
                if ci < len(chunks):
                    c = chunks[ci]
                    ps = P.psb[pi % 4]
                    p_ = pT[pi % 4]
                    pi += 1
                    mk.matmul(ps[:, 0:tn], kT[:, kv, c * 128:(c + 1) * 128], qT[:, h, t0:t0 + tn])
                    mk.act(p_[:, 0:tn], ps[:, 0:tn], AF.Exp)
                    pend.append((ci, c, p_))
                if ci >= LOOK:
                    cj, c, p_ = pend.pop(0)
                    mk.matmul(ps_o[:, 0:tn], vt[:, c, kv * 128:(kv + 1) * 128], p_[:, 0:tn],
                              start=(cj == 0), stop=(cj == len(chunks) - 1))
                    mk.matmul(ps_s[:, 0:tn], P.ones.all(), p_[:, 0:tn], start=(cj == 0),
                              stop=(cj == len(chunks) - 1))
            r = rcp[acc_i % 2]
            mk.recip(r[:, 0:tn], ps_s[:, 0:tn])
            mk.tt("dve", st[:, t0:t0 + tn], ps_o[:, 0:tn], r[:, 0:tn], ALU.mult)
        mk.dma("sp", oT_d[h * 128:(h + 1) * 128, :], st.all())
    P.ps_i = 0
    cos, sin, R = rope_tables()
    gnk = np.ascontiguousarray(np.stack([q_norm, k_norm], axis=1).astype(np.float32))
    in_maps = []
    for c in range(NCORES):
        q = c % 4
        in_maps.append({"qT": qT_list[c], "kT": kT_list[c], "v": v_list[c], "gn": gnk, "cos": cos, "sin": sin,
                        "cosq": np.ascontiguousarray(cos[:, q * NLAT:(q + 1) * NLAT]),
                        "sinq": np.ascontiguousarray(sin[:, q * NLAT:(q + 1) * NLAT]), "R": R})
    return [r["oT"] for r in P.run(in_maps)]


def to_fm(x_tok):
    return np.ascontiguousarray(x_tok.T)


def core_xT(x_lat, x_ctx, nctx):
    out = []
    for c in range(NCORES):
        b, q = c // 4, c % 4
        parts = [x_lat[b, q * NLAT:(q + 1) * NLAT]]
        if nctx:
            parts.append(x_ctx[b, q * nctx:(q + 1) * nctx])
        out.append(to_fm(np.concatenate(parts, axis=0)))
    return out


def layer_gqa(x_lat, x_ctx, mod, L, inp):
    modT = [mod_layout(mod, L, c // 4) for c in range(NCORES)]
    xT = core_xT(x_lat, x_ctx, NCL)
    specs = [("qT", 0, 2048, "fm", "bf16"), ("kT", 2048, 512, "fm", "bf16"), ("v", 2560, 512, "tm", "bf16")]
    ra = run_A(xT, modT, inp["norm_pre_mix"][L], inp["gqa_w_in"][0], specs, NCL)
    kT_list, v_list = [], []
    for c in range(NCORES):
        b = c // 4
        kT_list.append(np.ascontiguousarray(np.concatenate(
            [ra[4 * b + i]["kT"][:, 0:NLAT] for i in range(4)] + [ra[4 * b + i]["kT"][:, NLAT:] for i in range(4)],
            axis=1)))
        v_list.append(np.ascontiguousarray(np.concatenate(
            [ra[4 * b + i]["v"][0:NLAT] for i in range(4)] + [ra[4 * b + i]["v"][NLAT:] for i in range(4)], axis=0)))
    oT = run_B_gqa([r["qT"] for r in ra], kT_list, v_list, inp["gqa_q_norm"][0], inp["gqa_k_norm"][0])
    xmid = run_C1(oT, xT, modT, inp["norm_post_mix"][L], inp["gqa_w_out"][0], None, NCL)
    return xmid


def split_xT(xT_list, nctx):
    x_lat = np.zeros((2, SEQ, D), np.float32)
    x_ctx = np.zeros((2, 4 * nctx, D), np.float32) if nctx else None
    for c in range(NCORES):
        b, q = c // 4, c % 4
        x_lat[b, q * NLAT:(q + 1) * NLAT] = xT_list[c][:, 0:NLAT].T
        if nctx:
            x_ctx[b, q * nctx:(q + 1) * nctx] = xT_list[c][:, NLAT:NLAT + nctx].T
    return x_lat, x_ctx


def layer_ffn(xmid_lat, xmid_ctx, mod, L, inp, nctx):
    modT = [mod_layout(mod, L, c // 4, with_ctx=bool(nctx)) for c in range(NCORES)]
    xe_list, mask_list, groups = ffn_host_io(xmid_lat, xmid_ctx, nctx)
    xo = run_C2(xe_list, mask_list, modT, inp["norm_pre_ffn"][L], inp["norm_post_ffn"][L], inp["ffn_w_up"][L],
                inp["ffn_conv_w"][L], inp["ffn_conv_b"][L], inp["ffn_w_down"][L], groups)
    return split_xT(xo, nctx)


def run_B_conv(zT_list, mask_list, b_pw1, w_dw, b_dw, ln_w, ln_b, nctx):
    HW = 15
    Le = NLAT + 2 * HW
    Ce = nctx + 2 * HW
    Te = Le + Ce
    T = NLAT + nctx
    P = Prog("Bconv")
    mk = P.mk
    zT_d = P.inp("zT", [2 * D, Te])
    mask_d = P.inp("mask", [128, Te])
    bp_d = P.inp("bp", [128, 2 * KC])
    wdw_d = P.inp("wdw", [128, KC, 31])
    bdw_d = P.inp("bdw", [128, KC])
    lnw_d = P.inp("lnw", [128, KC])
    lnb_d = P.inp("lnb", [128, KC])
    sT_d = P.outp("sT", [D, T], BF16)
    id_d = P.inp("ident", [128, 128], BF16)
    maskt = mk.sb("maskt", [128, Te], F32)
    mk.dma("sp", maskt.all(), mask_d)
    bp = mk.sb("bp", [128, 2 * KC], F32)
    mk.dma("sp", bp.all(), bp_d)
    wdw = mk.sb("wdw", [128, KC, 31], F32)
    mk.dma("sp", wdw.all(), wdw_d)
    bdw = P.load_vec("bdw", bdw_d)
    lnw = P.load_vec("lnw", lnw_d)
    lnb = P.load_vec("lnb", lnb_d)
    onesf = mk.sb("onesf", [128, 128], F32)
    mk.memset("dve", onesf.all(), 1.0)
    vT = mk.sb("vT", [128, KC, T], F32)
    zb = [mk.sb(f"zb{i}", [128, Te], F32) for i in range(4)]
    ub = [mk.sb(f"ub{i}", [128, Te], BF16) for i in range(2)]
    sqf = [mk.sb(f"sqf{i}", [128, 512], F32) for i in range(2)]
    dgb = [mk.sb(f"dgb{i}", [128, 31, 128], BF16) for i in range(2)]
    identb = mk.sb("identb", [128, 128], BF16)
    mk.dma("sp", identb.all(), id_d)
    pcv = [0]
    zsrc = zT_d.rearrange("(kc p) t -> p kc t", p=128)
    tls = tiles_of(T)
    ps_sum = [P.psb[i] for i in range(len(tls))]
    ps_sq = [P.psb[3 + i] for i in range(len(tls))]
    for kc in range(KC):
        za = zb[(2 * kc) % 4]
        zg = zb[(2 * kc + 1) % 4]
        mk.dma("sp", za.all(), zsrc[:, kc, :])
        mk.dma("act", zg.all(), zsrc[:, KC + kc, :])
        mk.act(zg.all(), zg.all(), AF.Sigmoid, bias=bp[:, KC + kc:KC + kc + 1])
        mk.tt("pool", zg.all(), zg.all(), maskt.all(), ALU.mult)
        u = ub[kc % 2]
        mk.stt(u.all(), za.all(), bp[:, kc:kc + 1], zg.all(), ALU.add, ALU.mult)
        dg = dgb[kc % 2]
        for j in range(31):
            mk.act(dg[:, j, :], identb.all(), AF.Copy, scale=wdw[:, kc, j:j + 1])
        for (e0, o0, n) in ((0, 0, NLAT), (Le, NLAT, nctx)):
            if n == 0:
                continue
            for (t0, tn) in tiles_of(n):
                ps = P.psb[6 + (pcv[0] % 2)]
                pcv[0] += 1
                for j in range(31):
                    mk.matmul(ps[:, 0:tn], dg[:, j, :], u[:, e0 + j + t0:e0 + j + t0 + tn],
                              start=(j == 0), stop=(j == 30))
                mk.act(vT[:, kc, o0 + t0:o0 + t0 + tn], ps[:, 0:tn], AF.Identity, bias=bdw[:, kc:kc + 1])
        for ti, (t0, tn) in enumerate(tls):
            s_ = sqf[(kc * len(tls) + ti) % 2]
            mk.act(s_[:, 0:tn], vT[:, kc, t0:t0 + tn], AF.Square)
            mk.matmul(ps_sum[ti][:, 0:tn], onesf.all(), vT[:, kc, t0:t0 + tn], start=(kc == 0), stop=(kc == KC - 1))
            mk.matmul(ps_sq[ti][:, 0:tn], onesf.all(), s_[:, 0:tn], start=(kc == 0), stop=(kc == KC - 1))
    mean = mk.sb("mean", [128, T], F32)
    rstd = mk.sb("rstd", [128, T], F32)
    msq = mk.sb("msq", [128, T], F32)
    for ti, (t0, tn) in enumerate(tls):
        mk.ts("dve", mean[:, t0:t0 + tn], ps_sum[ti][:, 0:tn], 1.0 / D, ALU.mult)
        mk.tt("dve", msq[:, t0:t0 + tn], mean[:, t0:t0 + tn], mean[:, t0:t0 + tn], ALU.mult)
        mk.stt(msq[:, t0:t0 + tn], ps_sq[ti][:, 0:tn], 1.0 / D, msq[:, t0:t0 + tn], ALU.mult, ALU.subtract)
        mk.act(rstd[:, t0:t0 + tn], msq[:, t0:t0 + tn], AF.Sqrt, bias=P.eps_t[:, 0:1])
        mk.recip(rstd[:, t0:t0 + tn], rstd[:, t0:t0 + tn])
    sb_ = [mk.sb(f"sbo{i}", [128, T], BF16) for i in range(2)]
    tmp = [mk.sb(f"tmpn{i}", [128, T], F32) for i in range(2)]
    for kc in range(KC):
        t = tmp[kc % 2]
        mk.tt("pool", t.all(), vT[:, kc, :], mean.all(), ALU.subtract)
        mk.tt("dve", t.all(), t.all(), rstd.all(), ALU.mult)
        so = sb_[kc % 2]
        mk.act(so.all(), t.all(), AF.Silu, bias=lnb[:, kc:kc + 1], scale=lnw[:, kc:kc + 1])
        mk.dma("sp", sT_d[kc * 128:(kc + 1) * 128, :], so.all())
    bpk = np.ascontiguousarray(b_pw1.reshape(2 * KC, 128).T)
    wdwk = np.ascontiguousarray(w_dw.T.reshape(KC, 128, 31).transpose(1, 0, 2))
    in_maps = [{"zT": zT_list[c], "mask": mask_list[c], "bp": bpk, "wdw": wdwk, "bdw": vec_pk(b_dw),
                "lnw": vec_pk(ln_w), "lnb": vec_pk(ln_b), "ident": np.eye(128, dtype=np.float32).astype(NPBF)}
               for c in range(NCORES)]
    return [r["sT"] for r in P.run(in_maps)]


def layer_conv(x_lat, x_ctx, mod, L, inp):
    HW = 15
    modT = [mod_layout(mod, L, c // 4) for c in range(NCORES)]
    xT = core_xT(x_lat, x_ctx, NCL)
    ra = run_A(xT, modT, inp["norm_pre_mix"][L], inp["conv_w_pw1"][0], [("zT", 0, 2 * D, "fm", "f32")], NCL)
    zT_list, mask_list = [], []
    for b in range(2):
        zl = np.concatenate([ra[4 * b + i]["zT"][:, 0:NLAT] for i in range(4)], axis=1)
        zl = np.pad(zl, ((0, 0), (HW, HW)))
        ml = np.pad(np.ones(SEQ, np.float32), (HW, HW))
        zc = np.pad(np.concatenate([ra[4 * b + i]["zT"][:, NLAT:] for i in range(4)], axis=1), ((0, 0), (HW, HW)))
        mc = np.pad(np.ones(NCTX, np.float32), (HW, HW))
        for q in range(4):
            sl = slice(q * NLAT, q * NLAT + NLAT + 2 * HW)
            slc = slice(q * NCL, q * NCL + NCL + 2 * HW)
            zT_list.append(np.ascontiguousarray(np.concatenate([zl[:, sl], zc[:, slc]], axis=1)))
            m = np.concatenate([ml[sl], mc[slc]])
            mask_list.append(np.ascontiguousarray(np.broadcast_to(m[None, :], (128, m.shape[0]))))
    sT = run_B_conv(zT_list, mask_list, inp["conv_b_pw1"][0], inp["conv_w_dw"][0], inp["conv_b_dw"][0],
                    inp["conv_ln_w"][0], inp["conv_ln_b"][0], NCL)
    xmid = run_C1(sT, xT, modT, inp["norm_post_mix"][L], inp["conv_w_pw2"][0], inp["conv_b_pw2"][0], NCL)
    return xmid


def rview(v, pattern, **kw):
    return V(v.ap.rearrange(pattern, **kw), v.tile, v.lo, v.hi)


def nat_tables(rpb):
    p = np.arange(128)
    half, kc = p // 64, p % 64
    i = np.arange(8)
    qc = np.arange(64)
    dr = -8 + 2 * i[None, :] + half[:, None]
    cidx = kc[:, None] - qc[None, :] + 15
    cstart = np.clip(qc - 8, 0, 48)
    cval = (kc[:, None] >= cstart[None, :]) & (kc[:, None] < cstart[None, :] + 16)
    rv = dr >= -7
    B = rpb[:, np.clip(dr + 7, 0, 14)[:, :, None], np.clip(cidx, 0, 30)[:, None, :]]
    B = np.ascontiguousarray(B.transpose(1, 0, 2, 3)).astype(np.float32)
    M = (rv[:, :, None] & cval[:, None, :]).astype(np.float32)
    M = np.ascontiguousarray(np.broadcast_to(M[:, None], (128, 8, 8, 64)))
    return B, M


def nat_rowvalid(q):
    p = np.arange(128)
    half = p // 64
    out = np.zeros((128, 16, 8), np.float32)
    for rl in range(16):
        r = 16 * q + rl
        rs = min(max(r - 4, 0), 56)
        for i in range(8):
            kr = r - 8 + 2 * i + half
            out[:, rl, i] = ((kr >= rs) & (kr < rs + 8)).astype(np.float32)
    return out


def run_B_nat(qT_list, kTw_list, vw_list, kTc_list, vc_list, rpb):
    P = Prog("Bnat")
    mk = P.mk
    NW = 2048
    qT_d = P.inp("qT", [D, NLAT], BF16)
    kT_d = P.inp("kTw", [D, NW], BF16)
    v_d = P.inp("vw", [NW, D], BF16)
    kTc_d = P.inp("kTc", [D, NCTX], BF16)
    vc_d = P.inp("vc", [NCTX, D], BF16)
    B_d = P.inp("B", [128, 16, 8, 64])
    M_d = P.inp("M", [128, 8, 8, 64])
    rv_d = P.inp("rv", [128, 16, 8])
    oT_d = P.outp("oT", [D, NLAT], BF16)
    rv = mk.sb("rv", [128, 16, 8], F32)
    mk.dma("sp", rv.all(), rv_d)
    Mt = mk.sb("Mt", [128, 8, 8, 64], F32)
    mk.dma("sp", Mt.all(), M_d)
    Et = mk.sb("Et", [128, 8, 8, 64], F32)
    kT = mk.sb("kT", [128, 8, NW], BF16)
    qT = mk.sb("qT", [128, 8, NLAT], BF16)
    kTc = mk.sb("kTc", [128, 8, NCTX], BF16)
    vc = mk.sb("vc", [128, 2, 1024], BF16)
    vb = [mk.sb(f"vb{i}", [128, 8, 1024], BF16) for i in range(2)]
    pT = [mk.sb(f"pT{i}", [128, 512], BF16) for i in range(4)]
    rcp = [mk.sb(f"rcp{i}", [128, 512], F32) for i in range(2)]
    ost = mk.sb("ost", [128, 8, NLAT], BF16)
    scale = 128.0 ** -0.5
    pi = 0
    acc_i = 0
    vi = 0
    for g in range(2):
        hs = slice(g * 1024, (g + 1) * 1024)
        mk.dma("sp", Et.all(), B_d[:, g * 8:(g + 1) * 8, :, :])
        mk.act(Et.all(), Et.all(), AF.Exp)
        mk.tt("pool", Et.all(), Et.all(), Mt.all(), ALU.mult)
        for h0 in range(0, 8, 4):
            mk.dma("sp", kT[:, h0:h0 + 4, :], kT_d[hs, :].rearrange("(h p) t -> p h t", p=128)[:, h0:h0 + 4, :])
        mk.dma("act", qT.all(), qT_d[hs, :].rearrange("(h p) t -> p h t", p=128))
        mk.dma("act", kTc.all(), kTc_d[hs, :].rearrange("(h p) t -> p h t", p=128))
        mk.dma("act", vc.all(), vc_d[:, hs].rearrange("(c p) n -> p c n", p=128))
        for rl in range(16):
            vband = vb[vi % 2]
            vi += 1
            mk.dma("sp" if rl % 2 == 0 else "act", vband.all(),
                   v_d[rl * 64:rl * 64 + 1024, hs].rearrange("(c p) n -> p c n", p=128))
            ps_o = P.psb[4 + 2 * (acc_i % 2)]
            ps_s = P.psb[5 + 2 * (acc_i % 2)]
            acc_i += 1
            qs = slice(rl * 64, (rl + 1) * 64)
            LOOK = 2
            pend = []
            for ii in range(10 + LOOK):
                if ii < 10:
                    i = ii
                    ps = P.psb[pi % 4]
                    p_ = pT[pi % 4]
                    pi += 1
                    for h in range(8):
                        if i < 8:
                            lhs = kT[:, h, (rl + 2 * i) * 64:(rl + 2 * i) * 64 + 128]
                        else:
                            lhs = kTc[:, h, (i - 8) * 128:(i - 7) * 128]
                        mk.matmul(ps[:, h * 64:(h + 1) * 64], lhs, qT[:, h, qs])
                    mk.act(p_.all(), ps.all(), AF.Exp, scale=scale)
                    if i < 8:
                        mk.stt(rview(p_.all(), "p (h q) -> p h q", h=8), rview(p_.all(), "p (h q) -> p h q", h=8),
                               rv[:, rl, i:i + 1], Et[:, :, i, :], ALU.mult, ALU.mult)
                    pend.append((i, p_))
                if ii < LOOK:
                    continue
                i, p_ = pend.pop(0)
                for h in range(8):
                    vv = vband[:, i, h * 128:(h + 1) * 128] if i < 8 else vc[:, i - 8, h * 128:(h + 1) * 128]
                    mk.matmul(ps_o[:, h * 64:(h + 1) * 64], vv, p_[:, h * 64:(h + 1) * 64],
                              start=(i == 0 and h == 0), stop=(i == 9), skip=True)
                mk.matmul(ps_s.all(), P.ones.all(), p_.all(), start=(i == 0), stop=(i == 9))
            r = rcp[acc_i % 2]
            mk.recip(r.all(), ps_s.all())
            mk.tt("dve", ost[:, :, qs], rview(ps_o.all(), "p (h q) -> p h q", h=8),
                  rview(r.all(), "p (h q) -> p h q", h=8), ALU.mult)
        mk.dma("sp", oT_d[hs, :].rearrange("(h p) t -> p h t", p=128), ost.all())
    B, M = nat_tables(rpb)
    in_maps = [{"qT": qT_list[c], "kTw": kTw_list[c], "vw": vw_list[c], "kTc": kTc_list[c], "vc": vc_list[c],
                "B": B, "M": M, "rv": nat_rowvalid(c % 4)} for c in range(NCORES)]
    return [r["oT"] for r in P.run(in_maps)]


def layer_nat(x_lat, x_ctx, mod, L, inp):
    modT = [mod_layout(mod, L, c // 4) for c in range(NCORES)]
    xT = core_xT(x_lat, x_ctx, NCL)
    specs = [("qT", 0, 2048, "fm", "bf16"), ("kT", 2048, 2048, "fm", "bf16"), ("v", 4096, 2048, "tm", "bf16")]
    ra = run_A(xT, modT, inp["norm_pre_mix"][L], inp["nat_w_in"][0], specs, NCL)
    qT_list, kTw, vw, kTc, vcl = [], [], [], [], []
    for b in range(2):
        kg = np.concatenate([ra[4 * b + i]["kT"][:, 0:NLAT] for i in range(4)], axis=1)
        kg = np.pad(kg, ((0, 0), (512, 512)))
        vg = np.concatenate([ra[4 * b + i]["v"][0:NLAT] for i in range(4)], axis=0)
        vg = np.pad(vg, ((512, 512), (0, 0)))
        for q in range(4):
            c = 4 * b + q
            qT_list.append(np.ascontiguousarray(ra[c]["qT"][:, 0:NLAT]))
            kTw.append(np.ascontiguousarray(kg[:, q * 1024:q * 1024 + 2048]))
            vw.append(np.ascontiguousarray(vg[q * 1024:q * 1024 + 2048]))
            kTc.append(np.ascontiguousarray(np.concatenate([ra[4 * b + i]["kT"][:, NLAT:] for i in range(4)], axis=1)))
            vcl.append(np.ascontiguousarray(np.concatenate([ra[4 * b + i]["v"][NLAT:] for i in range(4)], axis=0)))
    oT = run_B_nat(qT_list, kTw, vw, kTc, vcl, inp["nat_rpb"][0])
    modT1 = [mod_layout(mod, L, c // 4, with_ctx=False) for c in range(NCORES)]
    xT1 = [np.ascontiguousarray(x[:, 0:NLAT]) for x in xT]
    xmid = run_C1(oT, xT1, modT1, inp["norm_post_mix"][L], inp["nat_w_out"][0], None, 0)
    return xmid


def mlstm_consts():
    blk = np.arange(128) // 64
    same = blk[:, None] == blk[None, :]
    idx = np.arange(128)
    Uf = (same & (idx[:, None] <= idx[None, :])).astype(np.float32)
    Ub = np.ascontiguousarray(Uf.T)
    I = np.eye(128, dtype=np.float32)
    out = np.zeros((2, 5, 128, 128), np.float32)
    for d, U in enumerate((Uf, Ub)):
        out[d, 0] = U
        out[d, 1] = -U
        out[d, 2] = (U - 1.0) * 30000.0
        out[d, 3] = U.T - I
        out[d, 4] = I
    return np.ascontiguousarray(out.transpose(2, 0, 1, 3))


def run_B_mlstm(qT_list, kT_list, ktm_list, vtm_list, otm_list, gtm_list, bg_list, gain_list):
    NT = NCTX + SEQ
    NSC = NT // 128
    P = Prog("Bmlstm")
    mk = P.mk
    qT_d = P.inp("qT", [2, 128, NT], BF16)
    kT_d = P.inp("kT", [2, 128, NT], BF16)
    ktm_d = P.inp("ktm", [NT, 2, 128], BF16)
    vtm_d = P.inp("vtm", [NT, 2, 256], BF16)
    otm_d = P.inp("otm", [NT, 2, 256])
    gtm_d = P.inp("gtm", [NT, 2, 4])
    bg_d = P.inp("bg", [128, 2, 4])
    gain_d = P.inp("gain", [128, 2, 256])
    cm_d = P.inp("cm", [128, 2, 5, 128])
    out_d = P.outp("hout", [NT, 2, 256], BF16)
    cm = mk.sb("cm", [128, 2, 5, 128], F32)
    mk.dma("sp", cm.all(), cm_d)
    bg = mk.sb("bg", [128, 2, 4], F32)
    mk.dma("sp", bg.all(), bg_d)
    gain = mk.sb("gain", [128, 2, 256], F32)
    mk.dma("sp", gain.all(), gain_d)
    onesf = mk.sb("onesf", [128, 128], F32)
    mk.memset("dve", onesf.all(), 1.0)
    qT = mk.sb("qT", [128, NT], BF16)
    kT = mk.sb("kT", [128, NT], BF16)
    ktm = mk.sb("ktm", [128, NSC, 128], BF16)
    vtm = mk.sb("vtm", [128, NSC, 257], BF16)
    otm = mk.sb("otm", [128, NSC, 256], F32)
    gt = mk.sb("gt", [128, NSC, 4], F32)
    tmpg = mk.sb("tmpg", [128, NSC, 4], F32)
    IG = mk.sb("IG", [128, 2, NSC], F32)
    LF = mk.sb("LF", [128, 2, NSC], F32)
    Hacc = mk.sb("Hacc", [128, NSC, 256], F32)
    Cst = [[mk.sb(f"Cst{d}{i}", [128, 257], F32) for i in range(2)] for d in range(2)]
    Cbf = [[mk.sb(f"Cbf{d}{i}", [128, 257], BF16) for i in range(3)] for d in range(2)]
    Qpad = [[mk.sb(f"Qpad{d}{i}", [128, 256], BF16) for i in range(2)] for d in range(2)]
    for d in range(2):
        for i in range(2):
            mk.memset("pool", Qpad[d][i].all(), 0.0)
    LFbc = [mk.sb(f"LFbc{i}", [128, 128], F32) for i in range(2)]
    Edec = [mk.sb(f"Edec{i}", [128, 128], F32) for i in range(2)]
    DmT = [mk.sb(f"DmT{i}", [128, 128], F32) for i in range(2)]
    wgt = [mk.sb(f"wgt{i}", [128, 1], F32) for i in range(2)]
    Kw = [mk.sb(f"Kw{i}", [128, 128], BF16) for i in range(2)]
    Sm = [mk.sb(f"Sm{i}", [128, 128], BF16) for i in range(2)]
    dn = [mk.sb(f"dn{i}", [128, 1], F32) for i in range(2)]
    ssq = mk.sb("ssq", [128, NSC], F32)
    rst = mk.sb("rst", [128, NSC], F32)
    sqj = mk.sb("sqj", [128, 256], F32)
    sg = [mk.sb(f"sg{i}", [128, 256], F32) for i in range(2)]
    hn = [mk.sb(f"hn{i}", [128, 256], F32) for i in range(2)]
    ob = [mk.sb(f"ob{i}", [128, 256], BF16) for i in range(2)]
    scale = 128.0 ** -0.5
    order_f = list(range(NSC))
    order_b = [1, 0] + list(range(NSC - 1, 1, -1))
    it = 0
    for hd in range(2):
        mk.dma("sp", qT.all(), qT_d[hd])
        mk.dma("act", kT.all(), kT_d[hd])
        mk.dma("sp", ktm.all(), ktm_d[:, hd, :].rearrange("(c p) n -> p c n", p=128))
        mk.dma("act", vtm[:, :, 0:256], vtm_d[:, hd, :].rearrange("(c p) n -> p c n", p=128))
        mk.memset("pool", vtm[:, :, 256:257], 1.0)
        mk.dma("sp", otm.all(), otm_d[:, hd, :].rearrange("(c p) n -> p c n", p=128))
        mk.dma("act", gt.all(), gtm_d[:, hd, :].rearrange("(c p) n -> p c n", p=128))
        for c in range(4):
            mk.act(gt[:, :, c:c + 1], gt[:, :, c:c + 1], AF.Identity, bias=bg[:, hd, c:c + 1])
        mk.act(tmpg.all(), gt.all(), AF.Exp, scale=-1.0)
        mk.act(tmpg.all(), tmpg.all(), AF.Ln, bias=onesf[:, 0:1])
        for d in range(2):
            mk.act(rview(IG[:, d, :], "p (c o) -> p c o", o=1), gt[:, :, 2 * d:2 * d + 1], AF.Copy)
            mk.act(rview(LF[:, d, :], "p (c o) -> p c o", o=1), tmpg[:, :, 2 * d + 1:2 * d + 2], AF.Copy, scale=-1.0)
        mk.memset("pool", Hacc.all(), 0.0)
        cur = [0, 0]
        cbi = [0, 0]
        for d in range(2):
            mk.memset("dve", Cst[d][0].all(), 0.0)
            mk.memset("pool", Cbf[d][0].all(), 0.0)
        for step in range(NSC):
            for d in range(2):
                sc = (order_f if d == 0 else order_b)[step]
                i2 = it % 2
                it += 1
                U, nU, NEG, SL, Id = (cm[:, d, k, :] for k in range(5))
                lfc = LF[:, d, sc:sc + 1]
                igc = IG[:, d, sc:sc + 1]
                tsl = slice(sc * 128, (sc + 1) * 128)
                mk.act(LFbc[i2].all(), onesf.all(), AF.Copy, scale=lfc)
                ps1 = P.psum()
                mk.matmul(ps1[:, 0:128], LFbc[i2].all(), U)
                mk.act(Edec[i2].all(), ps1[:, 0:128], AF.Exp)
                ps2 = P.psum()
                mk.matmul(ps2[:, 0:128], LFbc[i2].all(), U, start=True, stop=False)
                mk.matmul(ps2[:, 0:128], nU, LFbc[i2].all(), start=False, stop=False)
                mk.matmul(ps2[:, 0:128], Id, NEG, start=False, stop=True)
                mk.act(DmT[i2].all(), ps2[:, 0:128], AF.Exp, bias=igc)
                ps3 = P.psum()
                mk.matmul(ps3[:, 0:1], SL, lfc)
                mk.act(wgt[i2].all(), ps3[:, 0:1], AF.Exp, bias=igc)
                mk.ts("dve", Kw[i2].all(), ktm[:, sc, :], wgt[i2][:, 0:1], ALU.mult)
                ps4 = P.psum()
                mk.matmul(ps4[:, 0:128], kT[:, tsl], qT[:, tsl])
                mk.stt(Sm[i2].all(), ps4[:, 0:128], scale, DmT[i2].all(), ALU.mult, ALU.mult)
                qp = Qpad[d][step % 2]
                mk.stt(qp[:, 0:64], qT[:, sc * 128:sc * 128 + 64], scale, Edec[i2][:, 0:64], ALU.mult, ALU.mult)
                mk.stt(qp[:, 192:256], qT[:, sc * 128 + 64:sc * 128 + 128], scale, Edec[i2][:, 64:128],
                       ALU.mult, ALU.mult)
                first, second = ((0, 64), (64, 128)) if d == 0 else ((64, 128), (0, 64))
                gcol = (63, 127) if d == 0 else (64, 0)
                cb_in = Cbf[d][cbi[d] % 3]
                cs_in = Cst[d][cur[d] % 2]
                cs_mid = Cst[d][(cur[d] + 1) % 2]
                cb_mid = Cbf[d][(cbi[d] + 1) % 3]
                cb_out = Cbf[d][(cbi[d] + 2) % 3]
                ps6 = P.psum()
                mk.matmul(ps6[:, 0:257], Kw[i2][first[0]:first[1], :], vtm[first[0]:first[1], sc, :])
                mk.stt(cs_mid.all(), cs_in.all(), Edec[i2][:, gcol[0]:gcol[0] + 1], ps6[:, 0:257], ALU.mult, ALU.add)
                mk.copy("act", cb_mid.all(), cs_mid.all())
                ps7 = P.psum()
                mk.matmul(ps7[:, 0:257], Kw[i2][second[0]:second[1], :], vtm[second[0]:second[1], sc, :])
                mk.stt(cs_in.all(), cs_mid.all(), Edec[i2][:, gcol[1]:gcol[1] + 1], ps7[:, 0:257], ALU.mult, ALU.add)
                mk.copy("act", cb_out.all(), cs_in.all())
                cbi[d] += 2
                cA, cB = (cb_in, cb_mid) if d == 0 else (cb_mid, cb_in)
                ps5 = P.psum()
                mk.matmul(ps5[:, 0:257], Sm[i2].all(), vtm[:, sc, :], start=True, stop=False)
                mk.matmul(ps5[:, 0:257], qp[:, 0:128], cA.all(), start=False, stop=False)
                mk.matmul(ps5[:, 0:257], qp[:, 128:256], cB.all(), start=False, stop=True)
                mk.act(dn[i2].all(), ps5[:, 256:257], AF.Abs)
                mk.ts("dve", dn[i2].all(), dn[i2].all(), 1.0, ALU.max)
                mk.recip(dn[i2].all(), dn[i2].all())
                mk.stt(Hacc[:, sc, :], ps5[:, 0:256], dn[i2][:, 0:1], Hacc[:, sc, :], ALU.mult, ALU.add)
        mk.memset("dve", ssq.all(), 0.0)
        for sc in range(NSC):
            mk.act(sqj.all(), Hacc[:, sc, :], AF.Square, accum_out=ssq[:, sc:sc + 1])
        mk.act(rst.all(), ssq.all(), AF.Sqrt, bias=P.eps_t[:, 0:1], scale=1.0 / 256)
        mk.recip(rst.all(), rst.all())
        for sc in range(NSC):
            j = sc % 2
            mk.act(sg[j].all(), otm[:, sc, :], AF.Sigmoid)
            mk.stt(hn[j].all(), Hacc[:, sc, :], rst[:, sc:sc + 1], gain[:, hd, :], ALU.mult, ALU.mult)
            mk.tt("pool", ob[j].all(), hn[j].all(), sg[j].all(), ALU.mult)
            mk.dma("sp", out_d[sc * 128:(sc + 1) * 128, hd, :], ob[j].all())
    cmk = mlstm_consts()
    in_maps = [{"qT": qT_list[c], "kT": kT_list[c], "ktm": ktm_list[c], "vtm": vtm_list[c], "otm": otm_list[c],
                "gtm": gtm_list[c], "bg": bg_list[c], "gain": gain_list[c], "cm": cmk} for c in range(NCORES)]
    return [r["hout"] for r in P.run(in_maps)]


def layer_mlstm(x_lat, x_ctx, mod, L, inp):
    modT = [mod_layout(mod, L, c // 4) for c in range(NCORES)]
    xT = core_xT(x_lat, x_ctx, NCL)
    specs = [("qT", 0, 1024, "fm", "bf16"), ("kT", 1024, 1024, "fm", "bf16"), ("ktm", 1024, 1024, "tm", "bf16"),
             ("vtm", 2048, 2048, "tm", "bf16"), ("otm", 4096, 2048, "tm", "f32"), ("gtm", 6144, 32, "tm", "f32")]
    ra = run_A(xT, modT, inp["norm_pre_mix"][L], inp["mlstm_w_in"][0], specs, NCL)

    def glob_fm(name, b):
        return np.concatenate([ra[4 * b + i][name][:, NLAT:] for i in range(4)]
                              + [ra[4 * b + i][name][:, 0:NLAT] for i in range(4)], axis=1)

    def glob_tm(name, b):
        return np.concatenate([ra[4 * b + i][name][NLAT:] for i in range(4)]
                              + [ra[4 * b + i][name][0:NLAT] for i in range(4)], axis=0)

    lists = [[] for _ in range(8)]
    b_gate = inp["mlstm_b_gate"][0].reshape(4, 8)
    onorm = inp["mlstm_out_norm"][0].reshape(8, 256)
    for b in range(2):
        qg, kg = glob_fm("qT", b), glob_fm("kT", b)
        ktm, vtm, otm, gtm = (glob_tm(n, b) for n in ("ktm", "vtm", "otm", "gtm"))
        NT = qg.shape[1]
        for hp in range(4):
            hs = [2 * hp, 2 * hp + 1]
            lists[0].append(np.ascontiguousarray(qg.reshape(8, 128, NT)[hs]))
            lists[1].append(np.ascontiguousarray(kg.reshape(8, 128, NT)[hs]))
            lists[2].append(np.ascontiguousarray(ktm.reshape(NT, 8, 128)[:, hs]))
            lists[3].append(np.ascontiguousarray(vtm.reshape(NT, 8, 256)[:, hs]))
            lists[4].append(np.ascontiguousarray(otm.reshape(NT, 8, 256)[:, hs]))
            lists[5].append(np.ascontiguousarray(gtm.reshape(NT, 4, 8)[:, :, hs].transpose(0, 2, 1)))
            lists[6].append(np.ascontiguousarray(np.broadcast_to(b_gate[:, hs].T[None], (128, 2, 4))))
            lists[7].append(np.ascontiguousarray(np.broadcast_to(onorm[hs][None], (128, 2, 256))))
    ho = run_B_mlstm(*lists)
    oT = []
    for b in range(2):
        og = np.concatenate([ho[4 * b + hp] for hp in range(4)], axis=1)
        og = og.reshape(NCTX + SEQ, D)
        for q in range(4):
            tok = np.concatenate([og[NCTX + q * NLAT:NCTX + (q + 1) * NLAT], og[q * NCL:(q + 1) * NCL]], axis=0)
            oT.append(to_fm(tok))
    xmid = run_C1(oT, xT, modT, inp["norm_post_mix"][L], inp["mlstm_w_out"][0], None, NCL)
    return xmid


def kernel(**inputs):
    inp = {k: np.asarray(v) for k, v in inputs.items()}
    mod = run_mod(inp["c"], inp["c_ctx"], inp["mod_w"], inp["mod_b"])
    x_lat, x_ctx = inp["x"], inp["ctx"]
    layers = [layer_gqa, layer_mlstm, layer_conv, layer_nat]
    for L in range(4):
        nctx = NCL if L < 3 else 0
        xmid = layers[L](x_lat, x_ctx, mod, L, inp)
        xl, xc = split_xT(xmid, nctx)
        x_lat, x_ctx = layer_ffn(xl, xc, mod, L, inp, nctx)
    return np.ascontiguousarray(x_lat.astype(np.float32))
```

```python
import numpy as np
from contextlib import ExitStack
import ml_dtypes
import concourse.bass as bass
import concourse.mybir as mybir
from concourse.bass_utils import run_bass_kernel_spmd

F32 = mybir.dt.float32
BF16 = mybir.dt.bfloat16
AF = mybir.ActivationFunctionType
ALU = mybir.AluOpType
AX = mybir.AxisListType
NPBF = ml_dtypes.bfloat16

D = 2048
KC = 16
NCTX = 256
SEQ = 4096
NLAT = 1024
NCL = 64
DFF = 5632
EPS = 1e-6
NCORES = 8


class V:
    __slots__ = ("ap", "tile", "lo", "hi")

    def __init__(self, ap, tile, lo, hi):
        self.ap, self.tile, self.lo, self.hi = ap, tile, lo, hi


class Tile:
    def __init__(self, mk, t, shape, name):
        self.mk, self.t, self.shape, self.name = mk, t, list(shape), name
        st = []
        acc = 1
        for s in reversed(self.shape[1:]):
            st.append(acc)
            acc *= s
        self.strides = list(reversed(st))
        self.recs_w = []
        self.recs_r = []

    def __getitem__(self, idx):
        if not isinstance(idx, tuple):
            idx = (idx,)
        ap = self.t[idx]
        lo = 0
        hi = 0
        fidx = list(idx[1:]) + [slice(None)] * (len(self.shape) - len(idx))
        for s, n, stride in zip(fidx, self.shape[1:], self.strides):
            if isinstance(s, slice):
                a = 0 if s.start is None else s.start
                b = n if s.stop is None else s.stop
                step = 1 if s.step is None else s.step
                cnt = (b - a + step - 1) // step
                last = a + (cnt - 1) * step
            else:
                a = s
                last = s
            lo += a * stride
            hi += last * stride
        return V(ap, self, lo, hi + 1)

    def all(self):
        return self[tuple([slice(None)] * len(self.shape))]


class MK:
    SEM_ROLL = 30000

    def __init__(self, nc, n_dma_sems=32):
        self.nc = nc
        self.es = ExitStack()
        self.sem_es = ExitStack()
        self.eng = {"pe": nc.tensor, "act": nc.scalar, "dve": nc.vector, "pool": nc.gpsimd, "sp": nc.sync}
        self.sem = {}
        self.cnt = {}
        self.nsem = 0
        for e in self.eng:
            self._new_sem(e)
        self.waited = {e: {} for e in self.eng}
        self.dma_sems = [self.es.enter_context(nc.semaphore(f"dq{i}")) for i in range(n_dma_sems)]
        self.dma_val = [0] * n_dma_sems
        self.dma_rr = 0
        self.ninst = 0
        self.nwait = 0
        self.rr = 0
        self.stk = []

    def _new_sem(self, e):
        self.sem[e] = self.sem_es.enter_context(self.nc.semaphore(f"s_{e}_{self.nsem}"))
        self.nsem += 1
        self.cnt[e] = 0

    def sb(self, name, shape, dt=F32):
        t = self.es.enter_context(self.nc.sbuf_tensor("sb_" + name, list(shape), dt))
        return Tile(self, t, shape, name)

    def ps(self, name, shape, dt=F32):
        t = self.es.enter_context(self.nc.psum_tensor("ps_" + name, list(shape), dt))
        return Tile(self, t, shape, name)

    def push(self):
        self.stk.append(self.es)
        self.es = ExitStack()

    def pop(self):
        self.es.close()
        self.es = self.stk.pop()

    def barrier(self):
        for e in self.eng:
            for o in self.eng:
                if o != e and self.cnt[o] > 0:
                    self._wait(e, self.sem[o], self.cnt[o])
            for i, s_ in enumerate(self.dma_sems):
                if self.dma_val[i] > 0:
                    self._wait(e, s_, self.dma_val[i])

    def _wait(self, e, sem, val):
        w = self.waited[e]
        if w.get(sem, 0) >= val:
            return
        w[sem] = val
        self.eng[e].wait_ge(sem, val)
        self.nwait += 1

    def _deps(self, e, reads, writes):
        for v in reads:
            if not isinstance(v, V):
                continue
            for (lo, hi, sem, val, de) in v.tile.recs_w:
                if lo < v.hi and v.lo < hi and not (de == "pe" and e == "pe"):
                    self._wait(e, sem, val)
        for v in writes:
            if not isinstance(v, V):
                continue
            for (lo, hi, sem, val, de) in v.tile.recs_w:
                if lo < v.hi and v.lo < hi and not (de == "pe" and e == "pe"):
                    self._wait(e, sem, val)
            for (lo, hi, sem, val, de) in v.tile.recs_r:
                if lo < v.hi and v.lo < hi and not (de == "pe" and e == "pe"):
                    self._wait(e, sem, val)

    def _record(self, reads, writes, sem, val, e):
        for v in writes:
            if not isinstance(v, V):
                continue
            t = v.tile
            t.recs_w = [r for r in t.recs_w if not (v.lo <= r[0] and r[1] <= v.hi)]
            t.recs_r = [r for r in t.recs_r if not (v.lo <= r[0] and r[1] <= v.hi)]
            t.recs_w.append((v.lo, v.hi, sem, val, e))
        for v in reads:
            if not isinstance(v, V):
                continue
            t = v.tile
            t.recs_r = [r for r in t.recs_r if not (r[2] is sem and v.lo <= r[0] and r[1] <= v.hi)]
            t.recs_r.append((v.lo, v.hi, sem, val, e))

    @staticmethod
    def _ap(v):
        return v.ap if isinstance(v, V) else v

    def op(self, e, fn, reads, writes):
        self._deps(e, reads, writes)
        if self.cnt[e] >= self.SEM_ROLL:
            self._new_sem(e)
        ins = fn()
        self.cnt[e] += 1
        ins.then_inc(self.sem[e], 1)
        self._record(reads, writes, self.sem[e], self.cnt[e], e)
        self.ninst += 1
        return ins

    def dma(self, e, out, in_, **kw):
        self._deps(e, [in_], [out])
        i = self.dma_rr
        self.dma_rr = (self.dma_rr + 1) % len(self.dma_sems)
        s = self.dma_sems[i]
        if self.dma_val[i] > 0:
            self._wait(e, s, self.dma_val[i])
        self.dma_val[i] += 16
        self.eng[e].dma_start(out=self._ap(out), in_=self._ap(in_), **kw).then_inc(s, 16)
        self._record([in_], [out], s, self.dma_val[i], "dma")
        self.ninst += 1

    def finish(self, e="sp"):
        for i, s in enumerate(self.dma_sems):
            if self.dma_val[i] > 0:
                self._wait(e, s, self.dma_val[i])
        for o in self.eng:
            if o != e and self.cnt[o] > 0:
                self._wait(e, self.sem[o], self.cnt[o])

    def close(self):
        while self.stk:
            self.pop()
        self.es.close()
        self.sem_es.close()

    def matmul(self, out, lhsT, rhs, start=True, stop=True, skip=False):
        return self.op("pe", lambda: self.nc.tensor.matmul(out.ap, lhsT.ap, rhs.ap, start=start, stop=stop,
                                                           skip_group_check=skip),
                       [lhsT, rhs], [out])

    def act(self, out, in_, func, bias=None, scale=None, accum_out=None):
        kw = {}
        reads = [in_]
        writes = [out]
        if bias is not None:
            kw["bias"] = self._ap(bias)
            reads.append(bias)
        if scale is not None:
            kw["scale"] = self._ap(scale)
            reads.append(scale)
        if accum_out is not None:
            kw["accum_out"] = accum_out.ap
            writes.append(accum_out)
        return self.op("act", lambda: self.nc.scalar.activation(out.ap, in_.ap, func, **kw), reads, writes)

    def tt(self, e, out, a, b, op):
        return self.op(e, lambda: self.eng[e].tensor_tensor(out.ap, a.ap, b.ap, op), [a, b], [out])

    def ts(self, e, out, a, s1, op0, s2=None, op1=None):
        reads = [a, s1, s2]
        if op1 is None:
            s2, op1 = 0.0, ALU.add
        return self.op(e, lambda: self.eng[e].tensor_scalar(out.ap, a.ap, self._ap(s1), self._ap(s2), op0, op1),
                       reads, [out])

    def stt(self, out, a, s, b, op0, op1):
        return self.op("dve", lambda: self.nc.vector.scalar_tensor_tensor(out.ap, a.ap, self._ap(s), b.ap, op0, op1),
                       [a, s, b], [out])

    def copy(self, e, out, in_):
        if e == "act":
            return self.op(e, lambda: self.nc.scalar.copy(out.ap, in_.ap), [in_], [out])
        return self.op(e, lambda: self.eng[e].tensor_copy(out.ap, in_.ap), [in_], [out])

    def memset(self, e, out, val):
        return self.op(e, lambda: self.eng[e].memset(out.ap, val), [], [out])

    def recip(self, out, in_):
        return self.op("dve", lambda: self.nc.vector.reciprocal(out.ap, in_.ap), [in_], [out])

    def evac(self, out, in_):
        self.rr ^= 1
        return self.copy("act" if self.rr else "dve", out, in_)


def tiles_of(n, mx=512):
    k = (n + mx - 1) // mx
    base = n // k
    rem = n % k
    out = []
    o = 0
    for i in range(k):
        s = base + (1 if i < rem else 0)
        out.append((o, s))
        o += s
    return out


class Prog:
    def __init__(self, name):
        import time as _t
        self.t_start = _t.time()
        self.name = name
        self.nc = bass.Bass("TRN2", target_bir_lowering=False)
        self.mk = MK(self.nc)
        self.in_names = []
        self.out_names = []
        mk = self.mk
        self.psb = [mk.ps(f"psb{i}", [128, 512], F32) for i in range(8)]
        self.ps_i = 0
        self.ones = mk.sb("ones_bf", [128, 128], BF16)
        mk.memset("dve", self.ones.all(), 1.0)
        self.eps_t = mk.sb("eps_t", [128, 1], F32)
        mk.memset("dve", self.eps_t.all(), EPS)
        self.wq = 0

    def inp(self, name, shape, dt=F32):
        self.in_names.append(name)
        return self.nc.dram_tensor(name, list(shape), dt, kind="ExternalInput").ap()

    def outp(self, name, shape, dt=F32):
        self.out_names.append(name)
        return self.nc.dram_tensor(name, list(shape), dt, kind="ExternalOutput").ap()

    def rstd_of(self, out, ssq, dim, eps=EPS):
        self.mk.act(out, ssq, AF.Sqrt, bias=self.eps_t[0:out.ap.shape[0], 0:1] if eps == EPS else eps, scale=1.0 / dim)
        self.mk.recip(out, out)

    def psum(self):
        p = self.psb[self.ps_i]
        self.ps_i = (self.ps_i + 1) % 8
        return p

    def run(self, in_maps):
        import time as _t
        self.mk.finish()
        t0 = _t.time()
        import os as _os
        if _os.environ.get("KTRACE"):
            res = run_bass_kernel_spmd(self.nc, in_maps, core_ids=list(range(NCORES)), trace=True)
            print(f"[{self.name}] exec_time_ns={res.exec_time_ns}", flush=True)
        else:
            res = run_bass_kernel_spmd(self.nc, in_maps, core_ids=list(range(NCORES)))
        print(f"[{self.name}] ninst={self.mk.ninst} nwait={self.mk.nwait} build={t0 - self.t_start:.1f}s "
              f"run={_t.time() - t0:.1f}s", flush=True)
        self.mk.close()
        return res.results

    def load_w(self, tile, w_ap, col0, ncols, nk):
        src = w_ap[:, col0:col0 + ncols].rearrange("(kc p) n -> p kc n", p=128)
        step = 4
        for k0 in range(0, nk, step):
            k1 = min(nk, k0 + step)
            self.mk.dma("pool", tile[:, k0:k1, 0:ncols], src[:, k0:k1, :])

    def rstd_fm(self, xT, nk, T, rstd, sqbuf, dim):
        mk = self.mk
        for (t0, tn) in tiles_of(T):
            ps = self.psum()
            for kc in range(nk):
                sq = sqbuf[kc % len(sqbuf)]
                mk.act(sq[:, 0:tn], xT[:, kc, t0:t0 + tn], AF.Square)
                mk.matmul(ps[:, 0:tn], self.ones.all(), sq[:, 0:tn], start=(kc == 0), stop=(kc == nk - 1))
            self.rstd_of(rstd[:, t0:t0 + tn], ps[:, 0:tn], dim)

    def modulate_fm(self, hT, xT, rstd, segs, tmp):
        mk = self.mk
        for kc in range(KC):
            for (t0, tn, a, sh) in segs:
                t = tmp[kc % len(tmp)]
                mk.tt("dve", t[:, 0:tn], xT[:, kc, t0:t0 + tn], rstd[:, t0:t0 + tn], ALU.mult)
                mk.act(hT[:, kc, t0:t0 + tn], t[:, 0:tn], AF.Identity, bias=sh[:, kc:kc + 1], scale=a[:, kc:kc + 1])

    def load_mod(self, modT, nseg):
        t = self.mk.sb("modsb", [128, nseg, 6, KC], F32)
        self.mk.dma("sp", t.all(), modT)
        return t

    def load_vec(self, name, ap_kc):
        t = self.mk.sb(name, [128, KC], F32)
        self.mk.dma("sp", t.all(), ap_kc)
        return t


def vec_pk(v):
    return np.ascontiguousarray(v.reshape(-1, 128).T)


def run_mod(c, c_ctx, mod_w, mod_b):
    P = Prog("mod")
    mk = P.mk
    NCOL = 12288 // NCORES
    cT = P.inp("cT", [128, KC, 3])
    w = P.inp("w", [4, D, NCOL])
    b = P.inp("b", [3, 4, NCOL])
    out = P.outp("out", [3, 4, NCOL])
    cs = mk.sb("cs", [128, KC, 3], F32)
    sT = mk.sb("sT", [128, KC, 3], F32)
    mk.dma("sp", cs.all(), cT)
    mk.act(sT.all(), cs.all(), AF.Silu)
    bs = mk.sb("bs", [3, 4, NCOL], F32)
    mk.dma("sp", bs.all(), b)
    os_ = mk.sb("os", [3, 4, NCOL], F32)
    wb = [mk.sb(f"wb{i}", [128, KC, 512], F32) for i in range(2)]
    it = 0
    for l in range(4):
        for n0 in range(0, NCOL, 512):
            wt = wb[it % 2]
            it += 1
            src = w[l, :, n0:n0 + 512].rearrange("(kc p) n -> p kc n", p=128)
            for k0 in range(0, KC, 4):
                mk.dma("sp" if (k0 // 4) % 2 == 0 else "act", wt[:, k0:k0 + 4, :], src[:, k0:k0 + 4, :])
            ps = P.psum()
            for kc in range(KC):
                mk.matmul(ps[0:3, :], sT[:, kc, :], wt[:, kc, :], start=(kc == 0), stop=(kc == KC - 1))
            mk.tt("dve", os_[:, l, n0:n0 + 512], ps[0:3, :], bs[:, l, n0:n0 + 512], ALU.add)
    mk.dma("sp", out, os_.all())
    cstack = np.stack([c[0], c[1], c_ctx], axis=1)
    cT_np = np.ascontiguousarray(cstack.reshape(KC, 128, 3).transpose(1, 0, 2))
    in_maps = []
    for core in range(NCORES):
        sl = slice(core * NCOL, (core + 1) * NCOL)
        in_maps.append({"cT": cT_np, "w": np.ascontiguousarray(mod_w[:, :, sl]),
                        "b": np.ascontiguousarray(np.broadcast_to(mod_b[None, :, sl], (3, 4, NCOL)))})
    res = P.run(in_maps)
    mod = np.concatenate([r["out"] for r in res], axis=2)
    return mod


def mod_layout(mod, layer, b, with_ctx=True):
    rows = [b, 2] if with_ctx else [b]
    m = mod[rows, layer]
    m = m.reshape(len(rows), 6, KC, 128).transpose(3, 0, 1, 2)
    return np.ascontiguousarray(m)


def norm_mod_stream(P, xT_d, T, segs, normw_t, mod_t, ish, isc, hT):
    mk = P.mk
    nseg = mod_t.shape[1]
    a_t = mk.sb("nm_a", [128, nseg, KC], F32)
    for s in range(nseg):
        mk.stt(a_t[:, s, :], mod_t[:, s, isc, :], 1.0, normw_t.all(), ALU.add, ALU.mult)
    NXB = 4
    xb = [mk.sb(f"nm_xb{i}", [128, T], F32) for i in range(NXB)]
    sq = [mk.sb(f"nm_sq{i}", [128, 512], BF16) for i in range(6)]
    rstd = mk.sb("nm_rstd", [128, T], F32)
    tls = tiles_of(T)
    pss = [P.psum() for _ in tls]
    xsrc = xT_d.rearrange("(kc p) t -> p kc t", p=128)
    for kc in range(KC):
        x = xb[kc % NXB]
        mk.dma("sp", x.all(), xsrc[:, kc, :])
        for ti, (t0, tn) in enumerate(tls):
            s_ = sq[(kc * len(tls) + ti) % 6]
            if ti % 3 == 0:
                mk.act(s_[:, 0:tn], x[:, t0:t0 + tn], AF.Square)
            else:
                mk.tt("pool" if ti % 3 == 1 else "dve", s_[:, 0:tn], x[:, t0:t0 + tn], x[:, t0:t0 + tn], ALU.mult)
            mk.matmul(pss[ti][:, 0:tn], P.ones.all(), s_[:, 0:tn], start=(kc == 0), stop=(kc == KC - 1))
    for ti, (t0, tn) in enumerate(tls):
        P.rstd_of(rstd[:, t0:t0 + tn], pss[ti][:, 0:tn], D)
    for kc in range(KC):
        x = xb[kc % NXB]
        mk.dma("sp", x.all(), xsrc[:, kc, :])
        mk.tt("dve", x.all(), x.all(), rstd.all(), ALU.mult)
        for (t0, tn, s) in segs:
            mk.act(hT[:, kc, t0:t0 + tn], x[:, t0:t0 + tn], AF.Identity,
                   bias=mod_t[:, s, ish, kc:kc + 1], scale=a_t[:, s, kc:kc + 1])


def proj_fm(P, hT, T, w_ap, col0, ncols, out_ap, out_dt, wbufs, stg, nk=KC, post=None):
    mk = P.mk
    tls = tiles_of(T)
    bi = 0
    for b0 in range(0, ncols, 512):
        bn = min(512, ncols - b0)
        wt = wbufs[P.wq % len(wbufs)]
        P.wq += 1
        P.load_w(wt, w_ap, col0 + b0, bn, nk)
        for n0 in range(0, bn, 128):
            st = stg[bi % len(stg)]
            bi += 1
            for (t0, tn) in tls:
                ps = P.psum()
                for kc in range(nk):
                    mk.matmul(ps[:, 0:tn], wt[:, kc, n0:n0 + 128], hT[:, kc, t0:t0 + tn],
                              start=(kc == 0), stop=(kc == nk - 1))
                if post is None:
                    mk.evac(st[:, t0:t0 + tn], ps[:, 0:tn])
                else:
                    post(st, ps, b0 + n0, t0, tn)
            mk.dma("sp", out_ap[b0 + n0:b0 + n0 + 128, :], st[:, 0:T])


def proj_tm(P, hT, T, w_ap, col0, ncols, out_ap, wbufs, stg, nk=KC):
    mk = P.mk
    bi = 0
    for b0 in range(0, ncols, 512):
        bn = min(512, ncols - b0)
        wt = wbufs[P.wq % len(wbufs)]
        P.wq += 1
        P.load_w(wt, w_ap, col0 + b0, bn, nk)
        for t0 in range(0, T, 128):
            tn = min(128, T - t0)
            st = stg[bi % len(stg)]
            bi += 1
            ps = P.psum()
            for kc in range(nk):
                mk.matmul(ps[0:tn, 0:bn], hT[:, kc, t0:t0 + tn], wt[:, kc, 0:bn], start=(kc == 0), stop=(kc == nk - 1))
            mk.evac(st[0:tn, 0:bn], ps[0:tn, 0:bn])
            mk.dma("sp", out_ap[t0:t0 + tn, b0:b0 + bn], st[0:tn, 0:bn])


def run_A(xT_list, modT_list, normw, w_in, specs, nctx):
    T = NLAT + nctx
    N = w_in.shape[1]
    P = Prog("A")
    mk = P.mk
    xT_d = P.inp("xT", [D, T])
    modT_d = P.inp("modT", [128, 2 if nctx else 1, 6, KC])
    nw_d = P.inp("nw", [128, KC])
    w_d = P.inp("w", [D, N])
    mod_t = P.load_mod(modT_d, 2 if nctx else 1)
    nw_t = P.load_vec("nw_t", nw_d)
    hT = mk.sb("hT", [128, KC, T], BF16)
    segs = [(0, NLAT, 0)] + ([(NLAT, nctx, 1)] if nctx else [])
    norm_mod_stream(P, xT_d, T, segs, nw_t, mod_t, 0, 1, hT)
    wbufs = [mk.sb(f"wbuf{i}", [128, KC, 512], BF16) for i in range(2)]
    stg_fm_b = [mk.sb(f"sfb{i}", [128, T], BF16) for i in range(3)]
    stg_fm_f = [mk.sb(f"sff{i}", [128, T], F32) for i in range(3)]
    stg_tm_b = [mk.sb(f"stb{i}", [128, 512], BF16) for i in range(3)]
    stg_tm_f = [mk.sb(f"stf{i}", [128, 512], F32) for i in range(3)]
    for (name, col0, ncols, lay, dt) in specs:
        bdt = BF16 if dt == "bf16" else F32
        if lay == "fm":
            o = P.outp(name, [ncols, T], bdt)
            proj_fm(P, hT, T, w_d, col0, ncols, o, bdt, wbufs, stg_fm_b if dt == "bf16" else stg_fm_f)
        else:
            o = P.outp(name, [T, ncols], bdt)
            proj_tm(P, hT, T, w_d, col0, ncols, o, wbufs, stg_tm_b if dt == "bf16" else stg_tm_f)
    nwk = vec_pk(normw)
    in_maps = [{"xT": xT_list[c], "modT": modT_list[c], "nw": nwk, "w": w_in} for c in range(NCORES)]
    return P.run(in_maps)


def run_C1(oT_list, xT_list, modT_list, normw, w_out, bias, nctx):
    T = NLAT + nctx
    nseg = 2 if nctx else 1
    P = Prog("C1")
    mk = P.mk
    oT_d = P.inp("oT", [D, T], BF16)
    xT_d = P.inp("xT", [D, T])
    modT_d = P.inp("modT", [128, nseg, 6, KC])
    nw_d = P.inp("nw", [128, KC])
    b_d = P.inp("bias", [128, KC])
    w_d = P.inp("w", [D, D])
    out_d = P.outp("xmid", [D, T])
    mod_t = P.load_mod(modT_d, nseg)
    nw_t = P.load_vec("nw_t", nw_d)
    b_t = P.load_vec("b_t", b_d)
    gw = mk.sb("gw", [128, nseg, KC], F32)
    for s in range(nseg):
        mk.tt("dve", gw[:, s, :], mod_t[:, s, 2, :], nw_t.all(), ALU.mult)
    wres = mk.sb("wres", [128, KC, D], BF16)
    for b0 in range(0, D, 512):
        src = w_d[:, b0:b0 + 512].rearrange("(kc p) n -> p kc n", p=128)
        for k0 in range(0, KC, 4):
            mk.dma("pool", wres[:, k0:k0 + 4, b0:b0 + 512], src[:, k0:k0 + 4, :])
    ob = [mk.sb(f"ob{i}", [128, KC, 512], BF16) for i in range(2)]
    yTb = [mk.sb(f"yT{i}", [128, KC, 512], F32) for i in range(2)]
    sq = [mk.sb(f"sq{i}", [128, 512], BF16) for i in range(4)]
    rstd = mk.sb("rstd", [128, 512], F32)
    xb = [mk.sb(f"xb{i}", [128, 512], F32) for i in range(4)]
    osrc = oT_d.rearrange("(kc p) t -> p kc t", p=128)
    xsrc = xT_d.rearrange("(kc p) t -> p kc t", p=128)
    odst = out_d.rearrange("(kc p) t -> p kc t", p=128)
    tls = [(0, 512, 0), (512, 512, 0)] + ([(NLAT, nctx, 1)] if nctx else [])
    def load_o(ti):
        t0_, tn_, _ = tls[ti]
        for k0 in range(0, KC, 4):
            mk.dma("sp", ob[ti % 2][:, k0:k0 + 4, 0:tn_], osrc[:, k0:k0 + 4, t0_:t0_ + tn_])

    load_o(0)
    for ti, (t0, tn, seg) in enumerate(tls):
        o = ob[ti % 2]
        yT = yTb[ti % 2]
        if ti + 1 < len(tls):
            load_o(ti + 1)
        pss = P.psb[7]
        pend = []
        for n in range(KC + 2):
            if n < KC:
                ps = P.psb[(ti * KC + n) % 6]
                for kc in range(KC):
                    mk.matmul(ps[:, 0:tn], wres[:, kc, n * 128:(n + 1) * 128], o[:, kc, 0:tn],
                              start=(kc == 0), stop=(kc == KC - 1))
                mk.act(yT[:, n, 0:tn], ps[:, 0:tn], AF.Identity, bias=b_t[:, n:n + 1])
                s_ = sq[n % 4]
                mk.act(s_[:, 0:tn], yT[:, n, 0:tn], AF.Square)
                pend.append((n, s_))
            if n >= 2:
                m, s_ = pend.pop(0)
                mk.matmul(pss[:, 0:tn], P.ones.all(), s_[:, 0:tn], start=(m == 0), stop=(m == KC - 1))
        P.rstd_of(rstd[:, 0:tn], pss[:, 0:tn], D)
        for n in range(KC):
            x = xb[n % 4]
            mk.dma("sp", x[:, 0:tn], xsrc[:, n, t0:t0 + tn])
            mk.tt("dve", yT[:, n, 0:tn], yT[:, n, 0:tn], rstd[:, 0:tn], ALU.mult)
            mk.stt(x[:, 0:tn], yT[:, n, 0:tn], gw[:, seg, n:n + 1], x[:, 0:tn], ALU.mult, ALU.add)
            mk.dma("pool", odst[:, n, t0:t0 + tn], x[:, 0:tn])
    nwk = vec_pk(normw)
    bk = vec_pk(bias) if bias is not None else np.zeros((128, KC), np.float32)
    in_maps = [{"oT": oT_list[c], "xT": xT_list[c], "modT": modT_list[c], "nw": nwk, "bias": bk, "w": w_out}
               for c in range(NCORES)]
    return [r["xmid"] for r in P.run(in_maps)]


def run_C2_old(xe_list, mask_list, modT_list, nw_pre, nw_post, w_up, conv_w, conv_b, w_down, groups):
    ng = len(groups)
    nseg = 1 + max(s for _, s in groups)
    Te = sum(g + 2 for g, _ in groups)
    To = sum(g for g, _ in groups)
    NJ = DFF // 128
    P = Prog("C2")
    mk = P.mk
    xe_d = P.inp("xe", [D, Te])
    mask_d = P.inp("mask", [128, KC, 2 * ng])
    modT_d = P.inp("modT", [128, nseg, 6, KC])
    nw1_d = P.inp("nw1", [128, KC])
    nw2_d = P.inp("nw2", [128, KC])
    wu_d = P.inp("wu", [D, 2 * DFF])
    cw_d = P.inp("cw", [128, 2 * NJ, 3])
    cb_d = P.inp("cb", [128, 2 * NJ])
    wd_d = P.inp("wd", [DFF, D])
    out_d = P.outp("xo", [D, To])
    mod_t = P.load_mod(modT_d, nseg)
    nw1 = P.load_vec("nw1_t", nw1_d)
    nw2 = P.load_vec("nw2_t", nw2_d)
    mask_t = mk.sb("mask_t", [128, KC, 2 * ng], F32)
    mk.dma("sp", mask_t.all(), mask_d)
    cw = mk.sb("cw_t", [128, 2 * NJ, 3], F32)
    cb = mk.sb("cb_t", [128, 2 * NJ], F32)
    mk.dma("sp", cw.all(), cw_d)
    mk.dma("sp", cb.all(), cb_d)
    gw = mk.sb("gw", [128, nseg, KC], F32)
    for s in range(nseg):
        mk.tt("dve", gw[:, s, :], mod_t[:, s, 5, :], nw2.all(), ALU.mult)
    hT = mk.sb("hT", [128, KC, Te], BF16)
    segs = []
    o = 0
    for (G, s) in groups:
        segs.append((o, G + 2, s))
        o += G + 2
    norm_mod_stream(P, xe_d, Te, segs, nw1, mod_t, 3, 4, hT)
    o = 0
    for gi, (G, s) in enumerate(groups):
        mk.tt("dve", hT[:, :, o:o + 1], hT[:, :, o:o + 1], mask_t[:, :, 2 * gi:2 * gi + 1], ALU.mult)
        mk.tt("dve", hT[:, :, o + G + 1:o + G + 2], hT[:, :, o + G + 1:o + G + 2],
              mask_t[:, :, 2 * gi + 1:2 * gi + 2], ALU.mult)
        o += G + 2
    GM = max(g for g, _ in groups)
    actT = mk.sb("actT", [128, NJ, GM], BF16)
    wub = [mk.sb(f"wub{i}", [128, KC, 2, 128], BF16) for i in range(2)]
    ub = [mk.sb(f"ub{i}", [128, GM + 2], F32) for i in range(4)]
    cvb = [mk.sb(f"cvb{i}", [128, GM], F32) for i in range(4)]
    wdb = [mk.sb(f"wdb{i}", [128, NJ, 128], BF16) for i in range(2)]
    yT = mk.sb("yT", [128, KC, GM], F32)
    sq = [mk.sb(f"sq{i}", [128, 512], BF16) for i in range(2)]
    rstd = mk.sb("rstd", [128, 512], F32)
    xb = [mk.sb(f"xb{i}", [128, GM], F32) for i in range(3)]
    xsrc = xe_d.rearrange("(kc p) t -> p kc t", p=128)
    odst = out_d.rearrange("(kc p) t -> p kc t", p=128)
    wusrc = wu_d.rearrange("(kc p) n -> p kc n", p=128)
    wdsrc = wd_d.rearrange("(j p) n -> p j n", p=128)
    eo = 0
    oo = 0
    it = 0
    ui = 0
    for gi, (G, s) in enumerate(groups):
        Gx = G + 2
        tls = tiles_of(Gx)
        for j0 in range(0, NJ, 1):
            wt = wub[it % 2]
            it += 1
            for half, cbase in ((0, j0 * 128), (1, DFF + j0 * 128)):
                mk.dma("pool", wt[:, :, half, :], wusrc[:, :, cbase:cbase + 128])
            for jj in range(1):
                j = j0 + jj
                us = []
                for half in range(2):
                    u = ub[ui % 4]
                    ui += 1
                    for (t0, tn) in tls:
                        ps = P.psum()
                        for kc in range(KC):
                            mk.matmul(ps[:, 0:tn], wt[:, kc, half, jj * 128:(jj + 1) * 128],
                                      hT[:, kc, eo + t0:eo + t0 + tn], start=(kc == 0), stop=(kc == KC - 1))
                        mk.copy("act" if half == 0 else "dve", u[:, t0:t0 + tn], ps[:, 0:tn])
                    us.append(u)
                cs = []
                for half in range(2):
                    ch = half * NJ + j
                    u = us[half]
                    cv = cvb[(2 * j + half) % 4]
                    mk.act(cv[:, 0:G], u[:, 1:G + 1], AF.Identity, bias=cb[:, ch:ch + 1], scale=cw[:, ch, 1:2])
                    mk.stt(cv[:, 0:G], u[:, 0:G], cw[:, ch, 0:1], cv[:, 0:G], ALU.mult, ALU.add)
                    mk.stt(cv[:, 0:G], u[:, 2:G + 2], cw[:, ch, 2:3], cv[:, 0:G], ALU.mult, ALU.add)
                    cs.append(cv)
                mk.act(cs[1][:, 0:G], cs[1][:, 0:G], AF.Silu)
                mk.tt("dve", actT[:, j, 0:G], cs[0][:, 0:G], cs[1][:, 0:G], ALU.mult)
        pss = P.psum()
        for n0 in range(0, KC, 1):
            wd = wdb[n0 % 2]
            for j0 in range(0, NJ, 11):
                mk.dma("pool", wd[:, j0:j0 + 11, :], wdsrc[:, j0:j0 + 11, n0 * 128:n0 * 128 + 128])
            for nn in range(1):
                n = n0 + nn
                ps = P.psum()
                if ps is pss:
                    ps = P.psum()
                for j in range(NJ):
                    mk.matmul(ps[:, 0:G], wd[:, j, nn * 128:(nn + 1) * 128], actT[:, j, 0:G],
                              start=(j == 0), stop=(j == NJ - 1))
                mk.copy("act", yT[:, n, 0:G], ps[:, 0:G])
                s_ = sq[n % 2]
                mk.act(s_[:, 0:G], yT[:, n, 0:G], AF.Square)
                mk.matmul(pss[:, 0:G], P.ones.all(), s_[:, 0:G], start=(n == 0), stop=(n == KC - 1))
        P.rstd_of(rstd[:, 0:G], pss[:, 0:G], D)
        for n in range(KC):
            x = xb[n % 3]
            mk.dma("act", x[:, 0:G], xsrc[:, n, eo + 1:eo + 1 + G])
            mk.tt("dve", yT[:, n, 0:G], yT[:, n, 0:G], rstd[:, 0:G], ALU.mult)
            mk.stt(x[:, 0:G], yT[:, n, 0:G], gw[:, s, n:n + 1], x[:, 0:G], ALU.mult, ALU.add)
            mk.dma("sp", odst[:, n, oo:oo + G], x[:, 0:G])
        eo += Gx
        oo += G
    cwk = np.ascontiguousarray(conv_w.T.reshape(2 * NJ, 128, 3).transpose(1, 0, 2))
    cbk = np.ascontiguousarray(conv_b.reshape(2 * NJ, 128).T)
    in_maps = [{"xe": xe_list[c], "mask": mask_list[c], "modT": modT_list[c], "nw1": vec_pk(nw_pre),
                "nw2": vec_pk(nw_post), "wu": w_up, "cw": cwk, "cb": cbk, "wd": w_down} for c in range(NCORES)]
    return [r["xo"] for r in P.run(in_maps)]


def run_C2(xe_list, mask_list, modT_list, nw_pre, nw_post, w_up, conv_w, conv_b, w_down, groups):
    ng = len(groups)
    nseg = 1 + max(s for _, s in groups)
    Te = sum(g + 2 for g, _ in groups)
    To = sum(g for g, _ in groups)
    NJ = DFF // 128
    P = Prog("C2")
    mk = P.mk
    xe_d = P.inp("xe", [D, Te])
    mask_d = P.inp("mask", [128, KC, 2 * ng])
    modT_d = P.inp("modT", [128, nseg, 6, KC])
    nw1_d = P.inp("nw1", [128, KC])
    nw2_d = P.inp("nw2", [128, KC])
    wu_d = P.inp("wu", [D, 2 * DFF])
    cw_d = P.inp("cw", [128, 2 * NJ, 3])
    cb_d = P.inp("cb", [128, 2 * NJ])
    wd_d = P.inp("wd", [DFF, D])
    out_d = P.outp("xo", [D, To])
    ysc_d = P.nc.dram_tensor("ysc", [D, To], F32, kind="Internal").ap()
    mod_t = P.load_mod(modT_d, nseg)
    nw1 = P.load_vec("nw1_t", nw1_d)
    nw2 = P.load_vec("nw2_t", nw2_d)
    mask_t = mk.sb("mask_t", [128, KC, 2 * ng], F32)
    mk.dma("sp", mask_t.all(), mask_d)
    cw = mk.sb("cw_t", [128, 2 * NJ, 3], F32)
    cb = mk.sb("cb_t", [128, 2 * NJ], F32)
    mk.dma("sp", cw.all(), cw_d)
    mk.dma("sp", cb.all(), cb_d)
    gw = mk.sb("gw", [128, nseg, KC], F32)
    for s in range(nseg):
        mk.tt("dve", gw[:, s, :], mod_t[:, s, 5, :], nw2.all(), ALU.mult)
    actT = mk.sb("actT", [128, NJ, To], BF16)
    xsrc = xe_d.rearrange("(kc p) t -> p kc t", p=128)
    odst = out_d.rearrange("(kc p) t -> p kc t", p=128)
    ysc = ysc_d.rearrange("(kc p) t -> p kc t", p=128)
    wusrc = wu_d.rearrange("(kc p) n -> p kc n", p=128)
    wdsrc = wd_d.rearrange("(j p) n -> p j n", p=128)
    geo = []
    eo = oo = 0
    for (G, s) in groups:
        geo.append((eo, oo, G, s))
        eo += G + 2
        oo += G
    mk.push()
    hT = mk.sb("hT", [128, KC, Te], BF16)
    mk.push()
    segs = [(e0, G + 2, s) for (e0, o0, G, s) in geo]
    norm_mod_stream(P, xe_d, Te, segs, nw1, mod_t, 3, 4, hT)
    for gi, (e0, o0, G, s) in enumerate(geo):
        mk.tt("dve", hT[:, :, e0:e0 + 1], hT[:, :, e0:e0 + 1], mask_t[:, :, 2 * gi:2 * gi + 1], ALU.mult)
        mk.tt("dve", hT[:, :, e0 + G + 1:e0 + G + 2], hT[:, :, e0 + G + 1:e0 + G + 2],
              mask_t[:, :, 2 * gi + 1:2 * gi + 2], ALU.mult)
    mk.barrier()
    mk.pop()
    mk.push()
    wub = [mk.sb(f"wub{i}", [128, KC, 2, 128], BF16) for i in range(3)]
    ub = [mk.sb(f"ub{i}", [128, Te], F32) for i in range(4)]
    cvb = [mk.sb(f"cvb{i}", [128, To], F32) for i in range(2)]
    tle = tiles_of(Te)
    for j in range(NJ):
        wt = wub[j % 3]
        for half, cbase in ((0, j * 128), (1, DFF + j * 128)):
            mk.dma("pool", wt[:, :, half, :], wusrc[:, :, cbase:cbase + 128])
        us = []
        for half in range(2):
            u = ub[(2 * j + half) % 4]
            for (t0, tn) in tle:
                ps = P.psum()
                for kc in range(KC):
                    mk.matmul(ps[:, 0:tn], wt[:, kc, half, :], hT[:, kc, t0:t0 + tn],
                              start=(kc == 0), stop=(kc == KC - 1))
                mk.copy("act" if half == 0 else "dve", u[:, t0:t0 + tn], ps[:, 0:tn])
            us.append(u)
        for half in range(2):
            ch = half * NJ + j
            u = us[half]
            cv = cvb[half]
            for (e0, o0, G, s) in geo:
                mk.act(cv[:, o0:o0 + G], u[:, e0 + 1:e0 + 1 + G], AF.Identity, bias=cb[:, ch:ch + 1],
                       scale=cw[:, ch, 1:2])
                mk.stt(cv[:, o0:o0 + G], u[:, e0:e0 + G], cw[:, ch, 0:1], cv[:, o0:o0 + G], ALU.mult, ALU.add)
                mk.stt(cv[:, o0:o0 + G], u[:, e0 + 2:e0 + 2 + G], cw[:, ch, 2:3], cv[:, o0:o0 + G], ALU.mult, ALU.add)
        mk.act(cvb[1].all(), cvb[1].all(), AF.Silu)
        mk.tt("dve", actT[:, j, :], cvb[0].all(), cvb[1].all(), ALU.mult)
    mk.barrier()
    mk.pop()
    mk.pop()
    wdb = [mk.sb(f"wdb{i}", [128, NJ, 128], BF16) for i in range(2)]
    yst = [mk.sb(f"yst{i}", [128, To], F32) for i in range(2)]
    sq = [mk.sb(f"sq{i}", [128, 512], BF16) for i in range(4)]
    rstd = mk.sb("rstd", [128, To], F32)
    xb = [mk.sb(f"xb{i}", [128, Te], F32) for i in range(2)]
    tlo = tiles_of(To)
    pss = [P.psb[5 + i] for i in range(len(tlo))]
    pk = 0
    pend2 = []
    for n in range(KC):
        wd = wdb[n % 2]
        for j0 in range(0, NJ, 11):
            mk.dma("pool", wd[:, j0:j0 + 11, :], wdsrc[:, j0:j0 + 11, n * 128:(n + 1) * 128])
        y = yst[n % 2]
        for ti, (t0, tn) in enumerate(tlo):
            ps = P.psb[pk % 5]
            pk += 1
            for j in range(NJ):
                mk.matmul(ps[:, 0:tn], wd[:, j, :], actT[:, j, t0:t0 + tn], start=(j == 0), stop=(j == NJ - 1))
            mk.copy("act", y[:, t0:t0 + tn], ps[:, 0:tn])
            s_ = sq[(n * len(tlo) + ti) % 4]
            mk.act(s_[:, 0:tn], y[:, t0:t0 + tn], AF.Square)
            pend2.append((n, ti, tn, s_))
            if len(pend2) > 2:
                m_, ti_, tn_, sq_ = pend2.pop(0)
                mk.matmul(pss[ti_][:, 0:tn_], P.ones.all(), sq_[:, 0:tn_], start=(m_ == 0), stop=(m_ == KC - 1))
        mk.dma("sp", ysc[:, n, :], y.all())
    for (m_, ti_, tn_, sq_) in pend2:
        mk.matmul(pss[ti_][:, 0:tn_], P.ones.all(), sq_[:, 0:tn_], start=(m_ == 0), stop=(m_ == KC - 1))
    for ti, (t0, tn) in enumerate(tlo):
        P.rstd_of(rstd[:, t0:t0 + tn], pss[ti][:, 0:tn], D)
    mk.barrier()
    y3 = yst + [mk.sb(f"yst3{i}", [128, To], F32) for i in range(2)]
    x3 = xb + [mk.sb(f"xb3{i}", [128, Te], F32) for i in range(2)]
    for n in range(KC):
        y = y3[n % 4]
        x = x3[n % 4]
        mk.dma("sp", y.all(), ysc[:, n, :])
        mk.dma("sp", x.all(), xsrc[:, n, :])
        mk.tt("dve", y.all(), y.all(), rstd.all(), ALU.mult)
        for (e0, o0, G, s) in geo:
            mk.stt(y[:, o0:o0 + G], y[:, o0:o0 + G], gw[:, s, n:n + 1], x[:, e0 + 1:e0 + 1 + G], ALU.mult, ALU.add)
        mk.dma("pool", odst[:, n, :], y.all())
    P.ps_i = 0
    cwk = np.ascontiguousarray(conv_w.T.reshape(2 * NJ, 128, 3).transpose(1, 0, 2))
    cbk = np.ascontiguousarray(conv_b.reshape(2 * NJ, 128).T)
    in_maps = [{"xe": xe_list[c], "mask": mask_list[c], "modT": modT_list[c], "nw1": vec_pk(nw_pre),
                "nw2": vec_pk(nw_post), "wu": w_up, "cw": cwk, "cb": cbk, "wd": w_down} for c in range(NCORES)]
    return [r["xo"] for r in P.run(in_maps)]


def ffn_host_io(xmid_lat, xmid_ctx, nctx):
    xe_list, mask_list = [], []
    groups = [(512, 0), (512, 0)] + ([(nctx, 1)] if nctx else [])
    for c in range(NCORES):
        b, q = c // 4, c % 4
        cols = []
        m = []
        for g in range(2):
            p0 = q * NLAT + g * 512
            blk = np.zeros((514, D), np.float32)
            lo, hi = p0 - 1, p0 + 513
            slo, shi = max(lo, 0), min(hi, SEQ)
            blk[slo - lo:shi - lo] = xmid_lat[b, slo:shi]
            cols.append(blk)
            m += [1.0 if lo >= 0 else 0.0, 1.0 if hi <= SEQ else 0.0]
        if nctx:
            blk = np.zeros((nctx + 2, D), np.float32)
            lo, hi = q * nctx - 1, q * nctx + nctx + 1
            slo, shi = max(lo, 0), min(hi, NCTX)
            blk[slo - lo:shi - lo] = xmid_ctx[b, slo:shi]
            cols.append(blk)
            m += [1.0 if lo >= 0 else 0.0, 1.0 if hi <= NCTX else 0.0]
        xe = np.ascontiguousarray(np.concatenate(cols, axis=0).T)
        xe_list.append(xe)
        mask_list.append(np.ascontiguousarray(np.broadcast_to(np.array(m, np.float32)[None, None, :], (128, KC, len(m)))))
    return xe_list, mask_list, groups


def rope_tables():
    half = 32
    freqs = 10000.0 ** (-np.arange(half, dtype=np.float32) / half)
    pos = np.arange(SEQ)
    row, col = pos // 64, pos % 64
    ang = np.zeros((128, SEQ), np.float32)
    for d in range(128):
        p = row if d < 64 else col
        ang[d] = p.astype(np.float32) * freqs[(d % 64) % 32]
    R = np.zeros((128, 128), np.float32)
    for d in range(128):
        if (d % 64) < 32:
            R[d + 32, d] = -1.0
        else:
            R[d - 32, d] = 1.0
    return np.cos(ang).astype(np.float32), np.sin(ang).astype(np.float32), R.astype(NPBF)


def run_B_gqa(qT_list, kT_list, v_list, q_norm, k_norm):
    T = NLAT + NCL
    NK = SEQ + NCTX
    NCH = NK // 128
    P = Prog("Bgqa")
    mk = P.mk
    qT_d = P.inp("qT", [D, T], BF16)
    kT_d = P.inp("kT", [512, NK], BF16)
    v_d = P.inp("v", [NK, 512], BF16)
    gn_d = P.inp("gn", [128, 2])
    cos_d = P.inp("cos", [128, SEQ])
    sin_d = P.inp("sin", [128, SEQ])
    cosq_d = P.inp("cosq", [128, NLAT])
    sinq_d = P.inp("sinq", [128, NLAT])
    R_d = P.inp("R", [128, 128], BF16)
    oT_d = P.outp("oT", [D, T], BF16)
    gn = mk.sb("gn", [128, 2], F32)
    mk.dma("sp", gn.all(), gn_d)
    mk.ts("dve", gn[:, 0:1], gn[:, 0:1], 128.0 ** -0.5, ALU.mult)
    Rm = mk.sb("Rm", [128, 128], BF16)
    mk.dma("sp", Rm.all(), R_d)
    cosk = mk.sb("cosk", [128, SEQ], F32)
    sink = mk.sb("sink", [128, SEQ], F32)
    cosq = mk.sb("cosq", [128, NLAT], F32)
    sinq = mk.sb("sinq", [128, NLAT], F32)
    mk.dma("sp", cosk.all(), cos_d)
    mk.dma("act", sink.all(), sin_d)
    mk.dma("sp", cosq.all(), cosq_d)
    mk.dma("act", sinq.all(), sinq_d)
    kT = mk.sb("kT", [128, 4, NK], BF16)
    mk.dma("sp", kT.all(), kT_d.rearrange("(h p) t -> p h t", p=128))
    qT = mk.sb("qT", [128, 16, T], BF16)
    for h0 in range(0, 16, 4):
        mk.dma("act", qT[:, h0:h0 + 4, :], qT_d.rearrange("(h p) t -> p h t", p=128)[:, h0:h0 + 4, :])
    vt = mk.sb("vt", [128, NCH, 512], BF16)
    vsrc = v_d.rearrange("(c p) n -> p c n", p=128)
    for c0 in range(0, NCH, 17):
        mk.dma("sp", vt[:, c0:c0 + 17, :], vsrc[:, c0:c0 + 17, :])
    sqb = [mk.sb(f"sqb{i}", [128, 512], BF16) for i in range(2)]
    rsb = [mk.sb(f"rsb{i}", [128, 512], F32) for i in range(2)]
    knb = [mk.sb(f"knb{i}", [128, 512], BF16) for i in range(2)]
    t1b = [mk.sb(f"t1b{i}", [128, 512], F32) for i in range(2)]
    t2b = [mk.sb(f"t2b{i}", [128, 512], F32) for i in range(2)]
    cnt = [0]

    def normrope(buf, h, t0, tn, gcol, cs, sn, c0):
        i = cnt[0] % 2
        cnt[0] += 1
        x = buf[:, h, t0:t0 + tn]
        mk.act(sqb[i][:, 0:tn], x, AF.Square)
        ps = P.psum()
        mk.matmul(ps[:, 0:tn], P.ones.all(), sqb[i][:, 0:tn])
        mk.act(rsb[i][:, 0:tn], ps[:, 0:tn], AF.Ln, bias=P.eps_t[:, 0:1], scale=1.0 / 128)
        mk.act(rsb[i][:, 0:tn], rsb[i][:, 0:tn], AF.Exp, scale=-0.5)
        if cs is None:
            mk.stt(x, x, gcol, rsb[i][:, 0:tn], ALU.mult, ALU.mult)
            return
        mk.stt(knb[i][:, 0:tn], x, gcol, rsb[i][:, 0:tn], ALU.mult, ALU.mult)
        ps2 = P.psum()
        mk.matmul(ps2[:, 0:tn], Rm.all(), knb[i][:, 0:tn])
        mk.tt("pool", t1b[i][:, 0:tn], knb[i][:, 0:tn], cs[:, c0:c0 + tn], ALU.mult)
        mk.tt("dve", t2b[i][:, 0:tn], ps2[:, 0:tn], sn[:, c0:c0 + tn], ALU.mult)
        mk.tt("pool", x, t1b[i][:, 0:tn], t2b[i][:, 0:tn], ALU.add)

    for kv in range(4):
        for t0 in range(0, SEQ, 512):
            normrope(kT, kv, t0, 512, gn[:, 1:2], cosk, sink, t0)
        normrope(kT, kv, SEQ, NCTX, gn[:, 1:2], None, None, 0)
    pT = [mk.sb(f"pT{i}", [128, 512], BF16) for i in range(4)]
    rcp = [mk.sb(f"rcp{i}", [128, 512], F32) for i in range(2)]
    ost = [mk.sb(f"ost{i}", [128, T], BF16) for i in range(2)]
    pi = 0
    acc_i = 0
    for h in range(16):
        for t0 in (0, 512):
            normrope(qT, h, t0, 512, gn[:, 0:1], cosq, sinq, t0)
        normrope(qT, h, NLAT, NCL, gn[:, 0:1], None, None, 0)
    for h in range(16):
        kv = h // 4
        st = ost[h % 2]
        for (t0, tn, chunks) in ((0, 512, range(NCH)), (512, 512, range(NCH)), (NLAT, NCL, range(32, NCH))):
            ps_o = P.psb[4 + 2 * (acc_i % 2)]
            ps_s = P.psb[5 + 2 * (acc_i % 2)]
            acc_i += 1
            chunks = list(chunks)
            LOOK = 2
            pend = []
            for ci in range(len(chunks) + LOOK):
                if ci < len(chunks):
                    c = chunks[ci]
                    ps = P.psb[pi % 4]
                    p_ = pT[pi % 4]
                    pi += 1
                    mk.matmul(ps[:, 0:tn], kT[:, kv, c * 128:(c + 1) * 128], qT[:, h, t0:t0 + tn])
                    mk.act(p_[:, 0:tn], ps[:, 0:tn], AF.Exp)
                    pend.append((ci, c, p_))
                if ci >= LOOK:
                    cj, c, p_ = pend.pop(0)
                    mk.matmul(ps_o[:, 0:tn], vt[:, c, kv * 128:(kv + 1) * 128], p_[:, 0:tn],
                              start=(cj == 0), stop=(cj == len(chunks) - 1))
                    mk.matmul(ps_s[:, 0:tn], P.ones.all(), p_[:, 0:tn], start=(cj == 0),
                              stop=(cj == len(chunks) - 1))
            r = rcp[acc_i % 2]
            mk.recip(r[:, 0:tn], ps_s[:, 0:tn])
            mk.tt("dve", st[:, t0:t0 + tn], ps_o[:, 0:tn], r[:, 0:tn], ALU.mult)
        mk.dma("sp", oT_d[h * 128:(h + 1) * 128, :], st.all())
    P.ps_i = 0
    cos, sin, R = rope_tables()
    gnk = np.ascontiguousarray(np.stack([q_norm, k_norm], axis=1).astype(np.float32))
    in_maps = []
    for c in range(NCORES):
        q = c % 4
        in_maps.append({"qT": qT_list[c], "kT": kT_list[c], "v": v_list[c], "gn": gnk, "cos": cos, "sin": sin,
                        "cosq": np.ascontiguousarray(cos[:, q * NLAT:(q + 1) * NLAT]),
                        "sinq": np.ascontiguousarray(sin[:, q * NLAT:(q + 1) * NLAT]), "R": R})
    return [r["oT"] for r in P.run(in_maps)]


def to_fm(x_tok):
    return np.ascontiguousarray(x_tok.T)


def core_xT(x_lat, x_ctx, nctx):
    out = []
    for c in range(NCORES):
        b, q = c // 4, c % 4
        parts = [x_lat[b, q * NLAT:(q + 1) * NLAT]]
        if nctx:
            parts.append(x_ctx[b, q * nctx:(q + 1) * nctx])
        out.append(to_fm(np.concatenate(parts, axis=0)))
    return out


def layer_gqa(x_lat, x_ctx, mod, L, inp):
    modT = [mod_layout(mod, L, c // 4) for c in range(NCORES)]
    xT = core_xT(x_lat, x_ctx, NCL)
    specs = [("qT", 0, 2048, "fm", "bf16"), ("kT", 2048, 512, "fm", "bf16"), ("v", 2560, 512, "tm", "bf16")]
    ra = run_A(xT, modT, inp["norm_pre_mix"][L], inp["gqa_w_in"][0], specs, NCL)
    kT_list, v_list = [], []
    for c in range(NCORES):
        b = c // 4
        kT_list.append(np.ascontiguousarray(np.concatenate(
            [ra[4 * b + i]["kT"][:, 0:NLAT] for i in range(4)] + [ra[4 * b + i]["kT"][:, NLAT:] for i in range(4)],
            axis=1)))
        v_list.append(np.ascontiguousarray(np.concatenate(
            [ra[4 * b + i]["v"][0:NLAT] for i in range(4)] + [ra[4 * b + i]["v"][NLAT:] for i in range(4)], axis=0)))
    oT = run_B_gqa([r["qT"] for r in ra], kT_list, v_list, inp["gqa_q_norm"][0], inp["gqa_k_norm"][0])
    xmid = run_C1(oT, xT, modT, inp["norm_post_mix"][L], inp["gqa_w_out"][0], None, NCL)
    return xmid


def split_xT(xT_list, nctx):
    x_lat = np.zeros((2, SEQ, D), np.float32)
    x_ctx = np.zeros((2, 4 * nctx, D), np.float32) if nctx else None
    for c in range(NCORES):
        b, q = c // 4, c % 4
        x_lat[b, q * NLAT:(q + 1) * NLAT] = xT_list[c][:, 0:NLAT].T
        if nctx:
            x_ctx[b, q * nctx:(q + 1) * nctx] = xT_list[c][:, NLAT:NLAT + nctx].T
    return x_lat, x_ctx


def layer_ffn(xmid_lat, xmid_ctx, mod, L, inp, nctx):
    modT = [mod_layout(mod, L, c // 4, with_ctx=bool(nctx)) for c in range(NCORES)]
    xe_list, mask_list, groups = ffn_host_io(xmid_lat, xmid_ctx, nctx)
    xo = run_C2(xe_list, mask_list, modT, inp["norm_pre_ffn"][L], inp["norm_post_ffn"][L], inp["ffn_w_up"][L],
                inp["ffn_conv_w"][L], inp["ffn_conv_b"][L], inp["ffn_w_down"][L], groups)
    return split_xT(xo, nctx)


def run_B_conv(zT_list, mask_list, b_pw1, w_dw, b_dw, ln_w, ln_b, nctx):
    HW = 15
    Le = NLAT + 2 * HW
    Ce = nctx + 2 * HW
    Te = Le + Ce
    T = NLAT + nctx
    P = Prog("Bconv")
    mk = P.mk
    zT_d = P.inp("zT", [2 * D, Te])
    mask_d = P.inp("mask", [128, Te])
    bp_d = P.inp("bp", [128, 2 * KC])
    wdw_d = P.inp("wdw", [128, KC, 31])
    bdw_d = P.inp("bdw", [128, KC])
    lnw_d = P.inp("lnw", [128, KC])
    lnb_d = P.inp("lnb", [128, KC])
    sT_d = P.outp("sT", [D, T], BF16)
    id_d = P.inp("ident", [128, 128], BF16)
    maskt = mk.sb("maskt", [128, Te], F32)
    mk.dma("sp", maskt.all(), mask_d)
    bp = mk.sb("bp", [128, 2 * KC], F32)
    mk.dma("sp", bp.all(), bp_d)
    wdw = mk.sb("wdw", [128, KC, 31], F32)
    mk.dma("sp", wdw.all(), wdw_d)
    bdw = P.load_vec("bdw", bdw_d)
    lnw = P.load_vec("lnw", lnw_d)
    lnb = P.load_vec("lnb", lnb_d)
    onesf = mk.sb("onesf", [128, 128], F32)
    mk.memset("dve", onesf.all(), 1.0)
    vT = mk.sb("vT", [128, KC, T], F32)
    zb = [mk.sb(f"zb{i}", [128, Te], F32) for i in range(4)]
    ub = [mk.sb(f"ub{i}", [128, Te], BF16) for i in range(2)]
    sqf = [mk.sb(f"sqf{i}", [128, 512], F32) for i in range(2)]
    dgb = [mk.sb(f"dgb{i}", [128, 31, 128], BF16) for i in range(2)]
    identb = mk.sb("identb", [128, 128], BF16)
    mk.dma("sp", identb.all(), id_d)
    pcv = [0]
    zsrc = zT_d.rearrange("(kc p) t -> p kc t", p=128)
    tls = tiles_of(T)
    ps_sum = [P.psb[i] for i in range(len(tls))]
    ps_sq = [P.psb[3 + i] for i in range(len(tls))]
    for kc in range(KC):
        za = zb[(2 * kc) % 4]
        zg = zb[(2 * kc + 1) % 4]
        mk.dma("sp", za.all(), zsrc[:, kc, :])
        mk.dma("act", zg.all(), zsrc[:, KC + kc, :])
        mk.act(zg.all(), zg.all(), AF.Sigmoid, bias=bp[:, KC + kc:KC + kc + 1])
        mk.tt("pool", zg.all(), zg.all(), maskt.all(), ALU.mult)
        u = ub[kc % 2]
        mk.stt(u.all(), za.all(), bp[:, kc:kc + 1], zg.all(), ALU.add, ALU.mult)
        dg = dgb[kc % 2]
        for j in range(31):
            mk.act(dg[:, j, :], identb.all(), AF.Copy, scale=wdw[:, kc, j:j + 1])
        for (e0, o0, n) in ((0, 0, NLAT), (Le, NLAT, nctx)):
            if n == 0:
                continue
            for (t0, tn) in tiles_of(n):
                ps = P.psb[6 + (pcv[0] % 2)]
                pcv[0] += 1
                for j in range(31):
                    mk.matmul(ps[:, 0:tn], dg[:, j, :], u[:, e0 + j + t0:e0 + j + t0 + tn],
                              start=(j == 0), stop=(j == 30))
                mk.act(vT[:, kc, o0 + t0:o0 + t0 + tn], ps[:, 0:tn], AF.Identity, bias=bdw[:, kc:kc + 1])
        for ti, (t0, tn) in enumerate(tls):
            s_ = sqf[(kc * len(tls) + ti) % 2]
            mk.act(s_[:, 0:tn], vT[:, kc, t0:t0 + tn], AF.Square)
            mk.matmul(ps_sum[ti][:, 0:tn], onesf.all(), vT[:, kc, t0:t0 + tn], start=(kc == 0), stop=(kc == KC - 1))
            mk.matmul(ps_sq[ti][:, 0:tn], onesf.all(), s_[:, 0:tn], start=(kc == 0), stop=(kc == KC - 1))
    mean = mk.sb("mean", [128, T], F32)
    rstd = mk.sb("rstd", [128, T], F32)
    msq = mk.sb("msq", [128, T], F32)
    for ti, (t0, tn) in enumerate(tls):
        mk.ts("dve", mean[:, t0:t0 + tn], ps_sum[ti][:, 0:tn], 1.0 / D, ALU.mult)
        mk.tt("dve", msq[:, t0:t0 + tn], mean[:, t0:t0 + tn], mean[:, t0:t0 + tn], ALU.mult)
        mk.stt(msq[:, t0:t0 + tn], ps_sq[ti][:, 0:tn], 1.0 / D, msq[:, t0:t0 + tn], ALU.mult, ALU.subtract)
        mk.act(rstd[:, t0:t0 + tn], msq[:, t0:t0 + tn], AF.Sqrt, bias=P.eps_t[:, 0:1])
        mk.recip(rstd[:, t0:t0 + tn], rstd[:, t0:t0 + tn])
    sb_ = [mk.sb(f"sbo{i}", [128, T], BF16) for i in range(2)]
    tmp = [mk.sb(f"tmpn{i}", [128, T], F32) for i in range(2)]
    for kc in range(KC):
        t = tmp[kc % 2]
        mk.tt("pool", t.all(), vT[:, kc, :], mean.all(), ALU.subtract)
        mk.tt("dve", t.all(), t.all(), rstd.all(), ALU.mult)
        so = sb_[kc % 2]
        mk.act(so.all(), t.all(), AF.Silu, bias=lnb[:, kc:kc + 1], scale=lnw[:, kc:kc + 1])
        mk.dma("sp", sT_d[kc * 128:(kc + 1) * 128, :], so.all())
    bpk = np.ascontiguousarray(b_pw1.reshape(2 * KC, 128).T)
    wdwk = np.ascontiguousarray(w_dw.T.reshape(KC, 128, 31).transpose(1, 0, 2))
    in_maps = [{"zT": zT_list[c], "mask": mask_list[c], "bp": bpk, "wdw": wdwk, "bdw": vec_pk(b_dw),
                "lnw": vec_pk(ln_w), "lnb": vec_pk(ln_b), "ident": np.eye(128, dtype=np.float32).astype(NPBF)}
               for c in range(NCORES)]
    return [r["sT"] for r in P.run(in_maps)]


def layer_conv(x_lat, x_ctx, mod, L, inp):
    HW = 15
    modT = [mod_layout(mod, L, c // 4) for c in range(NCORES)]
    xT = core_xT(x_lat, x_ctx, NCL)
    ra = run_A(xT, modT, inp["norm_pre_mix"][L], inp["conv_w_pw1"][0], [("zT", 0, 2 * D, "fm", "f32")], NCL)
    zT_list, mask_list = [], []
    for b in range(2):
        zl = np.concatenate([ra[4 * b + i]["zT"][:, 0:NLAT] for i in range(4)], axis=1)
        zl = np.pad(zl, ((0, 0), (HW, HW)))
        ml = np.pad(np.ones(SEQ, np.float32), (HW, HW))
        zc = np.pad(np.concatenate([ra[4 * b + i]["zT"][:, NLAT:] for i in range(4)], axis=1), ((0, 0), (HW, HW)))
        mc = np.pad(np.ones(NCTX, np.float32), (HW, HW))
        for q in range(4):
            sl = slice(q * NLAT, q * NLAT + NLAT + 2 * HW)
            slc = slice(q * NCL, q * NCL + NCL + 2 * HW)
            zT_list.append(np.ascontiguousarray(np.concatenate([zl[:, sl], zc[:, slc]], axis=1)))
            m = np.concatenate([ml[sl], mc[slc]])
            mask_list.append(np.ascontiguousarray(np.broadcast_to(m[None, :], (128, m.shape[0]))))
    sT = run_B_conv(zT_list, mask_list, inp["conv_b_pw1"][0], inp["conv_w_dw"][0], inp["conv_b_dw"][0],
                    inp["conv_ln_w"][0], inp["conv_ln_b"][0], NCL)
    xmid = run_C1(sT, xT, modT, inp["norm_post_mix"][L], inp["conv_w_pw2"][0], inp["conv_b_pw2"][0], NCL)
    return xmid


def rview(v, pattern, **kw):
    return V(v.ap.rearrange(pattern, **kw), v.tile, v.lo, v.hi)


def nat_tables(rpb):
    p = np.arange(128)
    half, kc = p // 64, p % 64
    i = np.arange(8)
    qc = np.arange(64)
    dr = -8 + 2 * i[None, :] + half[:, None]
    cidx = kc[:, None] - qc[None, :] + 15
    cstart = np.clip(qc - 8, 0, 48)
    cval = (kc[:, None] >= cstart[None, :]) & (kc[:, None] < cstart[None, :] + 16)
    rv = dr >= -7
    B = rpb[:, np.clip(dr + 7, 0, 14)[:, :, None], np.clip(cidx, 0, 30)[:, None, :]]
    B = np.ascontiguousarray(B.transpose(1, 0, 2, 3)).astype(np.float32)
    M = (rv[:, :, None] & cval[:, None, :]).astype(np.float32)
    M = np.ascontiguousarray(np.broadcast_to(M[:, None], (128, 8, 8, 64)))
    return B, M


def nat_rowvalid(q):
    p = np.arange(128)
    half = p // 64
    out = np.zeros((128, 16, 8), np.float32)
    for rl in range(16):
        r = 16 * q + rl
        rs = min(max(r - 4, 0), 56)
        for i in range(8):
            kr = r - 8 + 2 * i + half
            out[:, rl, i] = ((kr >= rs) & (kr < rs + 8)).astype(np.float32)
    return out


def run_B_nat(qT_list, kTw_list, vw_list, kTc_list, vc_list, rpb):
    P = Prog("Bnat")
    mk = P.mk
    NW = 2048
    qT_d = P.inp("qT", [D, NLAT], BF16)
    kT_d = P.inp("kTw", [D, NW], BF16)
    v_d = P.inp("vw", [NW, D], BF16)
    kTc_d = P.inp("kTc", [D, NCTX], BF16)
    vc_d = P.inp("vc", [NCTX, D], BF16)
    B_d = P.inp("B", [128, 16, 8, 64])
    M_d = P.inp("M", [128, 8, 8, 64])
    rv_d = P.inp("rv", [128, 16, 8])
    oT_d = P.outp("oT", [D, NLAT], BF16)
    rv = mk.sb("rv", [128, 16, 8], F32)
    mk.dma("sp", rv.all(), rv_d)
    Mt = mk.sb("Mt", [128, 8, 8, 64], F32)
    mk.dma("sp", Mt.all(), M_d)
    Et = mk.sb("Et", [128, 8, 8, 64], F32)
    kT = mk.sb("kT", [128, 8, NW], BF16)
    qT = mk.sb("qT", [128, 8, NLAT], BF16)
    kTc = mk.sb("kTc", [128, 8, NCTX], BF16)
    vc = mk.sb("vc", [128, 2, 1024], BF16)
    vb = [mk.sb(f"vb{i}", [128, 8, 1024], BF16) for i in range(2)]
    pT = [mk.sb(f"pT{i}", [128, 512], BF16) for i in range(4)]
    rcp = [mk.sb(f"rcp{i}", [128, 512], F32) for i in range(2)]
    ost = mk.sb("ost", [128, 8, NLAT], BF16)
    scale = 128.0 ** -0.5
    pi = 0
    acc_i = 0
    vi = 0
    for g in range(2):
        hs = slice(g * 1024, (g + 1) * 1024)
        mk.dma("sp", Et.all(), B_d[:, g * 8:(g + 1) * 8, :, :])
        mk.act(Et.all(), Et.all(), AF.Exp)
        mk.tt("pool", Et.all(), Et.all(), Mt.all(), ALU.mult)
        for h0 in range(0, 8, 4):
            mk.dma("sp", kT[:, h0:h0 + 4, :], kT_d[hs, :].rearrange("(h p) t -> p h t", p=128)[:, h0:h0 + 4, :])
        mk.dma("act", qT.all(), qT_d[hs, :].rearrange("(h p) t -> p h t", p=128))
        mk.dma("act", kTc.all(), kTc_d[hs, :].rearrange("(h p) t -> p h t", p=128))
        mk.dma("act", vc.all(), vc_d[:, hs].rearrange("(c p) n -> p c n", p=128))
        for rl in range(16):
            vband = vb[vi % 2]
            vi += 1
            mk.dma("sp" if rl % 2 == 0 else "act", vband.all(),
                   v_d[rl * 64:rl * 64 + 1024, hs].rearrange("(c p) n -> p c n", p=128))
            ps_o = P.psb[4 + 2 * (acc_i % 2)]
            ps_s = P.psb[5 + 2 * (acc_i % 2)]
            acc_i += 1
            qs = slice(rl * 64, (rl + 1) * 64)
            LOOK = 2
            pend = []
            for ii in range(10 + LOOK):
                if ii < 10:
                    i = ii
                    ps = P.psb[pi % 4]
                    p_ = pT[pi % 4]
                    pi += 1
                    for h in range(8):
                        if i < 8:
                            lhs = kT[:, h, (rl + 2 * i) * 64:(rl + 2 * i) * 64 + 128]
                        else:
                            lhs = kTc[:, h, (i - 8) * 128:(i - 7) * 128]
                        mk.matmul(ps[:, h * 64:(h + 1) * 64], lhs, qT[:, h, qs])
                    mk.act(p_.all(), ps.all(), AF.Exp, scale=scale)
                    if i < 8:
                        mk.stt(rview(p_.all(), "p (h q) -> p h q", h=8), rview(p_.all(), "p (h q) -> p h q", h=8),
                               rv[:, rl, i:i + 1], Et[:, :, i, :], ALU.mult, ALU.mult)
                    pend.append((i, p_))
                if ii < LOOK:
                    continue
                i, p_ = pend.pop(0)
                for h in range(8):
                    vv = vband[:, i, h * 128:(h + 1) * 128] if i < 8 else vc[:, i - 8, h * 128:(h + 1) * 128]
                    mk.matmul(ps_o[:, h * 64:(h + 1) * 64], vv, p_[:, h * 64:(h + 1) * 64],
                              start=(i == 0 and h == 0), stop=(i == 9), skip=True)
                mk.matmul(ps_s.all(), P.ones.all(), p_.all(), start=(i == 0), stop=(i == 9))
            r = rcp[acc_i % 2]
            mk.recip(r.all(), ps_s.all())
            mk.tt("dve", ost[:, :, qs], rview(ps_o.all(), "p (h q) -> p h q", h=8),
                  rview(r.all(), "p (h q) -> p h q", h=8), ALU.mult)
        mk.dma("sp", oT_d[hs, :].rearrange("(h p) t -> p h t", p=128), ost.all())
    B, M = nat_tables(rpb)
    in_maps = [{"qT": qT_list[c], "kTw": kTw_list[c], "vw": vw_list[c], "kTc": kTc_list[c], "vc": vc_list[c],
                "B": B, "M": M, "rv": nat_rowvalid(c % 4)} for c in range(NCORES)]
    return [r["oT"] for r in P.run(in_maps)]


def layer_nat(x_lat, x_ctx, mod, L, inp):
    modT = [mod_layout(mod, L, c // 4) for c in range(NCORES)]
    xT = core_xT(x_lat, x_ctx, NCL)
    specs = [("qT", 0, 2048, "fm", "bf16"), ("kT", 2048, 2048, "fm", "bf16"), ("v", 4096, 2048, "tm", "bf16")]
    ra = run_A(xT, modT, inp["norm_pre_mix"][L], inp["nat_w_in"][0], specs, NCL)
    qT_list, kTw, vw, kTc, vcl = [], [], [], [], []
    for b in range(2):
        kg = np.concatenate([ra[4 * b + i]["kT"][:, 0:NLAT] for i in range(4)], axis=1)
        kg = np.pad(kg, ((0, 0), (512, 512)))
        vg = np.concatenate([ra[4 * b + i]["v"][0:NLAT] for i in range(4)], axis=0)
        vg = np.pad(vg, ((512, 512), (0, 0)))
        for q in range(4):
            c = 4 * b + q
            qT_list.append(np.ascontiguousarray(ra[c]["qT"][:, 0:NLAT]))
            kTw.append(np.ascontiguousarray(kg[:, q * 1024:q * 1024 + 2048]))
            vw.append(np.ascontiguousarray(vg[q * 1024:q * 1024 + 2048]))
            kTc.append(np.ascontiguousarray(np.concatenate([ra[4 * b + i]["kT"][:, NLAT:] for i in range(4)], axis=1)))
            vcl.append(np.ascontiguousarray(np.concatenate([ra[4 * b + i]["v"][NLAT:] for i in range(4)], axis=0)))
    oT = run_B_nat(qT_list, kTw, vw, kTc, vcl, inp["nat_rpb"][0])
    modT1 = [mod_layout(mod, L, c // 4, with_ctx=False) for c in range(NCORES)]
    xT1 = [np.ascontiguousarray(x[:, 0:NLAT]) for x in xT]
    xmid = run_C1(oT, xT1, modT1, inp["norm_post_mix"][L], inp["nat_w_out"][0], None, 0)
    return xmid


def mlstm_consts():
    blk = np.arange(128) // 64
    same = blk[:, None] == blk[None, :]
    idx = np.arange(128)
    Uf = (same & (idx[:, None] <= idx[None, :])).astype(np.float32)
    Ub = np.ascontiguousarray(Uf.T)
    I = np.eye(128, dtype=np.float32)
    out = np.zeros((2, 5, 128, 128), np.float32)
    for d, U in enumerate((Uf, Ub)):
        out[d, 0] = U
        out[d, 1] = -U
        out[d, 2] = (U - 1.0) * 30000.0
        out[d, 3] = U.T - I
        out[d, 4] = I
    return np.ascontiguousarray(out.transpose(2, 0, 1, 3))


def run_B_mlstm(qT_list, kT_list, ktm_list, vtm_list, otm_list, gtm_list, bg_list, gain_list):
    NT = NCTX + SEQ
    NSC = NT // 128
    P = Prog("Bmlstm")
    mk = P.mk
    qT_d = P.inp("qT", [2, 128, NT], BF16)
    kT_d = P.inp("kT", [2, 128, NT], BF16)
    ktm_d = P.inp("ktm", [NT, 2, 128], BF16)
    vtm_d = P.inp("vtm", [NT, 2, 256], BF16)
    otm_d = P.inp("otm", [NT, 2, 256])
    gtm_d = P.inp("gtm", [NT, 2, 4])
    bg_d = P.inp("bg", [128, 2, 4])
    gain_d = P.inp("gain", [128, 2, 256])
    cm_d = P.inp("cm", [128, 2, 5, 128])
    out_d = P.outp("hout", [NT, 2, 256], BF16)
    cm = mk.sb("cm", [128, 2, 5, 128], F32)
    mk.dma("sp", cm.all(), cm_d)
    bg = mk.sb("bg", [128, 2, 4], F32)
    mk.dma("sp", bg.all(), bg_d)
    gain = mk.sb("gain", [128, 2, 256], F32)
    mk.dma("sp", gain.all(), gain_d)
    onesf = mk.sb("onesf", [128, 128], F32)
    mk.memset("dve", onesf.all(), 1.0)
    qT = mk.sb("qT", [128, NT], BF16)
    kT = mk.sb("kT", [128, NT], BF16)
    ktm = mk.sb("ktm", [128, NSC, 128], BF16)
    vtm = mk.sb("vtm", [128, NSC, 257], BF16)
    otm = mk.sb("otm", [128, NSC, 256], F32)
    gt = mk.sb("gt", [128, NSC, 4], F32)
    tmpg = mk.sb("tmpg", [128, NSC, 4], F32)
    IG = mk.sb("IG", [128, 2, NSC], F32)
    LF = mk.sb("LF", [128, 2, NSC], F32)
    Hacc = mk.sb("Hacc", [128, NSC, 256], F32)
    Cst = [[mk.sb(f"Cst{d}{i}", [128, 257], F32) for i in range(2)] for d in range(2)]
    Cbf = [[mk.sb(f"Cbf{d}{i}", [128, 257], BF16) for i in range(3)] for d in range(2)]
    Qpad = [[mk.sb(f"Qpad{d}{i}", [128, 256], BF16) for i in range(2)] for d in range(2)]
    for d in range(2):
        for i in range(2):
            mk.memset("pool", Qpad[d][i].all(), 0.0)
    LFbc = [mk.sb(f"LFbc{i}", [128, 128], F32) for i in range(2)]
    Edec = [mk.sb(f"Edec{i}", [128, 128], F32) for i in range(2)]
    DmT = [mk.sb(f"DmT{i}", [128, 128], F32) for i in range(2)]
    wgt = [mk.sb(f"wgt{i}", [128, 1], F32) for i in range(2)]
    Kw = [mk.sb(f"Kw{i}", [128, 128], BF16) for i in range(2)]
    Sm = [mk.sb(f"Sm{i}", [128, 128], BF16) for i in range(2)]
    dn = [mk.sb(f"dn{i}", [128, 1], F32) for i in range(2)]
    ssq = mk.sb("ssq", [128, NSC], F32)
    rst = mk.sb("rst", [128, NSC], F32)
    sqj = mk.sb("sqj", [128, 256], F32)
    sg = [mk.sb(f"sg{i}", [128, 256], F32) for i in range(2)]
    hn = [mk.sb(f"hn{i}", [128, 256], F32) for i in range(2)]
    ob = [mk.sb(f"ob{i}", [128, 256], BF16) for i in range(2)]
    scale = 128.0 ** -0.5
    order_f = list(range(NSC))
    order_b = [1, 0] + list(range(NSC - 1, 1, -1))
    it = 0
    for hd in range(2):
        mk.dma("sp", qT.all(), qT_d[hd])
        mk.dma("act", kT.all(), kT_d[hd])
        mk.dma("sp", ktm.all(), ktm_d[:, hd, :].rearrange("(c p) n -> p c n", p=128))
        mk.dma("act", vtm[:, :, 0:256], vtm_d[:, hd, :].rearrange("(c p) n -> p c n", p=128))
        mk.memset("pool", vtm[:, :, 256:257], 1.0)
        mk.dma("sp", otm.all(), otm_d[:, hd, :].rearrange("(c p) n -> p c n", p=128))
        mk.dma("act", gt.all(), gtm_d[:, hd, :].rearrange("(c p) n -> p c n", p=128))
        for c in range(4):
            mk.act(gt[:, :, c:c + 1], gt[:, :, c:c + 1], AF.Identity, bias=bg[:, hd, c:c + 1])
        mk.act(tmpg.all(), gt.all(), AF.Exp, scale=-1.0)
        mk.act(tmpg.all(), tmpg.all(), AF.Ln, bias=onesf[:, 0:1])
        for d in range(2):
            mk.act(rview(IG[:, d, :], "p (c o) -> p c o", o=1), gt[:, :, 2 * d:2 * d + 1], AF.Copy)
            mk.act(rview(LF[:, d, :], "p (c o) -> p c o", o=1), tmpg[:, :, 2 * d + 1:2 * d + 2], AF.Copy, scale=-1.0)
        mk.memset("pool", Hacc.all(), 0.0)
        cur = [0, 0]
        cbi = [0, 0]
        for d in range(2):
            mk.memset("dve", Cst[d][0].all(), 0.0)
            mk.memset("pool", Cbf[d][0].all(), 0.0)
        for step in range(NSC):
            for d in range(2):
                sc = (order_f if d == 0 else order_b)[step]
                i2 = it % 2
                it += 1
                U, nU, NEG, SL, Id = (cm[:, d, k, :] for k in range(5))
                lfc = LF[:, d, sc:sc + 1]
                igc = IG[:, d, sc:sc + 1]
                tsl = slice(sc * 128, (sc + 1) * 128)
                mk.act(LFbc[i2].all(), onesf.all(), AF.Copy, scale=lfc)
                ps1 = P.psum()
                mk.matmul(ps1[:, 0:128], LFbc[i2].all(), U)
                mk.act(Edec[i2].all(), ps1[:, 0:128], AF.Exp)
                ps2 = P.psum()
                mk.matmul(ps2[:, 0:128], LFbc[i2].all(), U, start=True, stop=False)
                mk.matmul(ps2[:, 0:128], nU, LFbc[i2].all(), start=False, stop=False)
                mk.matmul(ps2[:, 0:128], Id, NEG, start=False, stop=True)
                mk.act(DmT[i2].all(), ps2[:, 0:128], AF.Exp, bias=igc)
                ps3 = P.psum()
                mk.matmul(ps3[:, 0:1], SL, lfc)
                mk.act(wgt[i2].all(), ps3[:, 0:1], AF.Exp, bias=igc)
                mk.ts("dve", Kw[i2].all(), ktm[:, sc, :], wgt[i2][:, 0:1], ALU.mult)
                ps4 = P.psum()
                mk.matmul(ps4[:, 0:128], kT[:, tsl], qT[:, tsl])
                mk.stt(Sm[i2].all(), ps4[:, 0:128], scale, DmT[i2].all(), ALU.mult, ALU.mult)
                qp = Qpad[d][step % 2]
                mk.stt(qp[:, 0:64], qT[:, sc * 128:sc * 128 + 64], scale, Edec[i2][:, 0:64], ALU.mult, ALU.mult)
                mk.stt(qp[:, 192:256], qT[:, sc * 128 + 64:sc * 128 + 128], scale, Edec[i2][:, 64:128],
                       ALU.mult, ALU.mult)
                first, second = ((0, 64), (64, 128)) if d == 0 else ((64, 128), (0, 64))
                gcol = (63, 127) if d == 0 else (64, 0)
                cb_in = Cbf[d][cbi[d] % 3]
                cs_in = Cst[d][cur[d] % 2]
                cs_mid = Cst[d][(cur[d] + 1) % 2]
                cb_mid = Cbf[d][(cbi[d] + 1) % 3]
                cb_out = Cbf[d][(cbi[d] + 2) % 3]
                ps6 = P.psum()
                mk.matmul(ps6[:, 0:257], Kw[i2][first[0]:first[1], :], vtm[first[0]:first[1], sc, :])
                mk.stt(cs_mid.all(), cs_in.all(), Edec[i2][:, gcol[0]:gcol[0] + 1], ps6[:, 0:257], ALU.mult, ALU.add)
                mk.copy("act", cb_mid.all(), cs_mid.all())
                ps7 = P.psum()
                mk.matmul(ps7[:, 0:257], Kw[i2][second[0]:second[1], :], vtm[second[0]:second[1], sc, :])
                mk.stt(cs_in.all(), cs_mid.all(), Edec[i2][:, gcol[1]:gcol[1] + 1], ps7[:, 0:257], ALU.mult, ALU.add)
                mk.copy("act", cb_out.all(), cs_in.all())
                cbi[d] += 2
                cA, cB = (cb_in, cb_mid) if d == 0 else (cb_mid, cb_in)
                ps5 = P.psum()
                mk.matmul(ps5[:, 0:257], Sm[i2].all(), vtm[:, sc, :], start=True, stop=False)
                mk.matmul(ps5[:, 0:257], qp[:, 0:128], cA.all(), start=False, stop=False)
                mk.matmul(ps5[:, 0:257], qp[:, 128:256], cB.all(), start=False, stop=True)
                mk.act(dn[i2].all(), ps5[:, 256:257], AF.Abs)
                mk.ts("dve", dn[i2].all(), dn[i2].all(), 1.0, ALU.max)
                mk.recip(dn[i2].all(), dn[i2].all())
                mk.stt(Hacc[:, sc, :], ps5[:, 0:256], dn[i2][:, 0:1], Hacc[:, sc, :], ALU.mult, ALU.add)
        mk.memset("dve", ssq.all(), 0.0)
        for sc in range(NSC):
            mk.act(sqj.all(), Hacc[:, sc, :], AF.Square, accum_out=ssq[:, sc:sc + 1])
        mk.act(rst.all(), ssq.all(), AF.Sqrt, bias=P.eps_t[:, 0:1], scale=1.0 / 256)
        mk.recip(rst.all(), rst.all())
        for sc in range(NSC):
            j = sc % 2
            mk.act(sg[j].all(), otm[:, sc, :], AF.Sigmoid)
            mk.stt(hn[j].all(), Hacc[:, sc, :], rst[:, sc:sc + 1], gain[:, hd, :], ALU.mult, ALU.mult)
            mk.tt("pool", ob[j].all(), hn[j].all(), sg[j].all(), ALU.mult)
            mk.dma("sp", out_d[sc * 128:(sc + 1) * 128, hd, :], ob[j].all())
    cmk = mlstm_consts()
    in_maps = [{"qT": qT_list[c], "kT": kT_list[c], "ktm": ktm_list[c], "vtm": vtm_list[c], "otm": otm_list[c],
                "gtm": gtm_list[c], "bg": bg_list[c], "gain": gain_list[c], "cm": cmk} for c in range(NCORES)]
    return [r["hout"] for r in P.run(in_maps)]


def layer_mlstm(x_lat, x_ctx, mod, L, inp):
    modT = [mod_layout(mod, L, c // 4) for c in range(NCORES)]
    xT = core_xT(x_lat, x_ctx, NCL)
    specs = [("qT", 0, 1024, "fm", "bf16"), ("kT", 1024, 1024, "fm", "bf16"), ("ktm", 1024, 1024, "tm", "bf16"),
             ("vtm", 2048, 2048, "tm", "bf16"), ("otm", 4096, 2048, "tm", "f32"), ("gtm", 6144, 32, "tm", "f32")]
    ra = run_A(xT, modT, inp["norm_pre_mix"][L], inp["mlstm_w_in"][0], specs, NCL)

    def glob_fm(name, b):
        return np.concatenate([ra[4 * b + i][name][:, NLAT:] for i in range(4)]
                              + [ra[4 * b + i][name][:, 0:NLAT] for i in range(4)], axis=1)

    def glob_tm(name, b):
        return np.concatenate([ra[4 * b + i][name][NLAT:] for i in range(4)]
                              + [ra[4 * b + i][name][0:NLAT] for i in range(4)], axis=0)

    lists = [[] for _ in range(8)]
    b_gate = inp["mlstm_b_gate"][0].reshape(4, 8)
    onorm = inp["mlstm_out_norm"][0].reshape(8, 256)
    for b in range(2):
        qg, kg = glob_fm("qT", b), glob_fm("kT", b)
        ktm, vtm, otm, gtm = (glob_tm(n, b) for n in ("ktm", "vtm", "otm", "gtm"))
        NT = qg.shape[1]
        for hp in range(4):
            hs = [2 * hp, 2 * hp + 1]
            lists[0].append(np.ascontiguousarray(qg.reshape(8, 128, NT)[hs]))
            lists[1].append(np.ascontiguousarray(kg.reshape(8, 128, NT)[hs]))
            lists[2].append(np.ascontiguousarray(ktm.reshape(NT, 8, 128)[:, hs]))
            lists[3].append(np.ascontiguousarray(vtm.reshape(NT, 8, 256)[:, hs]))
            lists[4].append(np.ascontiguousarray(otm.reshape(NT, 8, 256)[:, hs]))
            lists[5].append(np.ascontiguousarray(gtm.reshape(NT, 4, 8)[:, :, hs].transpose(0, 2, 1)))
            lists[6].append(np.ascontiguousarray(np.broadcast_to(b_gate[:, hs].T[None], (128, 2, 4))))
            lists[7].append(np.ascontiguousarray(np.broadcast_to(onorm[hs][None], (128, 2, 256))))
    ho = run_B_mlstm(*lists)
    oT = []
    for b in range(2):
        og = np.concatenate([ho[4 * b + hp] for hp in range(4)], axis=1)
        og = og.reshape(NCTX + SEQ, D)
        for q in range(4):
            tok = np.concatenate([og[NCTX + q * NLAT:NCTX + (q + 1) * NLAT], og[q * NCL:(q + 1) * NCL]], axis=0)
            oT.append(to_fm(tok))
    xmid = run_C1(oT, xT, modT, inp["norm_post_mix"][L], inp["mlstm_w_out"][0], None, NCL)
    return xmid


def kernel(**inputs):
    inp = {k: np.asarray(v) for k, v in inputs.items()}
    mod = run_mod(inp["c"], inp["c_ctx"], inp["mod_w"], inp["mod_b"])
    x_lat, x_ctx = inp["x"], inp["ctx"]
    layers = [layer_gqa, layer_mlstm, layer_conv, layer_nat]
    for L in range(4):
        nctx = NCL if L < 3 else 0
        xmid = layers[L](x_lat, x_ctx, mod, L, inp)
        xl, xc = split_xT(xmid, nctx)
        x_lat, x_ctx = layer_ffn(xl, xc, mod, L, inp, nctx)
    return np.ascontiguousarray(x_lat.astype(np.float32))
```
